# Optimizing a Trainium2 kernel written in Bass

```python
import jax, jax.numpy as jnp
from jax import lax
import numpy as np

D_MODEL = 1024
BATCH = 32
SEQ = 2048
DEPTH = 1

CHUNK = 64
PLE_DIM = 256
D_MIX = D_MODEL
D_CONV = D_MIX // 2
D_LRU = D_MIX - D_CONV
CONV_HEADS = 8
LRU_HEADS = 8
LRU_HEAD_DIM = D_LRU // LRU_HEADS
CONV_WIDTH = 31
LRU_CONV_WIDTH = 4
LRU_C = 8.0
N_GROUPS = 4
EXPERTS_PER_GROUP = 8
N_EXPERTS = N_GROUPS * EXPERTS_PER_GROUP
TOP_K = 2
D_EXPERT = D_MODEL // 2
ROUTE_BLOCK = 128
EPS = 1e-6

kernel_name = 'hymba_conformer_rglru_hmoe_block'


def _rmsnorm(x, g):
    xf = x.astype(jnp.float32)
    y = xf * lax.rsqrt(jnp.mean(xf * xf, axis=-1, keepdims=True) + EPS)
    return (y * g.astype(jnp.float32)).astype(x.dtype)


def _layernorm(x, g, b):
    xf = x.astype(jnp.float32)
    mu = jnp.mean(xf, axis=-1, keepdims=True)
    var = jnp.mean(jnp.square(xf - mu), axis=-1, keepdims=True)
    y = (xf - mu) * lax.rsqrt(var + EPS)
    return (y * g.astype(jnp.float32) + b.astype(jnp.float32)).astype(x.dtype)


def _causal_depthwise_conv(x, w, b):
    width = w.shape[0]
    y = lax.conv_general_dilated(
        x, w[:, None, :], window_strides=(1,), padding=((width - 1, 0),),
        dimension_numbers=('NWC', 'WIO', 'NWC'), feature_group_count=x.shape[-1])
    return y + b


def _conformer_conv(v, g, w_dw, b_dw, ln_g, ln_b):
    u = v * jax.nn.sigmoid(g)
    u = _causal_depthwise_conv(u, w_dw, b_dw)
    return jax.nn.silu(_layernorm(u, ln_g, ln_b))


def _rglru(x, w_r, b_r, w_i, b_i, lam):
    B, S, C = x.shape
    xh = x.reshape(B, S, LRU_HEADS, LRU_HEAD_DIM)
    r = jax.nn.sigmoid(jnp.einsum('bshi,hij->bshj', xh, w_r).reshape(B, S, C) + b_r)
    i_g = jax.nn.sigmoid(jnp.einsum('bshi,hij->bshj', xh, w_i).reshape(B, S, C) + b_i)
    log_a = -LRU_C * r.astype(jnp.float32) * jax.nn.softplus(-lam.astype(jnp.float32))
    a = jnp.exp(log_a)
    u = jnp.sqrt(-jnp.expm1(2.0 * log_a)) * (i_g * x).astype(jnp.float32)
    nc = S // CHUNK
    a_c = a.reshape(B, nc, CHUNK, C)
    u_c = u.reshape(B, nc, CHUNK, C)

    def combine(left, right):
        a_l, h_l = left
        a_r, h_r = right
        return a_l * a_r, a_r * h_l + h_r

    a_cum, h_loc = lax.associative_scan(combine, (a_c, u_c), axis=2)

    def step(h_prev, xs):
        ac, hl = xs
        h = hl + ac * h_prev[:, None, :]
        return h[:, -1, :], h

    _, h = lax.scan(step, jnp.zeros((B, C), jnp.float32),
                    (jnp.moveaxis(a_cum, 1, 0), jnp.moveaxis(h_loc, 1, 0)))
    return jnp.moveaxis(h, 0, 1).reshape(B, S, C).astype(x.dtype)


def _hier_route(h, w_group, b_group, w_expert, b_expert):
    T = h.shape[0]
    g_logits = (h @ w_group).astype(jnp.float32) + b_group.astype(jnp.float32)
    g_prob, g_idx = lax.top_k(jax.nn.softmax(g_logits, axis=-1), 1)
    e_logits = ((h @ w_expert).astype(jnp.float32) + b_expert.astype(jnp.float32))
    e_logits = e_logits.reshape(T, N_GROUPS, EXPERTS_PER_GROUP)
    e_sel = jnp.take_along_axis(e_logits, g_idx[:, :, None], axis=1)[:, 0]
    e_prob, e_loc = lax.top_k(jax.nn.softmax(e_sel, axis=-1), TOP_K)
    e_prob = e_prob / jnp.sum(e_prob, axis=-1, keepdims=True)
    gate = g_prob * e_prob
    expert_id = g_idx * EXPERTS_PER_GROUP + e_loc
    return expert_id, gate


def _routed_experts(h, expert_id, gate, w1, w3, w2):
    T, D = h.shape
    A = T * TOP_K
    flat_e = expert_id.reshape(A)
    flat_tok = jnp.repeat(jnp.arange(T, dtype=jnp.int32), TOP_K, total_repeat_length=A)
    flat_gate = gate.reshape(A)
    order = jnp.argsort(flat_e)
    se, st, sg = flat_e[order], flat_tok[order], flat_gate[order]
    counts = jnp.zeros((N_EXPERTS,), jnp.int32).at[flat_e].add(1)
    padded = (counts + ROUTE_BLOCK - 1) // ROUTE_BLOCK * ROUTE_BLOCK
    start = jnp.cumsum(counts) - counts
    pad_end = jnp.cumsum(padded)
    pad_start = pad_end - padded
    dest = pad_start[se] + (jnp.arange(A, dtype=jnp.int32) - start[se])
    n_blocks = -(-A // ROUTE_BLOCK) + N_EXPERTS
    P = n_blocks * ROUTE_BLOCK
    buf_tok = jnp.zeros((P,), jnp.int32).at[dest].set(st)
    buf_gate = jnp.zeros((P,), jnp.float32).at[dest].set(sg)
    block_start = jnp.arange(n_blocks, dtype=jnp.int32) * ROUTE_BLOCK
    block_e = jnp.minimum(jnp.searchsorted(pad_end, block_start, side='right'), N_EXPERTS - 1)

    def expert_block(args):
        tok, e, g = args
        xb = h[tok]
        y = (jax.nn.silu(xb @ w1[e]) * (xb @ w3[e])) @ w2[e]
        return (y * g[:, None]).astype(h.dtype)

    y = lax.map(expert_block, (buf_tok.reshape(n_blocks, ROUTE_BLOCK), block_e,
                               buf_gate.reshape(n_blocks, ROUTE_BLOCK)))
    return jnp.zeros_like(h).at[buf_tok].add(y.reshape(P, D))


def setup_inputs(seed: int = 0) -> dict:
    key = jax.random.key(seed)
    ks = jax.random.split(key, 32)
    f32 = jnp.float32
    L = DEPTH

    def nrm(k, shape, scale):
        return jax.random.normal(k, shape, f32) * scale

    def gain(k, shape):
        return 1.0 + 0.05 * jax.random.normal(k, shape, f32)

    u = jax.random.uniform(ks[14], (L, D_LRU), f32, 0.9, 0.999)
    s = u ** (1.0 / LRU_C)
    lru_lambda = jnp.log(s) - jnp.log1p(-s)
    return {
        'x': nrm(ks[0], (BATCH, SEQ, D_MODEL), 1.0),
        'p': nrm(ks[1], (L, BATCH, SEQ, PLE_DIM), 1.0),
        'g_mix': gain(ks[2], (L, D_MODEL)),
        'w_in': nrm(ks[3], (L, D_MODEL, 2 * D_CONV + 2 * D_LRU), D_MODEL ** -0.5),
        'conv_dw_w': nrm(ks[4], (L, CONV_WIDTH, D_CONV), CONV_WIDTH ** -0.5),
        'conv_dw_b': nrm(ks[5], (L, D_CONV), 0.02),
        'conv_ln_g': gain(ks[6], (L, D_CONV)),
        'conv_ln_b': nrm(ks[7], (L, D_CONV), 0.02),
        'lru_conv_w': nrm(ks[8], (L, LRU_CONV_WIDTH, D_LRU), LRU_CONV_WIDTH ** -0.5),
        'lru_conv_b': nrm(ks[9], (L, D_LRU), 0.02),
        'lru_w_r': nrm(ks[10], (L, LRU_HEADS, LRU_HEAD_DIM, LRU_HEAD_DIM), LRU_HEAD_DIM ** -0.5),
        'lru_b_r': nrm(ks[11], (L, D_LRU), 0.02),
        'lru_w_i': nrm(ks[12], (L, LRU_HEADS, LRU_HEAD_DIM, LRU_HEAD_DIM), LRU_HEAD_DIM ** -0.5),
        'lru_b_i': nrm(ks[13], (L, D_LRU), 0.02),
        'lru_lambda': lru_lambda,
        'w_out': nrm(ks[15], (L, D_MIX, D_MODEL), D_MIX ** -0.5),
        'g_ffn': gain(ks[16], (L, D_MODEL)),
        'w_group': nrm(ks[17], (L, D_MODEL, N_GROUPS), D_MODEL ** -0.5),
        'b_group': nrm(ks[18], (L, N_GROUPS), 0.01),
        'w_expert': nrm(ks[19], (L, D_MODEL, N_EXPERTS), D_MODEL ** -0.5),
        'b_expert': nrm(ks[20], (L, N_EXPERTS), 0.01),
        'w1': nrm(ks[21], (L, N_EXPERTS, D_MODEL, D_EXPERT), D_MODEL ** -0.5),
        'w3': nrm(ks[22], (L, N_EXPERTS, D_MODEL, D_EXPERT), D_MODEL ** -0.5),
        'w2': nrm(ks[23], (L, N_EXPERTS, D_EXPERT, D_MODEL), D_EXPERT ** -0.5),
        'g_ple': gain(ks[24], (L, D_MODEL)),
        'w_ple': nrm(ks[25], (L, PLE_DIM, D_MODEL), PLE_DIM ** -0.5),
        'g_ple_proj': gain(ks[26], (L, D_MODEL)),
        'w_ple_gate': nrm(ks[27], (L, D_MODEL, D_MODEL), D_MODEL ** -0.5),
        'g_final': gain(ks[28], (D_MODEL,)),
    }


def reference(x, p, g_mix, w_in, conv_dw_w, conv_dw_b, conv_ln_g, conv_ln_b,
              lru_conv_w, lru_conv_b, lru_w_r, lru_b_r, lru_w_i, lru_b_i, lru_lambda,
              w_out, g_ffn, w_group, b_group, w_expert, b_expert, w1, w3, w2,
              g_ple, w_ple, g_ple_proj, w_ple_gate, g_final):
    B, S, D = x.shape
    for i in range(DEPTH):
        h = _rmsnorm(x, g_mix[i])
        z = h @ w_in[i]
        conv_v, conv_g, lru_x, lru_g = jnp.split(
            z, [D_CONV, 2 * D_CONV, 2 * D_CONV + D_LRU], axis=-1)
        y_conv = _conformer_conv(conv_v, conv_g, conv_dw_w[i], conv_dw_b[i],
                                 conv_ln_g[i], conv_ln_b[i])
        xr = _causal_depthwise_conv(lru_x, lru_conv_w[i], lru_conv_b[i])
        y_lru = _rglru(xr, lru_w_r[i], lru_b_r[i], lru_w_i[i], lru_b_i[i],
                       lru_lambda[i]) * jax.nn.gelu(lru_g)
        x = x + jnp.concatenate([y_conv, y_lru], axis=-1) @ w_out[i]
        hf = _rmsnorm(x, g_ffn[i]).reshape(B * S, D)
        eid, gate = _hier_route(hf, w_group[i], b_group[i], w_expert[i], b_expert[i])
        x = x + _routed_experts(hf, eid, gate, w1[i], w3[i], w2[i]).reshape(B, S, D)
        e = _rmsnorm(p[i] @ w_ple[i], g_ple_proj[i])
        pg = jax.nn.sigmoid(_rmsnorm(x, g_ple[i]) @ w_ple_gate[i])
        x = x + pg * e
    return _rmsnorm(x, g_final)
```

```python
import numpy as np
from contextlib import ExitStack
import concourse.bass as bass
import concourse.mybir as mybir
from concourse.bass_utils import run_bass_kernel_spmd

F32 = mybir.dt.float32
BF16 = mybir.dt.bfloat16
I32 = mybir.dt.int32
ALU = mybir.AluOpType
AF = mybir.ActivationFunctionType
AX = mybir.AxisListType

N_CORES = 8
HOP = True
LOADLAG = 3
MC_OFF = 0
SEQ = 2048
D = 1024
EPS = 1e-6
Q = 4

CW = 0
CB = CW + 124
LG = CB + 4
LB = LG + 4
LW = LB + 4
LBB = LW + 16
BR = LBB + 4
BI = BR + 4
LAM = BI + 4
NCP = LAM + 4
D_CWH = 0
D_GH = 124
D_BH = 128
D_BRH = 132
D_BIH = 136
D_N4 = 140
D_N8 = 144
NDP = 148


class Src:
    def __init__(self, sem, name, is_dma):
        self.sem, self.name, self.is_dma, self.total = sem, name, is_dma, 0


class Eng(Src):
    def __init__(self, sem, name, blockname, same_wait=True):
        super().__init__(sem, name, False)
        self.blockname, self.ops, self.seen, self.same_wait = blockname, [], {}, same_wait


class Buf:
    def __init__(self, t, dsem=None):
        self.t, self.w, self.r, self.dsem = t, None, {}, dsem

    def __getitem__(self, k):
        return self.t[k]


class Prog:
    def __init__(self, nc, stack):
        self.nc, self.stack = nc, stack
        self.srcs = []
        mk = lambda n, b, sw=True: self._reg(Eng(self._sem("e_" + n), n, b, sw))
        self.pe = mk("pe", "tensor", False)
        self.act = mk("act", "scalar")
        self.dve = mk("dve", "vector")
        self.pool = mk("pool", "gpsimd")
        self.sp = mk("sp", "sync")
        self.engs = [self.pe, self.act, self.dve, self.pool, self.sp]
        self.nbuf = 0

    def _sem(self, name):
        return self.stack.enter_context(self.nc.semaphore(name))

    def _reg(self, s):
        self.srcs.append(s)
        return s

    def buf(self, stack, shape, dt, name=None, dma=False, psum=False):
        self.nbuf += 1
        name = "%s_%d" % (name or "b", self.nbuf)
        if psum:
            t = stack.enter_context(self.nc.psum_tensor(name, shape, dt))
        else:
            t = stack.enter_context(self.nc.sbuf_tensor(name, shape, dt))
        ds = self._reg(Src(self._sem("d_" + name), name, True)) if dma else None
        return Buf(t, ds)

    def _deps(self, eng, reads, writes):
        need = {}

        def add(src, val):
            if src.is_dma:
                val = src.total
            if need.get(src, 0) < val:
                need[src] = val

        for b in reads:
            if b.w is not None:
                add(*b.w)
        for b in writes:
            if b.w is not None:
                add(*b.w)
            for s, v in b.r.items():
                add(s, v)
        waits = []
        for src, val in need.items():
            if src is eng and not eng.same_wait:
                continue
            if eng.seen.get(src, 0) >= val:
                continue
            eng.seen[src] = val
            waits.append((src.sem, val))
        return waits

    def _mark(self, src, val, reads, writes):
        for b in reads:
            b.r[src] = val
        for b in writes:
            b.w = (src, val)
            b.r = {}

    def op(self, eng, fn, reads=(), writes=()):
        waits = self._deps(eng, reads, writes)
        eng.total += 1
        sem = eng.sem

        def emit(e):
            for s, v in waits:
                e.wait_ge(s, v)
            fn(e).then_inc(sem, 1)

        eng.ops.append(emit)
        self._mark(eng, eng.total, reads, writes)

    def dma(self, q, fn, sem_buf, reads=(), writes=()):
        src = sem_buf.dsem
        waits = self._deps(q, reads, writes)
        src.total += 16
        sem = src.sem

        def emit(e):
            for s, v in waits:
                e.wait_ge(s, v)
            fn(e).then_inc(sem, 16)

        q.ops.append(emit)
        self._mark(src, src.total, reads, writes)

    def barrier(self):
        for E in self.engs:
            waits = []
            for S in self.srcs:
                if S is E or S.total == 0:
                    continue
                if E.seen.get(S, 0) >= S.total:
                    continue
                E.seen[S] = S.total
                waits.append((S.sem, S.total))

            def emit(e, waits=waits):
                for s, v in waits:
                    e.wait_ge(s, v)

            E.ops.append(emit)

    def flush(self, block):
        for E in self.engs:
            if not E.ops:
                continue
            ops = E.ops
            E.ops = []

            def body(e, ops=ops):
                for f in ops:
                    f(e)

            getattr(block, E.blockname)(body)


def run_pipelined(gens, depth, skew):
    it = iter(gens)
    active, pending, tick = [], True, 0
    while pending or active:
        for g in list(active):
            try:
                next(g)
            except StopIteration:
                active.remove(g)
        if pending and tick % skew == 0 and len(active) < depth:
            try:
                g = next(it)
                active.append(g)
                next(g)
            except StopIteration:
                pending = False
        tick += 1


def build(NSEQ=4, CAP=640, debug=False):
    T = NSEQ * SEQ
    NT = T // 512
    NSUB = CAP // 128
    NSLOT = 32 * CAP
    TRASH = NSLOT
    nc = bass.Bass("TRN2", target_bir_lowering=False)

    def dr(name, shape, dt=F32, kind="ExternalInput"):
        return nc.dram_tensor(name, shape, dt, kind=kind).ap()

    x_d = dr("x", [T, D])
    p_d = dr("p", [T, 256])
    win_d = dr("w_in", [D, 2048])
    wout_d = dr("w_out", [D, D])
    wr_d = dr("w_route", [D, 36])
    gbd_d = dr("gate_bd", [4, 128, 256])
    cpar_d = dr("cpar", [128, NCP])
    gbc_d = dr("gbc", [3, 128, 8])
    rowbc_d = dr("rowbc", [2, 128, D])
    rb_d = dr("rbias", [128, Q * 36])
    iota_d = dr("iota_e", [128, Q * 32])
    ident_d = dr("ident", [128, 128])
    tri_d = dr("tri", [128, 128])
    w1_d = dr("w1", [32, D, 512])
    w3_d = dr("w3", [32, D, 512])
    w2_d = dr("w2", [32, 512, D])
    wple_d = dr("w_ple", [256, D])
    wpg_d = dr("w_ple_gate", [D, D])
    out_d = dr("out", [T, D], kind="ExternalOutput")
    sk = "ExternalOutput" if debug else "Internal"
    x1_d = dr("x1s", [T, D], kind=sk)
    hs_d = dr("hss", [NSLOT + 128, D], BF16, kind=sk)
    y_d = dr("yss", [NSLOT + 128, D], kind=sk)
    if debug:
        ri_d = dr("rinfo_o", [128, NT * 4 * Q], kind="ExternalOutput")

    IOA = bass.IndirectOffsetOnAxis

    with ExitStack() as top:
        P = Prog(nc, top)
        pe, act, dve, pool, sp = P.pe, P.act, P.dve, P.pool, P.sp
        block = top.enter_context(nc.Block())

        pmm = [P.buf(top, [128, 512], F32, "pmm", psum=True) for _ in range(4)]
        ptr = P.buf(top, [128, 1024], BF16, "ptr", psum=True)
        ptr2 = P.buf(top, [128, 1024], BF16, "ptr2", psum=True)
        ps1 = P.buf(top, [128, 512], F32, "ps1", psum=True)
        ps2 = P.buf(top, [128, 512], F32, "ps2", psum=True)
        pmm_i = [0]

        def next_pmm():
            b = pmm[pmm_i[0] % 4]
            pmm_i[0] += 1
            return b

        rinfo = P.buf(top, [128, NT, 4, Q], F32, "rinfo")
        sloti = P.buf(top, [128, NT, 2, Q], I32, "sloti")
        if debug:
            rinfo.dsem = P._reg(Src(P._sem("d_rinfo"), "rinfo", True))
        ident = P.buf(top, [128, 128], BF16, "ident", dma=True)
        P.dma(pool, lambda e: e.dma_start(out=ident[:, :], in_=ident_d), ident, writes=[ident])

        def transposes(src, n, dst_ps):
            def fn(e):
                ins = None
                for c in range(n):
                    ins = e.transpose(dst_ps[:, c * 128:(c + 1) * 128], src[:, c * 128:(c + 1) * 128], ident[:, :])
                return ins
            P.op(pe, fn, reads=[src, ident], writes=[dst_ps])

        def rstd_from_ss(ss, out, n):
            P.op(dve, lambda e: e.tensor_scalar(out, ss, 1.0 / n, EPS, ALU.mult, ALU.add), reads=[], writes=[])

        with ExitStack() as sa:
            B = lambda shape, dt=F32, name=None, dma=False: P.buf(sa, shape, dt, name, dma)
            win = B([128, 8, 2048], BF16, "win", dma=True)
            wout = B([128, 8, D], BF16, "wout", dma=True)
            wr = B([128, 8, 36], BF16, "wr", dma=True)
            gbd = B([128, 4, 256], BF16, "gbd", dma=True)
            tri = B([128, 128], BF16, "tri", dma=True)
            cpar = B([128, NCP], F32, "cpar", dma=True)
            dpar = B([128, NDP], F32, "dpar")
            gmixbc = B([128, 8], F32, "gmixbc", dma=True)
            gffnbc = B([128, 8], F32, "gffnbc", dma=True)
            rbias = B([128, Q, 36], F32, "rbias", dma=True)
            iota = B([128, Q, 32], F32, "iota", dma=True)
            ones = B([128, 128], BF16, "ones")
            cntbc = B([128, 32], F32, "cntbc")

            P.dma(sp, lambda e: e.dma_start(out=cpar[:, :], in_=cpar_d), cpar, writes=[cpar])
            P.dma(sp, lambda e: e.dma_start(out=gmixbc[:, :], in_=gbc_d[0]), gmixbc, writes=[gmixbc])
            P.dma(sp, lambda e: e.dma_start(out=gffnbc[:, :], in_=gbc_d[1]), gffnbc, writes=[gffnbc])
            P.dma(sp, lambda e: e.dma_start(out=rbias[:, :, :], in_=rb_d.rearrange("p (q n) -> p q n", q=Q)), rbias, writes=[rbias])
            P.dma(sp, lambda e: e.dma_start(out=iota[:, :, :], in_=iota_d.rearrange("p (q n) -> p q n", q=Q)), iota, writes=[iota])
            P.dma(pool, lambda e: e.dma_start(out=tri[:, :], in_=tri_d), tri, writes=[tri])
            for k in range(8):
                P.dma(pool, lambda e, k=k: e.dma_start(out=win[:, k, :], in_=win_d[k * 128:(k + 1) * 128, :]), win, writes=[win])
            P.dma(pool, lambda e: e.dma_start(out=gbd[:, :, :], in_=gbd_d.rearrange("c p n -> p c n")), gbd, writes=[gbd])
            for k in range(8):
                P.dma(pool, lambda e, k=k: e.dma_start(out=wout[:, k, :], in_=wout_d[k * 128:(k + 1) * 128, :]), wout, writes=[wout])
            P.dma(pool, lambda e: e.dma_start(out=wr[:, :, :], in_=wr_d.rearrange("(k p) n -> p k n", p=128)), wr, writes=[wr])

            P.op(dve, lambda e: e.memset(ones[:, :], 1.0), writes=[ones])
            P.op(dve, lambda e: e.memset(cntbc[:, :], 0.0), writes=[cntbc])
            for k in range(8):
                P.op(dve, lambda e, k=k: e.tensor_scalar(win[:, k, :], win[:, k, :], gmixbc[:, k:k + 1], None, ALU.mult), reads=[win, gmixbc], writes=[win])
                P.op(dve, lambda e, k=k: e.tensor_scalar(wr[:, k, :], wr[:, k, :], gffnbc[:, k:k + 1], None, ALU.mult), reads=[wr, gffnbc], writes=[wr])

            tsm = B([128, 8, 4], F32, "tsm")

            def dv(fn, reads, writes):
                P.op(dve, fn, reads=reads, writes=writes)

            dv(lambda e: e.tensor_scalar(dpar[:, D_CWH:D_CWH + 124], cpar[:, CW:CW + 124], 0.5, None, ALU.mult), [cpar], [dpar])
            dv(lambda e: e.tensor_scalar(dpar[:, D_GH:D_GH + 8], cpar[:, LG:LG + 8], 0.5, None, ALU.mult), [cpar], [dpar])
            dv(lambda e: e.tensor_scalar(dpar[:, D_BRH:D_BRH + 8], cpar[:, BR:BR + 8], 0.5, None, ALU.mult), [cpar], [dpar])
            z_, az, ee, LL, tt, mk_, zp = [tsm[:, i, :] for i in range(7)]
            dv(lambda e: e.tensor_scalar(z_, cpar[:, LAM:LAM + 4], -1.0, None, ALU.mult), [cpar], [tsm])
            dv(lambda e: e.tensor_tensor(az, z_, cpar[:, LAM:LAM + 4], ALU.max), [tsm, cpar], [tsm])
            P.op(act, lambda e: e.activation(out=ee, in_=az, func=AF.Exp, scale=-1.0), reads=[tsm], writes=[tsm])
            P.op(act, lambda e: e.activation(out=LL, in_=ee, func=AF.Ln, bias=1.0, scale=1.0), reads=[tsm], writes=[tsm])
            dv(lambda e: e.tensor_scalar(tt, ee, -0.25, 1.0 / 3.0, ALU.mult, ALU.add), [tsm], [tsm])
            dv(lambda e: e.tensor_tensor(tt, tt, ee, ALU.mult), [tsm], [tsm])
            dv(lambda e: e.tensor_scalar(tt, tt, -1.0, 0.5, ALU.mult, ALU.add), [tsm], [tsm])
            dv(lambda e: e.tensor_tensor(tt, tt, ee, ALU.mult), [tsm], [tsm])
            dv(lambda e: e.tensor_scalar(tt, tt, -1.0, 1.0, ALU.mult, ALU.add), [tsm], [tsm])
            dv(lambda e: e.tensor_tensor(tt, tt, ee, ALU.mult), [tsm], [tsm])
            dv(lambda e: e.tensor_single_scalar(mk_, ee, 0.05, ALU.is_lt), [tsm], [tsm])
            dv(lambda e: e.tensor_tensor(tt, tt, LL, ALU.subtract), [tsm], [tsm])
            dv(lambda e: e.tensor_tensor(tt, tt, mk_, ALU.mult), [tsm], [tsm])
            dv(lambda e: e.tensor_tensor(tt, tt, LL, ALU.add), [tsm], [tsm])
            dv(lambda e: e.tensor_single_scalar(zp, z_, 0.0, ALU.max), [tsm], [tsm])
            dv(lambda e: e.tensor_tensor(tt, tt, zp, ALU.add), [tsm], [tsm])
            dv(lambda e: e.tensor_scalar(dpar[:, D_N4:D_N4 + 4], tt, -4.0, None, ALU.mult), [tsm], [dpar])
            dv(lambda e: e.tensor_scalar(dpar[:, D_N8:D_N8 + 4], tt, -8.0, None, ALU.mult), [tsm], [dpar])

            xa = [B([128, D], F32, "xa", dma=True) for _ in range(2)]
            ssF = [B([128, 8], F32, "ssF") for _ in range(2)]
            xn = [B([128, D], BF16, "xn") for _ in range(2)]
            hT = B([128, 8, 512], BF16, "hT")
            gth = [B([128, 512], F32, "gth") for _ in range(2)]
            gvs = [B([128, 512], F32, "gvs") for _ in range(2)]
            ga = B([128, 512], F32, "ga")
            gb = B([128, 512], F32, "gb")
            ub = [[B([128, 542], BF16, "ub") for _ in range(4)] for _ in range(2)]
            xbuf = [[B([128, 515], BF16, "xbuf") for _ in range(4)] for _ in range(2)]
            qg = [[B([128, 512], BF16, "qg") for _ in range(4)] for _ in range(2)]
            acc2 = [[B([128, 512], F32, "acc") for _ in range(4)] for _ in range(2)]
            cvbf = [B([128, 512], BF16, "cvbf") for _ in range(4)]
            sqbf = [B([128, 512], BF16, "sqbf") for _ in range(4)]
            mean = B([128, 512], F32, "mean")
            msq = B([128, 512], F32, "msq")
            xr = [B([128, 512], F32, "xr") for _ in range(2)]
            xrbf = [B([128, 512], BF16, "xrbf") for _ in range(2)]
            t1 = [B([128, 512], F32, "t1") for _ in range(2)]
            t2 = [B([128, 512], F32, "t2") for _ in range(2)]
            t3 = [B([128, 512], F32, "t3") for _ in range(2)]
            lh, th = t1, t2
            ab = [B([128, 512], F32, "ab")] * 2
            hb = [B([128, 512], F32, "hb")] * 2
            carry = [B([128, 1], F32, "carry") for _ in range(4)]
            yT = [B([128, 8, 512], BF16, "yT") for _ in range(2)]
            xb = B([128, D], F32, "xb", dma=True)
            ssE = [B([128, 8], F32, "ssE") for _ in range(2)]
            x1 = [B([128, D], F32, "x1", dma=True) for _ in range(2)]
            hfn = [B([128, D], BF16, "hfn", dma=True) for _ in range(4)]
            hfT = [B([128, 8, 128], BF16, "hfT") for _ in range(2)]
            rs = B([128, 40, Q], F32, "rs")
            r36 = B([128, Q, 36], F32, "r36")
            r32 = [B([128, Q, 32], F32, "r32") for _ in range(5)]
            mbf = B([128, Q, 32], BF16, "mbf")
            r8 = [B([128, Q, 8], F32, "r8") for _ in range(4)]
            r4 = [B([128, Q, 4], F32, "r4") for _ in range(3)]
            print("phase A sbuf remaining", nc.sbuf_bytes_remaining)

            cwh = lambda c, k: dpar[:, D_CWH + c * 31 + k:D_CWH + c * 31 + k + 1]
            NJ = SEQ // 512

            def rsqA(ssb, i_v, i_l, i_o):
                P.op(act, lambda e: e.activation(out=ssb[:, i_l:i_l + 1], in_=ssb[:, i_v:i_v + 1], func=AF.Ln), reads=[ssb], writes=[ssb])
                P.op(act, lambda e: e.activation(out=ssb[:, i_o:i_o + 1], in_=ssb[:, i_l:i_l + 1], func=AF.Exp, scale=-0.5), reads=[ssb], writes=[ssb])

            def evac_scaled(dst3, ps, gvec):
                P.op(act, lambda e: e.activation(out=dst3.all(), in_=ps[:, :].rearrange("p (c j) -> p c j", c=8), func=AF.Copy),
                     reads=[ps], writes=[dst3.buf])

            class View3:
                def __init__(self, buf, fn, allfn=None):
                    self.buf, self.fn, self.all = buf, fn, allfn

                def __call__(self, c):
                    return self.fn(c)

            def gen_F(ti):
                s_, j = divmod(ti, NJ)
                row0 = ti * 512
                par = ti % 2
                ub_, xbuf_, qg_ = ub[par], xbuf[par], qg[par]
                if j == 0:
                    for c in range(4):
                        P.op(pool, lambda e, c=c: e.memset(ub_[c][:, 0:30], 0.0), writes=[ub_[c]])
                        P.op(pool, lambda e, c=c: e.memset(xbuf_[c][:, 0:3], 0.0), writes=[xbuf_[c]])
                for q in range(Q):
                    yield ("SEG" if q % 2 == 0 else "CHAIN")
                    xt, xnb, ss = xa[q % 2], xn[q % 2], ssF[q % 2]
                    r0 = row0 + q * 128
                    P.dma(sp, lambda e, xt=xt, r0=r0: e.dma_start(out=xt[:, :], in_=x_d[r0:r0 + 128, :]), xt, writes=[xt])
                    P.op(act, lambda e, xt=xt, ss=ss, xnb=xnb: e.activation(out=xnb[:, :], in_=xt[:, :], func=AF.Square, accum_out=ss[:, 0:1]),
                         reads=[xt], writes=[xnb, ss])
                    P.op(dve, lambda e, ss=ss: e.tensor_scalar(ss[:, 1:2], ss[:, 0:1], 1.0 / D, EPS, ALU.mult, ALU.add), reads=[ss], writes=[ss])
                    rsqA(ss, 1, 3, 2)
                    P.op(act, lambda e, xt=xt, xnb=xnb, ss=ss: e.activation(out=xnb[:, :], in_=xt[:, :], func=AF.Copy, scale=ss[:, 2:3]),
                         reads=[xt, ss], writes=[xnb])
                    transposes(xnb, 8, ptr)
                    evac_scaled(View3(hT, None, lambda q=q: hT[:, :, q * 128:(q + 1) * 128]), ptr, gmixbc)

                fbank = [pmm[0], ps2]

                def zmm(m, bi):
                    pb = fbank[bi % 2]

                    def fn(e, m=m, pb=pb):
                        ins = None
                        for k in range(8):
                            ins = e.matmul(pb[:, :], win[:, k, m * 128:(m + 1) * 128], hT[:, k, :], start=(k == 0), stop=(k == 7))
                        return ins
                    P.op(pe, fn, reads=[win, hT], writes=[pb])
                    return pb

                for c in range(4):
                    yield ("SEG" if c % 2 == 0 else "CHAIN")
                    pg = zmm(4 + c, c)
                    ta, vs = gth[c % 2], gvs[c % 2]
                    P.op(act, lambda e, pg=pg, ta=ta: e.activation(out=ta[:, :], in_=pg[:, :], func=AF.Tanh, scale=0.5), reads=[pg], writes=[ta])
                    pv = zmm(c, c)
                    P.op(act, lambda e, pv=pv, vs=vs: e.activation(out=vs[:, :], in_=pv[:, :], func=AF.Copy), reads=[pv], writes=[vs])
                    P.op(pool, lambda e, ta=ta, vs=vs: e.tensor_tensor(ta[:, :], ta[:, :], vs[:, :], ALU.mult), reads=[ta, vs], writes=[ta])
                    P.op(pool, lambda e, ta=ta, vs=vs, c=c: e.tensor_tensor(ub_[c][:, 30:542], ta[:, :], vs[:, :], ALU.add), reads=[ta, vs], writes=[ub_[c]])
                for c in range(4):
                    yield ("SEG" if c % 2 == 0 else "CHAIN")
                    px = zmm(8 + c, c)
                    P.op(act, lambda e, px=px, c=c: e.activation(out=xbuf_[c][:, 3:515], in_=px[:, :], func=AF.Copy), reads=[px], writes=[xbuf_[c]])
                for c in range(4):
                    yield "SEG"
                    pgl = zmm(12 + c, c)
                    P.op(act, lambda e, pgl=pgl: e.activation(out=ga[:, :], in_=pgl[:, :], func=AF.Copy), reads=[pgl], writes=[ga])
                    P.op(act, lambda e, pgl=pgl: e.activation(out=gb[:, :], in_=pgl[:, :], func=AF.Square), reads=[pgl], writes=[gb])
                    P.op(act, lambda e: e.activation(out=gb[:, :], in_=gb[:, :], func=AF.Identity, bias=1.0, scale=0.044715), reads=[gb], writes=[gb])
                    P.op(pool, lambda e: e.tensor_tensor(gb[:, :], gb[:, :], ga[:, :], ALU.mult), reads=[ga, gb], writes=[gb])
                    P.op(act, lambda e: e.activation(out=gb[:, :], in_=gb[:, :], func=AF.Tanh, scale=0.7978845608028654), reads=[gb], writes=[gb])
                    P.op(act, lambda e: e.activation(out=gb[:, :], in_=gb[:, :], func=AF.Identity, bias=1.0, scale=1.0), reads=[gb], writes=[gb])
                    P.op(pool, lambda e, c=c: e.tensor_tensor(qg_[c][:, :], gb[:, :], ga[:, :], ALU.mult), reads=[ga, gb], writes=[qg_[c]])

            def gen_Mc(ti):
                s_, j = divmod(ti, NJ)
                par = ti % 2
                ub_, xbuf_, qg_, yT_ = ub[par], xbuf[par], qg[par], yT[par]
                ubn, xbufn = ub[1 - par], xbuf[1 - par]
                acc = acc2[par]
                for k in range(31):
                    for c in range(4):
                        if k == 0:
                            P.op(dve, lambda e, c=c: e.tensor_scalar(acc[c][:, :], ub_[c][:, 0:512], cwh(c, 0), cpar[:, CB + c:CB + c + 1], ALU.mult, ALU.add),
                                 reads=[ub_[c], dpar, cpar], writes=[acc[c]])
                        else:
                            P.op(dve, lambda e, c=c, k=k: e.scalar_tensor_tensor(acc[c][:, :], ub_[c][:, k:k + 512], cwh(c, k), acc[c][:, :], ALU.mult, ALU.add),
                                 reads=[ub_[c], dpar, acc[c]], writes=[acc[c]])
                    yield
                for c in range(4):
                    if j < NJ - 1:
                        P.op(pool, lambda e, c=c: e.tensor_copy(ubn[c][:, 0:30], ub_[c][:, 512:542]), reads=[ub_[c]], writes=[ubn[c]])
                    P.op(act, lambda e, c=c: e.activation(out=cvbf[c][:, :], in_=acc[c][:, :], func=AF.Copy), reads=[acc[c]], writes=[cvbf[c]])
                    P.op(act, lambda e, c=c: e.activation(out=sqbf[c][:, :], in_=acc[c][:, :], func=AF.Square), reads=[acc[c]], writes=[sqbf[c]])

                def stat_mm(dst, srcs):
                    def fn(e):
                        ins = None
                        for c in range(4):
                            ins = e.matmul(dst[:, :], ones[:, :], srcs[c][:, :], start=(c == 0), stop=(c == 3))
                        return ins
                    P.op(pe, fn, reads=[ones] + srcs, writes=[dst])
                stat_mm(ps1, cvbf)
                P.op(act, lambda e: e.activation(out=mean[:, :], in_=ps1[:, :], func=AF.Copy, scale=1.0 / 512), reads=[ps1], writes=[mean])
                stat_mm(ps1, sqbf)
                P.op(pool, lambda e: e.tensor_tensor(msq[:, :], mean[:, :], mean[:, :], ALU.mult), reads=[mean], writes=[msq])
                P.op(dve, lambda e: e.scalar_tensor_tensor(msq[:, :], ps1[:, :], 1.0 / 512, msq[:, :], ALU.mult, ALU.subtract), reads=[ps1, msq], writes=[msq])
                P.op(dve, lambda e: e.tensor_scalar(msq[:, :], msq[:, :], EPS, None, ALU.add), reads=[msq], writes=[msq])
                P.op(act, lambda e: e.activation(out=msq[:, :], in_=msq[:, :], func=AF.Ln), reads=[msq], writes=[msq])
                P.op(act, lambda e: e.activation(out=msq[:, :], in_=msq[:, :], func=AF.Exp, scale=-0.5), reads=[msq], writes=[msq])
                yield
                for c in range(4):
                    P.op(pool, lambda e, c=c: e.tensor_tensor(acc[c][:, :], acc[c][:, :], mean[:, :], ALU.subtract), reads=[acc[c], mean], writes=[acc[c]])
                    P.op(pool, lambda e, c=c: e.tensor_tensor(acc[c][:, :], acc[c][:, :], msq[:, :], ALU.mult), reads=[acc[c], msq], writes=[acc[c]])
                for c in range(4):
                    t_ = (mean, msq)[c % 2]
                    P.op(act, lambda e, c=c: e.activation(out=acc[c][:, :], in_=acc[c][:, :], func=AF.Identity,
                                                          bias=dpar[:, D_BH + c:D_BH + c + 1], scale=dpar[:, D_GH + c:D_GH + c + 1]),
                         reads=[acc[c], dpar], writes=[acc[c]])
                    P.op(act, lambda e, c=c, t_=t_: e.activation(out=t_[:, :], in_=acc[c][:, :], func=AF.Tanh), reads=[acc[c]], writes=[t_])
                    P.op(dve, lambda e, c=c, t_=t_: e.scalar_tensor_tensor(yT_[:, c, :], t_[:, :], 1.0, acc[c][:, :], ALU.add, ALU.mult),
                         reads=[acc[c], t_], writes=[yT_])
            def gen_Ml(ti):
                s_, j = divmod(ti, NJ)
                par = ti % 2
                xbuf_, qg_, yT_ = xbuf[par], qg[par], yT[par]
                xbufn = xbuf[1 - par]
                if j == 0:
                    for c in range(4):
                        P.op(pool, lambda e, c=c: e.memset(carry[c][:, :], 0.0), writes=[carry[c]])
                for c in range(4):
                    xr_, xrb_ = xr[c % 2], xrbf[c % 2]
                    lw = lambda k, c=c: cpar[:, LW + c * 4 + k:LW + c * 4 + k + 1]
                    P.op(dve, lambda e, c=c, xr_=xr_, lw=lw: e.tensor_scalar(xr_[:, :], xbuf_[c][:, 0:512], lw(0), cpar[:, LBB + c:LBB + c + 1], ALU.mult, ALU.add),
                         reads=[xbuf_[c], cpar], writes=[xr_])
                    for k in range(1, 4):
                        P.op(dve, lambda e, c=c, k=k, xr_=xr_, lw=lw: e.scalar_tensor_tensor(xr_[:, :], xbuf_[c][:, k:k + 512], lw(k), xr_[:, :], ALU.mult, ALU.add),
                             reads=[xbuf_[c], cpar, xr_], writes=[xr_])
                    if j < NJ - 1:
                        P.op(pool, lambda e, c=c: e.tensor_copy(xbufn[c][:, 0:3], xbuf_[c][:, 512:515]), reads=[xbuf_[c]], writes=[xbufn[c]])
                    P.op(act, lambda e, xr_=xr_, xrb_=xrb_: e.activation(out=xrb_[:, :], in_=xr_[:, :], func=AF.Copy), reads=[xr_], writes=[xrb_])
                    pr, pi = pmm[1], pmm[1]
                    P.op(pe, lambda e, c=c, pr=pr, xrb_=xrb_: e.matmul(pr[:, :], gbd[:, c, 0:128], xrb_[:, :], start=True, stop=True), reads=[gbd, xrb_], writes=[pr])
                    a1, a2, a3, aa, hh = t1[c % 2], t2[c % 2], t3[c % 2], ab[c % 2], hb[c % 2]
                    dp = lambda o, c=c: dpar[:, o + c:o + c + 1]
                    yield
                    P.op(act, lambda e, pr=pr, a1=a1, dp=dp: e.activation(out=a1[:, :], in_=pr[:, :], func=AF.Tanh, bias=dp(D_BRH), scale=0.5), reads=[pr, dpar], writes=[a1])
                    P.op(pe, lambda e, c=c, pi=pi, xrb_=xrb_: e.matmul(pi[:, :], gbd[:, c, 128:256], xrb_[:, :], start=True, stop=True), reads=[gbd, xrb_], writes=[pi])
                    P.op(act, lambda e, pi=pi, a3=a3, dp=dp: e.activation(out=a3[:, :], in_=pi[:, :], func=AF.Tanh, bias=dp(D_BIH), scale=0.5), reads=[pi, dpar], writes=[a3])
                    P.op(act, lambda e, a1=a1, aa=aa, dp=dp: e.activation(out=aa[:, :], in_=a1[:, :], func=AF.Exp, bias=dp(D_N4), scale=dp(D_N4)), reads=[a1, dpar], writes=[aa])
                    P.op(act, lambda e, a1=a1, a2=a2, dp=dp: e.activation(out=a2[:, :], in_=a1[:, :], func=AF.Exp, bias=dp(D_N8), scale=dp(D_N8)), reads=[a1, dpar], writes=[a2])
                    P.op(dve, lambda e, a2=a2: e.tensor_scalar(a2[:, :], a2[:, :], 0.99999994, -1.0, ALU.min, ALU.mult), reads=[a2], writes=[a2])
                    P.op(act, lambda e, a2=a2: e.activation(out=a2[:, :], in_=a2[:, :], func=AF.Ln, bias=1.0, scale=1.0), reads=[a2], writes=[a2])
                    P.op(act, lambda e, a2=a2: e.activation(out=a2[:, :], in_=a2[:, :], func=AF.Exp, scale=0.5), reads=[a2], writes=[a2])
                    P.op(dve, lambda e, a3=a3, xr_=xr_: e.scalar_tensor_tensor(a3[:, :], a3[:, :], 1.0, xr_[:, :], ALU.add, ALU.mult), reads=[a3, xr_], writes=[a3])
                    yield
                    P.op(dve, lambda e, a2=a2, a3=a3: e.tensor_tensor(a3[:, :], a3[:, :], a2[:, :], ALU.mult), reads=[a2, a3], writes=[a3])
                    P.op(dve, lambda e, c=c, aa=aa, a3=a3, hh=hh: e.tensor_tensor_scan(hh[:, :], aa[:, :], a3[:, :], carry[c][:, 0:1], ALU.mult, ALU.add),
                         reads=[aa, a3, carry[c]], writes=[hh])
                    P.op(dve, lambda e, c=c, hh=hh: e.tensor_copy(carry[c][:, :], hh[:, 511:512]), reads=[hh], writes=[carry[c]])
                    P.op(dve, lambda e, c=c, hh=hh: e.scalar_tensor_tensor(yT_[:, 4 + c, :], hh[:, :], 0.25, qg_[c][:, :], ALU.mult, ALU.mult),
                         reads=[hh, qg_[c]], writes=[yT_])
                    yield

            def gen_E(ti):
                row0 = ti * 512
                yT_ = yT[ti % 2]
                for q in range(Q):
                    yield ("SEG" if q % 2 == 0 else "CHAIN")
                    r0 = row0 + q * 128
                    x1t, ss = x1[q % 2], ssE[q % 2]
                    hf = hfn[q]
                    hft = hfT[q % 2]
                    P.dma(sp, lambda e, r0=r0: e.dma_start(out=xb[:, :], in_=x_d[r0:r0 + 128, :]), xb, writes=[xb])
                    for h in range(2):
                        pb = pmm[2]

                        def fn(e, pb=pb, h=h, q=q):
                            ins = None
                            for k in range(8):
                                ins = e.matmul(pb[:, :], yT_[:, k, q * 128:(q + 1) * 128], wout[:, k, h * 512:(h + 1) * 512], start=(k == 0), stop=(k == 7))
                            return ins
                        P.op(pe, fn, reads=[yT_, wout], writes=[pb])
                        P.op(dve, lambda e, pb=pb, h=h, x1t=x1t: e.tensor_tensor(x1t[:, h * 512:(h + 1) * 512], pb[:, :], xb[:, h * 512:(h + 1) * 512], ALU.add),
                             reads=[pb, xb], writes=[x1t])
                    P.dma(sp, lambda e, x1t=x1t, r0=r0: e.dma_start(out=x1_d[r0:r0 + 128, :], in_=x1t[:, :]), x1t, reads=[x1t])
                    P.op(act, lambda e, x1t=x1t, ss=ss, hf=hf: e.activation(out=hf[:, :], in_=x1t[:, :], func=AF.Square, accum_out=ss[:, 0:1]), reads=[x1t], writes=[hf, ss])
                    P.op(dve, lambda e, ss=ss: e.tensor_scalar(ss[:, 1:2], ss[:, 0:1], 1.0 / D, EPS, ALU.mult, ALU.add), reads=[ss], writes=[ss])
                    rsqA(ss, 1, 3, 2)
                    P.op(act, lambda e, x1t=x1t, hf=hf, ss=ss: e.activation(out=hf[:, :], in_=x1t[:, :], func=AF.Copy, scale=ss[:, 2:3]), reads=[x1t, ss], writes=[hf])
                    transposes(hf, 8, ptr2)
                    evac_scaled(View3(hft, None, lambda hft=hft: hft[:, :, :]), ptr2, gffnbc)

                    def fnl(e, hft=hft, q=q):
                        ins = None
                        for k in range(8):
                            ins = e.matmul(pmm[3][:, q * 36:(q + 1) * 36], hft[:, k, :], wr[:, k, :], start=(k == 0), stop=(k == 7))
                        return ins
                    P.op(pe, fnl, reads=[hft, wr], writes=[pmm[3]])

                yield "SEG"
                S = lambda i: rs[:, i, :]
                bc = lambda ap, n: ap.unsqueeze(2).broadcast_to([128, Q, n])
                lgb = r36
                P.op(dve, lambda e: e.tensor_tensor(lgb[:, :, :], pmm[3][:, 0:Q * 36].rearrange("p (q n) -> p q n", q=Q), rbias[:, :, :], ALU.add),
                     reads=[pmm[3], rbias], writes=[lgb])
                gmask, gsh, gex = r4
                P.op(dve, lambda e: e.tensor_reduce(S(0), lgb[:, :, 0:4], AX.X, ALU.max), reads=[lgb], writes=[rs])
                P.op(dve, lambda e: e.tensor_tensor(gmask[:, :, :], lgb[:, :, 0:4], bc(S(0), 4), ALU.is_equal), reads=[lgb, rs], writes=[gmask])
                P.op(dve, lambda e: e.tensor_tensor(gsh[:, :, :], lgb[:, :, 0:4], bc(S(0), 4), ALU.subtract), reads=[lgb, rs], writes=[gsh])
                P.op(act, lambda e: e.activation(out=gex[:, :, :], in_=gsh[:, :, :], func=AF.Exp), reads=[gsh], writes=[gex])
                P.op(dve, lambda e: e.tensor_reduce(S(1), gex[:, :, :], AX.X, ALU.add), reads=[gex], writes=[rs])
                P.op(dve, lambda e: e.reciprocal(S(2), S(1)), reads=[rs], writes=[rs])
                le4 = lgb[:, :, 4:36].rearrange("p q (g j) -> p q g j", g=4)
                tmp32 = r32[0]
                P.op(dve, lambda e: e.tensor_tensor(tmp32[:, :, :].rearrange("p q (g j) -> p q g j", g=4), le4,
                                                    gmask[:, :, :].unsqueeze(3).broadcast_to([128, Q, 4, 8]), ALU.mult), reads=[lgb, gmask], writes=[tmp32])
                sel, top8, oh1, oh2 = r8
                P.op(dve, lambda e: e.tensor_reduce(sel[:, :, :], tmp32[:, :, :].rearrange("p q (g j) -> p q j g", g=4), AX.X, ALU.add), reads=[tmp32], writes=[sel])
                yield
                for q in range(Q):
                    P.op(dve, lambda e, q=q: e.max(top8[:, q, :], sel[:, q, :]), reads=[sel], writes=[top8])
                P.op(dve, lambda e: e.tensor_tensor(oh1[:, :, :], sel[:, :, :], top8[:, :, 0:1].broadcast_to([128, Q, 8]), ALU.is_equal), reads=[sel, top8], writes=[oh1])
                P.op(dve, lambda e: e.tensor_tensor(oh2[:, :, :], sel[:, :, :], top8[:, :, 1:2].broadcast_to([128, Q, 8]), ALU.is_equal), reads=[sel, top8], writes=[oh2])
                P.op(dve, lambda e: e.tensor_tensor(S(3), top8[:, :, 1], top8[:, :, 0], ALU.subtract), reads=[top8], writes=[rs])
                P.op(act, lambda e: e.activation(out=S(4), in_=S(3), func=AF.Exp), reads=[rs], writes=[rs])
                P.op(dve, lambda e: e.tensor_scalar(S(5), S(4), 1.0, None, ALU.add), reads=[rs], writes=[rs])
                P.op(dve, lambda e: e.reciprocal(S(6), S(5)), reads=[rs], writes=[rs])
                P.op(dve, lambda e: e.tensor_tensor(S(7), S(6), S(2), ALU.mult), reads=[rs], writes=[rs])
                P.op(dve, lambda e: e.tensor_tensor(S(8), S(2), S(7), ALU.subtract), reads=[rs], writes=[rs])
                E1, E2 = r32[1], r32[2]
                for Ek, oh in ((E1, oh1), (E2, oh2)):
                    P.op(dve, lambda e, Ek=Ek, oh=oh: e.tensor_tensor(Ek[:, :, :].rearrange("p q (g j) -> p q g j", g=4),
                                                                      gmask[:, :, :].unsqueeze(3).broadcast_to([128, Q, 4, 8]),
                                                                      oh[:, :, :].unsqueeze(2).broadcast_to([128, Q, 4, 8]), ALU.mult),
                         reads=[gmask, oh], writes=[Ek])
                P.op(dve, lambda e: e.tensor_tensor(mbf[:, :, :], E1[:, :, :], E2[:, :, :], ALU.add), reads=[E1, E2], writes=[mbf])

                def fnc(e):
                    ins = None
                    for q in range(Q):
                        ins = e.matmul(pmm[3][:, 160 + q * 32:160 + (q + 1) * 32], tri[:, :], mbf[:, q, :], start=True, stop=(q == 0))
                        for q2 in range(q):
                            ins = e.matmul(pmm[3][:, 160 + q * 32:160 + (q + 1) * 32], ones[:, :], mbf[:, q2, :], start=False, stop=(q2 == q - 1))
                    for q in range(Q):
                        ins = e.matmul(pmm[3][:, 288:320], ones[:, :], mbf[:, q, :], start=(q == 0), stop=(q == Q - 1))
                    return ins
                P.op(pe, fnc, reads=[tri, ones, mbf], writes=[pmm[3]])
                yield
                tot = r32[3]
                P.op(dve, lambda e: e.tensor_tensor(tot[:, :, :], pmm[3][:, 160:288].rearrange("p (q n) -> p q n", q=Q),
                                                    cntbc[:, :].unsqueeze(1).broadcast_to([128, Q, 32]), ALU.add), reads=[pmm[3], cntbc], writes=[tot])
                P.op(dve, lambda e: e.tensor_tensor(cntbc[:, :], cntbc[:, :], pmm[3][:, 288:320], ALU.add), reads=[pmm[3], cntbc], writes=[cntbc])
                tm = r32[4]
                for kk, Ek in ((0, E1), (1, E2)):
                    P.op(dve, lambda e, Ek=Ek: e.tensor_tensor(tm[:, :, :], Ek[:, :, :], tot[:, :, :], ALU.mult), reads=[Ek, tot], writes=[tm])
                    P.op(dve, lambda e, kk=kk: e.tensor_reduce(S(10 + kk), tm[:, :, :], AX.X, ALU.add), reads=[tm], writes=[rs])
                    P.op(dve, lambda e, Ek=Ek: e.tensor_tensor(tm[:, :, :], Ek[:, :, :], iota[:, :, :], ALU.mult), reads=[Ek, iota], writes=[tm])
                    P.op(dve, lambda e, kk=kk: e.tensor_reduce(S(12 + kk), tm[:, :, :], AX.X, ALU.add), reads=[tm], writes=[rs])
                    P.op(dve, lambda e, kk=kk: e.scalar_tensor_tensor(S(14 + kk), S(12 + kk), float(CAP), S(10 + kk), ALU.mult, ALU.add), reads=[rs], writes=[rs])
                    P.op(dve, lambda e, kk=kk: e.tensor_single_scalar(S(16 + kk), S(10 + kk), float(CAP), ALU.is_lt), reads=[rs], writes=[rs])
                    P.op(dve, lambda e, kk=kk: e.tensor_scalar(S(14 + kk), S(14 + kk), float(-TRASH), None, ALU.add), reads=[rs], writes=[rs])
                    P.op(dve, lambda e, kk=kk: e.tensor_tensor(S(14 + kk), S(14 + kk), S(16 + kk), ALU.mult), reads=[rs], writes=[rs])
                    P.op(dve, lambda e, kk=kk: e.tensor_scalar(S(14 + kk), S(14 + kk), float(TRASH), 0.0, ALU.add, ALU.max), reads=[rs], writes=[rs])
                    P.op(dve, lambda e, kk=kk: e.tensor_scalar(rinfo[:, ti, kk, :], S(14 + kk), float(TRASH), None, ALU.min), reads=[rs], writes=[rinfo])
                    P.op(dve, lambda e, kk=kk: e.tensor_tensor(rinfo[:, ti, 2 + kk, :], S(7 + kk), S(16 + kk), ALU.mult), reads=[rs], writes=[rinfo])
                    yield
                P.op(dve, lambda e: e.tensor_copy(sloti[:, ti, :, :], rinfo[:, ti, 0:2, :]), reads=[rinfo], writes=[sloti])
                for q in range(Q):
                    for kk in range(2):
                        P.dma(pool, lambda e, q=q, kk=kk: e.indirect_dma_start(
                            out=hs_d[:, :], out_offset=IOA(ap=sloti[:, ti, kk, q:q + 1], axis=0), in_=hfn[q][:, :], in_offset=None),
                            hfn[q], reads=[hfn[q], sloti])

            def collect(genfunc, ti):
                items = []
                orig_op, orig_dma = P.op, P.dma
                P.op = lambda eng, fn, reads=(), writes=(): items.append((orig_op, (eng, fn), dict(reads=reads, writes=writes), eng))
                P.dma = lambda q, fn, sem_buf, reads=(), writes=(): items.append((orig_dma, (q, fn, sem_buf), dict(reads=reads, writes=writes), q))
                try:
                    for tok in genfunc(ti):
                        items.append(tok)
                finally:
                    del P.op, P.dma
                return items

            def stages1(items):
                out, cur, prev = [], [], None
                for it in items:
                    if it is None or (HOP and prev is not None and it[3] is not prev):
                        out.append(cur)
                        cur = []
                    if it is None:
                        prev = None
                    else:
                        cur.append(it)
                        prev = it[3]
                out.append(cur)
                res = []
                for st in out:
                    if not st:
                        continue
                    res.append(st)
                    if LOADLAG and all(it[3] is sp and it[2]["writes"] for it in st):
                        res.extend([[] for _ in range(LOADLAG)])
                return res

            def zip_locked(chains):
                k = len(chains)
                if k == 1:
                    return chains[0]
                spans, wsets = [], []
                for L in chains:
                    fw, lr, ws = {}, {}, set()
                    for si, st in enumerate(L):
                        for (f, args, kw, eng) in st:
                            for bb in kw["writes"]:
                                fw.setdefault(id(bb), si)
                                ws.add(id(bb))
                            for bb in kw["reads"]:
                                lr[id(bb)] = si
                    spans.append({x: (fw[x], lr[x]) for x in fw if x in lr and lr[x] > fw[x]})
                    wsets.append(ws)
                out, pos, owner = [], [0] * k, {}
                while any(pos[c] < len(chains[c]) for c in range(k)):
                    progressed = False
                    for c in range(k):
                        if pos[c] >= len(chains[c]):
                            continue
                        st = chains[c][pos[c]]
                        W = set(id(bb) for (f, args, kw, eng) in st for bb in kw["writes"])
                        if any(owner.get(x) not in (None, c) for x in W):
                            continue
                        out.append(st)
                        progressed = True
                        for x in W:
                            if x in spans[c] and any(x in wsets[j] for j in range(k) if j != c):
                                owner[x] = c
                        for x in list(owner):
                            if owner[x] == c and pos[c] >= spans[c][x][1]:
                                owner[x] = None
                        pos[c] += 1
                    assert progressed, "chain lock deadlock"
                return out

            def stages(items):
                segs = [[[]]]
                for it in items:
                    if it == "SEG":
                        segs.append([[]])
                    elif it == "CHAIN":
                        segs[-1].append([])
                    else:
                        segs[-1][-1].append(it)
                out = []
                for seg in segs:
                    out += zip_locked([stages1(ch) for ch in seg if ch])  if any(seg) else []
                return out

            def spread(genfunc, ti, n, off=0):
                sts = stages(collect(genfunc, ti))
                assert len(sts) <= n - off, (len(sts), n, off)
                k = 0
                for t in range(n):
                    while t >= off and k < len(sts) and k * (n - off) < (t - off + 1) * len(sts):
                        for f, args, kw, eng in sts[k]:
                            f(*args, **kw)
                        k += 1
                    yield

            counts = [len(stages(collect(g, 1))) for g in (gen_F, gen_Mc, gen_Ml, gen_E)]
            NSTG = max(counts) + 2
            print("stages per stream", counts, NSTG)

            def both(g1, g2):
                for _ in g1:
                    next(g2)
                    yield

            def tile_gen(ti):
                yield from spread(gen_F, ti, NSTG)
                yield from both(spread(gen_Mc, ti, NSTG, MC_OFF), spread(gen_Ml, ti, NSTG))
                yield from spread(gen_E, ti, NSTG)

            run_pipelined((tile_gen(ti) for ti in range(NT)), depth=3, skew=NSTG)

            if debug:
                P.dma(sp, lambda e: e.dma_start(out=ri_d, in_=rinfo[:, :, :, :].rearrange("p a b c -> p (a b c)")), rinfo, reads=[rinfo])
            P.barrier()
            P.flush(block)

        with ExitStack() as sb:
            B = lambda shape, dt=F32, name=None, dma=False: P.buf(sb, shape, dt, name, dma)
            gffnbc = B([128, 8], F32, "gffnbc", dma=True)
            P.dma(sp, lambda e: e.dma_start(out=gffnbc[:, :], in_=gbc_d[1]), gffnbc, writes=[gffnbc])
            zt = B([128, D], F32, "zt", dma=True)
            P.op(dve, lambda e: e.memset(zt[:, :], 0.0), writes=[zt])
            P.dma(sp, lambda e: e.dma_start(out=y_d[NSLOT:NSLOT + 128, :], in_=zt[:, :]), zt, reads=[zt])
            w1 = [B([128, 8, 512], BF16, "w1") for _ in range(2)]
            w3 = [B([128, 8, 512], BF16, "w3") for _ in range(2)]
            w2 = [B([128, 4, D], BF16, "w2") for _ in range(2)]
            w3s = B([128, 8, 512], F32, "w3s", dma=True)
            w1s = B([128, 8, 512], F32, "w1s", dma=True)
            w2s = B([128, 4, D], F32, "w2s", dma=True)
            hst = [B([128, NSUB, D], BF16, "hst", dma=True) for _ in range(2)]
            hfTe = [B([128, 8, CAP], BF16, "hfTe") for _ in range(2)]
            actT = B([128, 4, CAP], BF16, "actT")
            tb = [B([128, 512], F32, "tb") for _ in range(2)]
            tc = [B([128, 512], F32, "tc") for _ in range(2)]
            yt = [B([128, D], F32, "yt", dma=True) for _ in range(3)]
            ntiles = [(0, 512)] if CAP == 512 else ([(n0, min(512, CAP - n0)) for n0 in range(0, CAP, 512)])
            yi = 0
            ci = 0

            def load_expert(ex):
                sl = ex % 2
                P.dma(sp, lambda e: e.dma_start(out=hst[sl][:, :, :], in_=hs_d[ex * CAP:(ex + 1) * CAP, :].rearrange("(s p) n -> p s n", p=128)),
                      hst[sl], writes=[hst[sl]])
                P.dma(sp, lambda e: e.dma_start(out=w1s[:, :, :], in_=w1_d[ex].rearrange("(k p) n -> p k n", p=128)), w1s, writes=[w1s])
                P.dma(sp, lambda e: e.dma_start(out=w3s[:, :, :], in_=w3_d[ex].rearrange("(k p) n -> p k n", p=128)), w3s, writes=[w3s])
                P.dma(sp, lambda e: e.dma_start(out=w2s[:, :, :], in_=w2_d[ex].rearrange("(k p) n -> p k n", p=128)), w2s, writes=[w2s])

            def cast_expert(ex):
                sl = ex % 2
                for k in range(8):
                    P.op(pool, lambda e, k=k: e.tensor_tensor(w1[sl][:, k, :], w1s[:, k, :], gffnbc[:, k:k + 1].broadcast_to([128, 512]), ALU.mult),
                         reads=[w1s, gffnbc], writes=[w1[sl]])
                for k in range(8):
                    P.op(act, lambda e, k=k: e.activation(out=w3[sl][:, k, :], in_=w3s[:, k, :], func=AF.Copy, scale=gffnbc[:, k:k + 1]), reads=[w3s, gffnbc], writes=[w3[sl]])
                for k in range(4):
                    P.op(act, lambda e, k=k: e.activation(out=w2[sl][:, k, :], in_=w2s[:, k, :], func=AF.Copy), reads=[w2s], writes=[w2[sl]])

            pool6 = pmm + [ps1, ps2]
            p6 = [0]

            def next6():
                bb = pool6[p6[0] % 6]
                p6[0] += 1
                return bb

            def do_T(ex):
                sl = ex % 2
                hT_e = hfTe[sl]
                for sbt in range(NSUB):
                    pt_ = ptr if sbt % 2 == 0 else ptr2

                    def fn(e, sbt=sbt, pt_=pt_, sl=sl):
                        ins = None
                        for c in range(8):
                            ins = e.transpose(pt_[:, c * 128:(c + 1) * 128], hst[sl][:, sbt, c * 128:(c + 1) * 128], ident[:, :])
                        return ins
                    P.op(pe, fn, reads=[hst[sl], ident], writes=[pt_])
                    P.op(dve, lambda e, sbt=sbt, pt_=pt_, hT_e=hT_e: e.tensor_copy(hT_e[:, :, sbt * 128:(sbt + 1) * 128], pt_[:, :].rearrange("p (c j) -> p c j", c=8)),
                         reads=[pt_], writes=[hT_e])

            def do_H(ex):
                sl = ex % 2
                hT_e = hfTe[sl]
                for (n0, nn) in ntiles:
                    for m in range(4):
                        p1, p3 = next6(), next6()
                        for (pb, wt) in ((p1, w1[sl]), (p3, w3[sl])):
                            def fn(e, pb=pb, wt=wt, m=m, n0=n0, nn=nn, hT_e=hT_e):
                                ins = None
                                for k in range(8):
                                    ins = e.matmul(pb[:, 0:nn], wt[:, k, m * 128:(m + 1) * 128], hT_e[:, k, n0:n0 + nn], start=(k == 0), stop=(k == 7))
                                return ins
                            P.op(pe, fn, reads=[wt, hT_e], writes=[pb])
                        tb_, tc_ = tb[ci_[0] % 2], tc[ci_[0] % 2]
                        ci_[0] += 1
                        P.op(act, lambda e, p1=p1, tb_=tb_, nn=nn: e.activation(out=tb_[:, 0:nn], in_=p1[:, 0:nn], func=AF.Tanh, scale=0.5), reads=[p1], writes=[tb_])
                        P.op(dve, lambda e, p1=p1, tb_=tb_, tc_=tc_, nn=nn: e.scalar_tensor_tensor(tc_[:, 0:nn], tb_[:, 0:nn], 1.0, p1[:, 0:nn], ALU.add, ALU.mult),
                             reads=[tb_, p1], writes=[tc_])
                        P.op(dve, lambda e, p3=p3, tc_=tc_, nn=nn, m=m, n0=n0: e.scalar_tensor_tensor(actT[:, m, n0:n0 + nn], tc_[:, 0:nn], 0.5, p3[:, 0:nn], ALU.mult, ALU.mult),
                             reads=[tc_, p3], writes=[actT])

            def do_Y(ex):
                sl = ex % 2
                for sbt in range(NSUB):
                    yb = yt[yi_[0] % 3]
                    yi_[0] += 1
                    for h in range(2):
                        pb = next6()

                        def fn(e, pb=pb, h=h, sbt=sbt, sl=sl):
                            ins = None
                            for m in range(4):
                                ins = e.matmul(pb[:, :], actT[:, m, sbt * 128:(sbt + 1) * 128], w2[sl][:, m, h * 512:(h + 1) * 512], start=(m == 0), stop=(m == 3))
                            return ins
                        P.op(pe, fn, reads=[actT, w2[sl]], writes=[pb])
                        P.op(act, lambda e, pb=pb, h=h, yb=yb: e.activation(out=yb[:, h * 512:(h + 1) * 512], in_=pb[:, :], func=AF.Copy), reads=[pb], writes=[yb])
                    r0 = ex * CAP + sbt * 128
                    P.dma(sp, lambda e, yb=yb, r0=r0: e.dma_start(out=y_d[r0:r0 + 128, :], in_=yb[:, :]), yb, reads=[yb])

            ci_, yi_ = [0], [0]
            load_expert(0)
            cast_expert(0)
            do_T(0)
            for ex in range(32):
                if ex + 1 < 32:
                    load_expert(ex + 1)
                do_H(ex)
                if ex + 1 < 32:
                    cast_expert(ex + 1)
                    do_T(ex + 1)
                do_Y(ex)
            P.barrier()
            P.flush(block)

        with ExitStack() as sc:
            B = lambda shape, dt=F32, name=None, dma=False: P.buf(sc, shape, dt, name, dma)
            gplebc = B([128, 8], F32, "gplebc", dma=True)
            gpp = B([128, D], F32, "gpp", dma=True)
            gfin = B([128, D], F32, "gfin", dma=True)
            wple = B([128, 2, D], BF16, "wple", dma=True)
            wpg = B([128, 8, D], BF16, "wpg", dma=True)
            P.dma(sp, lambda e: e.dma_start(out=gplebc[:, :], in_=gbc_d[2]), gplebc, writes=[gplebc])
            P.dma(sp, lambda e: e.dma_start(out=gpp[:, :], in_=rowbc_d[0]), gpp, writes=[gpp])
            P.dma(sp, lambda e: e.dma_start(out=gfin[:, :], in_=rowbc_d[1]), gfin, writes=[gfin])
            for k in range(2):
                P.dma(pool, lambda e, k=k: e.dma_start(out=wple[:, k, :], in_=wple_d[k * 128:(k + 1) * 128, :]), wple, writes=[wple])
            for k in range(8):
                P.dma(pool, lambda e, k=k: e.dma_start(out=wpg[:, k, :], in_=wpg_d[k * 128:(k + 1) * 128, :]), wpg, writes=[wpg])
            for k in range(8):
                P.op(dve, lambda e, k=k: e.tensor_scalar(wpg[:, k, :], wpg[:, k, :], gplebc[:, k:k + 1], None, ALU.mult), reads=[wpg, gplebc], writes=[wpg])
            x1t_ = [B([128, D], F32, "x1c", dma=True) for _ in range(4)]
            pt_b = [B([128, 256], F32, "pc", dma=True) for _ in range(4)]
            y1_ = [B([128, D], F32, "y1c", dma=True) for _ in range(4)]
            y2_ = [B([128, D], F32, "y2c", dma=True) for _ in range(4)]
            NP = 4
            junk = [B([128, D], BF16, "junkc") for _ in range(NP)]
            ssC_ = [B([128, 16], F32, "ssC") for _ in range(NP)]
            xn3 = [B([128, D], BF16, "xn3") for _ in range(NP)]
            x3T = [B([128, 8, 128], BF16, "x3T") for _ in range(NP)]
            pbf = [B([128, 256], BF16, "pbf") for _ in range(NP)]
            pT = [B([128, 2, 128], BF16, "pT") for _ in range(NP)]
            thg = [B([128, D], F32, "thg") for _ in range(NP)]
            te = [B([128, D], F32, "te") for _ in range(NP)]
            ob = [B([128, D], F32, "ob", dma=True) for _ in range(NP)]

            def rsq(ssb, i_v, i_l, i_o):
                P.op(act, lambda e: e.activation(out=ssb[:, i_l:i_l + 1], in_=ssb[:, i_v:i_v + 1], func=AF.Ln), reads=[ssb], writes=[ssb])
                P.op(act, lambda e: e.activation(out=ssb[:, i_o:i_o + 1], in_=ssb[:, i_l:i_l + 1], func=AF.Exp, scale=-0.5), reads=[ssb], writes=[ssb])

            def subtile_gen(st):
                ti, q = divmod(st, Q)
                r0 = st * 128
                i3 = st % NP
                xx, pp, y1, y2 = x1t_[i3], pt_b[i3], y1_[i3], y2_[i3]
                jk, ssC = junk[i3], ssC_[i3]
                xn_, x3_, pb_, pT_, tg_, te_, ob_ = xn3[i3], x3T[i3], pbf[i3], pT[i3], thg[i3], te[i3], ob[i3]
                P.dma(sp, lambda e: e.dma_start(out=xx[:, :], in_=x1_d[r0:r0 + 128, :]), xx, writes=[xx])
                P.dma(sp, lambda e: e.dma_start(out=pp[:, :], in_=p_d[r0:r0 + 128, :]), pp, writes=[pp])
                for (yy, kk) in ((y1, 0), (y2, 1)):
                    P.dma(pool, lambda e, yy=yy, kk=kk: e.indirect_dma_start(
                        out=yy[:, :], out_offset=None, in_=y_d[:, :], in_offset=IOA(ap=sloti[:, ti, kk, q:q + 1], axis=0)),
                        yy, reads=[sloti], writes=[yy])
                yield
                for (yy, kk) in ((y1, 0), (y2, 1)):
                    P.op(dve, lambda e, yy=yy, kk=kk: e.scalar_tensor_tensor(xx[:, :], yy[:, :], rinfo[:, ti, 2 + kk, q:q + 1], xx[:, :], ALU.mult, ALU.add),
                         reads=[yy, rinfo, xx], writes=[xx])
                P.op(act, lambda e: e.activation(out=jk[:, :], in_=xx[:, :], func=AF.Square, accum_out=ssC[:, 0:1]), reads=[xx], writes=[jk, ssC])
                P.op(dve, lambda e: e.tensor_scalar(ssC[:, 1:2], ssC[:, 0:1], 1.0 / D, EPS, ALU.mult, ALU.add), reads=[ssC], writes=[ssC])
                rsq(ssC, 1, 3, 2)
                P.op(act, lambda e: e.activation(out=xn_[:, :], in_=xx[:, :], func=AF.Copy, scale=ssC[:, 2:3]), reads=[xx, ssC], writes=[xn_])
                P.op(act, lambda e: e.activation(out=pb_[:, :], in_=pp[:, :], func=AF.Copy), reads=[pp], writes=[pb_])
                yield
                transposes(xn_, 8, ptr)
                P.op(dve, lambda e: e.tensor_copy(x3_[:, :, :], ptr[:, :].rearrange("p (c j) -> p c j", c=8)), reads=[ptr], writes=[x3_])
                transposes(pb_, 2, ptr2)
                P.op(act, lambda e: e.activation(out=pT_[:, :, :], in_=ptr2[:, 0:256].rearrange("p (c j) -> p c j", c=2), func=AF.Copy), reads=[ptr2], writes=[pT_])
                yield
                pes = []
                for h in range(2):
                    pg_ = next_pmm()

                    def fn(e, pg_=pg_, h=h):
                        ins = None
                        for k in range(8):
                            ins = e.matmul(pg_[:, :], x3_[:, k, :], wpg[:, k, h * 512:(h + 1) * 512], start=(k == 0), stop=(k == 7))
                        return ins
                    P.op(pe, fn, reads=[x3_, wpg], writes=[pg_])
                    P.op(act, lambda e, pg_=pg_, h=h: e.activation(out=tg_[:, h * 512:(h + 1) * 512], in_=pg_[:, :], func=AF.Tanh, scale=0.5), reads=[pg_], writes=[tg_])
                    pe_ = next_pmm()

                    def fn2(e, pe_=pe_, h=h):
                        ins = None
                        for k in range(2):
                            ins = e.matmul(pe_[:, :], pT_[:, k, :], wple[:, k, h * 512:(h + 1) * 512], start=(k == 0), stop=(k == 1))
                        return ins
                    P.op(pe, fn2, reads=[pT_, wple], writes=[pe_])
                    P.op(act, lambda e, pe_=pe_, h=h: e.activation(out=jk[:, 0:512], in_=pe_[:, :], func=AF.Square, accum_out=ssC[:, 4 + h:5 + h]), reads=[pe_], writes=[jk, ssC])
                    pes.append(pe_)
                yield
                P.op(dve, lambda e: e.tensor_tensor(ssC[:, 6:7], ssC[:, 4:5], ssC[:, 5:6], ALU.add), reads=[ssC], writes=[ssC])
                P.op(dve, lambda e: e.tensor_scalar(ssC[:, 7:8], ssC[:, 6:7], 4.0 / D, 4.0 * EPS, ALU.mult, ALU.add), reads=[ssC], writes=[ssC])
                rsq(ssC, 7, 9, 8)
                for h in range(2):
                    P.op(dve, lambda e, h=h, pe_=pes[h]: e.scalar_tensor_tensor(te_[:, h * 512:(h + 1) * 512], pe_[:, :], ssC[:, 8:9], gpp[:, h * 512:(h + 1) * 512], ALU.mult, ALU.mult),
                         reads=[pes[h], ssC, gpp], writes=[te_])
                P.op(dve, lambda e: e.scalar_tensor_tensor(te_[:, :], tg_[:, :], 1.0, te_[:, :], ALU.add, ALU.mult), reads=[tg_, te_], writes=[te_])
                P.op(pool, lambda e: e.tensor_tensor(xx[:, :], xx[:, :], te_[:, :], ALU.add), reads=[te_, xx], writes=[xx])
                P.op(act, lambda e: e.activation(out=jk[:, :], in_=xx[:, :], func=AF.Square, accum_out=ssC[:, 10:11]), reads=[xx], writes=[jk, ssC])
                P.op(dve, lambda e: e.tensor_scalar(ssC[:, 11:12], ssC[:, 10:11], 1.0 / D, EPS, ALU.mult, ALU.add), reads=[ssC], writes=[ssC])
                rsq(ssC, 11, 13, 12)
                yield
                P.op(dve, lambda e: e.scalar_tensor_tensor(ob_[:, :], xx[:, :], ssC[:, 12:13], gfin[:, :], ALU.mult, ALU.mult), reads=[xx, ssC, gfin], writes=[ob_])
                P.dma(sp, lambda e: e.dma_start(out=out_d[r0:r0 + 128, :], in_=ob_[:, :]), ob_, reads=[ob_])

            run_pipelined((subtile_gen(st) for st in range(T // 128)), depth=NP, skew=1)
            P.barrier()
            P.flush(block)
    return nc


def _chan(v):
    return np.ascontiguousarray(np.asarray(v, np.float32).reshape(4, 128).T)


def _gbc(g):
    return np.ascontiguousarray(np.asarray(g, np.float32).reshape(8, 128).T)


def prep_shared(inp):
    f = lambda a: np.ascontiguousarray(np.asarray(a, np.float32))
    cpar = np.zeros((128, NCP), np.float32)
    cw = f(inp["conv_dw_w"][0])
    for c in range(4):
        cpar[:, CW + c * 31:CW + (c + 1) * 31] = cw[:, c * 128:(c + 1) * 128].T
    cpar[:, CB:CB + 4] = _chan(inp["conv_dw_b"][0])
    cpar[:, LG:LG + 4] = _chan(inp["conv_ln_g"][0])
    cpar[:, LB:LB + 4] = _chan(inp["conv_ln_b"][0])
    lw = f(inp["lru_conv_w"][0])
    for c in range(4):
        cpar[:, LW + c * 4:LW + (c + 1) * 4] = lw[:, c * 128:(c + 1) * 128].T
    cpar[:, LBB:LBB + 4] = _chan(inp["lru_conv_b"][0])
    cpar[:, BR:BR + 4] = _chan(inp["lru_b_r"][0])
    cpar[:, BI:BI + 4] = _chan(inp["lru_b_i"][0])
    cpar[:, LAM:LAM + 4] = _chan(inp["lru_lambda"][0])
    wr_, wi_ = f(inp["lru_w_r"][0]), f(inp["lru_w_i"][0])
    gbd = np.zeros((4, 128, 256), np.float32)
    for c in range(4):
        for hh in range(2):
            gbd[c, hh * 64:(hh + 1) * 64, hh * 64:(hh + 1) * 64] = wr_[2 * c + hh]
            gbd[c, hh * 64:(hh + 1) * 64, 128 + hh * 64:128 + (hh + 1) * 64] = wi_[2 * c + hh]
    rb = np.concatenate([f(inp["b_group"][0]), f(inp["b_expert"][0])])
    shared = {
        "w_in": f(inp["w_in"][0]), "w_out": f(inp["w_out"][0]),
        "w_route": np.ascontiguousarray(np.concatenate([f(inp["w_group"][0]), f(inp["w_expert"][0])], axis=1)),
        "gate_bd": gbd, "cpar": cpar,
        "gbc": np.stack([_gbc(inp["g_mix"][0]), _gbc(inp["g_ffn"][0]), _gbc(inp["g_ple"][0])]),
        "rowbc": np.stack([np.ascontiguousarray(np.broadcast_to(f(inp["g_ple_proj"][0]), (128, D))),
                           np.ascontiguousarray(np.broadcast_to(f(inp["g_final"]), (128, D)))]),
        "rbias": np.ascontiguousarray(np.broadcast_to(np.tile(rb, Q), (128, Q * 36))),
        "iota_e": np.ascontiguousarray(np.broadcast_to(np.tile(np.arange(32, dtype=np.float32), Q), (128, Q * 32))),
        "ident": np.eye(128, dtype=np.float32),
        "tri": np.ascontiguousarray(np.triu(np.ones((128, 128), np.float32), 1)),
        "w1": f(inp["w1"][0]), "w3": f(inp["w3"][0]), "w2": f(inp["w2"][0]),
        "w_ple": f(inp["w_ple"][0]), "w_ple_gate": f(inp["w_ple_gate"][0]),
    }
    return shared


def kernel(**inputs):
    NSEQ, CAP = 4, 1024
    x = np.asarray(inputs["x"], np.float32)
    p = np.asarray(inputs["p"], np.float32)[0]
    shared = prep_shared(inputs)
    nc = build(NSEQ, CAP)
    in_maps = []
    for i in range(N_CORES):
        m = dict(shared)
        m["x"] = np.ascontiguousarray(x[i * NSEQ:(i + 1) * NSEQ].reshape(NSEQ * SEQ, D))
        m["p"] = np.ascontiguousarray(p[i * NSEQ:(i + 1) * NSEQ].reshape(NSEQ * SEQ, 256))
        in_maps.append(m)
    res = run_bass_kernel_spmd(nc, in_maps, core_ids=list(range(N_CORES)))
    out = np.concatenate([np.asarray(r["out"], np.float32).reshape(NSEQ, SEQ, D) for r in res.results], axis=0)
    return out
```

```python
import numpy as np
from contextlib import ExitStack
import concourse.bass as bass
import concourse.mybir as mybir
from concourse.bass_utils import run_bass_kernel_spmd

F32 = mybir.dt.float32
BF16 = mybir.dt.bfloat16
I32 = mybir.dt.int32
ALU = mybir.AluOpType
AF = mybir.ActivationFunctionType
AX = mybir.AxisListType

N_CORES = 8
HOP = True
NP_C = 6
LOADLAG = 3
MC_OFF = 0
SEQ = 2048
D = 1024
EPS = 1e-6
Q = 4

CW = 0
CB = CW + 124
LG = CB + 4
LB = LG + 4
LW = LB + 4
LBB = LW + 16
BR = LBB + 4
BI = BR + 4
LAM = BI + 4
NCP = LAM + 4
D_CWH = 0
D_GH = 124
D_BH = 128
D_BRH = 132
D_BIH = 136
D_N4 = 140
D_N8 = 144
NDP = 148


class Src:
    def __init__(self, sem, name, is_dma):
        self.sem, self.name, self.is_dma, self.total = sem, name, is_dma, 0


class Eng(Src):
    def __init__(self, sem, name, blockname, same_wait=True):
        super().__init__(sem, name, False)
        self.blockname, self.ops, self.seen, self.same_wait = blockname, [], {}, same_wait


class Buf:
    def __init__(self, t, dsem=None):
        self.t, self.w, self.r, self.dsem = t, None, {}, dsem

    def __getitem__(self, k):
        return self.t[k]


class Prog:
    def __init__(self, nc, stack):
        self.nc, self.stack = nc, stack
        self.srcs = []
        mk = lambda n, b, sw=True: self._reg(Eng(self._sem("e_" + n), n, b, sw))
        self.pe = mk("pe", "tensor", False)
        self.act = mk("act", "scalar")
        self.dve = mk("dve", "vector")
        self.pool = mk("pool", "gpsimd")
        self.sp = mk("sp", "sync")
        self.engs = [self.pe, self.act, self.dve, self.pool, self.sp]
        self.nbuf = 0

    def _sem(self, name):
        return self.stack.enter_context(self.nc.semaphore(name))

    def _reg(self, s):
        self.srcs.append(s)
        return s

    def buf(self, stack, shape, dt, name=None, dma=False, psum=False):
        self.nbuf += 1
        name = "%s_%d" % (name or "b", self.nbuf)
        if psum:
            t = stack.enter_context(self.nc.psum_tensor(name, shape, dt))
        else:
            t = stack.enter_context(self.nc.sbuf_tensor(name, shape, dt))
        ds = self._reg(Src(self._sem("d_" + name), name, True)) if dma else None
        return Buf(t, ds)

    def _deps(self, eng, reads, writes):
        need = {}

        def add(src, val):
            if src.is_dma:
                val = src.total
            if need.get(src, 0) < val:
                need[src] = val

        for b in reads:
            if b.w is not None:
                add(*b.w)
        for b in writes:
            if b.w is not None:
                add(*b.w)
            for s, v in b.r.items():
                add(s, v)
        waits = []
        for src, val in need.items():
            if src is eng and not eng.same_wait:
                continue
            if eng.seen.get(src, 0) >= val:
                continue
            eng.seen[src] = val
            waits.append((src.sem, val))
        return waits

    def _mark(self, src, val, reads, writes):
        for b in reads:
            b.r[src] = val
        for b in writes:
            b.w = (src, val)
            b.r = {}

    def op(self, eng, fn, reads=(), writes=()):
        waits = self._deps(eng, reads, writes)
        eng.total += 1
        sem = eng.sem

        def emit(e):
            for s, v in waits:
                e.wait_ge(s, v)
            fn(e).then_inc(sem, 1)

        eng.ops.append(emit)
        self._mark(eng, eng.total, reads, writes)

    def dma(self, q, fn, sem_buf, reads=(), writes=()):
        src = sem_buf.dsem
        waits = self._deps(q, reads, writes)
        src.total += 16
        sem = src.sem

        def emit(e):
            for s, v in waits:
                e.wait_ge(s, v)
            fn(e).then_inc(sem, 16)

        q.ops.append(emit)
        self._mark(src, src.total, reads, writes)

    def barrier(self):
        for E in self.engs:
            waits = []
            for S in self.srcs:
                if S is E or S.total == 0:
                    continue
                if E.seen.get(S, 0) >= S.total:
                    continue
                E.seen[S] = S.total
                waits.append((S.sem, S.total))

            def emit(e, waits=waits):
                for s, v in waits:
                    e.wait_ge(s, v)

            E.ops.append(emit)

    def flush(self, block):
        for E in self.engs:
            if not E.ops:
                continue
            ops = E.ops
            E.ops = []

            def body(e, ops=ops):
                for f in ops:
                    f(e)

            getattr(block, E.blockname)(body)


def run_pipelined(gens, depth, skew):
    it = iter(gens)
    active, pending, tick = [], True, 0
    while pending or active:
        for g in list(active):
            try:
                next(g)
            except StopIteration:
                active.remove(g)
        if pending and tick % skew == 0 and len(active) < depth:
            try:
                g = next(it)
                active.append(g)
                next(g)
            except StopIteration:
                pending = False
        tick += 1


def build(NSEQ=4, CAP=640, debug=False):
    T = NSEQ * SEQ
    NT = T // 512
    NSUB = CAP // 128
    NSLOT = 32 * CAP
    TRASH = NSLOT
    nc = bass.Bass("TRN2", target_bir_lowering=False)

    def dr(name, shape, dt=F32, kind="ExternalInput"):
        return nc.dram_tensor(name, shape, dt, kind=kind).ap()

    x_d = dr("x", [T, D])
    p_d = dr("p", [T, 256])
    win_d = dr("w_in", [D, 2048])
    wout_d = dr("w_out", [D, D])
    wr_d = dr("w_route", [D, 36])
    gbd_d = dr("gate_bd", [4, 128, 256])
    cpar_d = dr("cpar", [128, NCP])
    gbc_d = dr("gbc", [3, 128, 8])
    rowbc_d = dr("rowbc", [2, 128, D])
    rb_d = dr("rbias", [128, Q * 36])
    iota_d = dr("iota_e", [128, Q * 32])
    ident_d = dr("ident", [128, 128])
    tri_d = dr("tri", [128, 128])
    w1_d = dr("w1", [32, D, 512])
    w3_d = dr("w3", [32, D, 512])
    w2_d = dr("w2", [32, 512, D])
    wple_d = dr("w_ple", [256, D])
    wpg_d = dr("w_ple_gate", [D, D])
    out_d = dr("out", [T, D], kind="ExternalOutput")
    sk = "ExternalOutput" if debug else "Internal"
    x1_d = dr("x1s", [T, D], kind=sk)
    hs_d = dr("hss", [NSLOT + 128, D], BF16, kind=sk)
    y_d = dr("yss", [NSLOT + 128, D], kind=sk)
    if debug:
        ri_d = dr("rinfo_o", [128, NT * 4 * Q], kind="ExternalOutput")

    IOA = bass.IndirectOffsetOnAxis

    with ExitStack() as top:
        P = Prog(nc, top)
        pe, act, dve, pool, sp = P.pe, P.act, P.dve, P.pool, P.sp
        block = top.enter_context(nc.Block())

        pmm = [P.buf(top, [128, 512], F32, "pmm", psum=True) for _ in range(4)]
        ptr = P.buf(top, [128, 1024], BF16, "ptr", psum=True)
        ptr2 = P.buf(top, [128, 1024], BF16, "ptr2", psum=True)
        ps1 = P.buf(top, [128, 512], F32, "ps1", psum=True)
        ps2 = P.buf(top, [128, 512], F32, "ps2", psum=True)
        pmm_i = [0]

        def next_pmm():
            b = pmm[pmm_i[0] % 4]
            pmm_i[0] += 1
            return b

        rinfo = P.buf(top, [128, NT, 4, Q], F32, "rinfo")
        sloti = P.buf(top, [128, NT, 2, Q], I32, "sloti")
        if debug:
            rinfo.dsem = P._reg(Src(P._sem("d_rinfo"), "rinfo", True))
        ident = P.buf(top, [128, 128], BF16, "ident", dma=True)
        P.dma(pool, lambda e: e.dma_start(out=ident[:, :], in_=ident_d), ident, writes=[ident])

        def transposes(src, n, dst_ps):
            def fn(e):
                ins = None
                for c in range(n):
                    ins = e.transpose(dst_ps[:, c * 128:(c + 1) * 128], src[:, c * 128:(c + 1) * 128], ident[:, :])
                return ins
            P.op(pe, fn, reads=[src, ident], writes=[dst_ps])

        def rstd_from_ss(ss, out, n):
            P.op(dve, lambda e: e.tensor_scalar(out, ss, 1.0 / n, EPS, ALU.mult, ALU.add), reads=[], writes=[])

        with ExitStack() as sa:
            B = lambda shape, dt=F32, name=None, dma=False: P.buf(sa, shape, dt, name, dma)
            win = B([128, 8, 2048], BF16, "win", dma=True)
            wout = B([128, 8, D], BF16, "wout", dma=True)
            wr = B([128, 8, 36], BF16, "wr", dma=True)
            gbd = B([128, 4, 256], BF16, "gbd", dma=True)
            tri = B([128, 128], BF16, "tri", dma=True)
            cpar = B([128, NCP], F32, "cpar", dma=True)
            dpar = B([128, NDP], F32, "dpar")
            gmixbc = B([128, 8], F32, "gmixbc", dma=True)
            gffnbc = B([128, 8], F32, "gffnbc", dma=True)
            rbias = B([128, Q, 36], F32, "rbias", dma=True)
            iota = B([128, Q, 32], F32, "iota", dma=True)
            ones = B([128, 128], BF16, "ones")
            cntbc = B([128, 32], F32, "cntbc")

            P.dma(sp, lambda e: e.dma_start(out=cpar[:, :], in_=cpar_d), cpar, writes=[cpar])
            P.dma(sp, lambda e: e.dma_start(out=gmixbc[:, :], in_=gbc_d[0]), gmixbc, writes=[gmixbc])
            P.dma(sp, lambda e: e.dma_start(out=gffnbc[:, :], in_=gbc_d[1]), gffnbc, writes=[gffnbc])
            P.dma(sp, lambda e: e.dma_start(out=rbias[:, :, :], in_=rb_d.rearrange("p (q n) -> p q n", q=Q)), rbias, writes=[rbias])
            P.dma(sp, lambda e: e.dma_start(out=iota[:, :, :], in_=iota_d.rearrange("p (q n) -> p q n", q=Q)), iota, writes=[iota])
            P.dma(pool, lambda e: e.dma_start(out=tri[:, :], in_=tri_d), tri, writes=[tri])
            for k in range(8):
                P.dma(pool, lambda e, k=k: e.dma_start(out=win[:, k, :], in_=win_d[k * 128:(k + 1) * 128, :]), win, writes=[win])
            P.dma(pool, lambda e: e.dma_start(out=gbd[:, :, :], in_=gbd_d.rearrange("c p n -> p c n")), gbd, writes=[gbd])
            for k in range(8):
                P.dma(pool, lambda e, k=k: e.dma_start(out=wout[:, k, :], in_=wout_d[k * 128:(k + 1) * 128, :]), wout, writes=[wout])
            P.dma(pool, lambda e: e.dma_start(out=wr[:, :, :], in_=wr_d.rearrange("(k p) n -> p k n", p=128)), wr, writes=[wr])

            P.op(dve, lambda e: e.memset(ones[:, :], 1.0), writes=[ones])
            P.op(dve, lambda e: e.memset(cntbc[:, :], 0.0), writes=[cntbc])
            for k in range(8):
                P.op(dve, lambda e, k=k: e.tensor_scalar(win[:, k, :], win[:, k, :], gmixbc[:, k:k + 1], None, ALU.mult), reads=[win, gmixbc], writes=[win])
                P.op(dve, lambda e, k=k: e.tensor_scalar(wr[:, k, :], wr[:, k, :], gffnbc[:, k:k + 1], None, ALU.mult), reads=[wr, gffnbc], writes=[wr])

            tsm = B([128, 8, 4], F32, "tsm")

            def dv(fn, reads, writes):
                P.op(dve, fn, reads=reads, writes=writes)

            dv(lambda e: e.tensor_scalar(dpar[:, D_CWH:D_CWH + 124], cpar[:, CW:CW + 124], 0.5, None, ALU.mult), [cpar], [dpar])
            dv(lambda e: e.tensor_scalar(dpar[:, D_GH:D_GH + 8], cpar[:, LG:LG + 8], 0.5, None, ALU.mult), [cpar], [dpar])
            dv(lambda e: e.tensor_scalar(dpar[:, D_BRH:D_BRH + 8], cpar[:, BR:BR + 8], 0.5, None, ALU.mult), [cpar], [dpar])
            z_, az, ee, LL, tt, mk_, zp = [tsm[:, i, :] for i in range(7)]
            dv(lambda e: e.tensor_scalar(z_, cpar[:, LAM:LAM + 4], -1.0, None, ALU.mult), [cpar], [tsm])
            dv(lambda e: e.tensor_tensor(az, z_, cpar[:, LAM:LAM + 4], ALU.max), [tsm, cpar], [tsm])
            P.op(act, lambda e: e.activation(out=ee, in_=az, func=AF.Exp, scale=-1.0), reads=[tsm], writes=[tsm])
            P.op(act, lambda e: e.activation(out=LL, in_=ee, func=AF.Ln, bias=1.0, scale=1.0), reads=[tsm], writes=[tsm])
            dv(lambda e: e.tensor_scalar(tt, ee, -0.25, 1.0 / 3.0, ALU.mult, ALU.add), [tsm], [tsm])
            dv(lambda e: e.tensor_tensor(tt, tt, ee, ALU.mult), [tsm], [tsm])
            dv(lambda e: e.tensor_scalar(tt, tt, -1.0, 0.5, ALU.mult, ALU.add), [tsm], [tsm])
            dv(lambda e: e.tensor_tensor(tt, tt, ee, ALU.mult), [tsm], [tsm])
            dv(lambda e: e.tensor_scalar(tt, tt, -1.0, 1.0, ALU.mult, ALU.add), [tsm], [tsm])
            dv(lambda e: e.tensor_tensor(tt, tt, ee, ALU.mult), [tsm], [tsm])
            dv(lambda e: e.tensor_single_scalar(mk_, ee, 0.05, ALU.is_lt), [tsm], [tsm])
            dv(lambda e: e.tensor_tensor(tt, tt, LL, ALU.subtract), [tsm], [tsm])
            dv(lambda e: e.tensor_tensor(tt, tt, mk_, ALU.mult), [tsm], [tsm])
            dv(lambda e: e.tensor_tensor(tt, tt, LL, ALU.add), [tsm], [tsm])
            dv(lambda e: e.tensor_single_scalar(zp, z_, 0.0, ALU.max), [tsm], [tsm])
            dv(lambda e: e.tensor_tensor(tt, tt, zp, ALU.add), [tsm], [tsm])
            dv(lambda e: e.tensor_scalar(dpar[:, D_N4:D_N4 + 4], tt, -4.0, None, ALU.mult), [tsm], [dpar])
            dv(lambda e: e.tensor_scalar(dpar[:, D_N8:D_N8 + 4], tt, -8.0, None, ALU.mult), [tsm], [dpar])

            xa = [B([128, D], F32, "xa", dma=True) for _ in range(2)]
            ssF = [B([128, 8], F32, "ssF") for _ in range(2)]
            xn = [B([128, D], BF16, "xn") for _ in range(2)]
            hT = B([128, 8, 512], BF16, "hT")
            gth = [B([128, 512], F32, "gth") for _ in range(2)]
            gvs = [B([128, 512], F32, "gvs") for _ in range(2)]
            ga = B([128, 512], F32, "ga")
            gb = B([128, 512], F32, "gb")
            ub = [[B([128, 542], BF16, "ub") for _ in range(4)] for _ in range(2)]
            xbuf = [[B([128, 515], BF16, "xbuf") for _ in range(4)] for _ in range(2)]
            qg = [[B([128, 512], BF16, "qg") for _ in range(4)] for _ in range(2)]
            acc2 = [[B([128, 512], F32, "acc") for _ in range(4)] for _ in range(2)]
            cvbf = [B([128, 512], BF16, "cvbf") for _ in range(4)]
            sqbf = [B([128, 512], BF16, "sqbf") for _ in range(4)]
            mean = B([128, 512], F32, "mean")
            msq = B([128, 512], F32, "msq")
            xr = [B([128, 512], F32, "xr") for _ in range(2)]
            xrbf = [B([128, 512], BF16, "xrbf") for _ in range(2)]
            t1 = [B([128, 512], F32, "t1") for _ in range(2)]
            t2 = [B([128, 512], F32, "t2") for _ in range(2)]
            t3 = [B([128, 512], F32, "t3") for _ in range(2)]
            lh, th = t1, t2
            ab = [B([128, 512], F32, "ab")] * 2
            hb = [B([128, 512], F32, "hb")] * 2
            carry = [B([128, 1], F32, "carry") for _ in range(4)]
            yT = [B([128, 8, 512], BF16, "yT") for _ in range(2)]
            xb = B([128, D], F32, "xb", dma=True)
            ssE = [B([128, 8], F32, "ssE") for _ in range(2)]
            x1 = [B([128, D], F32, "x1", dma=True) for _ in range(2)]
            hfn = [B([128, D], BF16, "hfn", dma=True) for _ in range(4)]
            hfT = [B([128, 8, 128], BF16, "hfT") for _ in range(2)]
            rs = B([128, 40, Q], F32, "rs")
            r36 = B([128, Q, 36], F32, "r36")
            r32 = [B([128, Q, 32], F32, "r32") for _ in range(5)]
            mbf = B([128, Q, 32], BF16, "mbf")
            r8 = [B([128, Q, 8], F32, "r8") for _ in range(4)]
            r4 = [B([128, Q, 4], F32, "r4") for _ in range(3)]
            print("phase A sbuf remaining", nc.sbuf_bytes_remaining)

            cwh = lambda c, k: dpar[:, D_CWH + c * 31 + k:D_CWH + c * 31 + k + 1]
            NJ = SEQ // 512

            def rsqA(ssb, i_v, i_l, i_o):
                P.op(act, lambda e: e.activation(out=ssb[:, i_l:i_l + 1], in_=ssb[:, i_v:i_v + 1], func=AF.Ln), reads=[ssb], writes=[ssb])
                P.op(act, lambda e: e.activation(out=ssb[:, i_o:i_o + 1], in_=ssb[:, i_l:i_l + 1], func=AF.Exp, scale=-0.5), reads=[ssb], writes=[ssb])

            def evac_scaled(dst3, ps, gvec):
                P.op(act, lambda e: e.activation(out=dst3.all(), in_=ps[:, :].rearrange("p (c j) -> p c j", c=8), func=AF.Copy),
                     reads=[ps], writes=[dst3.buf])

            class View3:
                def __init__(self, buf, fn, allfn=None):
                    self.buf, self.fn, self.all = buf, fn, allfn

                def __call__(self, c):
                    return self.fn(c)

            def gen_F(ti):
                s_, j = divmod(ti, NJ)
                row0 = ti * 512
                par = ti % 2
                ub_, xbuf_, qg_ = ub[par], xbuf[par], qg[par]
                if j == 0:
                    for c in range(4):
                        P.op(pool, lambda e, c=c: e.memset(ub_[c][:, 0:30], 0.0), writes=[ub_[c]])
                        P.op(pool, lambda e, c=c: e.memset(xbuf_[c][:, 0:3], 0.0), writes=[xbuf_[c]])
                for q in range(Q):
                    yield ("SEG" if q % 2 == 0 else "CHAIN")
                    xt, xnb, ss = xa[q % 2], xn[q % 2], ssF[q % 2]
                    r0 = row0 + q * 128
                    P.dma(sp, lambda e, xt=xt, r0=r0: e.dma_start(out=xt[:, :], in_=x_d[r0:r0 + 128, :]), xt, writes=[xt])
                    P.op(act, lambda e, xt=xt, ss=ss, xnb=xnb: e.activation(out=xnb[:, :], in_=xt[:, :], func=AF.Square, accum_out=ss[:, 0:1]),
                         reads=[xt], writes=[xnb, ss])
                    P.op(dve, lambda e, ss=ss: e.tensor_scalar(ss[:, 1:2], ss[:, 0:1], 1.0 / D, EPS, ALU.mult, ALU.add), reads=[ss], writes=[ss])
                    rsqA(ss, 1, 3, 2)
                    P.op(act, lambda e, xt=xt, xnb=xnb, ss=ss: e.activation(out=xnb[:, :], in_=xt[:, :], func=AF.Copy, scale=ss[:, 2:3]),
                         reads=[xt, ss], writes=[xnb])
                    transposes(xnb, 8, ptr)
                    evac_scaled(View3(hT, None, lambda q=q: hT[:, :, q * 128:(q + 1) * 128]), ptr, gmixbc)

                fbank = [pmm[0], ps2]

                def zmm(m, bi):
                    pb = fbank[bi % 2]

                    def fn(e, m=m, pb=pb):
                        ins = None
                        for k in range(8):
                            ins = e.matmul(pb[:, :], win[:, k, m * 128:(m + 1) * 128], hT[:, k, :], start=(k == 0), stop=(k == 7))
                        return ins
                    P.op(pe, fn, reads=[win, hT], writes=[pb])
                    return pb

                for c in range(4):
                    yield ("SEG" if c % 2 == 0 else "CHAIN")
                    pg = zmm(4 + c, c)
                    ta, vs = gth[c % 2], gvs[c % 2]
                    P.op(act, lambda e, pg=pg, ta=ta: e.activation(out=ta[:, :], in_=pg[:, :], func=AF.Tanh, scale=0.5), reads=[pg], writes=[ta])
                    pv = zmm(c, c)
                    P.op(act, lambda e, pv=pv, vs=vs: e.activation(out=vs[:, :], in_=pv[:, :], func=AF.Copy), reads=[pv], writes=[vs])
                    P.op(pool, lambda e, ta=ta, vs=vs: e.tensor_tensor(ta[:, :], ta[:, :], vs[:, :], ALU.mult), reads=[ta, vs], writes=[ta])
                    P.op(pool, lambda e, ta=ta, vs=vs, c=c: e.tensor_tensor(ub_[c][:, 30:542], ta[:, :], vs[:, :], ALU.add), reads=[ta, vs], writes=[ub_[c]])
                for c in range(4):
                    yield ("SEG" if c % 2 == 0 else "CHAIN")
                    px = zmm(8 + c, c)
                    P.op(act, lambda e, px=px, c=c: e.activation(out=xbuf_[c][:, 3:515], in_=px[:, :], func=AF.Copy), reads=[px], writes=[xbuf_[c]])
                for c in range(4):
                    yield "SEG"
                    pgl = zmm(12 + c, c)
                    P.op(act, lambda e, pgl=pgl: e.activation(out=ga[:, :], in_=pgl[:, :], func=AF.Copy), reads=[pgl], writes=[ga])
                    P.op(act, lambda e, pgl=pgl: e.activation(out=gb[:, :], in_=pgl[:, :], func=AF.Square), reads=[pgl], writes=[gb])
                    P.op(act, lambda e: e.activation(out=gb[:, :], in_=gb[:, :], func=AF.Identity, bias=1.0, scale=0.044715), reads=[gb], writes=[gb])
                    P.op(pool, lambda e: e.tensor_tensor(gb[:, :], gb[:, :], ga[:, :], ALU.mult), reads=[ga, gb], writes=[gb])
                    P.op(act, lambda e: e.activation(out=gb[:, :], in_=gb[:, :], func=AF.Tanh, scale=0.7978845608028654), reads=[gb], writes=[gb])
                    P.op(act, lambda e: e.activation(out=gb[:, :], in_=gb[:, :], func=AF.Identity, bias=1.0, scale=1.0), reads=[gb], writes=[gb])
                    P.op(pool, lambda e, c=c: e.tensor_tensor(qg_[c][:, :], gb[:, :], ga[:, :], ALU.mult), reads=[ga, gb], writes=[qg_[c]])

            def gen_Mc(ti):
                s_, j = divmod(ti, NJ)
                par = ti % 2
                ub_, xbuf_, qg_, yT_ = ub[par], xbuf[par], qg[par], yT[par]
                ubn, xbufn = ub[1 - par], xbuf[1 - par]
                acc = acc2[par]
                for k in range(31):
                    for c in range(4):
                        if k == 0:
                            P.op(dve, lambda e, c=c: e.tensor_scalar(acc[c][:, :], ub_[c][:, 0:512], cwh(c, 0), cpar[:, CB + c:CB + c + 1], ALU.mult, ALU.add),
                                 reads=[ub_[c], dpar, cpar], writes=[acc[c]])
                        else:
                            P.op(dve, lambda e, c=c, k=k: e.scalar_tensor_tensor(acc[c][:, :], ub_[c][:, k:k + 512], cwh(c, k), acc[c][:, :], ALU.mult, ALU.add),
                                 reads=[ub_[c], dpar, acc[c]], writes=[acc[c]])
                    yield
                for c in range(4):
                    if j < NJ - 1:
                        P.op(pool, lambda e, c=c: e.tensor_copy(ubn[c][:, 0:30], ub_[c][:, 512:542]), reads=[ub_[c]], writes=[ubn[c]])
                    P.op(act, lambda e, c=c: e.activation(out=cvbf[c][:, :], in_=acc[c][:, :], func=AF.Copy), reads=[acc[c]], writes=[cvbf[c]])
                    P.op(act, lambda e, c=c: e.activation(out=sqbf[c][:, :], in_=acc[c][:, :], func=AF.Square), reads=[acc[c]], writes=[sqbf[c]])

                def stat_mm(dst, srcs):
                    def fn(e):
                        ins = None
                        for c in range(4):
                            ins = e.matmul(dst[:, :], ones[:, :], srcs[c][:, :], start=(c == 0), stop=(c == 3))
                        return ins
                    P.op(pe, fn, reads=[ones] + srcs, writes=[dst])
                stat_mm(ps1, cvbf)
                P.op(act, lambda e: e.activation(out=mean[:, :], in_=ps1[:, :], func=AF.Copy, scale=1.0 / 512), reads=[ps1], writes=[mean])
                stat_mm(ps1, sqbf)
                P.op(pool, lambda e: e.tensor_tensor(msq[:, :], mean[:, :], mean[:, :], ALU.mult), reads=[mean], writes=[msq])
                P.op(dve, lambda e: e.scalar_tensor_tensor(msq[:, :], ps1[:, :], 1.0 / 512, msq[:, :], ALU.mult, ALU.subtract), reads=[ps1, msq], writes=[msq])
                P.op(dve, lambda e: e.tensor_scalar(msq[:, :], msq[:, :], EPS, None, ALU.add), reads=[msq], writes=[msq])
                P.op(act, lambda e: e.activation(out=msq[:, :], in_=msq[:, :], func=AF.Ln), reads=[msq], writes=[msq])
                P.op(act, lambda e: e.activation(out=msq[:, :], in_=msq[:, :], func=AF.Exp, scale=-0.5), reads=[msq], writes=[msq])
                yield
                for c in range(4):
                    P.op(pool, lambda e, c=c: e.tensor_tensor(acc[c][:, :], acc[c][:, :], mean[:, :], ALU.subtract), reads=[acc[c], mean], writes=[acc[c]])
                    P.op(pool, lambda e, c=c: e.tensor_tensor(acc[c][:, :], acc[c][:, :], msq[:, :], ALU.mult), reads=[acc[c], msq], writes=[acc[c]])
                for c in range(4):
                    t_ = (mean, msq)[c % 2]
                    P.op(act, lambda e, c=c: e.activation(out=acc[c][:, :], in_=acc[c][:, :], func=AF.Identity,
                                                          bias=dpar[:, D_BH + c:D_BH + c + 1], scale=dpar[:, D_GH + c:D_GH + c + 1]),
                         reads=[acc[c], dpar], writes=[acc[c]])
                    P.op(act, lambda e, c=c, t_=t_: e.activation(out=t_[:, :], in_=acc[c][:, :], func=AF.Tanh), reads=[acc[c]], writes=[t_])
                    P.op(dve, lambda e, c=c, t_=t_: e.scalar_tensor_tensor(yT_[:, c, :], t_[:, :], 1.0, acc[c][:, :], ALU.add, ALU.mult),
                         reads=[acc[c], t_], writes=[yT_])
            def gen_Ml(ti):
                s_, j = divmod(ti, NJ)
                par = ti % 2
                xbuf_, qg_, yT_ = xbuf[par], qg[par], yT[par]
                xbufn = xbuf[1 - par]
                if j == 0:
                    for c in range(4):
                        P.op(pool, lambda e, c=c: e.memset(carry[c][:, :], 0.0), writes=[carry[c]])
                for c in range(4):
                    xr_, xrb_ = xr[c % 2], xrbf[c % 2]
                    lw = lambda k, c=c: cpar[:, LW + c * 4 + k:LW + c * 4 + k + 1]
                    P.op(dve, lambda e, c=c, xr_=xr_, lw=lw: e.tensor_scalar(xr_[:, :], xbuf_[c][:, 0:512], lw(0), cpar[:, LBB + c:LBB + c + 1], ALU.mult, ALU.add),
                         reads=[xbuf_[c], cpar], writes=[xr_])
                    for k in range(1, 4):
                        P.op(dve, lambda e, c=c, k=k, xr_=xr_, lw=lw: e.scalar_tensor_tensor(xr_[:, :], xbuf_[c][:, k:k + 512], lw(k), xr_[:, :], ALU.mult, ALU.add),
                             reads=[xbuf_[c], cpar, xr_], writes=[xr_])
                    if j < NJ - 1:
                        P.op(pool, lambda e, c=c: e.tensor_copy(xbufn[c][:, 0:3], xbuf_[c][:, 512:515]), reads=[xbuf_[c]], writes=[xbufn[c]])
                    P.op(act, lambda e, xr_=xr_, xrb_=xrb_: e.activation(out=xrb_[:, :], in_=xr_[:, :], func=AF.Copy), reads=[xr_], writes=[xrb_])
                    pr, pi = pmm[1], pmm[1]
                    P.op(pe, lambda e, c=c, pr=pr, xrb_=xrb_: e.matmul(pr[:, :], gbd[:, c, 0:128], xrb_[:, :], start=True, stop=True), reads=[gbd, xrb_], writes=[pr])
                    a1, a2, a3, aa, hh = t1[c % 2], t2[c % 2], t3[c % 2], ab[c % 2], hb[c % 2]
                    dp = lambda o, c=c: dpar[:, o + c:o + c + 1]
                    yield
                    P.op(act, lambda e, pr=pr, a1=a1, dp=dp: e.activation(out=a1[:, :], in_=pr[:, :], func=AF.Tanh, bias=dp(D_BRH), scale=0.5), reads=[pr, dpar], writes=[a1])
                    P.op(pe, lambda e, c=c, pi=pi, xrb_=xrb_: e.matmul(pi[:, :], gbd[:, c, 128:256], xrb_[:, :], start=True, stop=True), reads=[gbd, xrb_], writes=[pi])
                    P.op(act, lambda e, pi=pi, a3=a3, dp=dp: e.activation(out=a3[:, :], in_=pi[:, :], func=AF.Tanh, bias=dp(D_BIH), scale=0.5), reads=[pi, dpar], writes=[a3])
                    P.op(act, lambda e, a1=a1, aa=aa, dp=dp: e.activation(out=aa[:, :], in_=a1[:, :], func=AF.Exp, bias=dp(D_N4), scale=dp(D_N4)), reads=[a1, dpar], writes=[aa])
                    P.op(act, lambda e, a1=a1, a2=a2, dp=dp: e.activation(out=a2[:, :], in_=a1[:, :], func=AF.Exp, bias=dp(D_N8), scale=dp(D_N8)), reads=[a1, dpar], writes=[a2])
                    P.op(dve, lambda e, a2=a2: e.tensor_scalar(a2[:, :], a2[:, :], 0.99999994, -1.0, ALU.min, ALU.mult), reads=[a2], writes=[a2])
                    P.op(act, lambda e, a2=a2: e.activation(out=a2[:, :], in_=a2[:, :], func=AF.Ln, bias=1.0, scale=1.0), reads=[a2], writes=[a2])
                    P.op(act, lambda e, a2=a2: e.activation(out=a2[:, :], in_=a2[:, :], func=AF.Exp, scale=0.5), reads=[a2], writes=[a2])
                    P.op(dve, lambda e, a3=a3, xr_=xr_: e.scalar_tensor_tensor(a3[:, :], a3[:, :], 1.0, xr_[:, :], ALU.add, ALU.mult), reads=[a3, xr_], writes=[a3])
                    yield
                    P.op(dve, lambda e, a2=a2, a3=a3: e.tensor_tensor(a3[:, :], a3[:, :], a2[:, :], ALU.mult), reads=[a2, a3], writes=[a3])
                    P.op(dve, lambda e, c=c, aa=aa, a3=a3, hh=hh: e.tensor_tensor_scan(hh[:, :], aa[:, :], a3[:, :], carry[c][:, 0:1], ALU.mult, ALU.add),
                         reads=[aa, a3, carry[c]], writes=[hh])
                    P.op(dve, lambda e, c=c, hh=hh: e.tensor_copy(carry[c][:, :], hh[:, 511:512]), reads=[hh], writes=[carry[c]])
                    P.op(dve, lambda e, c=c, hh=hh: e.scalar_tensor_tensor(yT_[:, 4 + c, :], hh[:, :], 0.25, qg_[c][:, :], ALU.mult, ALU.mult),
                         reads=[hh, qg_[c]], writes=[yT_])
                    yield

            def gen_E(ti):
                row0 = ti * 512
                yT_ = yT[ti % 2]
                for q in range(Q):
                    yield ("SEG" if q % 2 == 0 else "CHAIN")
                    r0 = row0 + q * 128
                    x1t, ss = x1[q % 2], ssE[q % 2]
                    hf = hfn[q]
                    hft = hfT[q % 2]
                    P.dma(sp, lambda e, r0=r0: e.dma_start(out=xb[:, :], in_=x_d[r0:r0 + 128, :]), xb, writes=[xb])
                    for h in range(2):
                        pb = pmm[2]

                        def fn(e, pb=pb, h=h, q=q):
                            ins = None
                            for k in range(8):
                                ins = e.matmul(pb[:, :], yT_[:, k, q * 128:(q + 1) * 128], wout[:, k, h * 512:(h + 1) * 512], start=(k == 0), stop=(k == 7))
                            return ins
                        P.op(pe, fn, reads=[yT_, wout], writes=[pb])
                        P.op(dve, lambda e, pb=pb, h=h, x1t=x1t: e.tensor_tensor(x1t[:, h * 512:(h + 1) * 512], pb[:, :], xb[:, h * 512:(h + 1) * 512], ALU.add),
                             reads=[pb, xb], writes=[x1t])
                    P.dma(sp, lambda e, x1t=x1t, r0=r0: e.dma_start(out=x1_d[r0:r0 + 128, :], in_=x1t[:, :]), x1t, reads=[x1t])
                    P.op(act, lambda e, x1t=x1t, ss=ss, hf=hf: e.activation(out=hf[:, :], in_=x1t[:, :], func=AF.Square, accum_out=ss[:, 0:1]), reads=[x1t], writes=[hf, ss])
                    P.op(dve, lambda e, ss=ss: e.tensor_scalar(ss[:, 1:2], ss[:, 0:1], 1.0 / D, EPS, ALU.mult, ALU.add), reads=[ss], writes=[ss])
                    rsqA(ss, 1, 3, 2)
                    P.op(act, lambda e, x1t=x1t, hf=hf, ss=ss: e.activation(out=hf[:, :], in_=x1t[:, :], func=AF.Copy, scale=ss[:, 2:3]), reads=[x1t, ss], writes=[hf])
                    transposes(hf, 8, ptr2)
                    evac_scaled(View3(hft, None, lambda hft=hft: hft[:, :, :]), ptr2, gffnbc)

                    def fnl(e, hft=hft, q=q):
                        ins = None
                        for k in range(8):
                            ins = e.matmul(pmm[3][:, q * 36:(q + 1) * 36], hft[:, k, :], wr[:, k, :], start=(k == 0), stop=(k == 7))
                        return ins
                    P.op(pe, fnl, reads=[hft, wr], writes=[pmm[3]])

                yield "SEG"
                S = lambda i: rs[:, i, :]
                bc = lambda ap, n: ap.unsqueeze(2).broadcast_to([128, Q, n])
                lgb = r36
                P.op(dve, lambda e: e.tensor_tensor(lgb[:, :, :], pmm[3][:, 0:Q * 36].rearrange("p (q n) -> p q n", q=Q), rbias[:, :, :], ALU.add),
                     reads=[pmm[3], rbias], writes=[lgb])
                gmask, gsh, gex = r4
                P.op(dve, lambda e: e.tensor_reduce(S(0), lgb[:, :, 0:4], AX.X, ALU.max), reads=[lgb], writes=[rs])
                P.op(dve, lambda e: e.tensor_tensor(gmask[:, :, :], lgb[:, :, 0:4], bc(S(0), 4), ALU.is_equal), reads=[lgb, rs], writes=[gmask])
                P.op(dve, lambda e: e.tensor_tensor(gsh[:, :, :], lgb[:, :, 0:4], bc(S(0), 4), ALU.subtract), reads=[lgb, rs], writes=[gsh])
                P.op(act, lambda e: e.activation(out=gex[:, :, :], in_=gsh[:, :, :], func=AF.Exp), reads=[gsh], writes=[gex])
                P.op(dve, lambda e: e.tensor_reduce(S(1), gex[:, :, :], AX.X, ALU.add), reads=[gex], writes=[rs])
                P.op(dve, lambda e: e.reciprocal(S(2), S(1)), reads=[rs], writes=[rs])
                le4 = lgb[:, :, 4:36].rearrange("p q (g j) -> p q g j", g=4)
                tmp32 = r32[0]
                P.op(dve, lambda e: e.tensor_tensor(tmp32[:, :, :].rearrange("p q (g j) -> p q g j", g=4), le4,
                                                    gmask[:, :, :].unsqueeze(3).broadcast_to([128, Q, 4, 8]), ALU.mult), reads=[lgb, gmask], writes=[tmp32])
                sel, top8, oh1, oh2 = r8
                P.op(dve, lambda e: e.tensor_reduce(sel[:, :, :], tmp32[:, :, :].rearrange("p q (g j) -> p q j g", g=4), AX.X, ALU.add), reads=[tmp32], writes=[sel])
                yield
                for q in range(Q):
                    P.op(dve, lambda e, q=q: e.max(top8[:, q, :], sel[:, q, :]), reads=[sel], writes=[top8])
                P.op(dve, lambda e: e.tensor_tensor(oh1[:, :, :], sel[:, :, :], top8[:, :, 0:1].broadcast_to([128, Q, 8]), ALU.is_equal), reads=[sel, top8], writes=[oh1])
                P.op(dve, lambda e: e.tensor_tensor(oh2[:, :, :], sel[:, :, :], top8[:, :, 1:2].broadcast_to([128, Q, 8]), ALU.is_equal), reads=[sel, top8], writes=[oh2])
                P.op(dve, lambda e: e.tensor_tensor(S(3), top8[:, :, 1], top8[:, :, 0], ALU.subtract), reads=[top8], writes=[rs])
                P.op(act, lambda e: e.activation(out=S(4), in_=S(3), func=AF.Exp), reads=[rs], writes=[rs])
                P.op(dve, lambda e: e.tensor_scalar(S(5), S(4), 1.0, None, ALU.add), reads=[rs], writes=[rs])
                P.op(dve, lambda e: e.reciprocal(S(6), S(5)), reads=[rs], writes=[rs])
                P.op(dve, lambda e: e.tensor_tensor(S(7), S(6), S(2), ALU.mult), reads=[rs], writes=[rs])
                P.op(dve, lambda e: e.tensor_tensor(S(8), S(2), S(7), ALU.subtract), reads=[rs], writes=[rs])
                E1, E2 = r32[1], r32[2]
                for Ek, oh in ((E1, oh1), (E2, oh2)):
                    P.op(dve, lambda e, Ek=Ek, oh=oh: e.tensor_tensor(Ek[:, :, :].rearrange("p q (g j) -> p q g j", g=4),
                                                                      gmask[:, :, :].unsqueeze(3).broadcast_to([128, Q, 4, 8]),
                                                                      oh[:, :, :].unsqueeze(2).broadcast_to([128, Q, 4, 8]), ALU.mult),
                         reads=[gmask, oh], writes=[Ek])
                P.op(dve, lambda e: e.tensor_tensor(mbf[:, :, :], E1[:, :, :], E2[:, :, :], ALU.add), reads=[E1, E2], writes=[mbf])

                def fnc(e):
                    ins = None
                    for q in range(Q):
                        ins = e.matmul(pmm[3][:, 160 + q * 32:160 + (q + 1) * 32], tri[:, :], mbf[:, q, :], start=True, stop=(q == 0))
                        for q2 in range(q):
                            ins = e.matmul(pmm[3][:, 160 + q * 32:160 + (q + 1) * 32], ones[:, :], mbf[:, q2, :], start=False, stop=(q2 == q - 1))
                    for q in range(Q):
                        ins = e.matmul(pmm[3][:, 288:320], ones[:, :], mbf[:, q, :], start=(q == 0), stop=(q == Q - 1))
                    return ins
                P.op(pe, fnc, reads=[tri, ones, mbf], writes=[pmm[3]])
                yield
                tot = r32[3]
                P.op(dve, lambda e: e.tensor_tensor(tot[:, :, :], pmm[3][:, 160:288].rearrange("p (q n) -> p q n", q=Q),
                                                    cntbc[:, :].unsqueeze(1).broadcast_to([128, Q, 32]), ALU.add), reads=[pmm[3], cntbc], writes=[tot])
                P.op(dve, lambda e: e.tensor_tensor(cntbc[:, :], cntbc[:, :], pmm[3][:, 288:320], ALU.add), reads=[pmm[3], cntbc], writes=[cntbc])
                tm = r32[4]
                for kk, Ek in ((0, E1), (1, E2)):
                    P.op(dve, lambda e, Ek=Ek: e.tensor_tensor(tm[:, :, :], Ek[:, :, :], tot[:, :, :], ALU.mult), reads=[Ek, tot], writes=[tm])
                    P.op(dve, lambda e, kk=kk: e.tensor_reduce(S(10 + kk), tm[:, :, :], AX.X, ALU.add), reads=[tm], writes=[rs])
                    P.op(dve, lambda e, Ek=Ek: e.tensor_tensor(tm[:, :, :], Ek[:, :, :], iota[:, :, :], ALU.mult), reads=[Ek, iota], writes=[tm])
                    P.op(dve, lambda e, kk=kk: e.tensor_reduce(S(12 + kk), tm[:, :, :], AX.X, ALU.add), reads=[tm], writes=[rs])
                    P.op(dve, lambda e, kk=kk: e.scalar_tensor_tensor(S(14 + kk), S(12 + kk), float(CAP), S(10 + kk), ALU.mult, ALU.add), reads=[rs], writes=[rs])
                    P.op(dve, lambda e, kk=kk: e.tensor_single_scalar(S(16 + kk), S(10 + kk), float(CAP), ALU.is_lt), reads=[rs], writes=[rs])
                    P.op(dve, lambda e, kk=kk: e.tensor_scalar(S(14 + kk), S(14 + kk), float(-TRASH), None, ALU.add), reads=[rs], writes=[rs])
                    P.op(dve, lambda e, kk=kk: e.tensor_tensor(S(14 + kk), S(14 + kk), S(16 + kk), ALU.mult), reads=[rs], writes=[rs])
                    P.op(dve, lambda e, kk=kk: e.tensor_scalar(S(14 + kk), S(14 + kk), float(TRASH), 0.0, ALU.add, ALU.max), reads=[rs], writes=[rs])
                    P.op(dve, lambda e, kk=kk: e.tensor_scalar(rinfo[:, ti, kk, :], S(14 + kk), float(TRASH), None, ALU.min), reads=[rs], writes=[rinfo])
                    P.op(dve, lambda e, kk=kk: e.tensor_tensor(rinfo[:, ti, 2 + kk, :], S(7 + kk), S(16 + kk), ALU.mult), reads=[rs], writes=[rinfo])
                    yield
                P.op(dve, lambda e: e.tensor_copy(sloti[:, ti, :, :], rinfo[:, ti, 0:2, :]), reads=[rinfo], writes=[sloti])
                for q in range(Q):
                    for kk in range(2):
                        P.dma(pool, lambda e, q=q, kk=kk: e.indirect_dma_start(
                            out=hs_d[:, :], out_offset=IOA(ap=sloti[:, ti, kk, q:q + 1], axis=0), in_=hfn[q][:, :], in_offset=None),
                            hfn[q], reads=[hfn[q], sloti])

            def collect(genfunc, ti):
                items = []
                orig_op, orig_dma = P.op, P.dma
                P.op = lambda eng, fn, reads=(), writes=(): items.append((orig_op, (eng, fn), dict(reads=reads, writes=writes), eng))
                P.dma = lambda q, fn, sem_buf, reads=(), writes=(): items.append((orig_dma, (q, fn, sem_buf), dict(reads=reads, writes=writes), q))
                try:
                    for tok in genfunc(ti):
                        items.append(tok)
                finally:
                    del P.op, P.dma
                return items

            def stages1(items):
                out, cur, prev = [], [], None
                for it in items:
                    if it is None or (HOP and prev is not None and it[3] is not prev):
                        out.append(cur)
                        cur = []
                    if it is None:
                        prev = None
                    else:
                        cur.append(it)
                        prev = it[3]
                out.append(cur)
                res = []
                for st in out:
                    if not st:
                        continue
                    res.append(st)
                    if LOADLAG and all(it[3] is sp and it[2]["writes"] for it in st):
                        res.extend([[] for _ in range(LOADLAG)])
                return res

            def zip_locked(chains):
                k = len(chains)
                if k == 1:
                    return chains[0]
                spans, wsets = [], []
                for L in chains:
                    fw, lr, ws = {}, {}, set()
                    for si, st in enumerate(L):
                        for (f, args, kw, eng) in st:
                            for bb in kw["writes"]:
                                fw.setdefault(id(bb), si)
                                ws.add(id(bb))
                            for bb in kw["reads"]:
                                lr[id(bb)] = si
                    spans.append({x: (fw[x], lr[x]) for x in fw if x in lr and lr[x] > fw[x]})
                    wsets.append(ws)
                out, pos, owner = [], [0] * k, {}
                while any(pos[c] < len(chains[c]) for c in range(k)):
                    progressed = False
                    for c in range(k):
                        if pos[c] >= len(chains[c]):
                            continue
                        st = chains[c][pos[c]]
                        W = set(id(bb) for (f, args, kw, eng) in st for bb in kw["writes"])
                        if any(owner.get(x) not in (None, c) for x in W):
                            continue
                        out.append(st)
                        progressed = True
                        for x in W:
                            if x in spans[c] and any(x in wsets[j] for j in range(k) if j != c):
                                owner[x] = c
                        for x in list(owner):
                            if owner[x] == c and pos[c] >= spans[c][x][1]:
                                owner[x] = None
                        pos[c] += 1
                    assert progressed, "chain lock deadlock"
                return out

            def stages(items):
                segs = [[[]]]
                for it in items:
                    if it == "SEG":
                        segs.append([[]])
                    elif it == "CHAIN":
                        segs[-1].append([])
                    else:
                        segs[-1][-1].append(it)
                out = []
                for seg in segs:
                    out += zip_locked([stages1(ch) for ch in seg if ch])  if any(seg) else []
                return out

            def spread(genfunc, ti, n, off=0):
                sts = stages(collect(genfunc, ti))
                assert len(sts) <= n - off, (len(sts), n, off)
                k = 0
                for t in range(n):
                    while t >= off and k < len(sts) and k * (n - off) < (t - off + 1) * len(sts):
                        for f, args, kw, eng in sts[k]:
                            f(*args, **kw)
                        k += 1
                    yield

            counts = [len(stages(collect(g, 1))) for g in (gen_F, gen_Mc, gen_Ml, gen_E)]
            NSTG = max(counts) + 2
            print("stages per stream", counts, NSTG)

            def both(g1, g2):
                for _ in g1:
                    next(g2)
                    yield

            def tile_gen(ti):
                yield from spread(gen_F, ti, NSTG)
                yield from both(spread(gen_Mc, ti, NSTG, MC_OFF), spread(gen_Ml, ti, NSTG))
                yield from spread(gen_E, ti, NSTG)

            run_pipelined((tile_gen(ti) for ti in range(NT)), depth=3, skew=NSTG)

            if debug:
                P.dma(sp, lambda e: e.dma_start(out=ri_d, in_=rinfo[:, :, :, :].rearrange("p a b c -> p (a b c)")), rinfo, reads=[rinfo])
            P.barrier()
            P.flush(block)

        with ExitStack() as sb:
            B = lambda shape, dt=F32, name=None, dma=False: P.buf(sb, shape, dt, name, dma)
            gffnbc = B([128, 8], F32, "gffnbc", dma=True)
            P.dma(sp, lambda e: e.dma_start(out=gffnbc[:, :], in_=gbc_d[1]), gffnbc, writes=[gffnbc])
            zt = B([128, D], F32, "zt", dma=True)
            P.op(dve, lambda e: e.memset(zt[:, :], 0.0), writes=[zt])
            P.dma(sp, lambda e: e.dma_start(out=y_d[NSLOT:NSLOT + 128, :], in_=zt[:, :]), zt, reads=[zt])
            w1 = [B([128, 8, 512], BF16, "w1") for _ in range(2)]
            w3 = [B([128, 8, 512], BF16, "w3") for _ in range(2)]
            w2 = [B([128, 4, D], BF16, "w2") for _ in range(2)]
            w3s = B([128, 8, 512], F32, "w3s", dma=True)
            w1s = B([128, 8, 512], F32, "w1s", dma=True)
            w2s = B([128, 4, D], F32, "w2s", dma=True)
            hst = [B([128, NSUB, D], BF16, "hst", dma=True) for _ in range(2)]
            hfTe = [B([128, 8, CAP], BF16, "hfTe") for _ in range(2)]
            actT = B([128, 4, CAP], BF16, "actT")
            tb = [B([128, 512], F32, "tb") for _ in range(2)]
            tc = [B([128, 512], F32, "tc") for _ in range(2)]
            yt = [B([128, D], F32, "yt", dma=True) for _ in range(3)]
            ntiles = [(0, 512)] if CAP == 512 else ([(n0, min(512, CAP - n0)) for n0 in range(0, CAP, 512)])
            yi = 0
            ci = 0

            def load_expert(ex):
                sl = ex % 2
                P.dma(sp, lambda e: e.dma_start(out=hst[sl][:, :, :], in_=hs_d[ex * CAP:(ex + 1) * CAP, :].rearrange("(s p) n -> p s n", p=128)),
                      hst[sl], writes=[hst[sl]])
                P.dma(sp, lambda e: e.dma_start(out=w1s[:, :, :], in_=w1_d[ex].rearrange("(k p) n -> p k n", p=128)), w1s, writes=[w1s])
                P.dma(sp, lambda e: e.dma_start(out=w3s[:, :, :], in_=w3_d[ex].rearrange("(k p) n -> p k n", p=128)), w3s, writes=[w3s])
                P.dma(sp, lambda e: e.dma_start(out=w2s[:, :, :], in_=w2_d[ex].rearrange("(k p) n -> p k n", p=128)), w2s, writes=[w2s])

            def cast_expert(ex):
                sl = ex % 2
                for k in range(8):
                    P.op(pool, lambda e, k=k: e.tensor_tensor(w1[sl][:, k, :], w1s[:, k, :], gffnbc[:, k:k + 1].broadcast_to([128, 512]), ALU.mult),
                         reads=[w1s, gffnbc], writes=[w1[sl]])
                for k in range(8):
                    P.op(act, lambda e, k=k: e.activation(out=w3[sl][:, k, :], in_=w3s[:, k, :], func=AF.Copy, scale=gffnbc[:, k:k + 1]), reads=[w3s, gffnbc], writes=[w3[sl]])
                for k in range(4):
                    P.op(act, lambda e, k=k: e.activation(out=w2[sl][:, k, :], in_=w2s[:, k, :], func=AF.Copy), reads=[w2s], writes=[w2[sl]])

            pool6 = pmm + [ps1, ps2]
            p6 = [0]

            def next6():
                bb = pool6[p6[0] % 6]
                p6[0] += 1
                return bb

            def do_T(ex):
                sl = ex % 2
                hT_e = hfTe[sl]
                for sbt in range(NSUB):
                    pt_ = ptr if sbt % 2 == 0 else ptr2

                    def fn(e, sbt=sbt, pt_=pt_, sl=sl):
                        ins = None
                        for c in range(8):
                            ins = e.transpose(pt_[:, c * 128:(c + 1) * 128], hst[sl][:, sbt, c * 128:(c + 1) * 128], ident[:, :])
                        return ins
                    P.op(pe, fn, reads=[hst[sl], ident], writes=[pt_])
                    P.op(dve, lambda e, sbt=sbt, pt_=pt_, hT_e=hT_e: e.tensor_copy(hT_e[:, :, sbt * 128:(sbt + 1) * 128], pt_[:, :].rearrange("p (c j) -> p c j", c=8)),
                         reads=[pt_], writes=[hT_e])

            def do_H(ex):
                sl = ex % 2
                hT_e = hfTe[sl]
                for (n0, nn) in ntiles:
                    for m in range(4):
                        p1, p3 = next6(), next6()
                        for (pb, wt) in ((p1, w1[sl]), (p3, w3[sl])):
                            def fn(e, pb=pb, wt=wt, m=m, n0=n0, nn=nn, hT_e=hT_e):
                                ins = None
                                for k in range(8):
                                    ins = e.matmul(pb[:, 0:nn], wt[:, k, m * 128:(m + 1) * 128], hT_e[:, k, n0:n0 + nn], start=(k == 0), stop=(k == 7))
                                return ins
                            P.op(pe, fn, reads=[wt, hT_e], writes=[pb])
                        tb_, tc_ = tb[ci_[0] % 2], tc[ci_[0] % 2]
                        ci_[0] += 1
                        P.op(act, lambda e, p1=p1, tb_=tb_, nn=nn: e.activation(out=tb_[:, 0:nn], in_=p1[:, 0:nn], func=AF.Tanh, scale=0.5), reads=[p1], writes=[tb_])
                        P.op(dve, lambda e, p1=p1, tb_=tb_, tc_=tc_, nn=nn: e.scalar_tensor_tensor(tc_[:, 0:nn], tb_[:, 0:nn], 1.0, p1[:, 0:nn], ALU.add, ALU.mult),
                             reads=[tb_, p1], writes=[tc_])
                        P.op(dve, lambda e, p3=p3, tc_=tc_, nn=nn, m=m, n0=n0: e.scalar_tensor_tensor(actT[:, m, n0:n0 + nn], tc_[:, 0:nn], 0.5, p3[:, 0:nn], ALU.mult, ALU.mult),
                             reads=[tc_, p3], writes=[actT])

            def do_Y(ex):
                sl = ex % 2
                for sbt in range(NSUB):
                    yb = yt[yi_[0] % 3]
                    yi_[0] += 1
                    for h in range(2):
                        pb = next6()

                        def fn(e, pb=pb, h=h, sbt=sbt, sl=sl):
                            ins = None
                            for m in range(4):
                                ins = e.matmul(pb[:, :], actT[:, m, sbt * 128:(sbt + 1) * 128], w2[sl][:, m, h * 512:(h + 1) * 512], start=(m == 0), stop=(m == 3))
                            return ins
                        P.op(pe, fn, reads=[actT, w2[sl]], writes=[pb])
                        P.op(act, lambda e, pb=pb, h=h, yb=yb: e.activation(out=yb[:, h * 512:(h + 1) * 512], in_=pb[:, :], func=AF.Copy), reads=[pb], writes=[yb])
                    r0 = ex * CAP + sbt * 128
                    P.dma(sp, lambda e, yb=yb, r0=r0: e.dma_start(out=y_d[r0:r0 + 128, :], in_=yb[:, :]), yb, reads=[yb])

            ci_, yi_ = [0], [0]
            load_expert(0)
            cast_expert(0)
            do_T(0)
            for ex in range(32):
                if ex + 1 < 32:
                    load_expert(ex + 1)
                do_H(ex)
                if ex + 1 < 32:
                    cast_expert(ex + 1)
                    do_T(ex + 1)
                do_Y(ex)
            P.barrier()
            P.flush(block)

        with ExitStack() as sc:
            B = lambda shape, dt=F32, name=None, dma=False: P.buf(sc, shape, dt, name, dma)
            gplebc = B([128, 8], F32, "gplebc", dma=True)
            gpp = B([128, D], F32, "gpp", dma=True)
            gfin = B([128, D], F32, "gfin", dma=True)
            wple = B([128, 2, D], BF16, "wple", dma=True)
            wpg = B([128, 8, D], BF16, "wpg", dma=True)
            P.dma(sp, lambda e: e.dma_start(out=gplebc[:, :], in_=gbc_d[2]), gplebc, writes=[gplebc])
            P.dma(sp, lambda e: e.dma_start(out=gpp[:, :], in_=rowbc_d[0]), gpp, writes=[gpp])
            P.dma(sp, lambda e: e.dma_start(out=gfin[:, :], in_=rowbc_d[1]), gfin, writes=[gfin])
            for k in range(2):
                P.dma(pool, lambda e, k=k: e.dma_start(out=wple[:, k, :], in_=wple_d[k * 128:(k + 1) * 128, :]), wple, writes=[wple])
            for k in range(8):
                P.dma(pool, lambda e, k=k: e.dma_start(out=wpg[:, k, :], in_=wpg_d[k * 128:(k + 1) * 128, :]), wpg, writes=[wpg])
            for k in range(8):
                P.op(dve, lambda e, k=k: e.tensor_scalar(wpg[:, k, :], wpg[:, k, :], gplebc[:, k:k + 1], None, ALU.mult), reads=[wpg, gplebc], writes=[wpg])
            x1t_ = [B([128, D], F32, "x1c", dma=True) for _ in range(NP_C)]
            pt_b = [B([128, 256], F32, "pc", dma=True) for _ in range(NP_C)]
            y1_ = [B([128, D], F32, "y1c", dma=True) for _ in range(NP_C)]
            y2_ = [B([128, D], F32, "y2c", dma=True) for _ in range(NP_C)]
            NP = NP_C
            ssC_ = [B([128, 16], F32, "ssC") for _ in range(NP)]
            xn3 = [B([128, D], BF16, "xn3") for _ in range(NP)]
            junk, te, ob = xn3, y2_, x1t_
            x3T = [B([128, 8, 128], BF16, "x3T") for _ in range(NP)]
            pbf = [B([128, 256], BF16, "pbf") for _ in range(NP)]
            pT = [B([128, 2, 128], BF16, "pT") for _ in range(NP)]
            thg = [B([128, D], F32, "thg") for _ in range(NP)]

            def rsq(ssb, i_v, i_l, i_o):
                P.op(act, lambda e: e.activation(out=ssb[:, i_l:i_l + 1], in_=ssb[:, i_v:i_v + 1], func=AF.Ln), reads=[ssb], writes=[ssb])
                P.op(act, lambda e: e.activation(out=ssb[:, i_o:i_o + 1], in_=ssb[:, i_l:i_l + 1], func=AF.Exp, scale=-0.5), reads=[ssb], writes=[ssb])

            def subtile_gen(st):
                ti, q = divmod(st, Q)
                r0 = st * 128
                i3 = st % NP
                xx, pp, y1, y2 = x1t_[i3], pt_b[i3], y1_[i3], y2_[i3]
                jk, ssC = junk[i3], ssC_[i3]
                xn_, x3_, pb_, pT_, tg_, te_, ob_ = xn3[i3], x3T[i3], pbf[i3], pT[i3], thg[i3], te[i3], ob[i3]
                P.dma(sp, lambda e: e.dma_start(out=xx[:, :], in_=x1_d[r0:r0 + 128, :]), xx, writes=[xx])
                P.dma(sp, lambda e: e.dma_start(out=pp[:, :], in_=p_d[r0:r0 + 128, :]), pp, writes=[pp])
                for (yy, kk) in ((y1, 0), (y2, 1)):
                    P.dma(pool, lambda e, yy=yy, kk=kk: e.indirect_dma_start(
                        out=yy[:, :], out_offset=None, in_=y_d[:, :], in_offset=IOA(ap=sloti[:, ti, kk, q:q + 1], axis=0)),
                        yy, reads=[sloti], writes=[yy])
                yield
                for (yy, kk) in ((y1, 0), (y2, 1)):
                    P.op(dve, lambda e, yy=yy, kk=kk: e.scalar_tensor_tensor(xx[:, :], yy[:, :], rinfo[:, ti, 2 + kk, q:q + 1], xx[:, :], ALU.mult, ALU.add),
                         reads=[yy, rinfo, xx], writes=[xx])
                yield
                P.op(act, lambda e: e.activation(out=jk[:, :], in_=xx[:, :], func=AF.Square, accum_out=ssC[:, 0:1]), reads=[xx], writes=[jk, ssC])
                P.op(act, lambda e: e.activation(out=pb_[:, :], in_=pp[:, :], func=AF.Copy), reads=[pp], writes=[pb_])
                yield
                P.op(dve, lambda e: e.tensor_scalar(ssC[:, 1:2], ssC[:, 0:1], 1.0 / D, EPS, ALU.mult, ALU.add), reads=[ssC], writes=[ssC])
                yield
                rsq(ssC, 1, 3, 2)
                P.op(act, lambda e: e.activation(out=xn_[:, :], in_=xx[:, :], func=AF.Copy, scale=ssC[:, 2:3]), reads=[xx, ssC], writes=[xn_])
                yield
                transposes(xn_, 8, ptr)
                P.op(dve, lambda e: e.tensor_copy(x3_[:, :, :], ptr[:, :].rearrange("p (c j) -> p c j", c=8)), reads=[ptr], writes=[x3_])
                transposes(pb_, 2, ptr2)
                P.op(act, lambda e: e.activation(out=pT_[:, :, :], in_=ptr2[:, 0:256].rearrange("p (c j) -> p c j", c=2), func=AF.Copy), reads=[ptr2], writes=[pT_])
                yield
                pes = []
                for h in range(2):
                    pg_ = next_pmm()

                    def fn(e, pg_=pg_, h=h):
                        ins = None
                        for k in range(8):
                            ins = e.matmul(pg_[:, :], x3_[:, k, :], wpg[:, k, h * 512:(h + 1) * 512], start=(k == 0), stop=(k == 7))
                        return ins
                    P.op(pe, fn, reads=[x3_, wpg], writes=[pg_])
                    P.op(act, lambda e, pg_=pg_, h=h: e.activation(out=tg_[:, h * 512:(h + 1) * 512], in_=pg_[:, :], func=AF.Tanh, scale=0.5), reads=[pg_], writes=[tg_])
                    pe_ = next_pmm()

                    def fn2(e, pe_=pe_, h=h):
                        ins = None
                        for k in range(2):
                            ins = e.matmul(pe_[:, :], pT_[:, k, :], wple[:, k, h * 512:(h + 1) * 512], start=(k == 0), stop=(k == 1))
                        return ins
                    P.op(pe, fn2, reads=[pT_, wple], writes=[pe_])
                    P.op(act, lambda e, pe_=pe_, h=h: e.activation(out=jk[:, 0:512], in_=pe_[:, :], func=AF.Square, accum_out=ssC[:, 4 + h:5 + h]), reads=[pe_], writes=[jk, ssC])
                    pes.append(pe_)
                yield
                P.op(dve, lambda e: e.tensor_tensor(ssC[:, 6:7], ssC[:, 4:5], ssC[:, 5:6], ALU.add), reads=[ssC], writes=[ssC])
                P.op(dve, lambda e: e.tensor_scalar(ssC[:, 7:8], ssC[:, 6:7], 4.0 / D, 4.0 * EPS, ALU.mult, ALU.add), reads=[ssC], writes=[ssC])
                rsq(ssC, 7, 9, 8)
                yield
                for h in range(2):
                    P.op(dve, lambda e, h=h, pe_=pes[h]: e.scalar_tensor_tensor(te_[:, h * 512:(h + 1) * 512], pe_[:, :], ssC[:, 8:9], gpp[:, h * 512:(h + 1) * 512], ALU.mult, ALU.mult),
                         reads=[pes[h], ssC, gpp], writes=[te_])
                P.op(dve, lambda e: e.scalar_tensor_tensor(te_[:, :], tg_[:, :], 1.0, te_[:, :], ALU.add, ALU.mult), reads=[tg_, te_], writes=[te_])
                P.op(pool, lambda e: e.tensor_tensor(xx[:, :], xx[:, :], te_[:, :], ALU.add), reads=[te_, xx], writes=[xx])
                yield
                P.op(act, lambda e: e.activation(out=jk[:, :], in_=xx[:, :], func=AF.Square, accum_out=ssC[:, 10:11]), reads=[xx], writes=[jk, ssC])
                P.op(dve, lambda e: e.tensor_scalar(ssC[:, 11:12], ssC[:, 10:11], 1.0 / D, EPS, ALU.mult, ALU.add), reads=[ssC], writes=[ssC])
                rsq(ssC, 11, 13, 12)
                yield
                P.op(dve, lambda e: e.scalar_tensor_tensor(ob_[:, :], xx[:, :], ssC[:, 12:13], gfin[:, :], ALU.mult, ALU.mult), reads=[xx, ssC, gfin], writes=[ob_])
                P.dma(sp, lambda e: e.dma_start(out=out_d[r0:r0 + 128, :], in_=ob_[:, :]), ob_, reads=[ob_])

            run_pipelined((subtile_gen(st) for st in range(T // 128)), depth=NP, skew=2)
            P.barrier()
            P.flush(block)
    return nc


def _chan(v):
    return np.ascontiguousarray(np.asarray(v, np.float32).reshape(4, 128).T)


def _gbc(g):
    return np.ascontiguousarray(np.asarray(g, np.float32).reshape(8, 128).T)


def prep_shared(inp):
    f = lambda a: np.ascontiguousarray(np.asarray(a, np.float32))
    cpar = np.zeros((128, NCP), np.float32)
    cw = f(inp["conv_dw_w"][0])
    for c in range(4):
        cpar[:, CW + c * 31:CW + (c + 1) * 31] = cw[:, c * 128:(c + 1) * 128].T
    cpar[:, CB:CB + 4] = _chan(inp["conv_dw_b"][0])
    cpar[:, LG:LG + 4] = _chan(inp["conv_ln_g"][0])
    cpar[:, LB:LB + 4] = _chan(inp["conv_ln_b"][0])
    lw = f(inp["lru_conv_w"][0])
    for c in range(4):
        cpar[:, LW + c * 4:LW + (c + 1) * 4] = lw[:, c * 128:(c + 1) * 128].T
    cpar[:, LBB:LBB + 4] = _chan(inp["lru_conv_b"][0])
    cpar[:, BR:BR + 4] = _chan(inp["lru_b_r"][0])
    cpar[:, BI:BI + 4] = _chan(inp["lru_b_i"][0])
    cpar[:, LAM:LAM + 4] = _chan(inp["lru_lambda"][0])
    wr_, wi_ = f(inp["lru_w_r"][0]), f(inp["lru_w_i"][0])
    gbd = np.zeros((4, 128, 256), np.float32)
    for c in range(4):
        for hh in range(2):
            gbd[c, hh * 64:(hh + 1) * 64, hh * 64:(hh + 1) * 64] = wr_[2 * c + hh]
            gbd[c, hh * 64:(hh + 1) * 64, 128 + hh * 64:128 + (hh + 1) * 64] = wi_[2 * c + hh]
    rb = np.concatenate([f(inp["b_group"][0]), f(inp["b_expert"][0])])
    shared = {
        "w_in": f(inp["w_in"][0]), "w_out": f(inp["w_out"][0]),
        "w_route": np.ascontiguousarray(np.concatenate([f(inp["w_group"][0]), f(inp["w_expert"][0])], axis=1)),
        "gate_bd": gbd, "cpar": cpar,
        "gbc": np.stack([_gbc(inp["g_mix"][0]), _gbc(inp["g_ffn"][0]), _gbc(inp["g_ple"][0])]),
        "rowbc": np.stack([np.ascontiguousarray(np.broadcast_to(f(inp["g_ple_proj"][0]), (128, D))),
                           np.ascontiguousarray(np.broadcast_to(f(inp["g_final"]), (128, D)))]),
        "rbias": np.ascontiguousarray(np.broadcast_to(np.tile(rb, Q), (128, Q * 36))),
        "iota_e": np.ascontiguousarray(np.broadcast_to(np.tile(np.arange(32, dtype=np.float32), Q), (128, Q * 32))),
        "ident": np.eye(128, dtype=np.float32),
        "tri": np.ascontiguousarray(np.triu(np.ones((128, 128), np.float32), 1)),
        "w1": f(inp["w1"][0]), "w3": f(inp["w3"][0]), "w2": f(inp["w2"][0]),
        "w_ple": f(inp["w_ple"][0]), "w_ple_gate": f(inp["w_ple_gate"][0]),
    }
    return shared


def kernel(**inputs):
    NSEQ, CAP = 4, 1024
    x = np.asarray(inputs["x"], np.float32)
    p = np.asarray(inputs["p"], np.float32)[0]
    shared = prep_shared(inputs)
    nc = build(NSEQ, CAP)
    in_maps = []
    for i in range(N_CORES):
        m = dict(shared)
        m["x"] = np.ascontiguousarray(x[i * NSEQ:(i + 1) * NSEQ].reshape(NSEQ * SEQ, D))
        m["p"] = np.ascontiguousarray(p[i * NSEQ:(i + 1) * NSEQ].reshape(NSEQ * SEQ, 256))
        in_maps.append(m)
    res = run_bass_kernel_spmd(nc, in_maps, core_ids=list(range(N_CORES)))
    out = np.concatenate([np.asarray(r["out"], np.float32).reshape(NSEQ, SEQ, D) for r in res.results], axis=0)
    return out
```

```python
import numpy as np
from contextlib import ExitStack
import concourse.bass as bass
import concourse.mybir as mybir
from concourse.bass_utils import run_bass_kernel_spmd

F32 = mybir.dt.float32
BF16 = mybir.dt.bfloat16
I32 = mybir.dt.int32
ALU = mybir.AluOpType
AF = mybir.ActivationFunctionType
AX = mybir.AxisListType

N_CORES = 8
HOP = True
NP_C = 8
LOADLAG = 3
MC_OFF = 0
SEQ = 2048
D = 1024
EPS = 1e-6
Q = 4

CW = 0
CB = CW + 124
LG = CB + 4
LB = LG + 4
LW = LB + 4
LBB = LW + 16
BR = LBB + 4
BI = BR + 4
LAM = BI + 4
NCP = LAM + 4
D_CWH = 0
D_GH = 124
D_BH = 128
D_BRH = 132
D_BIH = 136
D_N4 = 140
D_N8 = 144
NDP = 148


class Src:
    def __init__(self, sem, name, is_dma):
        self.sem, self.name, self.is_dma, self.total = sem, name, is_dma, 0


class Eng(Src):
    def __init__(self, sem, name, blockname, same_wait=True):
        super().__init__(sem, name, False)
        self.blockname, self.ops, self.seen, self.same_wait = blockname, [], {}, same_wait


class Buf:
    def __init__(self, t, dsem=None):
        self.t, self.w, self.r, self.dsem = t, None, {}, dsem

    def __getitem__(self, k):
        return self.t[k]


class Prog:
    def __init__(self, nc, stack):
        self.nc, self.stack = nc, stack
        self.srcs = []
        mk = lambda n, b, sw=True: self._reg(Eng(self._sem("e_" + n), n, b, sw))
        self.pe = mk("pe", "tensor", False)
        self.act = mk("act", "scalar")
        self.dve = mk("dve", "vector")
        self.pool = mk("pool", "gpsimd")
        self.sp = mk("sp", "sync")
        self.engs = [self.pe, self.act, self.dve, self.pool, self.sp]
        self.nbuf = 0

    def _sem(self, name):
        return self.stack.enter_context(self.nc.semaphore(name))

    def _reg(self, s):
        self.srcs.append(s)
        return s

    def buf(self, stack, shape, dt, name=None, dma=False, psum=False):
        self.nbuf += 1
        name = "%s_%d" % (name or "b", self.nbuf)
        if psum:
            t = stack.enter_context(self.nc.psum_tensor(name, shape, dt))
        else:
            t = stack.enter_context(self.nc.sbuf_tensor(name, shape, dt))
        ds = self._reg(Src(self._sem("d_" + name), name, True)) if dma else None
        return Buf(t, ds)

    def _deps(self, eng, reads, writes):
        need = {}

        def add(src, val):
            if src.is_dma:
                val = src.total
            if need.get(src, 0) < val:
                need[src] = val

        for b in reads:
            if b.w is not None:
                add(*b.w)
        for b in writes:
            if b.w is not None:
                add(*b.w)
            for s, v in b.r.items():
                add(s, v)
        waits = []
        for src, val in need.items():
            if src is eng and not eng.same_wait:
                continue
            if eng.seen.get(src, 0) >= val:
                continue
            eng.seen[src] = val
            waits.append((src.sem, val))
        return waits

    def _mark(self, src, val, reads, writes):
        for b in reads:
            b.r[src] = val
        for b in writes:
            b.w = (src, val)
            b.r = {}

    def op(self, eng, fn, reads=(), writes=()):
        waits = self._deps(eng, reads, writes)
        eng.total += 1
        sem = eng.sem

        def emit(e):
            for s, v in waits:
                e.wait_ge(s, v)
            fn(e).then_inc(sem, 1)

        eng.ops.append(emit)
        self._mark(eng, eng.total, reads, writes)

    def dma(self, q, fn, sem_buf, reads=(), writes=()):
        src = sem_buf.dsem
        waits = self._deps(q, reads, writes)
        src.total += 16
        sem = src.sem

        def emit(e):
            for s, v in waits:
                e.wait_ge(s, v)
            fn(e).then_inc(sem, 16)

        q.ops.append(emit)
        self._mark(src, src.total, reads, writes)

    def barrier(self):
        for E in self.engs:
            waits = []
            for S in self.srcs:
                if S is E or S.total == 0:
                    continue
                if E.seen.get(S, 0) >= S.total:
                    continue
                E.seen[S] = S.total
                waits.append((S.sem, S.total))

            def emit(e, waits=waits):
                for s, v in waits:
                    e.wait_ge(s, v)

            E.ops.append(emit)

    def flush(self, block):
        for E in self.engs:
            if not E.ops:
                continue
            ops = E.ops
            E.ops = []

            def body(e, ops=ops):
                for f in ops:
                    f(e)

            getattr(block, E.blockname)(body)


def run_pipelined(gens, depth, skew):
    it = iter(gens)
    active, pending, tick = [], True, 0
    while pending or active:
        for g in list(active):
            try:
                next(g)
            except StopIteration:
                active.remove(g)
        if pending and tick % skew == 0 and len(active) < depth:
            try:
                g = next(it)
                active.append(g)
                next(g)
            except StopIteration:
                pending = False
        tick += 1


def build(NSEQ=4, CAP=640, debug=False):
    T = NSEQ * SEQ
    NT = T // 512
    NSUB = CAP // 128
    NSLOT = 32 * CAP
    TRASH = NSLOT
    nc = bass.Bass("TRN2", target_bir_lowering=False)

    def dr(name, shape, dt=F32, kind="ExternalInput"):
        return nc.dram_tensor(name, shape, dt, kind=kind).ap()

    x_d = dr("x", [T, D])
    p_d = dr("p", [T, 256])
    win_d = dr("w_in", [D, 2048])
    wout_d = dr("w_out", [D, D])
    wr_d = dr("w_route", [D, 36])
    gbd_d = dr("gate_bd", [4, 128, 256])
    cpar_d = dr("cpar", [128, NCP])
    gbc_d = dr("gbc", [3, 128, 8])
    rowbc_d = dr("rowbc", [2, 128, D])
    rb_d = dr("rbias", [128, Q * 36])
    iota_d = dr("iota_e", [128, Q * 32])
    ident_d = dr("ident", [128, 128])
    tri_d = dr("tri", [128, 128])
    w1_d = dr("w1", [32, D, 512])
    w3_d = dr("w3", [32, D, 512])
    w2_d = dr("w2", [32, 512, D])
    wple_d = dr("w_ple", [256, D])
    wpg_d = dr("w_ple_gate", [D, D])
    out_d = dr("out", [T, D], kind="ExternalOutput")
    sk = "ExternalOutput" if debug else "Internal"
    x1_d = dr("x1s", [T, D], kind=sk)
    hs_d = dr("hss", [NSLOT + 128, D], BF16, kind=sk)
    y_d = dr("yss", [NSLOT + 128, D], kind=sk)
    if debug:
        ri_d = dr("rinfo_o", [128, NT * 4 * Q], kind="ExternalOutput")

    IOA = bass.IndirectOffsetOnAxis

    with ExitStack() as top:
        P = Prog(nc, top)
        pe, act, dve, pool, sp = P.pe, P.act, P.dve, P.pool, P.sp
        block = top.enter_context(nc.Block())

        pmm = [P.buf(top, [128, 512], F32, "pmm", psum=True) for _ in range(4)]
        ptr = P.buf(top, [128, 1024], BF16, "ptr", psum=True)
        ptr2 = P.buf(top, [128, 1024], BF16, "ptr2", psum=True)
        ps1 = P.buf(top, [128, 512], F32, "ps1", psum=True)
        ps2 = P.buf(top, [128, 512], F32, "ps2", psum=True)
        pmm_i = [0]

        def next_pmm():
            b = pmm[pmm_i[0] % 4]
            pmm_i[0] += 1
            return b

        rinfo = P.buf(top, [128, NT, 4, Q], F32, "rinfo")
        sloti = P.buf(top, [128, NT, 2, Q], I32, "sloti")
        if debug:
            rinfo.dsem = P._reg(Src(P._sem("d_rinfo"), "rinfo", True))
        ident = P.buf(top, [128, 128], BF16, "ident", dma=True)
        P.dma(pool, lambda e: e.dma_start(out=ident[:, :], in_=ident_d), ident, writes=[ident])

        def transposes(src, n, dst_ps):
            def fn(e):
                ins = None
                for c in range(n):
                    ins = e.transpose(dst_ps[:, c * 128:(c + 1) * 128], src[:, c * 128:(c + 1) * 128], ident[:, :])
                return ins
            P.op(pe, fn, reads=[src, ident], writes=[dst_ps])

        def rstd_from_ss(ss, out, n):
            P.op(dve, lambda e: e.tensor_scalar(out, ss, 1.0 / n, EPS, ALU.mult, ALU.add), reads=[], writes=[])

        with ExitStack() as sa:
            B = lambda shape, dt=F32, name=None, dma=False: P.buf(sa, shape, dt, name, dma)
            win = B([128, 8, 2048], BF16, "win", dma=True)
            wout = B([128, 8, D], BF16, "wout", dma=True)
            wr = B([128, 8, 36], BF16, "wr", dma=True)
            gbd = B([128, 4, 256], BF16, "gbd", dma=True)
            tri = B([128, 128], BF16, "tri", dma=True)
            cpar = B([128, NCP], F32, "cpar", dma=True)
            dpar = B([128, NDP], F32, "dpar")
            gmixbc = B([128, 8], F32, "gmixbc", dma=True)
            gffnbc = B([128, 8], F32, "gffnbc", dma=True)
            rbias = B([128, Q, 36], F32, "rbias", dma=True)
            iota = B([128, Q, 32], F32, "iota", dma=True)
            ones = B([128, 128], BF16, "ones")
            cntbc = B([128, 32], F32, "cntbc")

            P.dma(sp, lambda e: e.dma_start(out=cpar[:, :], in_=cpar_d), cpar, writes=[cpar])
            P.dma(sp, lambda e: e.dma_start(out=gmixbc[:, :], in_=gbc_d[0]), gmixbc, writes=[gmixbc])
            P.dma(sp, lambda e: e.dma_start(out=gffnbc[:, :], in_=gbc_d[1]), gffnbc, writes=[gffnbc])
            P.dma(sp, lambda e: e.dma_start(out=rbias[:, :, :], in_=rb_d.rearrange("p (q n) -> p q n", q=Q)), rbias, writes=[rbias])
            P.dma(sp, lambda e: e.dma_start(out=iota[:, :, :], in_=iota_d.rearrange("p (q n) -> p q n", q=Q)), iota, writes=[iota])
            P.dma(pool, lambda e: e.dma_start(out=tri[:, :], in_=tri_d), tri, writes=[tri])
            for k in range(8):
                P.dma(pool, lambda e, k=k: e.dma_start(out=win[:, k, :], in_=win_d[k * 128:(k + 1) * 128, :]), win, writes=[win])
            P.dma(pool, lambda e: e.dma_start(out=gbd[:, :, :], in_=gbd_d.rearrange("c p n -> p c n")), gbd, writes=[gbd])
            for k in range(8):
                P.dma(pool, lambda e, k=k: e.dma_start(out=wout[:, k, :], in_=wout_d[k * 128:(k + 1) * 128, :]), wout, writes=[wout])
            P.dma(pool, lambda e: e.dma_start(out=wr[:, :, :], in_=wr_d.rearrange("(k p) n -> p k n", p=128)), wr, writes=[wr])

            P.op(dve, lambda e: e.memset(ones[:, :], 1.0), writes=[ones])
            P.op(dve, lambda e: e.memset(cntbc[:, :], 0.0), writes=[cntbc])
            for k in range(8):
                P.op(dve, lambda e, k=k: e.tensor_scalar(win[:, k, :], win[:, k, :], gmixbc[:, k:k + 1], None, ALU.mult), reads=[win, gmixbc], writes=[win])
                P.op(dve, lambda e, k=k: e.tensor_scalar(wr[:, k, :], wr[:, k, :], gffnbc[:, k:k + 1], None, ALU.mult), reads=[wr, gffnbc], writes=[wr])

            tsm = B([128, 8, 4], F32, "tsm")

            def dv(fn, reads, writes):
                P.op(dve, fn, reads=reads, writes=writes)

            dv(lambda e: e.tensor_scalar(dpar[:, D_CWH:D_CWH + 124], cpar[:, CW:CW + 124], 0.5, None, ALU.mult), [cpar], [dpar])
            dv(lambda e: e.tensor_scalar(dpar[:, D_GH:D_GH + 8], cpar[:, LG:LG + 8], 0.5, None, ALU.mult), [cpar], [dpar])
            dv(lambda e: e.tensor_scalar(dpar[:, D_BRH:D_BRH + 8], cpar[:, BR:BR + 8], 0.5, None, ALU.mult), [cpar], [dpar])
            z_, az, ee, LL, tt, mk_, zp = [tsm[:, i, :] for i in range(7)]
            dv(lambda e: e.tensor_scalar(z_, cpar[:, LAM:LAM + 4], -1.0, None, ALU.mult), [cpar], [tsm])
            dv(lambda e: e.tensor_tensor(az, z_, cpar[:, LAM:LAM + 4], ALU.max), [tsm, cpar], [tsm])
            P.op(act, lambda e: e.activation(out=ee, in_=az, func=AF.Exp, scale=-1.0), reads=[tsm], writes=[tsm])
            P.op(act, lambda e: e.activation(out=LL, in_=ee, func=AF.Ln, bias=1.0, scale=1.0), reads=[tsm], writes=[tsm])
            dv(lambda e: e.tensor_scalar(tt, ee, -0.25, 1.0 / 3.0, ALU.mult, ALU.add), [tsm], [tsm])
            dv(lambda e: e.tensor_tensor(tt, tt, ee, ALU.mult), [tsm], [tsm])
            dv(lambda e: e.tensor_scalar(tt, tt, -1.0, 0.5, ALU.mult, ALU.add), [tsm], [tsm])
            dv(lambda e: e.tensor_tensor(tt, tt, ee, ALU.mult), [tsm], [tsm])
            dv(lambda e: e.tensor_scalar(tt, tt, -1.0, 1.0, ALU.mult, ALU.add), [tsm], [tsm])
            dv(lambda e: e.tensor_tensor(tt, tt, ee, ALU.mult), [tsm], [tsm])
            dv(lambda e: e.tensor_single_scalar(mk_, ee, 0.05, ALU.is_lt), [tsm], [tsm])
            dv(lambda e: e.tensor_tensor(tt, tt, LL, ALU.subtract), [tsm], [tsm])
            dv(lambda e: e.tensor_tensor(tt, tt, mk_, ALU.mult), [tsm], [tsm])
            dv(lambda e: e.tensor_tensor(tt, tt, LL, ALU.add), [tsm], [tsm])
            dv(lambda e: e.tensor_single_scalar(zp, z_, 0.0, ALU.max), [tsm], [tsm])
            dv(lambda e: e.tensor_tensor(tt, tt, zp, ALU.add), [tsm], [tsm])
            dv(lambda e: e.tensor_scalar(dpar[:, D_N4:D_N4 + 4], tt, -4.0, None, ALU.mult), [tsm], [dpar])
            dv(lambda e: e.tensor_scalar(dpar[:, D_N8:D_N8 + 4], tt, -8.0, None, ALU.mult), [tsm], [dpar])

            xa = [B([128, D], F32, "xa", dma=True) for _ in range(2)]
            ssF = [B([128, 8], F32, "ssF") for _ in range(2)]
            xn = [B([128, D], BF16, "xn") for _ in range(2)]
            hT = B([128, 8, 512], BF16, "hT")
            gth = [B([128, 512], F32, "gth") for _ in range(2)]
            gvs = [B([128, 512], F32, "gvs") for _ in range(2)]
            ga = B([128, 512], F32, "ga")
            gb = B([128, 512], F32, "gb")
            ub = [[B([128, 542], BF16, "ub") for _ in range(4)] for _ in range(2)]
            xbuf = [[B([128, 515], BF16, "xbuf") for _ in range(4)] for _ in range(2)]
            qg = [[B([128, 512], BF16, "qg") for _ in range(4)] for _ in range(2)]
            acc2 = [[B([128, 512], F32, "acc") for _ in range(4)] for _ in range(2)]
            cvbf = [B([128, 512], BF16, "cvbf") for _ in range(4)]
            sqbf = [B([128, 512], BF16, "sqbf") for _ in range(4)]
            mean = B([128, 512], F32, "mean")
            msq = B([128, 512], F32, "msq")
            xr = [B([128, 512], F32, "xr") for _ in range(2)]
            xrbf = [B([128, 512], BF16, "xrbf") for _ in range(2)]
            t1 = [B([128, 512], F32, "t1") for _ in range(2)]
            t2 = [B([128, 512], F32, "t2") for _ in range(2)]
            t3 = [B([128, 512], F32, "t3") for _ in range(2)]
            lh, th = t1, t2
            ab = [B([128, 512], F32, "ab")] * 2
            hb = [B([128, 512], F32, "hb")] * 2
            carry = [B([128, 1], F32, "carry") for _ in range(4)]
            yT = [B([128, 8, 512], BF16, "yT") for _ in range(2)]
            xb = B([128, D], F32, "xb", dma=True)
            ssE = [B([128, 8], F32, "ssE") for _ in range(2)]
            x1 = [B([128, D], F32, "x1", dma=True) for _ in range(2)]
            hfn = [B([128, D], BF16, "hfn", dma=True) for _ in range(4)]
            hfT = [B([128, 8, 128], BF16, "hfT") for _ in range(2)]
            rs = B([128, 40, Q], F32, "rs")
            r36 = B([128, Q, 36], F32, "r36")
            r32 = [B([128, Q, 32], F32, "r32") for _ in range(5)]
            mbf = B([128, Q, 32], BF16, "mbf")
            r8 = [B([128, Q, 8], F32, "r8") for _ in range(4)]
            r4 = [B([128, Q, 4], F32, "r4") for _ in range(3)]

            cwh = lambda c, k: dpar[:, D_CWH + c * 31 + k:D_CWH + c * 31 + k + 1]
            NJ = SEQ // 512

            def rsqA(ssb, i_v, i_l, i_o):
                P.op(act, lambda e: e.activation(out=ssb[:, i_l:i_l + 1], in_=ssb[:, i_v:i_v + 1], func=AF.Ln), reads=[ssb], writes=[ssb])
                P.op(act, lambda e: e.activation(out=ssb[:, i_o:i_o + 1], in_=ssb[:, i_l:i_l + 1], func=AF.Exp, scale=-0.5), reads=[ssb], writes=[ssb])

            def evac_scaled(dst3, ps, gvec):
                P.op(act, lambda e: e.activation(out=dst3.all(), in_=ps[:, :].rearrange("p (c j) -> p c j", c=8), func=AF.Copy),
                     reads=[ps], writes=[dst3.buf])

            class View3:
                def __init__(self, buf, fn, allfn=None):
                    self.buf, self.fn, self.all = buf, fn, allfn

                def __call__(self, c):
                    return self.fn(c)

            def gen_F(ti):
                s_, j = divmod(ti, NJ)
                row0 = ti * 512
                par = ti % 2
                ub_, xbuf_, qg_ = ub[par], xbuf[par], qg[par]
                if j == 0:
                    for c in range(4):
                        P.op(pool, lambda e, c=c: e.memset(ub_[c][:, 0:30], 0.0), writes=[ub_[c]])
                        P.op(pool, lambda e, c=c: e.memset(xbuf_[c][:, 0:3], 0.0), writes=[xbuf_[c]])
                for q in range(Q):
                    yield ("SEG" if q % 2 == 0 else "CHAIN")
                    xt, xnb, ss = xa[q % 2], xn[q % 2], ssF[q % 2]
                    r0 = row0 + q * 128
                    P.dma(sp, lambda e, xt=xt, r0=r0: e.dma_start(out=xt[:, :], in_=x_d[r0:r0 + 128, :]), xt, writes=[xt])
                    P.op(act, lambda e, xt=xt, ss=ss, xnb=xnb: e.activation(out=xnb[:, :], in_=xt[:, :], func=AF.Square, accum_out=ss[:, 0:1]),
                         reads=[xt], writes=[xnb, ss])
                    P.op(dve, lambda e, ss=ss: e.tensor_scalar(ss[:, 1:2], ss[:, 0:1], 1.0 / D, EPS, ALU.mult, ALU.add), reads=[ss], writes=[ss])
                    rsqA(ss, 1, 3, 2)
                    P.op(act, lambda e, xt=xt, xnb=xnb, ss=ss: e.activation(out=xnb[:, :], in_=xt[:, :], func=AF.Copy, scale=ss[:, 2:3]),
                         reads=[xt, ss], writes=[xnb])
                    transposes(xnb, 8, ptr)
                    evac_scaled(View3(hT, None, lambda q=q: hT[:, :, q * 128:(q + 1) * 128]), ptr, gmixbc)

                fbank = [pmm[0], ps2]

                def zmm(m, bi):
                    pb = fbank[bi % 2]

                    def fn(e, m=m, pb=pb):
                        ins = None
                        for k in range(8):
                            ins = e.matmul(pb[:, :], win[:, k, m * 128:(m + 1) * 128], hT[:, k, :], start=(k == 0), stop=(k == 7))
                        return ins
                    P.op(pe, fn, reads=[win, hT], writes=[pb])
                    return pb

                for c in range(4):
                    yield ("SEG" if c % 2 == 0 else "CHAIN")
                    pg = zmm(4 + c, c)
                    ta, vs = gth[c % 2], gvs[c % 2]
                    P.op(act, lambda e, pg=pg, ta=ta: e.activation(out=ta[:, :], in_=pg[:, :], func=AF.Tanh, scale=0.5), reads=[pg], writes=[ta])
                    pv = zmm(c, c)
                    P.op(act, lambda e, pv=pv, vs=vs: e.activation(out=vs[:, :], in_=pv[:, :], func=AF.Copy), reads=[pv], writes=[vs])
                    P.op(pool, lambda e, ta=ta, vs=vs: e.tensor_tensor(ta[:, :], ta[:, :], vs[:, :], ALU.mult), reads=[ta, vs], writes=[ta])
                    P.op(pool, lambda e, ta=ta, vs=vs, c=c: e.tensor_tensor(ub_[c][:, 30:542], ta[:, :], vs[:, :], ALU.add), reads=[ta, vs], writes=[ub_[c]])
                for c in range(4):
                    yield ("SEG" if c % 2 == 0 else "CHAIN")
                    px = zmm(8 + c, c)
                    P.op(act, lambda e, px=px, c=c: e.activation(out=xbuf_[c][:, 3:515], in_=px[:, :], func=AF.Copy), reads=[px], writes=[xbuf_[c]])
                for c in range(4):
                    yield "SEG"
                    pgl = zmm(12 + c, c)
                    P.op(act, lambda e, pgl=pgl: e.activation(out=ga[:, :], in_=pgl[:, :], func=AF.Copy), reads=[pgl], writes=[ga])
                    P.op(act, lambda e, pgl=pgl: e.activation(out=gb[:, :], in_=pgl[:, :], func=AF.Square), reads=[pgl], writes=[gb])
                    P.op(act, lambda e: e.activation(out=gb[:, :], in_=gb[:, :], func=AF.Identity, bias=1.0, scale=0.044715), reads=[gb], writes=[gb])
                    P.op(pool, lambda e: e.tensor_tensor(gb[:, :], gb[:, :], ga[:, :], ALU.mult), reads=[ga, gb], writes=[gb])
                    P.op(act, lambda e: e.activation(out=gb[:, :], in_=gb[:, :], func=AF.Tanh, scale=0.7978845608028654), reads=[gb], writes=[gb])
                    P.op(act, lambda e: e.activation(out=gb[:, :], in_=gb[:, :], func=AF.Identity, bias=1.0, scale=1.0), reads=[gb], writes=[gb])
                    P.op(pool, lambda e, c=c: e.tensor_tensor(qg_[c][:, :], gb[:, :], ga[:, :], ALU.mult), reads=[ga, gb], writes=[qg_[c]])

            def gen_Mc(ti):
                s_, j = divmod(ti, NJ)
                par = ti % 2
                ub_, xbuf_, qg_, yT_ = ub[par], xbuf[par], qg[par], yT[par]
                ubn, xbufn = ub[1 - par], xbuf[1 - par]
                acc = acc2[par]
                for k in range(31):
                    for c in range(4):
                        if k == 0:
                            P.op(dve, lambda e, c=c: e.tensor_scalar(acc[c][:, :], ub_[c][:, 0:512], cwh(c, 0), cpar[:, CB + c:CB + c + 1], ALU.mult, ALU.add),
                                 reads=[ub_[c], dpar, cpar], writes=[acc[c]])
                        else:
                            P.op(dve, lambda e, c=c, k=k: e.scalar_tensor_tensor(acc[c][:, :], ub_[c][:, k:k + 512], cwh(c, k), acc[c][:, :], ALU.mult, ALU.add),
                                 reads=[ub_[c], dpar, acc[c]], writes=[acc[c]])
                    yield
                for c in range(4):
                    if j < NJ - 1:
                        P.op(pool, lambda e, c=c: e.tensor_copy(ubn[c][:, 0:30], ub_[c][:, 512:542]), reads=[ub_[c]], writes=[ubn[c]])
                    P.op(act, lambda e, c=c: e.activation(out=cvbf[c][:, :], in_=acc[c][:, :], func=AF.Copy), reads=[acc[c]], writes=[cvbf[c]])
                    P.op(act, lambda e, c=c: e.activation(out=sqbf[c][:, :], in_=acc[c][:, :], func=AF.Square), reads=[acc[c]], writes=[sqbf[c]])

                def stat_mm(dst, srcs):
                    def fn(e):
                        ins = None
                        for c in range(4):
                            ins = e.matmul(dst[:, :], ones[:, :], srcs[c][:, :], start=(c == 0), stop=(c == 3))
                        return ins
                    P.op(pe, fn, reads=[ones] + srcs, writes=[dst])
                stat_mm(ps1, cvbf)
                P.op(act, lambda e: e.activation(out=mean[:, :], in_=ps1[:, :], func=AF.Copy, scale=1.0 / 512), reads=[ps1], writes=[mean])
                stat_mm(ps1, sqbf)
                P.op(pool, lambda e: e.tensor_tensor(msq[:, :], mean[:, :], mean[:, :], ALU.mult), reads=[mean], writes=[msq])
                P.op(dve, lambda e: e.scalar_tensor_tensor(msq[:, :], ps1[:, :], 1.0 / 512, msq[:, :], ALU.mult, ALU.subtract), reads=[ps1, msq], writes=[msq])
                P.op(dve, lambda e: e.tensor_scalar(msq[:, :], msq[:, :], EPS, None, ALU.add), reads=[msq], writes=[msq])
                P.op(act, lambda e: e.activation(out=msq[:, :], in_=msq[:, :], func=AF.Ln), reads=[msq], writes=[msq])
                P.op(act, lambda e: e.activation(out=msq[:, :], in_=msq[:, :], func=AF.Exp, scale=-0.5), reads=[msq], writes=[msq])
                yield
                for c in range(4):
                    P.op(pool, lambda e, c=c: e.tensor_tensor(acc[c][:, :], acc[c][:, :], mean[:, :], ALU.subtract), reads=[acc[c], mean], writes=[acc[c]])
                    P.op(pool, lambda e, c=c: e.tensor_tensor(acc[c][:, :], acc[c][:, :], msq[:, :], ALU.mult), reads=[acc[c], msq], writes=[acc[c]])
                for c in range(4):
                    t_ = (mean, msq)[c % 2]
                    P.op(act, lambda e, c=c: e.activation(out=acc[c][:, :], in_=acc[c][:, :], func=AF.Identity,
                                                          bias=dpar[:, D_BH + c:D_BH + c + 1], scale=dpar[:, D_GH + c:D_GH + c + 1]),
                         reads=[acc[c], dpar], writes=[acc[c]])
                    P.op(act, lambda e, c=c, t_=t_: e.activation(out=t_[:, :], in_=acc[c][:, :], func=AF.Tanh), reads=[acc[c]], writes=[t_])
                    P.op(dve, lambda e, c=c, t_=t_: e.scalar_tensor_tensor(yT_[:, c, :], t_[:, :], 1.0, acc[c][:, :], ALU.add, ALU.mult),
                         reads=[acc[c], t_], writes=[yT_])
            def gen_Ml(ti):
                s_, j = divmod(ti, NJ)
                par = ti % 2
                xbuf_, qg_, yT_ = xbuf[par], qg[par], yT[par]
                xbufn = xbuf[1 - par]
                if j == 0:
                    for c in range(4):
                        P.op(pool, lambda e, c=c: e.memset(carry[c][:, :], 0.0), writes=[carry[c]])
                for c in range(4):
                    xr_, xrb_ = xr[c % 2], xrbf[c % 2]
                    lw = lambda k, c=c: cpar[:, LW + c * 4 + k:LW + c * 4 + k + 1]
                    P.op(dve, lambda e, c=c, xr_=xr_, lw=lw: e.tensor_scalar(xr_[:, :], xbuf_[c][:, 0:512], lw(0), cpar[:, LBB + c:LBB + c + 1], ALU.mult, ALU.add),
                         reads=[xbuf_[c], cpar], writes=[xr_])
                    for k in range(1, 4):
                        P.op(dve, lambda e, c=c, k=k, xr_=xr_, lw=lw: e.scalar_tensor_tensor(xr_[:, :], xbuf_[c][:, k:k + 512], lw(k), xr_[:, :], ALU.mult, ALU.add),
                             reads=[xbuf_[c], cpar, xr_], writes=[xr_])
                    if j < NJ - 1:
                        P.op(pool, lambda e, c=c: e.tensor_copy(xbufn[c][:, 0:3], xbuf_[c][:, 512:515]), reads=[xbuf_[c]], writes=[xbufn[c]])
                    P.op(act, lambda e, xr_=xr_, xrb_=xrb_: e.activation(out=xrb_[:, :], in_=xr_[:, :], func=AF.Copy), reads=[xr_], writes=[xrb_])
                    pr, pi = pmm[1], pmm[1]
                    P.op(pe, lambda e, c=c, pr=pr, xrb_=xrb_: e.matmul(pr[:, :], gbd[:, c, 0:128], xrb_[:, :], start=True, stop=True), reads=[gbd, xrb_], writes=[pr])
                    a1, a2, a3, aa, hh = t1[c % 2], t2[c % 2], t3[c % 2], ab[c % 2], hb[c % 2]
                    dp = lambda o, c=c: dpar[:, o + c:o + c + 1]
                    yield
                    P.op(act, lambda e, pr=pr, a1=a1, dp=dp: e.activation(out=a1[:, :], in_=pr[:, :], func=AF.Tanh, bias=dp(D_BRH), scale=0.5), reads=[pr, dpar], writes=[a1])
                    P.op(pe, lambda e, c=c, pi=pi, xrb_=xrb_: e.matmul(pi[:, :], gbd[:, c, 128:256], xrb_[:, :], start=True, stop=True), reads=[gbd, xrb_], writes=[pi])
                    P.op(act, lambda e, pi=pi, a3=a3, dp=dp: e.activation(out=a3[:, :], in_=pi[:, :], func=AF.Tanh, bias=dp(D_BIH), scale=0.5), reads=[pi, dpar], writes=[a3])
                    P.op(act, lambda e, a1=a1, aa=aa, dp=dp: e.activation(out=aa[:, :], in_=a1[:, :], func=AF.Exp, bias=dp(D_N4), scale=dp(D_N4)), reads=[a1, dpar], writes=[aa])
                    P.op(act, lambda e, a1=a1, a2=a2, dp=dp: e.activation(out=a2[:, :], in_=a1[:, :], func=AF.Exp, bias=dp(D_N8), scale=dp(D_N8)), reads=[a1, dpar], writes=[a2])
                    P.op(dve, lambda e, a2=a2: e.tensor_scalar(a2[:, :], a2[:, :], 0.99999994, -1.0, ALU.min, ALU.mult), reads=[a2], writes=[a2])
                    P.op(act, lambda e, a2=a2: e.activation(out=a2[:, :], in_=a2[:, :], func=AF.Ln, bias=1.0, scale=1.0), reads=[a2], writes=[a2])
                    P.op(act, lambda e, a2=a2: e.activation(out=a2[:, :], in_=a2[:, :], func=AF.Exp, scale=0.5), reads=[a2], writes=[a2])
                    P.op(dve, lambda e, a3=a3, xr_=xr_: e.scalar_tensor_tensor(a3[:, :], a3[:, :], 1.0, xr_[:, :], ALU.add, ALU.mult), reads=[a3, xr_], writes=[a3])
                    yield
                    P.op(dve, lambda e, a2=a2, a3=a3: e.tensor_tensor(a3[:, :], a3[:, :], a2[:, :], ALU.mult), reads=[a2, a3], writes=[a3])
                    P.op(dve, lambda e, c=c, aa=aa, a3=a3, hh=hh: e.tensor_tensor_scan(hh[:, :], aa[:, :], a3[:, :], carry[c][:, 0:1], ALU.mult, ALU.add),
                         reads=[aa, a3, carry[c]], writes=[hh])
                    P.op(dve, lambda e, c=c, hh=hh: e.tensor_copy(carry[c][:, :], hh[:, 511:512]), reads=[hh], writes=[carry[c]])
                    P.op(dve, lambda e, c=c, hh=hh: e.scalar_tensor_tensor(yT_[:, 4 + c, :], hh[:, :], 0.25, qg_[c][:, :], ALU.mult, ALU.mult),
                         reads=[hh, qg_[c]], writes=[yT_])
                    yield

            def gen_E(ti):
                row0 = ti * 512
                yT_ = yT[ti % 2]
                for q in range(Q):
                    yield ("SEG" if q % 2 == 0 else "CHAIN")
                    r0 = row0 + q * 128
                    x1t, ss = x1[q % 2], ssE[q % 2]
                    hf = hfn[q]
                    hft = hfT[q % 2]
                    P.dma(sp, lambda e, r0=r0: e.dma_start(out=xb[:, :], in_=x_d[r0:r0 + 128, :]), xb, writes=[xb])
                    for h in range(2):
                        pb = pmm[2]

                        def fn(e, pb=pb, h=h, q=q):
                            ins = None
                            for k in range(8):
                                ins = e.matmul(pb[:, :], yT_[:, k, q * 128:(q + 1) * 128], wout[:, k, h * 512:(h + 1) * 512], start=(k == 0), stop=(k == 7))
                            return ins
                        P.op(pe, fn, reads=[yT_, wout], writes=[pb])
                        P.op(dve, lambda e, pb=pb, h=h, x1t=x1t: e.tensor_tensor(x1t[:, h * 512:(h + 1) * 512], pb[:, :], xb[:, h * 512:(h + 1) * 512], ALU.add),
                             reads=[pb, xb], writes=[x1t])
                    P.dma(sp, lambda e, x1t=x1t, r0=r0: e.dma_start(out=x1_d[r0:r0 + 128, :], in_=x1t[:, :]), x1t, reads=[x1t])
                    P.op(act, lambda e, x1t=x1t, ss=ss, hf=hf: e.activation(out=hf[:, :], in_=x1t[:, :], func=AF.Square, accum_out=ss[:, 0:1]), reads=[x1t], writes=[hf, ss])
                    P.op(dve, lambda e, ss=ss: e.tensor_scalar(ss[:, 1:2], ss[:, 0:1], 1.0 / D, EPS, ALU.mult, ALU.add), reads=[ss], writes=[ss])
                    rsqA(ss, 1, 3, 2)
                    P.op(act, lambda e, x1t=x1t, hf=hf, ss=ss: e.activation(out=hf[:, :], in_=x1t[:, :], func=AF.Copy, scale=ss[:, 2:3]), reads=[x1t, ss], writes=[hf])
                    transposes(hf, 8, ptr2)
                    evac_scaled(View3(hft, None, lambda hft=hft: hft[:, :, :]), ptr2, gffnbc)

                    def fnl(e, hft=hft, q=q):
                        ins = None
                        for k in range(8):
                            ins = e.matmul(pmm[3][:, q * 36:(q + 1) * 36], hft[:, k, :], wr[:, k, :], start=(k == 0), stop=(k == 7))
                        return ins
                    P.op(pe, fnl, reads=[hft, wr], writes=[pmm[3]])

                yield "SEG"
                S = lambda i: rs[:, i, :]
                bc = lambda ap, n: ap.unsqueeze(2).broadcast_to([128, Q, n])
                lgb = r36
                P.op(dve, lambda e: e.tensor_tensor(lgb[:, :, :], pmm[3][:, 0:Q * 36].rearrange("p (q n) -> p q n", q=Q), rbias[:, :, :], ALU.add),
                     reads=[pmm[3], rbias], writes=[lgb])
                gmask, gsh, gex = r4
                P.op(dve, lambda e: e.tensor_reduce(S(0), lgb[:, :, 0:4], AX.X, ALU.max), reads=[lgb], writes=[rs])
                P.op(dve, lambda e: e.tensor_tensor(gmask[:, :, :], lgb[:, :, 0:4], bc(S(0), 4), ALU.is_equal), reads=[lgb, rs], writes=[gmask])
                P.op(dve, lambda e: e.tensor_tensor(gsh[:, :, :], lgb[:, :, 0:4], bc(S(0), 4), ALU.subtract), reads=[lgb, rs], writes=[gsh])
                P.op(act, lambda e: e.activation(out=gex[:, :, :], in_=gsh[:, :, :], func=AF.Exp), reads=[gsh], writes=[gex])
                P.op(dve, lambda e: e.tensor_reduce(S(1), gex[:, :, :], AX.X, ALU.add), reads=[gex], writes=[rs])
                P.op(dve, lambda e: e.reciprocal(S(2), S(1)), reads=[rs], writes=[rs])
                le4 = lgb[:, :, 4:36].rearrange("p q (g j) -> p q g j", g=4)
                tmp32 = r32[0]
                P.op(dve, lambda e: e.tensor_tensor(tmp32[:, :, :].rearrange("p q (g j) -> p q g j", g=4), le4,
                                                    gmask[:, :, :].unsqueeze(3).broadcast_to([128, Q, 4, 8]), ALU.mult), reads=[lgb, gmask], writes=[tmp32])
                sel, top8, oh1, oh2 = r8
                P.op(dve, lambda e: e.tensor_reduce(sel[:, :, :], tmp32[:, :, :].rearrange("p q (g j) -> p q j g", g=4), AX.X, ALU.add), reads=[tmp32], writes=[sel])
                yield
                for q in range(Q):
                    P.op(dve, lambda e, q=q: e.max(top8[:, q, :], sel[:, q, :]), reads=[sel], writes=[top8])
                P.op(dve, lambda e: e.tensor_tensor(oh1[:, :, :], sel[:, :, :], top8[:, :, 0:1].broadcast_to([128, Q, 8]), ALU.is_equal), reads=[sel, top8], writes=[oh1])
                P.op(dve, lambda e: e.tensor_tensor(oh2[:, :, :], sel[:, :, :], top8[:, :, 1:2].broadcast_to([128, Q, 8]), ALU.is_equal), reads=[sel, top8], writes=[oh2])
                P.op(dve, lambda e: e.tensor_tensor(S(3), top8[:, :, 1], top8[:, :, 0], ALU.subtract), reads=[top8], writes=[rs])
                P.op(act, lambda e: e.activation(out=S(4), in_=S(3), func=AF.Exp), reads=[rs], writes=[rs])
                P.op(dve, lambda e: e.tensor_scalar(S(5), S(4), 1.0, None, ALU.add), reads=[rs], writes=[rs])
                P.op(dve, lambda e: e.reciprocal(S(6), S(5)), reads=[rs], writes=[rs])
                P.op(dve, lambda e: e.tensor_tensor(S(7), S(6), S(2), ALU.mult), reads=[rs], writes=[rs])
                P.op(dve, lambda e: e.tensor_tensor(S(8), S(2), S(7), ALU.subtract), reads=[rs], writes=[rs])
                E1, E2 = r32[1], r32[2]
                for Ek, oh in ((E1, oh1), (E2, oh2)):
                    P.op(dve, lambda e, Ek=Ek, oh=oh: e.tensor_tensor(Ek[:, :, :].rearrange("p q (g j) -> p q g j", g=4),
                                                                      gmask[:, :, :].unsqueeze(3).broadcast_to([128, Q, 4, 8]),
                                                                      oh[:, :, :].unsqueeze(2).broadcast_to([128, Q, 4, 8]), ALU.mult),
                         reads=[gmask, oh], writes=[Ek])
                P.op(dve, lambda e: e.tensor_tensor(mbf[:, :, :], E1[:, :, :], E2[:, :, :], ALU.add), reads=[E1, E2], writes=[mbf])

                def fnc(e):
                    ins = None
                    for q in range(Q):
                        ins = e.matmul(pmm[3][:, 160 + q * 32:160 + (q + 1) * 32], tri[:, :], mbf[:, q, :], start=True, stop=(q == 0))
                        for q2 in range(q):
                            ins = e.matmul(pmm[3][:, 160 + q * 32:160 + (q + 1) * 32], ones[:, :], mbf[:, q2, :], start=False, stop=(q2 == q - 1))
                    for q in range(Q):
                        ins = e.matmul(pmm[3][:, 288:320], ones[:, :], mbf[:, q, :], start=(q == 0), stop=(q == Q - 1))
                    return ins
                P.op(pe, fnc, reads=[tri, ones, mbf], writes=[pmm[3]])
                yield
                tot = r32[3]
                P.op(dve, lambda e: e.tensor_tensor(tot[:, :, :], pmm[3][:, 160:288].rearrange("p (q n) -> p q n", q=Q),
                                                    cntbc[:, :].unsqueeze(1).broadcast_to([128, Q, 32]), ALU.add), reads=[pmm[3], cntbc], writes=[tot])
                P.op(dve, lambda e: e.tensor_tensor(cntbc[:, :], cntbc[:, :], pmm[3][:, 288:320], ALU.add), reads=[pmm[3], cntbc], writes=[cntbc])
                tm = r32[4]
                for kk, Ek in ((0, E1), (1, E2)):
                    P.op(dve, lambda e, Ek=Ek: e.tensor_tensor(tm[:, :, :], Ek[:, :, :], tot[:, :, :], ALU.mult), reads=[Ek, tot], writes=[tm])
                    P.op(dve, lambda e, kk=kk: e.tensor_reduce(S(10 + kk), tm[:, :, :], AX.X, ALU.add), reads=[tm], writes=[rs])
                    P.op(dve, lambda e, Ek=Ek: e.tensor_tensor(tm[:, :, :], Ek[:, :, :], iota[:, :, :], ALU.mult), reads=[Ek, iota], writes=[tm])
                    P.op(dve, lambda e, kk=kk: e.tensor_reduce(S(12 + kk), tm[:, :, :], AX.X, ALU.add), reads=[tm], writes=[rs])
                    P.op(dve, lambda e, kk=kk: e.scalar_tensor_tensor(S(14 + kk), S(12 + kk), float(CAP), S(10 + kk), ALU.mult, ALU.add), reads=[rs], writes=[rs])
                    P.op(dve, lambda e, kk=kk: e.tensor_single_scalar(S(16 + kk), S(10 + kk), float(CAP), ALU.is_lt), reads=[rs], writes=[rs])
                    P.op(dve, lambda e, kk=kk: e.tensor_scalar(S(14 + kk), S(14 + kk), float(-TRASH), None, ALU.add), reads=[rs], writes=[rs])
                    P.op(dve, lambda e, kk=kk: e.tensor_tensor(S(14 + kk), S(14 + kk), S(16 + kk), ALU.mult), reads=[rs], writes=[rs])
                    P.op(dve, lambda e, kk=kk: e.tensor_scalar(S(14 + kk), S(14 + kk), float(TRASH), 0.0, ALU.add, ALU.max), reads=[rs], writes=[rs])
                    P.op(dve, lambda e, kk=kk: e.tensor_scalar(rinfo[:, ti, kk, :], S(14 + kk), float(TRASH), None, ALU.min), reads=[rs], writes=[rinfo])
                    P.op(dve, lambda e, kk=kk: e.tensor_tensor(rinfo[:, ti, 2 + kk, :], S(7 + kk), S(16 + kk), ALU.mult), reads=[rs], writes=[rinfo])
                    yield
                P.op(dve, lambda e: e.tensor_copy(sloti[:, ti, :, :], rinfo[:, ti, 0:2, :]), reads=[rinfo], writes=[sloti])
                for q in range(Q):
                    for kk in range(2):
                        P.dma(pool, lambda e, q=q, kk=kk: e.indirect_dma_start(
                            out=hs_d[:, :], out_offset=IOA(ap=sloti[:, ti, kk, q:q + 1], axis=0), in_=hfn[q][:, :], in_offset=None),
                            hfn[q], reads=[hfn[q], sloti])

            def collect(genfunc, ti):
                items = []
                orig_op, orig_dma = P.op, P.dma
                P.op = lambda eng, fn, reads=(), writes=(): items.append((orig_op, (eng, fn), dict(reads=reads, writes=writes), eng))
                P.dma = lambda q, fn, sem_buf, reads=(), writes=(): items.append((orig_dma, (q, fn, sem_buf), dict(reads=reads, writes=writes), q))
                try:
                    for tok in genfunc(ti):
                        items.append(tok)
                finally:
                    del P.op, P.dma
                return items

            def stages1(items):
                out, cur, prev = [], [], None
                for it in items:
                    if it is None or (HOP and prev is not None and it[3] is not prev):
                        out.append(cur)
                        cur = []
                    if it is None:
                        prev = None
                    else:
                        cur.append(it)
                        prev = it[3]
                out.append(cur)
                res = []
                for st in out:
                    if not st:
                        continue
                    res.append(st)
                    if LOADLAG and all(it[3] is sp and it[2]["writes"] for it in st):
                        res.extend([[] for _ in range(LOADLAG)])
                return res

            def zip_locked(chains):
                k = len(chains)
                if k == 1:
                    return chains[0]
                spans, wsets = [], []
                for L in chains:
                    fw, lr, ws = {}, {}, set()
                    for si, st in enumerate(L):
                        for (f, args, kw, eng) in st:
                            for bb in kw["writes"]:
                                fw.setdefault(id(bb), si)
                                ws.add(id(bb))
                            for bb in kw["reads"]:
                                lr[id(bb)] = si
                    spans.append({x: (fw[x], lr[x]) for x in fw if x in lr and lr[x] > fw[x]})
                    wsets.append(ws)
                out, pos, owner = [], [0] * k, {}
                while any(pos[c] < len(chains[c]) for c in range(k)):
                    progressed = False
                    for c in range(k):
                        if pos[c] >= len(chains[c]):
                            continue
                        st = chains[c][pos[c]]
                        W = set(id(bb) for (f, args, kw, eng) in st for bb in kw["writes"])
                        if any(owner.get(x) not in (None, c) for x in W):
                            continue
                        out.append(st)
                        progressed = True
                        for x in W:
                            if x in spans[c] and any(x in wsets[j] for j in range(k) if j != c):
                                owner[x] = c
                        for x in list(owner):
                            if owner[x] == c and pos[c] >= spans[c][x][1]:
                                owner[x] = None
                        pos[c] += 1
                    assert progressed, "chain lock deadlock"
                return out

            def stages(items):
                segs = [[[]]]
                for it in items:
                    if it == "SEG":
                        segs.append([[]])
                    elif it == "CHAIN":
                        segs[-1].append([])
                    else:
                        segs[-1][-1].append(it)
                out = []
                for seg in segs:
                    out += zip_locked([stages1(ch) for ch in seg if ch])  if any(seg) else []
                return out

            def spread(genfunc, ti, n, off=0):
                sts = stages(collect(genfunc, ti))
                assert len(sts) <= n - off, (len(sts), n, off)
                k = 0
                for t in range(n):
                    while t >= off and k < len(sts) and k * (n - off) < (t - off + 1) * len(sts):
                        for f, args, kw, eng in sts[k]:
                            f(*args, **kw)
                        k += 1
                    yield

            counts = [len(stages(collect(g, 1))) for g in (gen_F, gen_Mc, gen_Ml, gen_E)]
            NSTG = max(counts) + 2

            def both(g1, g2):
                for _ in g1:
                    next(g2)
                    yield

            def tile_gen(ti):
                yield from spread(gen_F, ti, NSTG)
                yield from both(spread(gen_Mc, ti, NSTG, MC_OFF), spread(gen_Ml, ti, NSTG))
                yield from spread(gen_E, ti, NSTG)

            run_pipelined((tile_gen(ti) for ti in range(NT)), depth=3, skew=NSTG)

            if debug:
                P.dma(sp, lambda e: e.dma_start(out=ri_d, in_=rinfo[:, :, :, :].rearrange("p a b c -> p (a b c)")), rinfo, reads=[rinfo])
            P.barrier()
            P.flush(block)

        with ExitStack() as sb:
            B = lambda shape, dt=F32, name=None, dma=False: P.buf(sb, shape, dt, name, dma)
            gffnbc = B([128, 8], F32, "gffnbc", dma=True)
            P.dma(sp, lambda e: e.dma_start(out=gffnbc[:, :], in_=gbc_d[1]), gffnbc, writes=[gffnbc])
            zt = B([128, D], F32, "zt", dma=True)
            P.op(dve, lambda e: e.memset(zt[:, :], 0.0), writes=[zt])
            P.dma(sp, lambda e: e.dma_start(out=y_d[NSLOT:NSLOT + 128, :], in_=zt[:, :]), zt, reads=[zt])
            w1 = [B([128, 8, 512], BF16, "w1") for _ in range(2)]
            w3 = [B([128, 8, 512], BF16, "w3") for _ in range(2)]
            w2 = [B([128, 4, D], BF16, "w2") for _ in range(2)]
            w3s = B([128, 8, 512], F32, "w3s", dma=True)
            w1s = B([128, 8, 512], F32, "w1s", dma=True)
            w2s = B([128, 4, D], F32, "w2s", dma=True)
            hst = [B([128, NSUB, D], BF16, "hst", dma=True) for _ in range(2)]
            hfTe = [B([128, 8, CAP], BF16, "hfTe") for _ in range(2)]
            actT = B([128, 4, CAP], BF16, "actT")
            tb = [B([128, 512], F32, "tb") for _ in range(2)]
            tc = [B([128, 512], F32, "tc") for _ in range(2)]
            yt = [B([128, D], F32, "yt", dma=True) for _ in range(3)]
            ntiles = [(0, 512)] if CAP == 512 else ([(n0, min(512, CAP - n0)) for n0 in range(0, CAP, 512)])
            yi = 0
            ci = 0

            def load_expert(ex):
                sl = ex % 2
                P.dma(sp, lambda e: e.dma_start(out=hst[sl][:, :, :], in_=hs_d[ex * CAP:(ex + 1) * CAP, :].rearrange("(s p) n -> p s n", p=128)),
                      hst[sl], writes=[hst[sl]])
                P.dma(sp, lambda e: e.dma_start(out=w1s[:, :, :], in_=w1_d[ex].rearrange("(k p) n -> p k n", p=128)), w1s, writes=[w1s])
                P.dma(sp, lambda e: e.dma_start(out=w3s[:, :, :], in_=w3_d[ex].rearrange("(k p) n -> p k n", p=128)), w3s, writes=[w3s])
                P.dma(sp, lambda e: e.dma_start(out=w2s[:, :, :], in_=w2_d[ex].rearrange("(k p) n -> p k n", p=128)), w2s, writes=[w2s])

            def cast_expert(ex):
                sl = ex % 2
                for k in range(8):
                    P.op(pool, lambda e, k=k: e.tensor_tensor(w1[sl][:, k, :], w1s[:, k, :], gffnbc[:, k:k + 1].broadcast_to([128, 512]), ALU.mult),
                         reads=[w1s, gffnbc], writes=[w1[sl]])
                for k in range(8):
                    P.op(act, lambda e, k=k: e.activation(out=w3[sl][:, k, :], in_=w3s[:, k, :], func=AF.Copy, scale=gffnbc[:, k:k + 1]), reads=[w3s, gffnbc], writes=[w3[sl]])
                for k in range(4):
                    P.op(act, lambda e, k=k: e.activation(out=w2[sl][:, k, :], in_=w2s[:, k, :], func=AF.Copy), reads=[w2s], writes=[w2[sl]])

            pool6 = pmm + [ps1, ps2]
            p6 = [0]

            def next6():
                bb = pool6[p6[0] % 6]
                p6[0] += 1
                return bb

            def do_T(ex):
                sl = ex % 2
                hT_e = hfTe[sl]
                for sbt in range(NSUB):
                    pt_ = ptr if sbt % 2 == 0 else ptr2

                    def fn(e, sbt=sbt, pt_=pt_, sl=sl):
                        ins = None
                        for c in range(8):
                            ins = e.transpose(pt_[:, c * 128:(c + 1) * 128], hst[sl][:, sbt, c * 128:(c + 1) * 128], ident[:, :])
                        return ins
                    P.op(pe, fn, reads=[hst[sl], ident], writes=[pt_])
                    P.op(dve, lambda e, sbt=sbt, pt_=pt_, hT_e=hT_e: e.tensor_copy(hT_e[:, :, sbt * 128:(sbt + 1) * 128], pt_[:, :].rearrange("p (c j) -> p c j", c=8)),
                         reads=[pt_], writes=[hT_e])

            def do_H(ex):
                sl = ex % 2
                hT_e = hfTe[sl]
                for (n0, nn) in ntiles:
                    for m in range(4):
                        p1, p3 = next6(), next6()
                        for (pb, wt) in ((p1, w1[sl]), (p3, w3[sl])):
                            def fn(e, pb=pb, wt=wt, m=m, n0=n0, nn=nn, hT_e=hT_e):
                                ins = None
                                for k in range(8):
                                    ins = e.matmul(pb[:, 0:nn], wt[:, k, m * 128:(m + 1) * 128], hT_e[:, k, n0:n0 + nn], start=(k == 0), stop=(k == 7))
                                return ins
                            P.op(pe, fn, reads=[wt, hT_e], writes=[pb])
                        tb_, tc_ = tb[ci_[0] % 2], tc[ci_[0] % 2]
                        ci_[0] += 1
                        P.op(act, lambda e, p1=p1, tb_=tb_, nn=nn: e.activation(out=tb_[:, 0:nn], in_=p1[:, 0:nn], func=AF.Tanh, scale=0.5), reads=[p1], writes=[tb_])
                        P.op(dve, lambda e, p1=p1, tb_=tb_, tc_=tc_, nn=nn: e.scalar_tensor_tensor(tc_[:, 0:nn], tb_[:, 0:nn], 1.0, p1[:, 0:nn], ALU.add, ALU.mult),
                             reads=[tb_, p1], writes=[tc_])
                        P.op(dve, lambda e, p3=p3, tc_=tc_, nn=nn, m=m, n0=n0: e.scalar_tensor_tensor(actT[:, m, n0:n0 + nn], tc_[:, 0:nn], 0.5, p3[:, 0:nn], ALU.mult, ALU.mult),
                             reads=[tc_, p3], writes=[actT])

            def do_Y(ex):
                sl = ex % 2
                for sbt in range(NSUB):
                    yb = yt[yi_[0] % 3]
                    yi_[0] += 1
                    for h in range(2):
                        pb = next6()

                        def fn(e, pb=pb, h=h, sbt=sbt, sl=sl):
                            ins = None
                            for m in range(4):
                                ins = e.matmul(pb[:, :], actT[:, m, sbt * 128:(sbt + 1) * 128], w2[sl][:, m, h * 512:(h + 1) * 512], start=(m == 0), stop=(m == 3))
                            return ins
                        P.op(pe, fn, reads=[actT, w2[sl]], writes=[pb])
                        P.op(act, lambda e, pb=pb, h=h, yb=yb: e.activation(out=yb[:, h * 512:(h + 1) * 512], in_=pb[:, :], func=AF.Copy), reads=[pb], writes=[yb])
                    r0 = ex * CAP + sbt * 128
                    P.dma(sp, lambda e, yb=yb, r0=r0: e.dma_start(out=y_d[r0:r0 + 128, :], in_=yb[:, :]), yb, reads=[yb])

            ci_, yi_ = [0], [0]
            load_expert(0)
            cast_expert(0)
            do_T(0)
            for ex in range(32):
                if ex + 1 < 32:
                    load_expert(ex + 1)
                do_H(ex)
                if ex + 1 < 32:
                    cast_expert(ex + 1)
                    do_T(ex + 1)
                do_Y(ex)
            P.barrier()
            P.flush(block)

        with ExitStack() as sc:
            B = lambda shape, dt=F32, name=None, dma=False: P.buf(sc, shape, dt, name, dma)
            gplebc = B([128, 8], F32, "gplebc", dma=True)
            gpp = B([128, D], F32, "gpp", dma=True)
            gfin = B([128, D], F32, "gfin", dma=True)
            wple = B([128, 2, D], BF16, "wple", dma=True)
            wpg = B([128, 8, D], BF16, "wpg", dma=True)
            P.dma(sp, lambda e: e.dma_start(out=gplebc[:, :], in_=gbc_d[2]), gplebc, writes=[gplebc])
            P.dma(sp, lambda e: e.dma_start(out=gpp[:, :], in_=rowbc_d[0]), gpp, writes=[gpp])
            P.dma(sp, lambda e: e.dma_start(out=gfin[:, :], in_=rowbc_d[1]), gfin, writes=[gfin])
            for k in range(2):
                P.dma(pool, lambda e, k=k: e.dma_start(out=wple[:, k, :], in_=wple_d[k * 128:(k + 1) * 128, :]), wple, writes=[wple])
            for k in range(8):
                P.dma(pool, lambda e, k=k: e.dma_start(out=wpg[:, k, :], in_=wpg_d[k * 128:(k + 1) * 128, :]), wpg, writes=[wpg])
            for k in range(8):
                P.op(dve, lambda e, k=k: e.tensor_scalar(wpg[:, k, :], wpg[:, k, :], gplebc[:, k:k + 1], None, ALU.mult), reads=[wpg, gplebc], writes=[wpg])
            x1t_ = [B([128, D], F32, "x1c", dma=True) for _ in range(NP_C)]
            pt_b = [B([128, 256], F32, "pc", dma=True) for _ in range(NP_C)]
            y1_ = [B([128, D], F32, "y1c", dma=True) for _ in range(NP_C)]
            y2_ = [B([128, D], F32, "y2c", dma=True) for _ in range(NP_C)]
            NP = NP_C
            ssC_ = [B([128, 16], F32, "ssC") for _ in range(NP)]
            xn3 = [B([128, D], BF16, "xn3") for _ in range(NP)]
            junk, te, ob, thg = xn3, y2_, x1t_, y1_
            x3T = [B([128, 8, 128], BF16, "x3T") for _ in range(NP)]
            pbf = [B([128, 256], BF16, "pbf") for _ in range(NP)]
            pT = [B([128, 2, 128], BF16, "pT") for _ in range(NP)]

            def rsq(ssb, i_v, i_l, i_o):
                P.op(act, lambda e: e.activation(out=ssb[:, i_l:i_l + 1], in_=ssb[:, i_v:i_v + 1], func=AF.Ln), reads=[ssb], writes=[ssb])
                P.op(act, lambda e: e.activation(out=ssb[:, i_o:i_o + 1], in_=ssb[:, i_l:i_l + 1], func=AF.Exp, scale=-0.5), reads=[ssb], writes=[ssb])

            def subtile_gen(st):
                ti, q = divmod(st, Q)
                r0 = st * 128
                i3 = st % NP
                xx, pp, y1, y2 = x1t_[i3], pt_b[i3], y1_[i3], y2_[i3]
                jk, ssC = junk[i3], ssC_[i3]
                xn_, x3_, pb_, pT_, tg_, te_, ob_ = xn3[i3], x3T[i3], pbf[i3], pT[i3], thg[i3], te[i3], ob[i3]
                P.dma(sp, lambda e: e.dma_start(out=xx[:, :], in_=x1_d[r0:r0 + 128, :]), xx, writes=[xx])
                P.dma(sp, lambda e: e.dma_start(out=pp[:, :], in_=p_d[r0:r0 + 128, :]), pp, writes=[pp])
                for (yy, kk) in ((y1, 0), (y2, 1)):
                    P.dma(pool, lambda e, yy=yy, kk=kk: e.indirect_dma_start(
                        out=yy[:, :], out_offset=None, in_=y_d[:, :], in_offset=IOA(ap=sloti[:, ti, kk, q:q + 1], axis=0)),
                        yy, reads=[sloti], writes=[yy])
                yield
                for (yy, kk) in ((y1, 0), (y2, 1)):
                    P.op(dve, lambda e, yy=yy, kk=kk: e.scalar_tensor_tensor(xx[:, :], yy[:, :], rinfo[:, ti, 2 + kk, q:q + 1], xx[:, :], ALU.mult, ALU.add),
                         reads=[yy, rinfo, xx], writes=[xx])
                yield
                P.op(act, lambda e: e.activation(out=jk[:, :], in_=xx[:, :], func=AF.Square, accum_out=ssC[:, 0:1]), reads=[xx], writes=[jk, ssC])
                P.op(act, lambda e: e.activation(out=pb_[:, :], in_=pp[:, :], func=AF.Copy), reads=[pp], writes=[pb_])
                yield
                P.op(dve, lambda e: e.tensor_scalar(ssC[:, 1:2], ssC[:, 0:1], 1.0 / D, EPS, ALU.mult, ALU.add), reads=[ssC], writes=[ssC])
                yield
                rsq(ssC, 1, 3, 2)
                P.op(act, lambda e: e.activation(out=xn_[:, :], in_=xx[:, :], func=AF.Copy, scale=ssC[:, 2:3]), reads=[xx, ssC], writes=[xn_])
                yield
                transposes(xn_, 8, ptr)
                P.op(dve, lambda e: e.tensor_copy(x3_[:, :, :], ptr[:, :].rearrange("p (c j) -> p c j", c=8)), reads=[ptr], writes=[x3_])
                transposes(pb_, 2, ptr2)
                P.op(act, lambda e: e.activation(out=pT_[:, :, :], in_=ptr2[:, 0:256].rearrange("p (c j) -> p c j", c=2), func=AF.Copy), reads=[ptr2], writes=[pT_])
                yield
                pes = []
                for h in range(2):
                    pg_ = next_pmm()

                    def fn(e, pg_=pg_, h=h):
                        ins = None
                        for k in range(8):
                            ins = e.matmul(pg_[:, :], x3_[:, k, :], wpg[:, k, h * 512:(h + 1) * 512], start=(k == 0), stop=(k == 7))
                        return ins
                    P.op(pe, fn, reads=[x3_, wpg], writes=[pg_])
                    P.op(act, lambda e, pg_=pg_, h=h: e.activation(out=tg_[:, h * 512:(h + 1) * 512], in_=pg_[:, :], func=AF.Tanh, scale=0.5), reads=[pg_], writes=[tg_])
                    pe_ = next_pmm()

                    def fn2(e, pe_=pe_, h=h):
                        ins = None
                        for k in range(2):
                            ins = e.matmul(pe_[:, :], pT_[:, k, :], wple[:, k, h * 512:(h + 1) * 512], start=(k == 0), stop=(k == 1))
                        return ins
                    P.op(pe, fn2, reads=[pT_, wple], writes=[pe_])
                    P.op(act, lambda e, pe_=pe_, h=h: e.activation(out=jk[:, 0:512], in_=pe_[:, :], func=AF.Square, accum_out=ssC[:, 4 + h:5 + h]), reads=[pe_], writes=[jk, ssC])
                    P.op(act, lambda e, pe_=pe_, h=h: e.activation(out=te_[:, h * 512:(h + 1) * 512], in_=pe_[:, :], func=AF.Copy), reads=[pe_], writes=[te_])
                    pes.append(pe_)
                yield
                P.op(dve, lambda e: e.tensor_tensor(ssC[:, 6:7], ssC[:, 4:5], ssC[:, 5:6], ALU.add), reads=[ssC], writes=[ssC])
                P.op(dve, lambda e: e.tensor_scalar(ssC[:, 7:8], ssC[:, 6:7], 4.0 / D, 4.0 * EPS, ALU.mult, ALU.add), reads=[ssC], writes=[ssC])
                yield
                rsq(ssC, 7, 9, 8)
                yield
                P.op(dve, lambda e: e.scalar_tensor_tensor(te_[:, :], te_[:, :], ssC[:, 8:9], gpp[:, :], ALU.mult, ALU.mult), reads=[te_, ssC, gpp], writes=[te_])
                P.op(dve, lambda e: e.scalar_tensor_tensor(te_[:, :], tg_[:, :], 1.0, te_[:, :], ALU.add, ALU.mult), reads=[tg_, te_], writes=[te_])
                yield
                P.op(pool, lambda e: e.tensor_tensor(xx[:, :], xx[:, :], te_[:, :], ALU.add), reads=[te_, xx], writes=[xx])
                yield
                P.op(act, lambda e: e.activation(out=jk[:, :], in_=xx[:, :], func=AF.Square, accum_out=ssC[:, 10:11]), reads=[xx], writes=[jk, ssC])
                yield
                P.op(dve, lambda e: e.tensor_scalar(ssC[:, 11:12], ssC[:, 10:11], 1.0 / D, EPS, ALU.mult, ALU.add), reads=[ssC], writes=[ssC])
                yield
                rsq(ssC, 11, 13, 12)
                yield
                P.op(dve, lambda e: e.scalar_tensor_tensor(ob_[:, :], xx[:, :], ssC[:, 12:13], gfin[:, :], ALU.mult, ALU.mult), reads=[xx, ssC, gfin], writes=[ob_])
                P.dma(sp, lambda e: e.dma_start(out=out_d[r0:r0 + 128, :], in_=ob_[:, :]), ob_, reads=[ob_])

            run_pipelined((subtile_gen(st) for st in range(T // 128)), depth=NP, skew=2)
            P.barrier()
            P.flush(block)
    return nc


def _chan(v):
    return np.ascontiguousarray(np.asarray(v, np.float32).reshape(4, 128).T)


def _gbc(g):
    return np.ascontiguousarray(np.asarray(g, np.float32).reshape(8, 128).T)


def prep_shared(inp):
    f = lambda a: np.ascontiguousarray(np.asarray(a, np.float32))
    cpar = np.zeros((128, NCP), np.float32)
    cw = f(inp["conv_dw_w"][0])
    for c in range(4):
        cpar[:, CW + c * 31:CW + (c + 1) * 31] = cw[:, c * 128:(c + 1) * 128].T
    cpar[:, CB:CB + 4] = _chan(inp["conv_dw_b"][0])
    cpar[:, LG:LG + 4] = _chan(inp["conv_ln_g"][0])
    cpar[:, LB:LB + 4] = _chan(inp["conv_ln_b"][0])
    lw = f(inp["lru_conv_w"][0])
    for c in range(4):
        cpar[:, LW + c * 4:LW + (c + 1) * 4] = lw[:, c * 128:(c + 1) * 128].T
    cpar[:, LBB:LBB + 4] = _chan(inp["lru_conv_b"][0])
    cpar[:, BR:BR + 4] = _chan(inp["lru_b_r"][0])
    cpar[:, BI:BI + 4] = _chan(inp["lru_b_i"][0])
    cpar[:, LAM:LAM + 4] = _chan(inp["lru_lambda"][0])
    wr_, wi_ = f(inp["lru_w_r"][0]), f(inp["lru_w_i"][0])
    gbd = np.zeros((4, 128, 256), np.float32)
    for c in range(4):
        for hh in range(2):
            gbd[c, hh * 64:(hh + 1) * 64, hh * 64:(hh + 1) * 64] = wr_[2 * c + hh]
            gbd[c, hh * 64:(hh + 1) * 64, 128 + hh * 64:128 + (hh + 1) * 64] = wi_[2 * c + hh]
    rb = np.concatenate([f(inp["b_group"][0]), f(inp["b_expert"][0])])
    shared = {
        "w_in": f(inp["w_in"][0]), "w_out": f(inp["w_out"][0]),
        "w_route": np.ascontiguousarray(np.concatenate([f(inp["w_group"][0]), f(inp["w_expert"][0])], axis=1)),
        "gate_bd": gbd, "cpar": cpar,
        "gbc": np.stack([_gbc(inp["g_mix"][0]), _gbc(inp["g_ffn"][0]), _gbc(inp["g_ple"][0])]),
        "rowbc": np.stack([np.ascontiguousarray(np.broadcast_to(f(inp["g_ple_proj"][0]), (128, D))),
                           np.ascontiguousarray(np.broadcast_to(f(inp["g_final"]), (128, D)))]),
        "rbias": np.ascontiguousarray(np.broadcast_to(np.tile(rb, Q), (128, Q * 36))),
        "iota_e": np.ascontiguousarray(np.broadcast_to(np.tile(np.arange(32, dtype=np.float32), Q), (128, Q * 32))),
        "ident": np.eye(128, dtype=np.float32),
        "tri": np.ascontiguousarray(np.triu(np.ones((128, 128), np.float32), 1)),
        "w1": f(inp["w1"][0]), "w3": f(inp["w3"][0]), "w2": f(inp["w2"][0]),
        "w_ple": f(inp["w_ple"][0]), "w_ple_gate": f(inp["w_ple_gate"][0]),
    }
    return shared


def kernel(**inputs):
    NSEQ, CAP = 4, 1024
    x = np.asarray(inputs["x"], np.float32)
    p = np.asarray(inputs["p"], np.float32)[0]
    shared = prep_shared(inputs)
    nc = build(NSEQ, CAP)
    in_maps = []
    for i in range(N_CORES):
        m = dict(shared)
        m["x"] = np.ascontiguousarray(x[i * NSEQ:(i + 1) * NSEQ].reshape(NSEQ * SEQ, D))
        m["p"] = np.ascontiguousarray(p[i * NSEQ:(i + 1) * NSEQ].reshape(NSEQ * SEQ, 256))
        in_maps.append(m)
    res = run_bass_kernel_spmd(nc, in_maps, core_ids=list(range(N_CORES)))
    out = np.concatenate([np.asarray(r["out"], np.float32).reshape(NSEQ, SEQ, D) for r in res.results], axis=0)
    return out
```

```python
import numpy as np
from contextlib import ExitStack
import concourse.bass as bass
import concourse.mybir as mybir
from concourse.bass_utils import run_bass_kernel_spmd

F32 = mybir.dt.float32
BF16 = mybir.dt.bfloat16
I32 = mybir.dt.int32
ALU = mybir.AluOpType
AF = mybir.ActivationFunctionType
AX = mybir.AxisListType

N_CORES = 8
HOP = True
NP_C = 8
LOADLAG = 3
MC_OFF = 0
SEQ = 2048
D = 1024
EPS = 1e-6
Q = 4

CW = 0
CB = CW + 124
LG = CB + 4
LB = LG + 4
LW = LB + 4
LBB = LW + 16
BR = LBB + 4
BI = BR + 4
LAM = BI + 4
NCP = LAM + 4
D_CWH = 0
D_GH = 124
D_BH = 128
D_BRH = 132
D_BIH = 136
D_N4 = 140
D_N8 = 144
NDP = 148


class Src:
    def __init__(self, sem, name, is_dma):
        self.sem, self.name, self.is_dma, self.total = sem, name, is_dma, 0


class Eng(Src):
    def __init__(self, sem, name, blockname, same_wait=True):
        super().__init__(sem, name, False)
        self.blockname, self.ops, self.seen, self.same_wait = blockname, [], {}, same_wait


class Buf:
    def __init__(self, t, dsem=None):
        self.t, self.w, self.r, self.dsem = t, None, {}, dsem

    def __getitem__(self, k):
        return self.t[k]


class Prog:
    def __init__(self, nc, stack):
        self.nc, self.stack = nc, stack
        self.srcs = []
        mk = lambda n, b, sw=True: self._reg(Eng(self._sem("e_" + n), n, b, sw))
        self.pe = mk("pe", "tensor", False)
        self.act = mk("act", "scalar")
        self.dve = mk("dve", "vector")
        self.pool = mk("pool", "gpsimd")
        self.sp = mk("sp", "sync")
        self.engs = [self.pe, self.act, self.dve, self.pool, self.sp]
        self.nbuf = 0

    def _sem(self, name):
        return self.stack.enter_context(self.nc.semaphore(name))

    def _reg(self, s):
        self.srcs.append(s)
        return s

    def buf(self, stack, shape, dt, name=None, dma=False, psum=False):
        self.nbuf += 1
        name = "%s_%d" % (name or "b", self.nbuf)
        if psum:
            t = stack.enter_context(self.nc.psum_tensor(name, shape, dt))
        else:
            t = stack.enter_context(self.nc.sbuf_tensor(name, shape, dt))
        ds = self._reg(Src(self._sem("d_" + name), name, True)) if dma else None
        return Buf(t, ds)

    def _deps(self, eng, reads, writes):
        need = {}

        def add(src, val):
            if src.is_dma:
                val = src.total
            if need.get(src, 0) < val:
                need[src] = val

        for b in reads:
            if b.w is not None:
                add(*b.w)
        for b in writes:
            if b.w is not None:
                add(*b.w)
            for s, v in b.r.items():
                add(s, v)
        waits = []
        for src, val in need.items():
            if src is eng and not eng.same_wait:
                continue
            if eng.seen.get(src, 0) >= val:
                continue
            eng.seen[src] = val
            waits.append((src.sem, val))
        return waits

    def _mark(self, src, val, reads, writes):
        for b in reads:
            b.r[src] = val
        for b in writes:
            b.w = (src, val)
            b.r = {}

    def op(self, eng, fn, reads=(), writes=()):
        waits = self._deps(eng, reads, writes)
        eng.total += 1
        sem = eng.sem

        def emit(e):
            for s, v in waits:
                e.wait_ge(s, v)
            fn(e).then_inc(sem, 1)

        eng.ops.append(emit)
        self._mark(eng, eng.total, reads, writes)

    def dma(self, q, fn, sem_buf, reads=(), writes=()):
        src = sem_buf.dsem
        waits = self._deps(q, reads, writes)
        src.total += 16
        sem = src.sem

        def emit(e):
            for s, v in waits:
                e.wait_ge(s, v)
            fn(e).then_inc(sem, 16)

        q.ops.append(emit)
        self._mark(src, src.total, reads, writes)

    def barrier(self):
        for E in self.engs:
            waits = []
            for S in self.srcs:
                if S is E or S.total == 0:
                    continue
                if E.seen.get(S, 0) >= S.total:
                    continue
                E.seen[S] = S.total
                waits.append((S.sem, S.total))

            def emit(e, waits=waits):
                for s, v in waits:
                    e.wait_ge(s, v)

            E.ops.append(emit)

    def flush(self, block):
        for E in self.engs:
            if not E.ops:
                continue
            ops = E.ops
            E.ops = []

            def body(e, ops=ops):
                for f in ops:
                    f(e)

            getattr(block, E.blockname)(body)


def run_pipelined(gens, depth, skew):
    it = iter(gens)
    active, pending, tick = [], True, 0
    while pending or active:
        for g in list(active):
            try:
                next(g)
            except StopIteration:
                active.remove(g)
        if pending and tick % skew == 0 and len(active) < depth:
            try:
                g = next(it)
                active.append(g)
                next(g)
            except StopIteration:
                pending = False
        tick += 1


def build(NSEQ=4, CAP=640, debug=False):
    T = NSEQ * SEQ
    NT = T // 512
    NSUB = CAP // 128
    NSLOT = 32 * CAP
    TRASH = NSLOT
    nc = bass.Bass("TRN2", target_bir_lowering=False)

    def dr(name, shape, dt=F32, kind="ExternalInput"):
        return nc.dram_tensor(name, shape, dt, kind=kind).ap()

    x_d = dr("x", [T, D])
    p_d = dr("p", [T, 256])
    win_d = dr("w_in", [D, 2048])
    wout_d = dr("w_out", [D, D])
    wr_d = dr("w_route", [D, 36])
    gbd_d = dr("gate_bd", [4, 128, 256])
    cpar_d = dr("cpar", [128, NCP])
    gbc_d = dr("gbc", [3, 128, 8])
    rowbc_d = dr("rowbc", [2, 128, D])
    rb_d = dr("rbias", [128, Q * 36])
    iota_d = dr("iota_e", [128, Q * 32])
    ident_d = dr("ident", [128, 128])
    tri_d = dr("tri", [128, 128])
    w1_d = dr("w1", [32, D, 512])
    w3_d = dr("w3", [32, D, 512])
    w2_d = dr("w2", [32, 512, D])
    wple_d = dr("w_ple", [256, D])
    wpg_d = dr("w_ple_gate", [D, D])
    out_d = dr("out", [T, D], kind="ExternalOutput")
    sk = "ExternalOutput" if debug else "Internal"
    x1_d = dr("x1s", [T, D], kind=sk)
    hs_d = dr("hss", [NSLOT + 128, D], BF16, kind=sk)
    y_d = dr("yss", [NSLOT + 128, D], kind=sk)
    if debug:
        ri_d = dr("rinfo_o", [128, NT * 4 * Q], kind="ExternalOutput")

    IOA = bass.IndirectOffsetOnAxis

    with ExitStack() as top:
        P = Prog(nc, top)
        pe, act, dve, pool, sp = P.pe, P.act, P.dve, P.pool, P.sp
        block = top.enter_context(nc.Block())

        pmm = [P.buf(top, [128, 512], F32, "pmm", psum=True) for _ in range(4)]
        ptr = P.buf(top, [128, 1024], BF16, "ptr", psum=True)
        ptr2 = P.buf(top, [128, 1024], BF16, "ptr2", psum=True)
        ps1 = P.buf(top, [128, 512], F32, "ps1", psum=True)
        ps2 = P.buf(top, [128, 512], F32, "ps2", psum=True)
        pmm_i = [0]

        def next_pmm():
            b = pmm[pmm_i[0] % 4]
            pmm_i[0] += 1
            return b

        rinfo = P.buf(top, [128, NT, 4, Q], F32, "rinfo")
        sloti = P.buf(top, [128, NT, 2, Q], I32, "sloti")
        if debug:
            rinfo.dsem = P._reg(Src(P._sem("d_rinfo"), "rinfo", True))
        ident = P.buf(top, [128, 128], BF16, "ident", dma=True)
        P.dma(pool, lambda e: e.dma_start(out=ident[:, :], in_=ident_d), ident, writes=[ident])

        def transposes(src, n, dst_ps):
            def fn(e):
                ins = None
                for c in range(n):
                    ins = e.transpose(dst_ps[:, c * 128:(c + 1) * 128], src[:, c * 128:(c + 1) * 128], ident[:, :])
                return ins
            P.op(pe, fn, reads=[src, ident], writes=[dst_ps])

        def rstd_from_ss(ss, out, n):
            P.op(dve, lambda e: e.tensor_scalar(out, ss, 1.0 / n, EPS, ALU.mult, ALU.add), reads=[], writes=[])

        with ExitStack() as sa:
            B = lambda shape, dt=F32, name=None, dma=False: P.buf(sa, shape, dt, name, dma)
            win = B([128, 8, 2048], BF16, "win", dma=True)
            wout = B([128, 8, D], BF16, "wout", dma=True)
            wr = B([128, 8, 36], BF16, "wr", dma=True)
            gbd = B([128, 4, 256], BF16, "gbd", dma=True)
            tri = B([128, 128], BF16, "tri", dma=True)
            cpar = B([128, NCP], F32, "cpar", dma=True)
            dpar = B([128, NDP], F32, "dpar")
            gmixbc = B([128, 8], F32, "gmixbc", dma=True)
            gffnbc = B([128, 8], F32, "gffnbc", dma=True)
            rbias = B([128, Q, 36], F32, "rbias", dma=True)
            iota = B([128, Q, 32], F32, "iota", dma=True)
            ones = B([128, 128], BF16, "ones")
            identF = B([128, 128], F32, "identF", dma=True)
            P.dma(sp, lambda e: e.dma_start(out=identF[:, :], in_=ident_d), identF, writes=[identF])
            cntbc = B([128, 32], F32, "cntbc")

            P.dma(sp, lambda e: e.dma_start(out=cpar[:, :], in_=cpar_d), cpar, writes=[cpar])
            P.dma(sp, lambda e: e.dma_start(out=gmixbc[:, :], in_=gbc_d[0]), gmixbc, writes=[gmixbc])
            P.dma(sp, lambda e: e.dma_start(out=gffnbc[:, :], in_=gbc_d[1]), gffnbc, writes=[gffnbc])
            P.dma(sp, lambda e: e.dma_start(out=rbias[:, :, :], in_=rb_d.rearrange("p (q n) -> p q n", q=Q)), rbias, writes=[rbias])
            P.dma(sp, lambda e: e.dma_start(out=iota[:, :, :], in_=iota_d.rearrange("p (q n) -> p q n", q=Q)), iota, writes=[iota])
            P.dma(pool, lambda e: e.dma_start(out=tri[:, :], in_=tri_d), tri, writes=[tri])
            for k in range(8):
                P.dma(pool, lambda e, k=k: e.dma_start(out=win[:, k, :], in_=win_d[k * 128:(k + 1) * 128, :]), win, writes=[win])
            P.dma(pool, lambda e: e.dma_start(out=gbd[:, :, :], in_=gbd_d.rearrange("c p n -> p c n")), gbd, writes=[gbd])
            for k in range(8):
                P.dma(pool, lambda e, k=k: e.dma_start(out=wout[:, k, :], in_=wout_d[k * 128:(k + 1) * 128, :]), wout, writes=[wout])
            P.dma(pool, lambda e: e.dma_start(out=wr[:, :, :], in_=wr_d.rearrange("(k p) n -> p k n", p=128)), wr, writes=[wr])

            P.op(dve, lambda e: e.memset(ones[:, :], 1.0), writes=[ones])
            P.op(dve, lambda e: e.memset(cntbc[:, :], 0.0), writes=[cntbc])
            for k in range(8):
                P.op(dve, lambda e, k=k: e.tensor_scalar(win[:, k, :], win[:, k, :], gmixbc[:, k:k + 1], None, ALU.mult), reads=[win, gmixbc], writes=[win])
                P.op(dve, lambda e, k=k: e.tensor_scalar(wr[:, k, :], wr[:, k, :], gffnbc[:, k:k + 1], None, ALU.mult), reads=[wr, gffnbc], writes=[wr])

            tsm = B([128, 8, 4], F32, "tsm")

            def dv(fn, reads, writes):
                P.op(dve, fn, reads=reads, writes=writes)

            dv(lambda e: e.tensor_scalar(dpar[:, D_CWH:D_CWH + 124], cpar[:, CW:CW + 124], 0.5, None, ALU.mult), [cpar], [dpar])
            dv(lambda e: e.tensor_scalar(dpar[:, D_GH:D_GH + 8], cpar[:, LG:LG + 8], 0.5, None, ALU.mult), [cpar], [dpar])
            dv(lambda e: e.tensor_scalar(dpar[:, D_BRH:D_BRH + 8], cpar[:, BR:BR + 8], 0.5, None, ALU.mult), [cpar], [dpar])
            z_, az, ee, LL, tt, mk_, zp = [tsm[:, i, :] for i in range(7)]
            dv(lambda e: e.tensor_scalar(z_, cpar[:, LAM:LAM + 4], -1.0, None, ALU.mult), [cpar], [tsm])
            dv(lambda e: e.tensor_tensor(az, z_, cpar[:, LAM:LAM + 4], ALU.max), [tsm, cpar], [tsm])
            P.op(act, lambda e: e.activation(out=ee, in_=az, func=AF.Exp, scale=-1.0), reads=[tsm], writes=[tsm])
            P.op(act, lambda e: e.activation(out=LL, in_=ee, func=AF.Ln, bias=1.0, scale=1.0), reads=[tsm], writes=[tsm])
            dv(lambda e: e.tensor_scalar(tt, ee, -0.25, 1.0 / 3.0, ALU.mult, ALU.add), [tsm], [tsm])
            dv(lambda e: e.tensor_tensor(tt, tt, ee, ALU.mult), [tsm], [tsm])
            dv(lambda e: e.tensor_scalar(tt, tt, -1.0, 0.5, ALU.mult, ALU.add), [tsm], [tsm])
            dv(lambda e: e.tensor_tensor(tt, tt, ee, ALU.mult), [tsm], [tsm])
            dv(lambda e: e.tensor_scalar(tt, tt, -1.0, 1.0, ALU.mult, ALU.add), [tsm], [tsm])
            dv(lambda e: e.tensor_tensor(tt, tt, ee, ALU.mult), [tsm], [tsm])
            dv(lambda e: e.tensor_single_scalar(mk_, ee, 0.05, ALU.is_lt), [tsm], [tsm])
            dv(lambda e: e.tensor_tensor(tt, tt, LL, ALU.subtract), [tsm], [tsm])
            dv(lambda e: e.tensor_tensor(tt, tt, mk_, ALU.mult), [tsm], [tsm])
            dv(lambda e: e.tensor_tensor(tt, tt, LL, ALU.add), [tsm], [tsm])
            dv(lambda e: e.tensor_single_scalar(zp, z_, 0.0, ALU.max), [tsm], [tsm])
            dv(lambda e: e.tensor_tensor(tt, tt, zp, ALU.add), [tsm], [tsm])
            dv(lambda e: e.tensor_scalar(dpar[:, D_N4:D_N4 + 4], tt, -4.0, None, ALU.mult), [tsm], [dpar])
            dv(lambda e: e.tensor_scalar(dpar[:, D_N8:D_N8 + 4], tt, -8.0, None, ALU.mult), [tsm], [dpar])

            xa = [B([128, D], F32, "xa", dma=True) for _ in range(2)]
            ssF = [B([128, 8], F32, "ssF") for _ in range(2)]
            xn = [B([128, D], BF16, "xn") for _ in range(2)]
            hT = B([128, 8, 512], BF16, "hT")
            gth = [B([128, 512], F32, "gth") for _ in range(2)]
            gvs = [B([128, 512], F32, "gvs") for _ in range(2)]
            ga = B([128, 512], F32, "ga")
            gb = B([128, 512], F32, "gb")
            ub = [[B([128, 542], BF16, "ub") for _ in range(4)] for _ in range(2)]
            xbuf = [[B([128, 515], BF16, "xbuf") for _ in range(4)] for _ in range(2)]
            qg = [[B([128, 512], BF16, "qg") for _ in range(4)] for _ in range(2)]
            acc2 = [[B([128, 512], F32, "acc") for _ in range(4)] for _ in range(2)]
            cvbf = [B([128, 512], BF16, "cvbf") for _ in range(4)]
            sqbf = [B([128, 512], BF16, "sqbf") for _ in range(4)]
            mean = B([128, 512], F32, "mean")
            msq = B([128, 512], F32, "msq")
            xr = [B([128, 512], F32, "xr") for _ in range(2)]
            xrbf = [B([128, 512], BF16, "xrbf") for _ in range(2)]
            t1 = [B([128, 512], F32, "t1") for _ in range(2)]
            t2 = [B([128, 512], F32, "t2") for _ in range(2)]
            t3 = [B([128, 512], F32, "t3") for _ in range(2)]
            lh, th = t1, t2
            ab = [B([128, 512], F32, "ab")] * 2
            hb = [B([128, 512], F32, "hb")] * 2
            carry = [B([128, 1], F32, "carry") for _ in range(4)]
            yT = [B([128, 8, 512], BF16, "yT") for _ in range(2)]
            xb = B([128, D], F32, "xb", dma=True)
            ssE = [B([128, 8], F32, "ssE") for _ in range(2)]
            x1 = [B([128, D], F32, "x1", dma=True) for _ in range(2)]
            hfn = [B([128, D], BF16, "hfn", dma=True) for _ in range(4)]
            hfT = [B([128, 8, 128], BF16, "hfT") for _ in range(2)]
            rs = B([128, 40, Q], F32, "rs")
            r36 = B([128, Q, 36], F32, "r36")
            r32 = [B([128, Q, 32], F32, "r32") for _ in range(5)]
            mbf = B([128, Q, 32], BF16, "mbf")
            r8 = [B([128, Q, 8], F32, "r8") for _ in range(4)]
            r4 = [B([128, Q, 4], F32, "r4") for _ in range(3)]

            cwh = lambda c, k: dpar[:, D_CWH + c * 31 + k:D_CWH + c * 31 + k + 1]
            NJ = SEQ // 512

            def rsqA(ssb, i_v, i_l, i_o):
                P.op(act, lambda e: e.activation(out=ssb[:, i_l:i_l + 1], in_=ssb[:, i_v:i_v + 1], func=AF.Ln), reads=[ssb], writes=[ssb])
                P.op(act, lambda e: e.activation(out=ssb[:, i_o:i_o + 1], in_=ssb[:, i_l:i_l + 1], func=AF.Exp, scale=-0.5), reads=[ssb], writes=[ssb])

            def evac_scaled(dst3, ps, gvec):
                P.op(act, lambda e: e.activation(out=dst3.all(), in_=ps[:, :].rearrange("p (c j) -> p c j", c=8), func=AF.Copy),
                     reads=[ps], writes=[dst3.buf])

            class View3:
                def __init__(self, buf, fn, allfn=None):
                    self.buf, self.fn, self.all = buf, fn, allfn

                def __call__(self, c):
                    return self.fn(c)

            def gen_F(ti):
                s_, j = divmod(ti, NJ)
                row0 = ti * 512
                par = ti % 2
                ub_, xbuf_, qg_ = ub[par], xbuf[par], qg[par]
                if j == 0:
                    for c in range(4):
                        P.op(pool, lambda e, c=c: e.memset(ub_[c][:, 0:30], 0.0), writes=[ub_[c]])
                        P.op(pool, lambda e, c=c: e.memset(xbuf_[c][:, 0:3], 0.0), writes=[xbuf_[c]])
                for q in range(Q):
                    yield ("SEG" if q % 2 == 0 else "CHAIN")
                    xt, xnb, ss = xa[q % 2], xn[q % 2], ssF[q % 2]
                    r0 = row0 + q * 128
                    P.dma(sp, lambda e, xt=xt, r0=r0: e.dma_start(out=xt[:, :], in_=x_d[r0:r0 + 128, :]), xt, writes=[xt])
                    P.op(act, lambda e, xt=xt, ss=ss, xnb=xnb: e.activation(out=xnb[:, :], in_=xt[:, :], func=AF.Square, accum_out=ss[:, 0:1]),
                         reads=[xt], writes=[xnb, ss])
                    P.op(dve, lambda e, ss=ss: e.tensor_scalar(ss[:, 1:2], ss[:, 0:1], 1.0 / D, EPS, ALU.mult, ALU.add), reads=[ss], writes=[ss])
                    rsqA(ss, 1, 3, 2)
                    P.op(act, lambda e, xt=xt, xnb=xnb, ss=ss: e.activation(out=xnb[:, :], in_=xt[:, :], func=AF.Copy, scale=ss[:, 2:3]),
                         reads=[xt, ss], writes=[xnb])
                    transposes(xnb, 8, ptr)
                    evac_scaled(View3(hT, None, lambda q=q: hT[:, :, q * 128:(q + 1) * 128]), ptr, gmixbc)

                fbank = [pmm[0], ps2]

                def zmm(m, bi):
                    pb = fbank[bi % 2]

                    def fn(e, m=m, pb=pb):
                        ins = None
                        for k in range(8):
                            ins = e.matmul(pb[:, :], win[:, k, m * 128:(m + 1) * 128], hT[:, k, :], start=(k == 0), stop=(k == 7))
                        return ins
                    P.op(pe, fn, reads=[win, hT], writes=[pb])
                    return pb

                for c in range(4):
                    yield ("SEG" if c % 2 == 0 else "CHAIN")
                    pg = zmm(4 + c, c)
                    ta, vs = gth[c % 2], gvs[c % 2]
                    P.op(act, lambda e, pg=pg, ta=ta: e.activation(out=ta[:, :], in_=pg[:, :], func=AF.Tanh, scale=0.5), reads=[pg], writes=[ta])
                    pv = zmm(c, c)
                    P.op(act, lambda e, pv=pv, vs=vs: e.activation(out=vs[:, :], in_=pv[:, :], func=AF.Copy), reads=[pv], writes=[vs])
                    P.op(pool, lambda e, ta=ta, vs=vs: e.tensor_tensor(ta[:, :], ta[:, :], vs[:, :], ALU.mult), reads=[ta, vs], writes=[ta])
                    P.op(pool, lambda e, ta=ta, vs=vs, c=c: e.tensor_tensor(ub_[c][:, 30:542], ta[:, :], vs[:, :], ALU.add), reads=[ta, vs], writes=[ub_[c]])
                for c in range(4):
                    yield ("SEG" if c % 2 == 0 else "CHAIN")
                    px = zmm(8 + c, c)
                    P.op(act, lambda e, px=px, c=c: e.activation(out=xbuf_[c][:, 3:515], in_=px[:, :], func=AF.Copy), reads=[px], writes=[xbuf_[c]])
                for c in range(4):
                    yield "SEG"
                    pgl = zmm(12 + c, c)
                    P.op(act, lambda e, pgl=pgl: e.activation(out=ga[:, :], in_=pgl[:, :], func=AF.Copy), reads=[pgl], writes=[ga])
                    P.op(act, lambda e, pgl=pgl: e.activation(out=gb[:, :], in_=pgl[:, :], func=AF.Square), reads=[pgl], writes=[gb])
                    P.op(act, lambda e: e.activation(out=gb[:, :], in_=gb[:, :], func=AF.Identity, bias=1.0, scale=0.044715), reads=[gb], writes=[gb])
                    P.op(pool, lambda e: e.tensor_tensor(gb[:, :], gb[:, :], ga[:, :], ALU.mult), reads=[ga, gb], writes=[gb])
                    P.op(act, lambda e: e.activation(out=gb[:, :], in_=gb[:, :], func=AF.Tanh, scale=0.7978845608028654), reads=[gb], writes=[gb])
                    P.op(act, lambda e: e.activation(out=gb[:, :], in_=gb[:, :], func=AF.Identity, bias=1.0, scale=1.0), reads=[gb], writes=[gb])
                    P.op(pool, lambda e, c=c: e.tensor_tensor(qg_[c][:, :], gb[:, :], ga[:, :], ALU.mult), reads=[ga, gb], writes=[qg_[c]])

            def gen_Mc(ti):
                s_, j = divmod(ti, NJ)
                par = ti % 2
                ub_, xbuf_, qg_, yT_ = ub[par], xbuf[par], qg[par], yT[par]
                ubn, xbufn = ub[1 - par], xbuf[1 - par]
                acc = acc2[par]
                for k in range(31):
                    for c in range(4):
                        if k == 0:
                            P.op(dve, lambda e, c=c: e.tensor_scalar(acc[c][:, :], ub_[c][:, 0:512], cwh(c, 0), cpar[:, CB + c:CB + c + 1], ALU.mult, ALU.add),
                                 reads=[ub_[c], dpar, cpar], writes=[acc[c]])
                        else:
                            P.op(dve, lambda e, c=c, k=k: e.scalar_tensor_tensor(acc[c][:, :], ub_[c][:, k:k + 512], cwh(c, k), acc[c][:, :], ALU.mult, ALU.add),
                                 reads=[ub_[c], dpar, acc[c]], writes=[acc[c]])
                    yield
                for c in range(4):
                    if j < NJ - 1:
                        P.op(pool, lambda e, c=c: e.tensor_copy(ubn[c][:, 0:30], ub_[c][:, 512:542]), reads=[ub_[c]], writes=[ubn[c]])
                    P.op(act, lambda e, c=c: e.activation(out=cvbf[c][:, :], in_=acc[c][:, :], func=AF.Copy), reads=[acc[c]], writes=[cvbf[c]])
                    P.op(act, lambda e, c=c: e.activation(out=sqbf[c][:, :], in_=acc[c][:, :], func=AF.Square), reads=[acc[c]], writes=[sqbf[c]])

                def stat_mm(dst, srcs):
                    def fn(e):
                        ins = None
                        for c in range(4):
                            ins = e.matmul(dst[:, :], ones[:, :], srcs[c][:, :], start=(c == 0), stop=(c == 3))
                        return ins
                    P.op(pe, fn, reads=[ones] + srcs, writes=[dst])
                stat_mm(ps1, cvbf)
                P.op(act, lambda e: e.activation(out=mean[:, :], in_=ps1[:, :], func=AF.Copy, scale=1.0 / 512), reads=[ps1], writes=[mean])
                stat_mm(ps1, sqbf)
                P.op(pool, lambda e: e.tensor_tensor(msq[:, :], mean[:, :], mean[:, :], ALU.mult), reads=[mean], writes=[msq])
                P.op(dve, lambda e: e.scalar_tensor_tensor(msq[:, :], ps1[:, :], 1.0 / 512, msq[:, :], ALU.mult, ALU.subtract), reads=[ps1, msq], writes=[msq])
                P.op(dve, lambda e: e.tensor_scalar(msq[:, :], msq[:, :], EPS, None, ALU.add), reads=[msq], writes=[msq])
                P.op(act, lambda e: e.activation(out=msq[:, :], in_=msq[:, :], func=AF.Ln), reads=[msq], writes=[msq])
                P.op(act, lambda e: e.activation(out=msq[:, :], in_=msq[:, :], func=AF.Exp, scale=-0.5), reads=[msq], writes=[msq])
                yield
                for c in range(4):
                    P.op(pool, lambda e, c=c: e.tensor_tensor(acc[c][:, :], acc[c][:, :], mean[:, :], ALU.subtract), reads=[acc[c], mean], writes=[acc[c]])
                    P.op(pool, lambda e, c=c: e.tensor_tensor(acc[c][:, :], acc[c][:, :], msq[:, :], ALU.mult), reads=[acc[c], msq], writes=[acc[c]])
                for c in range(4):
                    t_ = (mean, msq)[c % 2]
                    P.op(act, lambda e, c=c: e.activation(out=acc[c][:, :], in_=acc[c][:, :], func=AF.Identity,
                                                          bias=dpar[:, D_BH + c:D_BH + c + 1], scale=dpar[:, D_GH + c:D_GH + c + 1]),
                         reads=[acc[c], dpar], writes=[acc[c]])
                    P.op(act, lambda e, c=c, t_=t_: e.activation(out=t_[:, :], in_=acc[c][:, :], func=AF.Tanh), reads=[acc[c]], writes=[t_])
                    P.op(dve, lambda e, c=c, t_=t_: e.scalar_tensor_tensor(yT_[:, c, :], t_[:, :], 1.0, acc[c][:, :], ALU.add, ALU.mult),
                         reads=[acc[c], t_], writes=[yT_])
            def gen_Ml(ti):
                s_, j = divmod(ti, NJ)
                par = ti % 2
                xbuf_, qg_, yT_ = xbuf[par], qg[par], yT[par]
                xbufn = xbuf[1 - par]
                if j == 0:
                    for c in range(4):
                        P.op(pool, lambda e, c=c: e.memset(carry[c][:, :], 0.0), writes=[carry[c]])
                for c in range(4):
                    xr_, xrb_ = xr[c % 2], xrbf[c % 2]
                    lw = lambda k, c=c: cpar[:, LW + c * 4 + k:LW + c * 4 + k + 1]
                    P.op(dve, lambda e, c=c, xr_=xr_, lw=lw: e.tensor_scalar(xr_[:, :], xbuf_[c][:, 0:512], lw(0), cpar[:, LBB + c:LBB + c + 1], ALU.mult, ALU.add),
                         reads=[xbuf_[c], cpar], writes=[xr_])
                    for k in range(1, 4):
                        P.op(dve, lambda e, c=c, k=k, xr_=xr_, lw=lw: e.scalar_tensor_tensor(xr_[:, :], xbuf_[c][:, k:k + 512], lw(k), xr_[:, :], ALU.mult, ALU.add),
                             reads=[xbuf_[c], cpar, xr_], writes=[xr_])
                    if j < NJ - 1:
                        P.op(pool, lambda e, c=c: e.tensor_copy(xbufn[c][:, 0:3], xbuf_[c][:, 512:515]), reads=[xbuf_[c]], writes=[xbufn[c]])
                    P.op(act, lambda e, xr_=xr_, xrb_=xrb_: e.activation(out=xrb_[:, :], in_=xr_[:, :], func=AF.Copy), reads=[xr_], writes=[xrb_])
                    pr, pi = pmm[1], pmm[1]
                    P.op(pe, lambda e, c=c, pr=pr, xrb_=xrb_: e.matmul(pr[:, :], gbd[:, c, 0:128], xrb_[:, :], start=True, stop=True), reads=[gbd, xrb_], writes=[pr])
                    a1, a2, a3, aa, hh = t1[c % 2], t2[c % 2], t3[c % 2], ab[c % 2], hb[c % 2]
                    dp = lambda o, c=c: dpar[:, o + c:o + c + 1]
                    yield
                    P.op(act, lambda e, pr=pr, a1=a1, dp=dp: e.activation(out=a1[:, :], in_=pr[:, :], func=AF.Tanh, bias=dp(D_BRH), scale=0.5), reads=[pr, dpar], writes=[a1])
                    P.op(pe, lambda e, c=c, pi=pi, xrb_=xrb_: e.matmul(pi[:, :], gbd[:, c, 128:256], xrb_[:, :], start=True, stop=True), reads=[gbd, xrb_], writes=[pi])
                    P.op(act, lambda e, pi=pi, a3=a3, dp=dp: e.activation(out=a3[:, :], in_=pi[:, :], func=AF.Tanh, bias=dp(D_BIH), scale=0.5), reads=[pi, dpar], writes=[a3])
                    P.op(act, lambda e, a1=a1, aa=aa, dp=dp: e.activation(out=aa[:, :], in_=a1[:, :], func=AF.Exp, bias=dp(D_N4), scale=dp(D_N4)), reads=[a1, dpar], writes=[aa])
                    P.op(act, lambda e, a1=a1, a2=a2, dp=dp: e.activation(out=a2[:, :], in_=a1[:, :], func=AF.Exp, bias=dp(D_N8), scale=dp(D_N8)), reads=[a1, dpar], writes=[a2])
                    P.op(dve, lambda e, a2=a2: e.tensor_scalar(a2[:, :], a2[:, :], 0.99999994, -1.0, ALU.min, ALU.mult), reads=[a2], writes=[a2])
                    P.op(act, lambda e, a2=a2: e.activation(out=a2[:, :], in_=a2[:, :], func=AF.Ln, bias=1.0, scale=1.0), reads=[a2], writes=[a2])
                    P.op(act, lambda e, a2=a2: e.activation(out=a2[:, :], in_=a2[:, :], func=AF.Exp, scale=0.5), reads=[a2], writes=[a2])
                    P.op(dve, lambda e, a3=a3, xr_=xr_: e.scalar_tensor_tensor(a3[:, :], a3[:, :], 1.0, xr_[:, :], ALU.add, ALU.mult), reads=[a3, xr_], writes=[a3])
                    yield
                    P.op(dve, lambda e, a2=a2, a3=a3: e.tensor_tensor(a3[:, :], a3[:, :], a2[:, :], ALU.mult), reads=[a2, a3], writes=[a3])
                    P.op(dve, lambda e, c=c, aa=aa, a3=a3, hh=hh: e.tensor_tensor_scan(hh[:, :], aa[:, :], a3[:, :], carry[c][:, 0:1], ALU.mult, ALU.add),
                         reads=[aa, a3, carry[c]], writes=[hh])
                    P.op(dve, lambda e, c=c, hh=hh: e.tensor_copy(carry[c][:, :], hh[:, 511:512]), reads=[hh], writes=[carry[c]])
                    P.op(dve, lambda e, c=c, hh=hh: e.scalar_tensor_tensor(yT_[:, 4 + c, :], hh[:, :], 0.25, qg_[c][:, :], ALU.mult, ALU.mult),
                         reads=[hh, qg_[c]], writes=[yT_])
                    yield

            def gen_E(ti):
                row0 = ti * 512
                yT_ = yT[ti % 2]
                for q in range(Q):
                    yield ("SEG" if q % 2 == 0 else "CHAIN")
                    r0 = row0 + q * 128
                    x1t, ss = x1[q % 2], ssE[q % 2]
                    hf = hfn[q]
                    hft = hfT[q % 2]
                    P.dma(sp, lambda e, r0=r0: e.dma_start(out=xb[:, :], in_=x_d[r0:r0 + 128, :]), xb, writes=[xb])
                    for h in range(2):
                        pb = pmm[2]

                        def fn(e, pb=pb, h=h, q=q):
                            ins = e.matmul(pb[:, :], identF[:, :], xb[:, h * 512:(h + 1) * 512], start=True, stop=False)
                            for k in range(8):
                                ins = e.matmul(pb[:, :], yT_[:, k, q * 128:(q + 1) * 128], wout[:, k, h * 512:(h + 1) * 512], start=False, stop=(k == 7))
                            return ins
                        P.op(pe, fn, reads=[yT_, wout, identF, xb], writes=[pb])
                        P.op(act, lambda e, pb=pb, h=h, x1t=x1t: e.activation(out=x1t[:, h * 512:(h + 1) * 512], in_=pb[:, :], func=AF.Copy),
                             reads=[pb], writes=[x1t])
                    P.dma(sp, lambda e, x1t=x1t, r0=r0: e.dma_start(out=x1_d[r0:r0 + 128, :], in_=x1t[:, :]), x1t, reads=[x1t])
                    P.op(act, lambda e, x1t=x1t, ss=ss, hf=hf: e.activation(out=hf[:, :], in_=x1t[:, :], func=AF.Square, accum_out=ss[:, 0:1]), reads=[x1t], writes=[hf, ss])
                    P.op(dve, lambda e, ss=ss: e.tensor_scalar(ss[:, 1:2], ss[:, 0:1], 1.0 / D, EPS, ALU.mult, ALU.add), reads=[ss], writes=[ss])
                    rsqA(ss, 1, 3, 2)
                    P.op(act, lambda e, x1t=x1t, hf=hf, ss=ss: e.activation(out=hf[:, :], in_=x1t[:, :], func=AF.Copy, scale=ss[:, 2:3]), reads=[x1t, ss], writes=[hf])
                    transposes(hf, 8, ptr2)
                    evac_scaled(View3(hft, None, lambda hft=hft: hft[:, :, :]), ptr2, gffnbc)

                    def fnl(e, hft=hft, q=q):
                        ins = None
                        for k in range(8):
                            ins = e.matmul(pmm[3][:, q * 36:(q + 1) * 36], hft[:, k, :], wr[:, k, :], start=(k == 0), stop=(k == 7))
                        return ins
                    P.op(pe, fnl, reads=[hft, wr], writes=[pmm[3]])

                yield "SEG"
                S = lambda i: rs[:, i, :]
                bc = lambda ap, n: ap.unsqueeze(2).broadcast_to([128, Q, n])
                lgb = r36
                P.op(dve, lambda e: e.tensor_tensor(lgb[:, :, :], pmm[3][:, 0:Q * 36].rearrange("p (q n) -> p q n", q=Q), rbias[:, :, :], ALU.add),
                     reads=[pmm[3], rbias], writes=[lgb])
                gmask, gsh, gex = r4
                P.op(dve, lambda e: e.tensor_reduce(S(0), lgb[:, :, 0:4], AX.X, ALU.max), reads=[lgb], writes=[rs])
                P.op(dve, lambda e: e.tensor_tensor(gmask[:, :, :], lgb[:, :, 0:4], bc(S(0), 4), ALU.is_equal), reads=[lgb, rs], writes=[gmask])
                P.op(dve, lambda e: e.tensor_tensor(gsh[:, :, :], lgb[:, :, 0:4], bc(S(0), 4), ALU.subtract), reads=[lgb, rs], writes=[gsh])
                P.op(act, lambda e: e.activation(out=gex[:, :, :], in_=gsh[:, :, :], func=AF.Exp), reads=[gsh], writes=[gex])
                P.op(dve, lambda e: e.tensor_reduce(S(1), gex[:, :, :], AX.X, ALU.add), reads=[gex], writes=[rs])
                P.op(dve, lambda e: e.reciprocal(S(2), S(1)), reads=[rs], writes=[rs])
                le4 = lgb[:, :, 4:36].rearrange("p q (g j) -> p q g j", g=4)
                tmp32 = r32[0]
                P.op(dve, lambda e: e.tensor_tensor(tmp32[:, :, :].rearrange("p q (g j) -> p q g j", g=4), le4,
                                                    gmask[:, :, :].unsqueeze(3).broadcast_to([128, Q, 4, 8]), ALU.mult), reads=[lgb, gmask], writes=[tmp32])
                sel, top8, oh1, oh2 = r8
                P.op(dve, lambda e: e.tensor_reduce(sel[:, :, :], tmp32[:, :, :].rearrange("p q (g j) -> p q j g", g=4), AX.X, ALU.add), reads=[tmp32], writes=[sel])
                yield
                for q in range(Q):
                    P.op(dve, lambda e, q=q: e.max(top8[:, q, :], sel[:, q, :]), reads=[sel], writes=[top8])
                P.op(dve, lambda e: e.tensor_tensor(oh1[:, :, :], sel[:, :, :], top8[:, :, 0:1].broadcast_to([128, Q, 8]), ALU.is_equal), reads=[sel, top8], writes=[oh1])
                P.op(dve, lambda e: e.tensor_tensor(oh2[:, :, :], sel[:, :, :], top8[:, :, 1:2].broadcast_to([128, Q, 8]), ALU.is_equal), reads=[sel, top8], writes=[oh2])
                P.op(dve, lambda e: e.tensor_tensor(S(3), top8[:, :, 1], top8[:, :, 0], ALU.subtract), reads=[top8], writes=[rs])
                P.op(act, lambda e: e.activation(out=S(4), in_=S(3), func=AF.Exp), reads=[rs], writes=[rs])
                P.op(dve, lambda e: e.tensor_scalar(S(5), S(4), 1.0, None, ALU.add), reads=[rs], writes=[rs])
                P.op(dve, lambda e: e.reciprocal(S(6), S(5)), reads=[rs], writes=[rs])
                P.op(dve, lambda e: e.tensor_tensor(S(7), S(6), S(2), ALU.mult), reads=[rs], writes=[rs])
                P.op(dve, lambda e: e.tensor_tensor(S(8), S(2), S(7), ALU.subtract), reads=[rs], writes=[rs])
                E1, E2 = r32[1], r32[2]
                for Ek, oh in ((E1, oh1), (E2, oh2)):
                    P.op(dve, lambda e, Ek=Ek, oh=oh: e.tensor_tensor(Ek[:, :, :].rearrange("p q (g j) -> p q g j", g=4),
                                                                      gmask[:, :, :].unsqueeze(3).broadcast_to([128, Q, 4, 8]),
                                                                      oh[:, :, :].unsqueeze(2).broadcast_to([128, Q, 4, 8]), ALU.mult),
                         reads=[gmask, oh], writes=[Ek])
                P.op(dve, lambda e: e.tensor_tensor(mbf[:, :, :], E1[:, :, :], E2[:, :, :], ALU.add), reads=[E1, E2], writes=[mbf])

                def fnc(e):
                    ins = None
                    for q in range(Q):
                        ins = e.matmul(pmm[3][:, 160 + q * 32:160 + (q + 1) * 32], tri[:, :], mbf[:, q, :], start=True, stop=(q == 0))
                        for q2 in range(q):
                            ins = e.matmul(pmm[3][:, 160 + q * 32:160 + (q + 1) * 32], ones[:, :], mbf[:, q2, :], start=False, stop=(q2 == q - 1))
                    for q in range(Q):
                        ins = e.matmul(pmm[3][:, 288:320], ones[:, :], mbf[:, q, :], start=(q == 0), stop=(q == Q - 1))
                    return ins
                P.op(pe, fnc, reads=[tri, ones, mbf], writes=[pmm[3]])
                yield
                tot = r32[3]
                P.op(dve, lambda e: e.tensor_tensor(tot[:, :, :], pmm[3][:, 160:288].rearrange("p (q n) -> p q n", q=Q),
                                                    cntbc[:, :].unsqueeze(1).broadcast_to([128, Q, 32]), ALU.add), reads=[pmm[3], cntbc], writes=[tot])
                P.op(dve, lambda e: e.tensor_tensor(cntbc[:, :], cntbc[:, :], pmm[3][:, 288:320], ALU.add), reads=[pmm[3], cntbc], writes=[cntbc])
                tm = r32[4]
                for kk, Ek in ((0, E1), (1, E2)):
                    P.op(dve, lambda e, Ek=Ek: e.tensor_tensor(tm[:, :, :], Ek[:, :, :], tot[:, :, :], ALU.mult), reads=[Ek, tot], writes=[tm])
                    P.op(dve, lambda e, kk=kk: e.tensor_reduce(S(10 + kk), tm[:, :, :], AX.X, ALU.add), reads=[tm], writes=[rs])
                    P.op(dve, lambda e, Ek=Ek: e.tensor_tensor(tm[:, :, :], Ek[:, :, :], iota[:, :, :], ALU.mult), reads=[Ek, iota], writes=[tm])
                    P.op(dve, lambda e, kk=kk: e.tensor_reduce(S(12 + kk), tm[:, :, :], AX.X, ALU.add), reads=[tm], writes=[rs])
                    P.op(dve, lambda e, kk=kk: e.scalar_tensor_tensor(S(14 + kk), S(12 + kk), float(CAP), S(10 + kk), ALU.mult, ALU.add), reads=[rs], writes=[rs])
                    P.op(dve, lambda e, kk=kk: e.tensor_single_scalar(S(16 + kk), S(10 + kk), float(CAP), ALU.is_lt), reads=[rs], writes=[rs])
                    P.op(dve, lambda e, kk=kk: e.tensor_scalar(S(14 + kk), S(14 + kk), float(-TRASH), None, ALU.add), reads=[rs], writes=[rs])
                    P.op(dve, lambda e, kk=kk: e.tensor_tensor(S(14 + kk), S(14 + kk), S(16 + kk), ALU.mult), reads=[rs], writes=[rs])
                    P.op(dve, lambda e, kk=kk: e.tensor_scalar(S(14 + kk), S(14 + kk), float(TRASH), 0.0, ALU.add, ALU.max), reads=[rs], writes=[rs])
                    P.op(dve, lambda e, kk=kk: e.tensor_scalar(rinfo[:, ti, kk, :], S(14 + kk), float(TRASH), None, ALU.min), reads=[rs], writes=[rinfo])
                    P.op(dve, lambda e, kk=kk: e.tensor_tensor(rinfo[:, ti, 2 + kk, :], S(7 + kk), S(16 + kk), ALU.mult), reads=[rs], writes=[rinfo])
                    yield
                P.op(dve, lambda e: e.tensor_copy(sloti[:, ti, :, :], rinfo[:, ti, 0:2, :]), reads=[rinfo], writes=[sloti])
                for q in range(Q):
                    for kk in range(2):
                        P.dma(pool, lambda e, q=q, kk=kk: e.indirect_dma_start(
                            out=hs_d[:, :], out_offset=IOA(ap=sloti[:, ti, kk, q:q + 1], axis=0), in_=hfn[q][:, :], in_offset=None),
                            hfn[q], reads=[hfn[q], sloti])

            def collect(genfunc, ti):
                items = []
                orig_op, orig_dma = P.op, P.dma
                P.op = lambda eng, fn, reads=(), writes=(): items.append((orig_op, (eng, fn), dict(reads=reads, writes=writes), eng))
                P.dma = lambda q, fn, sem_buf, reads=(), writes=(): items.append((orig_dma, (q, fn, sem_buf), dict(reads=reads, writes=writes), q))
                try:
                    for tok in genfunc(ti):
                        items.append(tok)
                finally:
                    del P.op, P.dma
                return items

            def stages1(items):
                out, cur, prev = [], [], None
                for it in items:
                    if it is None or (HOP and prev is not None and it[3] is not prev):
                        out.append(cur)
                        cur = []
                    if it is None:
                        prev = None
                    else:
                        cur.append(it)
                        prev = it[3]
                out.append(cur)
                res = []
                for st in out:
                    if not st:
                        continue
                    res.append(st)
                    if LOADLAG and all(it[3] is sp and it[2]["writes"] for it in st):
                        res.extend([[] for _ in range(LOADLAG)])
                return res

            def zip_locked(chains):
                k = len(chains)
                if k == 1:
                    return chains[0]
                spans, wsets = [], []
                for L in chains:
                    fw, lr, ws = {}, {}, set()
                    for si, st in enumerate(L):
                        for (f, args, kw, eng) in st:
                            for bb in kw["writes"]:
                                fw.setdefault(id(bb), si)
                                ws.add(id(bb))
                            for bb in kw["reads"]:
                                lr[id(bb)] = si
                    spans.append({x: (fw[x], lr[x]) for x in fw if x in lr and lr[x] > fw[x]})
                    wsets.append(ws)
                out, pos, owner = [], [0] * k, {}
                while any(pos[c] < len(chains[c]) for c in range(k)):
                    progressed = False
                    for c in range(k):
                        if pos[c] >= len(chains[c]):
                            continue
                        st = chains[c][pos[c]]
                        W = set(id(bb) for (f, args, kw, eng) in st for bb in kw["writes"])
                        if any(owner.get(x) not in (None, c) for x in W):
                            continue
                        out.append(st)
                        progressed = True
                        for x in W:
                            if x in spans[c] and any(x in wsets[j] for j in range(k) if j != c):
                                owner[x] = c
                        for x in list(owner):
                            if owner[x] == c and pos[c] >= spans[c][x][1]:
                                owner[x] = None
                        pos[c] += 1
                    assert progressed, "chain lock deadlock"
                return out

            def stages(items):
                segs = [[[]]]
                for it in items:
                    if it == "SEG":
                        segs.append([[]])
                    elif it == "CHAIN":
                        segs[-1].append([])
                    else:
                        segs[-1][-1].append(it)
                out = []
                for seg in segs:
                    out += zip_locked([stages1(ch) for ch in seg if ch])  if any(seg) else []
                return out

            def spread(genfunc, ti, n, off=0):
                sts = stages(collect(genfunc, ti))
                assert len(sts) <= n - off, (len(sts), n, off)
                k = 0
                for t in range(n):
                    while t >= off and k < len(sts) and k * (n - off) < (t - off + 1) * len(sts):
                        for f, args, kw, eng in sts[k]:
                            f(*args, **kw)
                        k += 1
                    yield

            counts = [len(stages(collect(g, 1))) for g in (gen_F, gen_Mc, gen_Ml, gen_E)]
            NSTG = max(counts) + 2

            def both(g1, g2):
                for _ in g1:
                    next(g2)
                    yield

            def tile_gen(ti):
                yield from spread(gen_F, ti, NSTG)
                yield from both(spread(gen_Mc, ti, NSTG, MC_OFF), spread(gen_Ml, ti, NSTG))
                yield from spread(gen_E, ti, NSTG)

            run_pipelined((tile_gen(ti) for ti in range(NT)), depth=3, skew=NSTG)

            if debug:
                P.dma(sp, lambda e: e.dma_start(out=ri_d, in_=rinfo[:, :, :, :].rearrange("p a b c -> p (a b c)")), rinfo, reads=[rinfo])
            P.barrier()
            P.flush(block)

        with ExitStack() as sb:
            B = lambda shape, dt=F32, name=None, dma=False: P.buf(sb, shape, dt, name, dma)
            gffnbc = B([128, 8], F32, "gffnbc", dma=True)
            P.dma(sp, lambda e: e.dma_start(out=gffnbc[:, :], in_=gbc_d[1]), gffnbc, writes=[gffnbc])
            zt = B([128, D], F32, "zt", dma=True)
            P.op(dve, lambda e: e.memset(zt[:, :], 0.0), writes=[zt])
            P.dma(sp, lambda e: e.dma_start(out=y_d[NSLOT:NSLOT + 128, :], in_=zt[:, :]), zt, reads=[zt])
            w1 = [B([128, 8, 512], BF16, "w1") for _ in range(2)]
            w3 = [B([128, 8, 512], BF16, "w3") for _ in range(2)]
            w2 = [B([128, 4, D], BF16, "w2") for _ in range(2)]
            w3s = B([128, 8, 512], F32, "w3s", dma=True)
            w1s = B([128, 8, 512], F32, "w1s", dma=True)
            w2s = B([128, 4, D], F32, "w2s", dma=True)
            hst = [B([128, NSUB, D], BF16, "hst", dma=True) for _ in range(2)]
            hfTe = [B([128, 8, CAP], BF16, "hfTe") for _ in range(2)]
            actT = B([128, 4, CAP], BF16, "actT")
            tb = [B([128, 512], F32, "tb") for _ in range(2)]
            tc = [B([128, 512], F32, "tc") for _ in range(2)]
            yt = [B([128, D], F32, "yt", dma=True) for _ in range(3)]
            ntiles = [(0, 512)] if CAP == 512 else ([(n0, min(512, CAP - n0)) for n0 in range(0, CAP, 512)])
            yi = 0
            ci = 0

            def load_expert(ex):
                sl = ex % 2
                P.dma(sp, lambda e: e.dma_start(out=hst[sl][:, :, :], in_=hs_d[ex * CAP:(ex + 1) * CAP, :].rearrange("(s p) n -> p s n", p=128)),
                      hst[sl], writes=[hst[sl]])
                P.dma(sp, lambda e: e.dma_start(out=w1s[:, :, :], in_=w1_d[ex].rearrange("(k p) n -> p k n", p=128)), w1s, writes=[w1s])
                P.dma(sp, lambda e: e.dma_start(out=w3s[:, :, :], in_=w3_d[ex].rearrange("(k p) n -> p k n", p=128)), w3s, writes=[w3s])
                P.dma(sp, lambda e: e.dma_start(out=w2s[:, :, :], in_=w2_d[ex].rearrange("(k p) n -> p k n", p=128)), w2s, writes=[w2s])

            def cast_expert(ex):
                sl = ex % 2
                for k in range(8):
                    P.op(pool, lambda e, k=k: e.tensor_tensor(w1[sl][:, k, :], w1s[:, k, :], gffnbc[:, k:k + 1].broadcast_to([128, 512]), ALU.mult),
                         reads=[w1s, gffnbc], writes=[w1[sl]])
                for k in range(8):
                    P.op(act, lambda e, k=k: e.activation(out=w3[sl][:, k, :], in_=w3s[:, k, :], func=AF.Copy, scale=gffnbc[:, k:k + 1]), reads=[w3s, gffnbc], writes=[w3[sl]])
                for k in range(4):
                    P.op(act, lambda e, k=k: e.activation(out=w2[sl][:, k, :], in_=w2s[:, k, :], func=AF.Copy), reads=[w2s], writes=[w2[sl]])

            pool6 = pmm + [ps1, ps2]
            p6 = [0]

            def next6():
                bb = pool6[p6[0] % 6]
                p6[0] += 1
                return bb

            def do_T(ex):
                sl = ex % 2
                hT_e = hfTe[sl]
                for sbt in range(NSUB):
                    pt_ = ptr if sbt % 2 == 0 else ptr2

                    def fn(e, sbt=sbt, pt_=pt_, sl=sl):
                        ins = None
                        for c in range(8):
                            ins = e.transpose(pt_[:, c * 128:(c + 1) * 128], hst[sl][:, sbt, c * 128:(c + 1) * 128], ident[:, :])
                        return ins
                    P.op(pe, fn, reads=[hst[sl], ident], writes=[pt_])
                    P.op(dve, lambda e, sbt=sbt, pt_=pt_, hT_e=hT_e: e.tensor_copy(hT_e[:, :, sbt * 128:(sbt + 1) * 128], pt_[:, :].rearrange("p (c j) -> p c j", c=8)),
                         reads=[pt_], writes=[hT_e])

            def do_H(ex):
                sl = ex % 2
                hT_e = hfTe[sl]
                for (n0, nn) in ntiles:
                    for m in range(4):
                        p1, p3 = next6(), next6()
                        for (pb, wt) in ((p1, w1[sl]), (p3, w3[sl])):
                            def fn(e, pb=pb, wt=wt, m=m, n0=n0, nn=nn, hT_e=hT_e):
                                ins = None
                                for k in range(8):
                                    ins = e.matmul(pb[:, 0:nn], wt[:, k, m * 128:(m + 1) * 128], hT_e[:, k, n0:n0 + nn], start=(k == 0), stop=(k == 7))
                                return ins
                            P.op(pe, fn, reads=[wt, hT_e], writes=[pb])
                        tb_, tc_ = tb[ci_[0] % 2], tc[ci_[0] % 2]
                        ci_[0] += 1
                        P.op(act, lambda e, p1=p1, tb_=tb_, nn=nn: e.activation(out=tb_[:, 0:nn], in_=p1[:, 0:nn], func=AF.Tanh, scale=0.5), reads=[p1], writes=[tb_])
                        P.op(dve, lambda e, p1=p1, tb_=tb_, tc_=tc_, nn=nn: e.scalar_tensor_tensor(tc_[:, 0:nn], tb_[:, 0:nn], 1.0, p1[:, 0:nn], ALU.add, ALU.mult),
                             reads=[tb_, p1], writes=[tc_])
                        P.op(dve, lambda e, p3=p3, tc_=tc_, nn=nn, m=m, n0=n0: e.scalar_tensor_tensor(actT[:, m, n0:n0 + nn], tc_[:, 0:nn], 0.5, p3[:, 0:nn], ALU.mult, ALU.mult),
                             reads=[tc_, p3], writes=[actT])

            def do_Y(ex):
                sl = ex % 2
                for sbt in range(NSUB):
                    yb = yt[yi_[0] % 3]
                    yi_[0] += 1
                    for h in range(2):
                        pb = next6()

                        def fn(e, pb=pb, h=h, sbt=sbt, sl=sl):
                            ins = None
                            for m in range(4):
                                ins = e.matmul(pb[:, :], actT[:, m, sbt * 128:(sbt + 1) * 128], w2[sl][:, m, h * 512:(h + 1) * 512], start=(m == 0), stop=(m == 3))
                            return ins
                        P.op(pe, fn, reads=[actT, w2[sl]], writes=[pb])
                        P.op(act, lambda e, pb=pb, h=h, yb=yb: e.activation(out=yb[:, h * 512:(h + 1) * 512], in_=pb[:, :], func=AF.Copy), reads=[pb], writes=[yb])
                    r0 = ex * CAP + sbt * 128
                    P.dma(sp, lambda e, yb=yb, r0=r0: e.dma_start(out=y_d[r0:r0 + 128, :], in_=yb[:, :]), yb, reads=[yb])

            ci_, yi_ = [0], [0]
            load_expert(0)
            cast_expert(0)
            do_T(0)
            for ex in range(32):
                if ex + 1 < 32:
                    load_expert(ex + 1)
                do_H(ex)
                if ex + 1 < 32:
                    cast_expert(ex + 1)
                    do_T(ex + 1)
                do_Y(ex)
            P.barrier()
            P.flush(block)

        with ExitStack() as sc:
            B = lambda shape, dt=F32, name=None, dma=False: P.buf(sc, shape, dt, name, dma)
            gplebc = B([128, 8], F32, "gplebc", dma=True)
            gpp = B([128, D], F32, "gpp", dma=True)
            gfin = B([128, D], F32, "gfin", dma=True)
            wple = B([128, 2, D], BF16, "wple", dma=True)
            wpg = B([128, 8, D], BF16, "wpg", dma=True)
            P.dma(sp, lambda e: e.dma_start(out=gplebc[:, :], in_=gbc_d[2]), gplebc, writes=[gplebc])
            P.dma(sp, lambda e: e.dma_start(out=gpp[:, :], in_=rowbc_d[0]), gpp, writes=[gpp])
            P.dma(sp, lambda e: e.dma_start(out=gfin[:, :], in_=rowbc_d[1]), gfin, writes=[gfin])
            for k in range(2):
                P.dma(pool, lambda e, k=k: e.dma_start(out=wple[:, k, :], in_=wple_d[k * 128:(k + 1) * 128, :]), wple, writes=[wple])
            for k in range(8):
                P.dma(pool, lambda e, k=k: e.dma_start(out=wpg[:, k, :], in_=wpg_d[k * 128:(k + 1) * 128, :]), wpg, writes=[wpg])
            for k in range(8):
                P.op(dve, lambda e, k=k: e.tensor_scalar(wpg[:, k, :], wpg[:, k, :], gplebc[:, k:k + 1], None, ALU.mult), reads=[wpg, gplebc], writes=[wpg])
            x1t_ = [B([128, D], F32, "x1c", dma=True) for _ in range(NP_C)]
            pt_b = [B([128, 256], F32, "pc", dma=True) for _ in range(NP_C)]
            y1_ = [B([128, D], F32, "y1c", dma=True) for _ in range(NP_C)]
            y2_ = [B([128, D], F32, "y2c", dma=True) for _ in range(NP_C)]
            NP = NP_C
            ssC_ = [B([128, 16], F32, "ssC") for _ in range(NP)]
            xn3 = [B([128, D], BF16, "xn3") for _ in range(NP)]
            junk, te, ob, thg = xn3, y2_, x1t_, y1_
            x3T = [B([128, 8, 128], BF16, "x3T") for _ in range(NP)]
            pbf = [B([128, 256], BF16, "pbf") for _ in range(NP)]
            pT = [B([128, 2, 128], BF16, "pT") for _ in range(NP)]

            def rsq(ssb, i_v, i_l, i_o):
                P.op(act, lambda e: e.activation(out=ssb[:, i_l:i_l + 1], in_=ssb[:, i_v:i_v + 1], func=AF.Ln), reads=[ssb], writes=[ssb])
                P.op(act, lambda e: e.activation(out=ssb[:, i_o:i_o + 1], in_=ssb[:, i_l:i_l + 1], func=AF.Exp, scale=-0.5), reads=[ssb], writes=[ssb])

            def subtile_gen(st):
                ti, q = divmod(st, Q)
                r0 = st * 128
                i3 = st % NP
                xx, pp, y1, y2 = x1t_[i3], pt_b[i3], y1_[i3], y2_[i3]
                jk, ssC = junk[i3], ssC_[i3]
                xn_, x3_, pb_, pT_, tg_, te_, ob_ = xn3[i3], x3T[i3], pbf[i3], pT[i3], thg[i3], te[i3], ob[i3]
                P.dma(sp, lambda e: e.dma_start(out=xx[:, :], in_=x1_d[r0:r0 + 128, :]), xx, writes=[xx])
                P.dma(sp, lambda e: e.dma_start(out=pp[:, :], in_=p_d[r0:r0 + 128, :]), pp, writes=[pp])
                for (yy, kk) in ((y1, 0), (y2, 1)):
                    P.dma(pool, lambda e, yy=yy, kk=kk: e.indirect_dma_start(
                        out=yy[:, :], out_offset=None, in_=y_d[:, :], in_offset=IOA(ap=sloti[:, ti, kk, q:q + 1], axis=0)),
                        yy, reads=[sloti], writes=[yy])
                yield
                for (yy, kk) in ((y1, 0), (y2, 1)):
                    P.op(dve, lambda e, yy=yy, kk=kk: e.scalar_tensor_tensor(xx[:, :], yy[:, :], rinfo[:, ti, 2 + kk, q:q + 1], xx[:, :], ALU.mult, ALU.add),
                         reads=[yy, rinfo, xx], writes=[xx])
                yield
                P.op(act, lambda e: e.activation(out=jk[:, :], in_=xx[:, :], func=AF.Square, accum_out=ssC[:, 0:1]), reads=[xx], writes=[jk, ssC])
                P.op(act, lambda e: e.activation(out=pb_[:, :], in_=pp[:, :], func=AF.Copy), reads=[pp], writes=[pb_])
                yield
                P.op(dve, lambda e: e.tensor_scalar(ssC[:, 1:2], ssC[:, 0:1], 1.0 / D, EPS, ALU.mult, ALU.add), reads=[ssC], writes=[ssC])
                yield
                rsq(ssC, 1, 3, 2)
                P.op(act, lambda e: e.activation(out=xn_[:, :], in_=xx[:, :], func=AF.Copy, scale=ssC[:, 2:3]), reads=[xx, ssC], writes=[xn_])
                yield
                transposes(xn_, 8, ptr)
                P.op(dve, lambda e: e.tensor_copy(x3_[:, :, :], ptr[:, :].rearrange("p (c j) -> p c j", c=8)), reads=[ptr], writes=[x3_])
                transposes(pb_, 2, ptr2)
                P.op(act, lambda e: e.activation(out=pT_[:, :, :], in_=ptr2[:, 0:256].rearrange("p (c j) -> p c j", c=2), func=AF.Copy), reads=[ptr2], writes=[pT_])
                yield
                pes = []
                for h in range(2):
                    pg_ = next_pmm()

                    def fn(e, pg_=pg_, h=h):
                        ins = None
                        for k in range(8):
                            ins = e.matmul(pg_[:, :], x3_[:, k, :], wpg[:, k, h * 512:(h + 1) * 512], start=(k == 0), stop=(k == 7))
                        return ins
                    P.op(pe, fn, reads=[x3_, wpg], writes=[pg_])
                    P.op(act, lambda e, pg_=pg_, h=h: e.activation(out=tg_[:, h * 512:(h + 1) * 512], in_=pg_[:, :], func=AF.Tanh, scale=0.5), reads=[pg_], writes=[tg_])
                    pe_ = next_pmm()

                    def fn2(e, pe_=pe_, h=h):
                        ins = None
                        for k in range(2):
                            ins = e.matmul(pe_[:, :], pT_[:, k, :], wple[:, k, h * 512:(h + 1) * 512], start=(k == 0), stop=(k == 1))
                        return ins
                    P.op(pe, fn2, reads=[pT_, wple], writes=[pe_])
                    P.op(act, lambda e, pe_=pe_, h=h: e.activation(out=jk[:, 0:512], in_=pe_[:, :], func=AF.Square, accum_out=ssC[:, 4 + h:5 + h]), reads=[pe_], writes=[jk, ssC])
                    P.op(act, lambda e, pe_=pe_, h=h: e.activation(out=te_[:, h * 512:(h + 1) * 512], in_=pe_[:, :], func=AF.Copy), reads=[pe_], writes=[te_])
                    pes.append(pe_)
                yield
                P.op(dve, lambda e: e.tensor_tensor(ssC[:, 6:7], ssC[:, 4:5], ssC[:, 5:6], ALU.add), reads=[ssC], writes=[ssC])
                P.op(dve, lambda e: e.tensor_scalar(ssC[:, 7:8], ssC[:, 6:7], 4.0 / D, 4.0 * EPS, ALU.mult, ALU.add), reads=[ssC], writes=[ssC])
                yield
                rsq(ssC, 7, 9, 8)
                yield
                P.op(dve, lambda e: e.scalar_tensor_tensor(te_[:, :], te_[:, :], ssC[:, 8:9], gpp[:, :], ALU.mult, ALU.mult), reads=[te_, ssC, gpp], writes=[te_])
                P.op(dve, lambda e: e.scalar_tensor_tensor(te_[:, :], tg_[:, :], 1.0, te_[:, :], ALU.add, ALU.mult), reads=[tg_, te_], writes=[te_])
                yield
                P.op(pool, lambda e: e.tensor_tensor(xx[:, :], xx[:, :], te_[:, :], ALU.add), reads=[te_, xx], writes=[xx])
                yield
                P.op(act, lambda e: e.activation(out=jk[:, :], in_=xx[:, :], func=AF.Square, accum_out=ssC[:, 10:11]), reads=[xx], writes=[jk, ssC])
                yield
                P.op(dve, lambda e: e.tensor_scalar(ssC[:, 11:12], ssC[:, 10:11], 1.0 / D, EPS, ALU.mult, ALU.add), reads=[ssC], writes=[ssC])
                yield
                rsq(ssC, 11, 13, 12)
                yield
                P.op(dve, lambda e: e.scalar_tensor_tensor(ob_[:, :], xx[:, :], ssC[:, 12:13], gfin[:, :], ALU.mult, ALU.mult), reads=[xx, ssC, gfin], writes=[ob_])
                P.dma(sp, lambda e: e.dma_start(out=out_d[r0:r0 + 128, :], in_=ob_[:, :]), ob_, reads=[ob_])

            run_pipelined((subtile_gen(st) for st in range(T // 128)), depth=NP, skew=2)
            P.barrier()
            P.flush(block)
    return nc


def _chan(v):
    return np.ascontiguousarray(np.asarray(v, np.float32).reshape(4, 128).T)


def _gbc(g):
    return np.ascontiguousarray(np.asarray(g, np.float32).reshape(8, 128).T)


def prep_shared(inp):
    f = lambda a: np.ascontiguousarray(np.asarray(a, np.float32))
    cpar = np.zeros((128, NCP), np.float32)
    cw = f(inp["conv_dw_w"][0])
    for c in range(4):
        cpar[:, CW + c * 31:CW + (c + 1) * 31] = cw[:, c * 128:(c + 1) * 128].T
    cpar[:, CB:CB + 4] = _chan(inp["conv_dw_b"][0])
    cpar[:, LG:LG + 4] = _chan(inp["conv_ln_g"][0])
    cpar[:, LB:LB + 4] = _chan(inp["conv_ln_b"][0])
    lw = f(inp["lru_conv_w"][0])
    for c in range(4):
        cpar[:, LW + c * 4:LW + (c + 1) * 4] = lw[:, c * 128:(c + 1) * 128].T
    cpar[:, LBB:LBB + 4] = _chan(inp["lru_conv_b"][0])
    cpar[:, BR:BR + 4] = _chan(inp["lru_b_r"][0])
    cpar[:, BI:BI + 4] = _chan(inp["lru_b_i"][0])
    cpar[:, LAM:LAM + 4] = _chan(inp["lru_lambda"][0])
    wr_, wi_ = f(inp["lru_w_r"][0]), f(inp["lru_w_i"][0])
    gbd = np.zeros((4, 128, 256), np.float32)
    for c in range(4):
        for hh in range(2):
            gbd[c, hh * 64:(hh + 1) * 64, hh * 64:(hh + 1) * 64] = wr_[2 * c + hh]
            gbd[c, hh * 64:(hh + 1) * 64, 128 + hh * 64:128 + (hh + 1) * 64] = wi_[2 * c + hh]
    rb = np.concatenate([f(inp["b_group"][0]), f(inp["b_expert"][0])])
    shared = {
        "w_in": f(inp["w_in"][0]), "w_out": f(inp["w_out"][0]),
        "w_route": np.ascontiguousarray(np.concatenate([f(inp["w_group"][0]), f(inp["w_expert"][0])], axis=1)),
        "gate_bd": gbd, "cpar": cpar,
        "gbc": np.stack([_gbc(inp["g_mix"][0]), _gbc(inp["g_ffn"][0]), _gbc(inp["g_ple"][0])]),
        "rowbc": np.stack([np.ascontiguousarray(np.broadcast_to(f(inp["g_ple_proj"][0]), (128, D))),
                           np.ascontiguousarray(np.broadcast_to(f(inp["g_final"]), (128, D)))]),
        "rbias": np.ascontiguousarray(np.broadcast_to(np.tile(rb, Q), (128, Q * 36))),
        "iota_e": np.ascontiguousarray(np.broadcast_to(np.tile(np.arange(32, dtype=np.float32), Q), (128, Q * 32))),
        "ident": np.eye(128, dtype=np.float32),
        "tri": np.ascontiguousarray(np.triu(np.ones((128, 128), np.float32), 1)),
        "w1": f(inp["w1"][0]), "w3": f(inp["w3"][0]), "w2": f(inp["w2"][0]),
        "w_ple": f(inp["w_ple"][0]), "w_ple_gate": f(inp["w_ple_gate"][0]),
    }
    return shared


def kernel(**inputs):
    NSEQ, CAP = 4, 1024
    x = np.asarray(inputs["x"], np.float32)
    p = np.asarray(inputs["p"], np.float32)[0]
    shared = prep_shared(inputs)
    nc = build(NSEQ, CAP)
    in_maps = []
    for i in range(N_CORES):
        m = dict(shared)
        m["x"] = np.ascontiguousarray(x[i * NSEQ:(i + 1) * NSEQ].reshape(NSEQ * SEQ, D))
        m["p"] = np.ascontiguousarray(p[i * NSEQ:(i + 1) * NSEQ].reshape(NSEQ * SEQ, 256))
        in_maps.append(m)
    res = run_bass_kernel_spmd(nc, in_maps, core_ids=list(range(N_CORES)))
    out = np.concatenate([np.asarray(r["out"], np.float32).reshape(NSEQ, SEQ, D) for r in res.results], axis=0)
    return out
```

```python
import numpy as np
from contextlib import ExitStack
import concourse.bass as bass
import concourse.mybir as mybir
from concourse.bass_utils import run_bass_kernel_spmd

F32 = mybir.dt.float32
BF16 = mybir.dt.bfloat16
I32 = mybir.dt.int32
ALU = mybir.AluOpType
AF = mybir.ActivationFunctionType
AX = mybir.AxisListType

N_CORES = 8
HOP = True
NP_C = 8
LOADLAG = 3
MC_OFF = 0
SEQ = 2048
D = 1024
EPS = 1e-6
Q = 4

CW = 0
CB = CW + 124
LG = CB + 4
LB = LG + 4
LW = LB + 4
LBB = LW + 16
BR = LBB + 4
BI = BR + 4
LAM = BI + 4
NCP = LAM + 4
D_CWH = 0
D_GH = 124
D_BH = 128
D_BRH = 132
D_BIH = 136
D_N4 = 140
D_N8 = 144
NDP = 148


class Src:
    def __init__(self, sem, name, is_dma):
        self.sem, self.name, self.is_dma, self.total = sem, name, is_dma, 0


class Eng(Src):
    def __init__(self, sem, name, blockname, same_wait=True):
        super().__init__(sem, name, False)
        self.blockname, self.ops, self.seen, self.same_wait = blockname, [], {}, same_wait


class Buf:
    def __init__(self, t, dsem=None):
        self.t, self.w, self.r, self.dsem = t, None, {}, dsem

    def __getitem__(self, k):
        return self.t[k]


class Prog:
    def __init__(self, nc, stack):
        self.nc, self.stack = nc, stack
        self.srcs = []
        mk = lambda n, b, sw=True: self._reg(Eng(self._sem("e_" + n), n, b, sw))
        self.pe = mk("pe", "tensor", False)
        self.act = mk("act", "scalar")
        self.dve = mk("dve", "vector")
        self.pool = mk("pool", "gpsimd")
        self.sp = mk("sp", "sync")
        self.engs = [self.pe, self.act, self.dve, self.pool, self.sp]
        self.nbuf = 0

    def _sem(self, name):
        return self.stack.enter_context(self.nc.semaphore(name))

    def _reg(self, s):
        self.srcs.append(s)
        return s

    def buf(self, stack, shape, dt, name=None, dma=False, psum=False):
        self.nbuf += 1
        name = "%s_%d" % (name or "b", self.nbuf)
        if psum:
            t = stack.enter_context(self.nc.psum_tensor(name, shape, dt))
        else:
            t = stack.enter_context(self.nc.sbuf_tensor(name, shape, dt))
        ds = self._reg(Src(self._sem("d_" + name), name, True)) if dma else None
        return Buf(t, ds)

    def _deps(self, eng, reads, writes):
        need = {}

        def add(src, val):
            if src.is_dma:
                val = src.total
            if need.get(src, 0) < val:
                need[src] = val

        for b in reads:
            if b.w is not None:
                add(*b.w)
        for b in writes:
            if b.w is not None:
                add(*b.w)
            for s, v in b.r.items():
                add(s, v)
        waits = []
        for src, val in need.items():
            if src is eng and not eng.same_wait:
                continue
            if eng.seen.get(src, 0) >= val:
                continue
            eng.seen[src] = val
            waits.append((src.sem, val))
        return waits

    def _mark(self, src, val, reads, writes):
        for b in reads:
            b.r[src] = val
        for b in writes:
            b.w = (src, val)
            b.r = {}

    def op(self, eng, fn, reads=(), writes=()):
        waits = self._deps(eng, reads, writes)
        eng.total += 1
        sem = eng.sem

        def emit(e):
            for s, v in waits:
                e.wait_ge(s, v)
            fn(e).then_inc(sem, 1)

        eng.ops.append(emit)
        self._mark(eng, eng.total, reads, writes)

    def dma(self, q, fn, sem_buf, reads=(), writes=()):
        src = sem_buf.dsem
        waits = self._deps(q, reads, writes)
        src.total += 16
        sem = src.sem

        def emit(e):
            for s, v in waits:
                e.wait_ge(s, v)
            fn(e).then_inc(sem, 16)

        q.ops.append(emit)
        self._mark(src, src.total, reads, writes)

    def barrier(self):
        for E in self.engs:
            waits = []
            for S in self.srcs:
                if S is E or S.total == 0:
                    continue
                if E.seen.get(S, 0) >= S.total:
                    continue
                E.seen[S] = S.total
                waits.append((S.sem, S.total))

            def emit(e, waits=waits):
                for s, v in waits:
                    e.wait_ge(s, v)

            E.ops.append(emit)

    def flush(self, block):
        for E in self.engs:
            if not E.ops:
                continue
            ops = E.ops
            E.ops = []

            def body(e, ops=ops):
                for f in ops:
                    f(e)

            getattr(block, E.blockname)(body)


def run_pipelined(gens, depth, skew):
    it = iter(gens)
    active, pending, tick = [], True, 0
    while pending or active:
        for g in list(active):
            try:
                next(g)
            except StopIteration:
                active.remove(g)
        if pending and tick % skew == 0 and len(active) < depth:
            try:
                g = next(it)
                active.append(g)
                next(g)
            except StopIteration:
                pending = False
        tick += 1


def build(NSEQ=4, CAP=640, debug=False):
    T = NSEQ * SEQ
    NT = T // 512
    NSUB = CAP // 128
    NSLOT = 32 * CAP
    TRASH = NSLOT
    nc = bass.Bass("TRN2", target_bir_lowering=False)

    def dr(name, shape, dt=F32, kind="ExternalInput"):
        return nc.dram_tensor(name, shape, dt, kind=kind).ap()

    x_d = dr("x", [T, D])
    p_d = dr("p", [T, 256])
    win_d = dr("w_in", [D, 2048])
    wout_d = dr("w_out", [D, D])
    wr_d = dr("w_route", [D, 36])
    gbd_d = dr("gate_bd", [4, 128, 256])
    cpar_d = dr("cpar", [128, NCP])
    gbc_d = dr("gbc", [3, 128, 8])
    rowbc_d = dr("rowbc", [2, 128, D])
    rb_d = dr("rbias", [128, Q * 36])
    iota_d = dr("iota_e", [128, Q * 32])
    ident_d = dr("ident", [128, 128])
    tri_d = dr("tri", [128, 128])
    w1_d = dr("w1", [32, D, 512])
    w3_d = dr("w3", [32, D, 512])
    w2_d = dr("w2", [32, 512, D])
    wple_d = dr("w_ple", [256, D])
    wpg_d = dr("w_ple_gate", [D, D])
    out_d = dr("out", [T, D], kind="ExternalOutput")
    sk = "ExternalOutput" if debug else "Internal"
    x1_d = dr("x1s", [T, D], kind=sk)
    hs_d = dr("hss", [NSLOT + 128, D], BF16, kind=sk)
    y_d = dr("yss", [NSLOT + 128, D], kind=sk)
    if debug:
        ri_d = dr("rinfo_o", [128, NT * 4 * Q], kind="ExternalOutput")

    IOA = bass.IndirectOffsetOnAxis

    with ExitStack() as top:
        P = Prog(nc, top)
        pe, act, dve, pool, sp = P.pe, P.act, P.dve, P.pool, P.sp
        block = top.enter_context(nc.Block())

        pmm = [P.buf(top, [128, 512], F32, "pmm", psum=True) for _ in range(4)]
        ptr = P.buf(top, [128, 1024], BF16, "ptr", psum=True)
        ptr2 = P.buf(top, [128, 1024], BF16, "ptr2", psum=True)
        ps1 = P.buf(top, [128, 512], F32, "ps1", psum=True)
        ps2 = P.buf(top, [128, 512], F32, "ps2", psum=True)
        pmm_i = [0]

        def next_pmm():
            b = pmm[pmm_i[0] % 4]
            pmm_i[0] += 1
            return b

        rinfo = P.buf(top, [128, NT, 4, Q], F32, "rinfo")
        sloti = P.buf(top, [128, NT, 2, Q], I32, "sloti")
        if debug:
            rinfo.dsem = P._reg(Src(P._sem("d_rinfo"), "rinfo", True))
        ident = P.buf(top, [128, 128], BF16, "ident", dma=True)
        P.dma(pool, lambda e: e.dma_start(out=ident[:, :], in_=ident_d), ident, writes=[ident])

        def transposes(src, n, dst_ps):
            def fn(e):
                ins = None
                for c in range(n):
                    ins = e.transpose(dst_ps[:, c * 128:(c + 1) * 128], src[:, c * 128:(c + 1) * 128], ident[:, :])
                return ins
            P.op(pe, fn, reads=[src, ident], writes=[dst_ps])

        def rstd_from_ss(ss, out, n):
            P.op(dve, lambda e: e.tensor_scalar(out, ss, 1.0 / n, EPS, ALU.mult, ALU.add), reads=[], writes=[])

        with ExitStack() as sa:
            B = lambda shape, dt=F32, name=None, dma=False: P.buf(sa, shape, dt, name, dma)
            win = B([128, 8, 2048], BF16, "win", dma=True)
            wout = B([128, 8, D], BF16, "wout", dma=True)
            wr = B([128, 8, 36], BF16, "wr", dma=True)
            gbd = B([128, 4, 256], BF16, "gbd", dma=True)
            tri = B([128, 128], BF16, "tri", dma=True)
            cpar = B([128, NCP], F32, "cpar", dma=True)
            dpar = B([128, NDP], F32, "dpar")
            gmixbc = B([128, 8], F32, "gmixbc", dma=True)
            gffnbc = B([128, 8], F32, "gffnbc", dma=True)
            rbias = B([128, Q, 36], F32, "rbias", dma=True)
            iota = B([128, Q, 32], F32, "iota", dma=True)
            ones = B([128, 128], BF16, "ones")
            identF = B([128, 128], F32, "identF", dma=True)
            P.dma(sp, lambda e: e.dma_start(out=identF[:, :], in_=ident_d), identF, writes=[identF])
            cntbc = B([128, 32], F32, "cntbc")

            P.dma(sp, lambda e: e.dma_start(out=cpar[:, :], in_=cpar_d), cpar, writes=[cpar])
            P.dma(sp, lambda e: e.dma_start(out=gmixbc[:, :], in_=gbc_d[0]), gmixbc, writes=[gmixbc])
            P.dma(sp, lambda e: e.dma_start(out=gffnbc[:, :], in_=gbc_d[1]), gffnbc, writes=[gffnbc])
            P.dma(sp, lambda e: e.dma_start(out=rbias[:, :, :], in_=rb_d.rearrange("p (q n) -> p q n", q=Q)), rbias, writes=[rbias])
            P.dma(sp, lambda e: e.dma_start(out=iota[:, :, :], in_=iota_d.rearrange("p (q n) -> p q n", q=Q)), iota, writes=[iota])
            P.dma(pool, lambda e: e.dma_start(out=tri[:, :], in_=tri_d), tri, writes=[tri])
            for k in range(8):
                P.dma(pool, lambda e, k=k: e.dma_start(out=win[:, k, :], in_=win_d[k * 128:(k + 1) * 128, :]), win, writes=[win])
            P.dma(pool, lambda e: e.dma_start(out=gbd[:, :, :], in_=gbd_d.rearrange("c p n -> p c n")), gbd, writes=[gbd])
            for k in range(8):
                P.dma(pool, lambda e, k=k: e.dma_start(out=wout[:, k, :], in_=wout_d[k * 128:(k + 1) * 128, :]), wout, writes=[wout])
            P.dma(pool, lambda e: e.dma_start(out=wr[:, :, :], in_=wr_d.rearrange("(k p) n -> p k n", p=128)), wr, writes=[wr])

            P.op(dve, lambda e: e.memset(ones[:, :], 1.0), writes=[ones])
            P.op(dve, lambda e: e.memset(cntbc[:, :], 0.0), writes=[cntbc])
            for k in range(8):
                P.op(dve, lambda e, k=k: e.tensor_scalar(win[:, k, :], win[:, k, :], gmixbc[:, k:k + 1], None, ALU.mult), reads=[win, gmixbc], writes=[win])
                P.op(dve, lambda e, k=k: e.tensor_scalar(wr[:, k, :], wr[:, k, :], gffnbc[:, k:k + 1], None, ALU.mult), reads=[wr, gffnbc], writes=[wr])

            tsm = B([128, 8, 4], F32, "tsm")

            def dv(fn, reads, writes):
                P.op(dve, fn, reads=reads, writes=writes)

            dv(lambda e: e.tensor_scalar(dpar[:, D_CWH:D_CWH + 124], cpar[:, CW:CW + 124], 0.5, None, ALU.mult), [cpar], [dpar])
            dv(lambda e: e.tensor_scalar(dpar[:, D_GH:D_GH + 8], cpar[:, LG:LG + 8], 0.5, None, ALU.mult), [cpar], [dpar])
            dv(lambda e: e.tensor_scalar(dpar[:, D_BRH:D_BRH + 8], cpar[:, BR:BR + 8], 0.5, None, ALU.mult), [cpar], [dpar])
            z_, az, ee, LL, tt, mk_, zp = [tsm[:, i, :] for i in range(7)]
            dv(lambda e: e.tensor_scalar(z_, cpar[:, LAM:LAM + 4], -1.0, None, ALU.mult), [cpar], [tsm])
            dv(lambda e: e.tensor_tensor(az, z_, cpar[:, LAM:LAM + 4], ALU.max), [tsm, cpar], [tsm])
            P.op(act, lambda e: e.activation(out=ee, in_=az, func=AF.Exp, scale=-1.0), reads=[tsm], writes=[tsm])
            P.op(act, lambda e: e.activation(out=LL, in_=ee, func=AF.Ln, bias=1.0, scale=1.0), reads=[tsm], writes=[tsm])
            dv(lambda e: e.tensor_scalar(tt, ee, -0.25, 1.0 / 3.0, ALU.mult, ALU.add), [tsm], [tsm])
            dv(lambda e: e.tensor_tensor(tt, tt, ee, ALU.mult), [tsm], [tsm])
            dv(lambda e: e.tensor_scalar(tt, tt, -1.0, 0.5, ALU.mult, ALU.add), [tsm], [tsm])
            dv(lambda e: e.tensor_tensor(tt, tt, ee, ALU.mult), [tsm], [tsm])
            dv(lambda e: e.tensor_scalar(tt, tt, -1.0, 1.0, ALU.mult, ALU.add), [tsm], [tsm])
            dv(lambda e: e.tensor_tensor(tt, tt, ee, ALU.mult), [tsm], [tsm])
            dv(lambda e: e.tensor_single_scalar(mk_, ee, 0.05, ALU.is_lt), [tsm], [tsm])
            dv(lambda e: e.tensor_tensor(tt, tt, LL, ALU.subtract), [tsm], [tsm])
            dv(lambda e: e.tensor_tensor(tt, tt, mk_, ALU.mult), [tsm], [tsm])
            dv(lambda e: e.tensor_tensor(tt, tt, LL, ALU.add), [tsm], [tsm])
            dv(lambda e: e.tensor_single_scalar(zp, z_, 0.0, ALU.max), [tsm], [tsm])
            dv(lambda e: e.tensor_tensor(tt, tt, zp, ALU.add), [tsm], [tsm])
            dv(lambda e: e.tensor_scalar(dpar[:, D_N4:D_N4 + 4], tt, -4.0, None, ALU.mult), [tsm], [dpar])
            dv(lambda e: e.tensor_scalar(dpar[:, D_N8:D_N8 + 4], tt, -8.0, None, ALU.mult), [tsm], [dpar])

            xa = [B([128, D], F32, "xa", dma=True) for _ in range(2)]
            ssF = [B([128, 8], F32, "ssF") for _ in range(2)]
            xn = [B([128, D], BF16, "xn") for _ in range(2)]
            hT = B([128, 8, 512], BF16, "hT")
            gth = [B([128, 512], F32, "gth") for _ in range(2)]
            gvs = [B([128, 512], F32, "gvs") for _ in range(2)]
            ga = B([128, 512], F32, "ga")
            gb = B([128, 512], F32, "gb")
            ub = [[B([128, 542], BF16, "ub") for _ in range(4)] for _ in range(2)]
            xbuf = [[B([128, 515], BF16, "xbuf") for _ in range(4)] for _ in range(2)]
            qg = [[B([128, 512], BF16, "qg") for _ in range(4)] for _ in range(2)]
            acc2 = [[B([128, 512], F32, "acc") for _ in range(4)] for _ in range(2)]
            cvbf = [B([128, 512], BF16, "cvbf") for _ in range(4)]
            sqbf = [B([128, 512], BF16, "sqbf") for _ in range(4)]
            mean = B([128, 512], F32, "mean")
            msq = B([128, 512], F32, "msq")
            xr = [B([128, 512], F32, "xr") for _ in range(2)]
            xrbf = [B([128, 512], BF16, "xrbf") for _ in range(2)]
            t1 = [B([128, 512], F32, "t1") for _ in range(2)]
            t2 = [B([128, 512], F32, "t2") for _ in range(2)]
            t3 = [B([128, 512], F32, "t3") for _ in range(2)]
            lh, th = t1, t2
            ab = [B([128, 512], F32, "ab")] * 2
            hb = [B([128, 512], F32, "hb")] * 2
            carry = [B([128, 1], F32, "carry") for _ in range(4)]
            yT = [B([128, 8, 512], BF16, "yT") for _ in range(2)]
            xb = B([128, D], F32, "xb", dma=True)
            ssE = [B([128, 8], F32, "ssE") for _ in range(2)]
            x1 = [B([128, D], F32, "x1", dma=True) for _ in range(2)]
            hfn = [B([128, D], BF16, "hfn", dma=True) for _ in range(4)]
            hfT = [B([128, 8, 128], BF16, "hfT") for _ in range(2)]
            rs = B([128, 40, Q], F32, "rs")
            r36 = B([128, Q, 36], F32, "r36")
            r32 = [B([128, Q, 32], F32, "r32") for _ in range(5)]
            mbf = B([128, Q, 32], BF16, "mbf")
            r8 = [B([128, Q, 8], F32, "r8") for _ in range(4)]
            r4 = [B([128, Q, 4], F32, "r4") for _ in range(3)]

            cwh = lambda c, k: dpar[:, D_CWH + c * 31 + k:D_CWH + c * 31 + k + 1]
            NJ = SEQ // 512

            def rsqA(ssb, i_v, i_l, i_o):
                P.op(act, lambda e: e.activation(out=ssb[:, i_l:i_l + 1], in_=ssb[:, i_v:i_v + 1], func=AF.Ln), reads=[ssb], writes=[ssb])
                P.op(act, lambda e: e.activation(out=ssb[:, i_o:i_o + 1], in_=ssb[:, i_l:i_l + 1], func=AF.Exp, scale=-0.5), reads=[ssb], writes=[ssb])

            def evac_scaled(dst3, ps, gvec):
                P.op(act, lambda e: e.activation(out=dst3.all(), in_=ps[:, :].rearrange("p (c j) -> p c j", c=8), func=AF.Copy),
                     reads=[ps], writes=[dst3.buf])

            class View3:
                def __init__(self, buf, fn, allfn=None):
                    self.buf, self.fn, self.all = buf, fn, allfn

                def __call__(self, c):
                    return self.fn(c)

            def gen_F(ti):
                s_, j = divmod(ti, NJ)
                row0 = ti * 512
                par = ti % 2
                ub_, xbuf_, qg_ = ub[par], xbuf[par], qg[par]
                if j == 0:
                    for c in range(4):
                        P.op(pool, lambda e, c=c: e.memset(ub_[c][:, 0:30], 0.0), writes=[ub_[c]])
                        P.op(pool, lambda e, c=c: e.memset(xbuf_[c][:, 0:3], 0.0), writes=[xbuf_[c]])
                for q in range(Q):
                    yield ("SEG" if q % 2 == 0 else "CHAIN")
                    xt, xnb, ss = xa[q % 2], xn[q % 2], ssF[q % 2]
                    r0 = row0 + q * 128
                    P.dma(sp, lambda e, xt=xt, r0=r0: e.dma_start(out=xt[:, :], in_=x_d[r0:r0 + 128, :]), xt, writes=[xt])
                    P.op(act, lambda e, xt=xt, ss=ss, xnb=xnb: e.activation(out=xnb[:, :], in_=xt[:, :], func=AF.Square, accum_out=ss[:, 0:1]),
                         reads=[xt], writes=[xnb, ss])
                    P.op(dve, lambda e, ss=ss: e.tensor_scalar(ss[:, 1:2], ss[:, 0:1], 1.0 / D, EPS, ALU.mult, ALU.add), reads=[ss], writes=[ss])
                    rsqA(ss, 1, 3, 2)
                    P.op(act, lambda e, xt=xt, xnb=xnb, ss=ss: e.activation(out=xnb[:, :], in_=xt[:, :], func=AF.Copy, scale=ss[:, 2:3]),
                         reads=[xt, ss], writes=[xnb])
                    transposes(xnb, 8, ptr)
                    evac_scaled(View3(hT, None, lambda q=q: hT[:, :, q * 128:(q + 1) * 128]), ptr, gmixbc)

                fbank = [pmm[0], ps2]

                def zmm(m, bi):
                    pb = fbank[bi % 2]

                    def fn(e, m=m, pb=pb):
                        ins = None
                        for k in range(8):
                            ins = e.matmul(pb[:, :], win[:, k, m * 128:(m + 1) * 128], hT[:, k, :], start=(k == 0), stop=(k == 7))
                        return ins
                    P.op(pe, fn, reads=[win, hT], writes=[pb])
                    return pb

                for c in range(4):
                    yield ("SEG" if c % 2 == 0 else "CHAIN")
                    pg = zmm(4 + c, c)
                    ta, vs = gth[c % 2], gvs[c % 2]
                    P.op(act, lambda e, pg=pg, ta=ta: e.activation(out=ta[:, :], in_=pg[:, :], func=AF.Tanh, scale=0.5), reads=[pg], writes=[ta])
                    pv = zmm(c, c)
                    P.op(act, lambda e, pv=pv, vs=vs: e.activation(out=vs[:, :], in_=pv[:, :], func=AF.Copy), reads=[pv], writes=[vs])
                    P.op(pool, lambda e, ta=ta, vs=vs: e.tensor_tensor(ta[:, :], ta[:, :], vs[:, :], ALU.mult), reads=[ta, vs], writes=[ta])
                    P.op(pool, lambda e, ta=ta, vs=vs, c=c: e.tensor_tensor(ub_[c][:, 30:542], ta[:, :], vs[:, :], ALU.add), reads=[ta, vs], writes=[ub_[c]])
                for c in range(4):
                    yield ("SEG" if c % 2 == 0 else "CHAIN")
                    px = zmm(8 + c, c)
                    P.op(act, lambda e, px=px, c=c: e.activation(out=xbuf_[c][:, 3:515], in_=px[:, :], func=AF.Copy), reads=[px], writes=[xbuf_[c]])
                for c in range(4):
                    yield "SEG"
                    pgl = zmm(12 + c, c)
                    P.op(act, lambda e, pgl=pgl: e.activation(out=ga[:, :], in_=pgl[:, :], func=AF.Copy), reads=[pgl], writes=[ga])
                    P.op(act, lambda e, pgl=pgl: e.activation(out=gb[:, :], in_=pgl[:, :], func=AF.Square), reads=[pgl], writes=[gb])
                    P.op(act, lambda e: e.activation(out=gb[:, :], in_=gb[:, :], func=AF.Identity, bias=1.0, scale=0.044715), reads=[gb], writes=[gb])
                    P.op(pool, lambda e: e.tensor_tensor(gb[:, :], gb[:, :], ga[:, :], ALU.mult), reads=[ga, gb], writes=[gb])
                    P.op(act, lambda e: e.activation(out=gb[:, :], in_=gb[:, :], func=AF.Tanh, scale=0.7978845608028654), reads=[gb], writes=[gb])
                    P.op(act, lambda e: e.activation(out=gb[:, :], in_=gb[:, :], func=AF.Identity, bias=1.0, scale=1.0), reads=[gb], writes=[gb])
                    P.op(pool, lambda e, c=c: e.tensor_tensor(qg_[c][:, :], gb[:, :], ga[:, :], ALU.mult), reads=[ga, gb], writes=[qg_[c]])

            def gen_Mc(ti):
                s_, j = divmod(ti, NJ)
                par = ti % 2
                ub_, xbuf_, qg_, yT_ = ub[par], xbuf[par], qg[par], yT[par]
                ubn, xbufn = ub[1 - par], xbuf[1 - par]
                acc = acc2[par]
                for k in range(31):
                    for c in range(4):
                        if k == 0:
                            P.op(dve, lambda e, c=c: e.tensor_scalar(acc[c][:, :], ub_[c][:, 0:512], cwh(c, 0), cpar[:, CB + c:CB + c + 1], ALU.mult, ALU.add),
                                 reads=[ub_[c], dpar, cpar], writes=[acc[c]])
                        else:
                            P.op(dve, lambda e, c=c, k=k: e.scalar_tensor_tensor(acc[c][:, :], ub_[c][:, k:k + 512], cwh(c, k), acc[c][:, :], ALU.mult, ALU.add),
                                 reads=[ub_[c], dpar, acc[c]], writes=[acc[c]])
                    yield
                for c in range(4):
                    if j < NJ - 1:
                        P.op(pool, lambda e, c=c: e.tensor_copy(ubn[c][:, 0:30], ub_[c][:, 512:542]), reads=[ub_[c]], writes=[ubn[c]])
                    P.op(act, lambda e, c=c: e.activation(out=cvbf[c][:, :], in_=acc[c][:, :], func=AF.Copy), reads=[acc[c]], writes=[cvbf[c]])
                    P.op(act, lambda e, c=c: e.activation(out=sqbf[c][:, :], in_=acc[c][:, :], func=AF.Square), reads=[acc[c]], writes=[sqbf[c]])

                def stat_mm(dst, srcs):
                    def fn(e):
                        ins = None
                        for c in range(4):
                            ins = e.matmul(dst[:, :], ones[:, :], srcs[c][:, :], start=(c == 0), stop=(c == 3))
                        return ins
                    P.op(pe, fn, reads=[ones] + srcs, writes=[dst])
                stat_mm(ps1, cvbf)
                P.op(act, lambda e: e.activation(out=mean[:, :], in_=ps1[:, :], func=AF.Copy, scale=1.0 / 512), reads=[ps1], writes=[mean])
                stat_mm(ps1, sqbf)
                P.op(pool, lambda e: e.tensor_tensor(msq[:, :], mean[:, :], mean[:, :], ALU.mult), reads=[mean], writes=[msq])
                P.op(dve, lambda e: e.scalar_tensor_tensor(msq[:, :], ps1[:, :], 1.0 / 512, msq[:, :], ALU.mult, ALU.subtract), reads=[ps1, msq], writes=[msq])
                P.op(dve, lambda e: e.tensor_scalar(msq[:, :], msq[:, :], EPS, None, ALU.add), reads=[msq], writes=[msq])
                P.op(act, lambda e: e.activation(out=msq[:, :], in_=msq[:, :], func=AF.Ln), reads=[msq], writes=[msq])
                P.op(act, lambda e: e.activation(out=msq[:, :], in_=msq[:, :], func=AF.Exp, scale=-0.5), reads=[msq], writes=[msq])
                yield
                for c in range(4):
                    P.op(pool, lambda e, c=c: e.tensor_tensor(acc[c][:, :], acc[c][:, :], mean[:, :], ALU.subtract), reads=[acc[c], mean], writes=[acc[c]])
                    P.op(pool, lambda e, c=c: e.tensor_tensor(acc[c][:, :], acc[c][:, :], msq[:, :], ALU.mult), reads=[acc[c], msq], writes=[acc[c]])
                for c in range(4):
                    t_ = (mean, msq)[c % 2]
                    P.op(act, lambda e, c=c: e.activation(out=acc[c][:, :], in_=acc[c][:, :], func=AF.Identity,
                                                          bias=dpar[:, D_BH + c:D_BH + c + 1], scale=dpar[:, D_GH + c:D_GH + c + 1]),
                         reads=[acc[c], dpar], writes=[acc[c]])
                    P.op(act, lambda e, c=c, t_=t_: e.activation(out=t_[:, :], in_=acc[c][:, :], func=AF.Tanh), reads=[acc[c]], writes=[t_])
                    P.op(dve, lambda e, c=c, t_=t_: e.scalar_tensor_tensor(yT_[:, c, :], t_[:, :], 1.0, acc[c][:, :], ALU.add, ALU.mult),
                         reads=[acc[c], t_], writes=[yT_])
            def gen_Ml(ti):
                s_, j = divmod(ti, NJ)
                par = ti % 2
                xbuf_, qg_, yT_ = xbuf[par], qg[par], yT[par]
                xbufn = xbuf[1 - par]
                if j == 0:
                    for c in range(4):
                        P.op(pool, lambda e, c=c: e.memset(carry[c][:, :], 0.0), writes=[carry[c]])
                for c in range(4):
                    xr_, xrb_ = xr[c % 2], xrbf[c % 2]
                    lw = lambda k, c=c: cpar[:, LW + c * 4 + k:LW + c * 4 + k + 1]
                    P.op(dve, lambda e, c=c, xr_=xr_, lw=lw: e.tensor_scalar(xr_[:, :], xbuf_[c][:, 0:512], lw(0), cpar[:, LBB + c:LBB + c + 1], ALU.mult, ALU.add),
                         reads=[xbuf_[c], cpar], writes=[xr_])
                    for k in range(1, 4):
                        P.op(dve, lambda e, c=c, k=k, xr_=xr_, lw=lw: e.scalar_tensor_tensor(xr_[:, :], xbuf_[c][:, k:k + 512], lw(k), xr_[:, :], ALU.mult, ALU.add),
                             reads=[xbuf_[c], cpar, xr_], writes=[xr_])
                    if j < NJ - 1:
                        P.op(pool, lambda e, c=c: e.tensor_copy(xbufn[c][:, 0:3], xbuf_[c][:, 512:515]), reads=[xbuf_[c]], writes=[xbufn[c]])
                    P.op(act, lambda e, xr_=xr_, xrb_=xrb_: e.activation(out=xrb_[:, :], in_=xr_[:, :], func=AF.Copy), reads=[xr_], writes=[xrb_])
                    pr, pi = pmm[1], pmm[1]
                    P.op(pe, lambda e, c=c, pr=pr, xrb_=xrb_: e.matmul(pr[:, :], gbd[:, c, 0:128], xrb_[:, :], start=True, stop=True), reads=[gbd, xrb_], writes=[pr])
                    a1, a2, a3, aa, hh = t1[c % 2], t2[c % 2], t3[c % 2], ab[c % 2], hb[c % 2]
                    dp = lambda o, c=c: dpar[:, o + c:o + c + 1]
                    yield
                    P.op(act, lambda e, pr=pr, a1=a1, dp=dp: e.activation(out=a1[:, :], in_=pr[:, :], func=AF.Tanh, bias=dp(D_BRH), scale=0.5), reads=[pr, dpar], writes=[a1])
                    P.op(pe, lambda e, c=c, pi=pi, xrb_=xrb_: e.matmul(pi[:, :], gbd[:, c, 128:256], xrb_[:, :], start=True, stop=True), reads=[gbd, xrb_], writes=[pi])
                    P.op(act, lambda e, pi=pi, a3=a3, dp=dp: e.activation(out=a3[:, :], in_=pi[:, :], func=AF.Tanh, bias=dp(D_BIH), scale=0.5), reads=[pi, dpar], writes=[a3])
                    P.op(act, lambda e, a1=a1, aa=aa, dp=dp: e.activation(out=aa[:, :], in_=a1[:, :], func=AF.Exp, bias=dp(D_N4), scale=dp(D_N4)), reads=[a1, dpar], writes=[aa])
                    P.op(act, lambda e, a1=a1, a2=a2, dp=dp: e.activation(out=a2[:, :], in_=a1[:, :], func=AF.Exp, bias=dp(D_N8), scale=dp(D_N8)), reads=[a1, dpar], writes=[a2])
                    P.op(dve, lambda e, a2=a2: e.tensor_scalar(a2[:, :], a2[:, :], 0.99999994, -1.0, ALU.min, ALU.mult), reads=[a2], writes=[a2])
                    P.op(act, lambda e, a2=a2: e.activation(out=a2[:, :], in_=a2[:, :], func=AF.Ln, bias=1.0, scale=1.0), reads=[a2], writes=[a2])
                    P.op(act, lambda e, a2=a2: e.activation(out=a2[:, :], in_=a2[:, :], func=AF.Exp, scale=0.5), reads=[a2], writes=[a2])
                    P.op(dve, lambda e, a3=a3, xr_=xr_: e.scalar_tensor_tensor(a3[:, :], a3[:, :], 1.0, xr_[:, :], ALU.add, ALU.mult), reads=[a3, xr_], writes=[a3])
                    yield
                    P.op(dve, lambda e, a2=a2, a3=a3: e.tensor_tensor(a3[:, :], a3[:, :], a2[:, :], ALU.mult), reads=[a2, a3], writes=[a3])
                    P.op(dve, lambda e, c=c, aa=aa, a3=a3, hh=hh: e.tensor_tensor_scan(hh[:, :], aa[:, :], a3[:, :], carry[c][:, 0:1], ALU.mult, ALU.add),
                         reads=[aa, a3, carry[c]], writes=[hh])
                    P.op(dve, lambda e, c=c, hh=hh: e.tensor_copy(carry[c][:, :], hh[:, 511:512]), reads=[hh], writes=[carry[c]])
                    P.op(dve, lambda e, c=c, hh=hh: e.scalar_tensor_tensor(yT_[:, 4 + c, :], hh[:, :], 0.25, qg_[c][:, :], ALU.mult, ALU.mult),
                         reads=[hh, qg_[c]], writes=[yT_])
                    yield

            def gen_E(ti):
                row0 = ti * 512
                yT_ = yT[ti % 2]
                for q in range(Q):
                    yield ("SEG" if q % 2 == 0 else "CHAIN")
                    r0 = row0 + q * 128
                    x1t, ss = x1[q % 2], ssE[q % 2]
                    hf = hfn[q]
                    hft = hfT[q % 2]
                    P.dma(sp, lambda e, r0=r0: e.dma_start(out=xb[:, :], in_=x_d[r0:r0 + 128, :]), xb, writes=[xb])
                    for h in range(2):
                        pb = pmm[2]

                        def fn(e, pb=pb, h=h, q=q):
                            ins = e.matmul(pb[:, :], identF[:, :], xb[:, h * 512:(h + 1) * 512], start=True, stop=False)
                            for k in range(8):
                                ins = e.matmul(pb[:, :], yT_[:, k, q * 128:(q + 1) * 128], wout[:, k, h * 512:(h + 1) * 512], start=False, stop=(k == 7))
                            return ins
                        P.op(pe, fn, reads=[yT_, wout, identF, xb], writes=[pb])
                        P.op(act, lambda e, pb=pb, h=h, x1t=x1t: e.activation(out=x1t[:, h * 512:(h + 1) * 512], in_=pb[:, :], func=AF.Copy),
                             reads=[pb], writes=[x1t])
                    P.dma(sp, lambda e, x1t=x1t, r0=r0: e.dma_start(out=x1_d[r0:r0 + 128, :], in_=x1t[:, :]), x1t, reads=[x1t])
                    P.op(act, lambda e, x1t=x1t, ss=ss, hf=hf: e.activation(out=hf[:, :], in_=x1t[:, :], func=AF.Square, accum_out=ss[:, 0:1]), reads=[x1t], writes=[hf, ss])
                    P.op(dve, lambda e, ss=ss: e.tensor_scalar(ss[:, 1:2], ss[:, 0:1], 1.0 / D, EPS, ALU.mult, ALU.add), reads=[ss], writes=[ss])
                    rsqA(ss, 1, 3, 2)
                    P.op(act, lambda e, x1t=x1t, hf=hf, ss=ss: e.activation(out=hf[:, :], in_=x1t[:, :], func=AF.Copy, scale=ss[:, 2:3]), reads=[x1t, ss], writes=[hf])
                    transposes(hf, 8, ptr2)
                    evac_scaled(View3(hft, None, lambda hft=hft: hft[:, :, :]), ptr2, gffnbc)

                    def fnl(e, hft=hft, q=q):
                        ins = None
                        for k in range(8):
                            ins = e.matmul(pmm[3][:, q * 36:(q + 1) * 36], hft[:, k, :], wr[:, k, :], start=(k == 0), stop=(k == 7))
                        return ins
                    P.op(pe, fnl, reads=[hft, wr], writes=[pmm[3]])

                yield "SEG"
                S = lambda i: rs[:, i, :]
                bc = lambda ap, n: ap.unsqueeze(2).broadcast_to([128, Q, n])
                lgb = r36
                P.op(dve, lambda e: e.tensor_tensor(lgb[:, :, :], pmm[3][:, 0:Q * 36].rearrange("p (q n) -> p q n", q=Q), rbias[:, :, :], ALU.add),
                     reads=[pmm[3], rbias], writes=[lgb])
                gmask, gsh, gex = r4
                P.op(dve, lambda e: e.tensor_reduce(S(0), lgb[:, :, 0:4], AX.X, ALU.max), reads=[lgb], writes=[rs])
                P.op(dve, lambda e: e.tensor_tensor(gmask[:, :, :], lgb[:, :, 0:4], bc(S(0), 4), ALU.is_equal), reads=[lgb, rs], writes=[gmask])
                P.op(dve, lambda e: e.tensor_tensor(gsh[:, :, :], lgb[:, :, 0:4], bc(S(0), 4), ALU.subtract), reads=[lgb, rs], writes=[gsh])
                P.op(act, lambda e: e.activation(out=gex[:, :, :], in_=gsh[:, :, :], func=AF.Exp), reads=[gsh], writes=[gex])
                P.op(dve, lambda e: e.tensor_reduce(S(1), gex[:, :, :], AX.X, ALU.add), reads=[gex], writes=[rs])
                P.op(dve, lambda e: e.reciprocal(S(2), S(1)), reads=[rs], writes=[rs])
                le4 = lgb[:, :, 4:36].rearrange("p q (g j) -> p q g j", g=4)
                tmp32 = r32[0]
                P.op(dve, lambda e: e.tensor_tensor(tmp32[:, :, :].rearrange("p q (g j) -> p q g j", g=4), le4,
                                                    gmask[:, :, :].unsqueeze(3).broadcast_to([128, Q, 4, 8]), ALU.mult), reads=[lgb, gmask], writes=[tmp32])
                sel, top8, oh1, oh2 = r8
                P.op(dve, lambda e: e.tensor_reduce(sel[:, :, :], tmp32[:, :, :].rearrange("p q (g j) -> p q j g", g=4), AX.X, ALU.add), reads=[tmp32], writes=[sel])
                yield
                for q in range(Q):
                    P.op(dve, lambda e, q=q: e.max(top8[:, q, :], sel[:, q, :]), reads=[sel], writes=[top8])
                P.op(dve, lambda e: e.tensor_tensor(oh1[:, :, :], sel[:, :, :], top8[:, :, 0:1].broadcast_to([128, Q, 8]), ALU.is_equal), reads=[sel, top8], writes=[oh1])
                P.op(dve, lambda e: e.tensor_tensor(oh2[:, :, :], sel[:, :, :], top8[:, :, 1:2].broadcast_to([128, Q, 8]), ALU.is_equal), reads=[sel, top8], writes=[oh2])
                P.op(dve, lambda e: e.tensor_tensor(S(3), top8[:, :, 1], top8[:, :, 0], ALU.subtract), reads=[top8], writes=[rs])
                P.op(act, lambda e: e.activation(out=S(4), in_=S(3), func=AF.Exp), reads=[rs], writes=[rs])
                P.op(dve, lambda e: e.tensor_scalar(S(5), S(4), 1.0, None, ALU.add), reads=[rs], writes=[rs])
                P.op(dve, lambda e: e.reciprocal(S(6), S(5)), reads=[rs], writes=[rs])
                P.op(dve, lambda e: e.tensor_tensor(S(7), S(6), S(2), ALU.mult), reads=[rs], writes=[rs])
                P.op(dve, lambda e: e.tensor_tensor(S(8), S(2), S(7), ALU.subtract), reads=[rs], writes=[rs])
                E1, E2 = r32[1], r32[2]
                for Ek, oh in ((E1, oh1), (E2, oh2)):
                    P.op(dve, lambda e, Ek=Ek, oh=oh: e.tensor_tensor(Ek[:, :, :].rearrange("p q (g j) -> p q g j", g=4),
                                                                      gmask[:, :, :].unsqueeze(3).broadcast_to([128, Q, 4, 8]),
                                                                      oh[:, :, :].unsqueeze(2).broadcast_to([128, Q, 4, 8]), ALU.mult),
                         reads=[gmask, oh], writes=[Ek])
                P.op(dve, lambda e: e.tensor_tensor(mbf[:, :, :], E1[:, :, :], E2[:, :, :], ALU.add), reads=[E1, E2], writes=[mbf])

                def fnc(e):
                    ins = None
                    for q in range(Q):
                        ins = e.matmul(pmm[3][:, 160 + q * 32:160 + (q + 1) * 32], tri[:, :], mbf[:, q, :], start=True, stop=(q == 0))
                        for q2 in range(q):
                            ins = e.matmul(pmm[3][:, 160 + q * 32:160 + (q + 1) * 32], ones[:, :], mbf[:, q2, :], start=False, stop=(q2 == q - 1))
                    for q in range(Q):
                        ins = e.matmul(pmm[3][:, 288:320], ones[:, :], mbf[:, q, :], start=(q == 0), stop=(q == Q - 1))
                    return ins
                P.op(pe, fnc, reads=[tri, ones, mbf], writes=[pmm[3]])
                yield
                tot = r32[3]
                P.op(dve, lambda e: e.tensor_tensor(tot[:, :, :], pmm[3][:, 160:288].rearrange("p (q n) -> p q n", q=Q),
                                                    cntbc[:, :].unsqueeze(1).broadcast_to([128, Q, 32]), ALU.add), reads=[pmm[3], cntbc], writes=[tot])
                P.op(dve, lambda e: e.tensor_tensor(cntbc[:, :], cntbc[:, :], pmm[3][:, 288:320], ALU.add), reads=[pmm[3], cntbc], writes=[cntbc])
                tm = r32[4]
                for kk, Ek in ((0, E1), (1, E2)):
                    P.op(dve, lambda e, Ek=Ek: e.tensor_tensor(tm[:, :, :], Ek[:, :, :], tot[:, :, :], ALU.mult), reads=[Ek, tot], writes=[tm])
                    P.op(dve, lambda e, kk=kk: e.tensor_reduce(S(10 + kk), tm[:, :, :], AX.X, ALU.add), reads=[tm], writes=[rs])
                    P.op(dve, lambda e, Ek=Ek: e.tensor_tensor(tm[:, :, :], Ek[:, :, :], iota[:, :, :], ALU.mult), reads=[Ek, iota], writes=[tm])
                    P.op(dve, lambda e, kk=kk: e.tensor_reduce(S(12 + kk), tm[:, :, :], AX.X, ALU.add), reads=[tm], writes=[rs])
                    P.op(dve, lambda e, kk=kk: e.scalar_tensor_tensor(S(14 + kk), S(12 + kk), float(CAP), S(10 + kk), ALU.mult, ALU.add), reads=[rs], writes=[rs])
                    P.op(dve, lambda e, kk=kk: e.tensor_single_scalar(S(16 + kk), S(10 + kk), float(CAP), ALU.is_lt), reads=[rs], writes=[rs])
                    P.op(dve, lambda e, kk=kk: e.tensor_scalar(S(14 + kk), S(14 + kk), float(-TRASH), None, ALU.add), reads=[rs], writes=[rs])
                    P.op(dve, lambda e, kk=kk: e.tensor_tensor(S(14 + kk), S(14 + kk), S(16 + kk), ALU.mult), reads=[rs], writes=[rs])
                    P.op(dve, lambda e, kk=kk: e.tensor_scalar(S(14 + kk), S(14 + kk), float(TRASH), 0.0, ALU.add, ALU.max), reads=[rs], writes=[rs])
                    P.op(dve, lambda e, kk=kk: e.tensor_scalar(rinfo[:, ti, kk, :], S(14 + kk), float(TRASH), None, ALU.min), reads=[rs], writes=[rinfo])
                    P.op(dve, lambda e, kk=kk: e.tensor_tensor(rinfo[:, ti, 2 + kk, :], S(7 + kk), S(16 + kk), ALU.mult), reads=[rs], writes=[rinfo])
                    yield
                P.op(dve, lambda e: e.tensor_copy(sloti[:, ti, :, :], rinfo[:, ti, 0:2, :]), reads=[rinfo], writes=[sloti])
                for q in range(Q):
                    for kk in range(2):
                        P.dma(pool, lambda e, q=q, kk=kk: e.indirect_dma_start(
                            out=hs_d[:, :], out_offset=IOA(ap=sloti[:, ti, kk, q:q + 1], axis=0), in_=hfn[q][:, :], in_offset=None),
                            hfn[q], reads=[hfn[q], sloti])

            def collect(genfunc, ti):
                items = []
                orig_op, orig_dma = P.op, P.dma
                P.op = lambda eng, fn, reads=(), writes=(): items.append((orig_op, (eng, fn), dict(reads=reads, writes=writes), eng))
                P.dma = lambda q, fn, sem_buf, reads=(), writes=(): items.append((orig_dma, (q, fn, sem_buf), dict(reads=reads, writes=writes), q))
                try:
                    for tok in genfunc(ti):
                        items.append(tok)
                finally:
                    del P.op, P.dma
                return items

            def stages1(items):
                out, cur, prev = [], [], None
                for it in items:
                    if it is None or (HOP and prev is not None and it[3] is not prev):
                        out.append(cur)
                        cur = []
                    if it is None:
                        prev = None
                    else:
                        cur.append(it)
                        prev = it[3]
                out.append(cur)
                res = []
                for st in out:
                    if not st:
                        continue
                    res.append(st)
                    if LOADLAG and all(it[3] is sp and it[2]["writes"] for it in st):
                        res.extend([[] for _ in range(LOADLAG)])
                return res

            def zip_locked(chains):
                k = len(chains)
                if k == 1:
                    return chains[0]
                spans, wsets = [], []
                for L in chains:
                    fw, lr, ws = {}, {}, set()
                    for si, st in enumerate(L):
                        for (f, args, kw, eng) in st:
                            for bb in kw["writes"]:
                                fw.setdefault(id(bb), si)
                                ws.add(id(bb))
                            for bb in kw["reads"]:
                                lr[id(bb)] = si
                    spans.append({x: (fw[x], lr[x]) for x in fw if x in lr and lr[x] > fw[x]})
                    wsets.append(ws)
                out, pos, owner = [], [0] * k, {}
                while any(pos[c] < len(chains[c]) for c in range(k)):
                    progressed = False
                    for c in range(k):
                        if pos[c] >= len(chains[c]):
                            continue
                        st = chains[c][pos[c]]
                        W = set(id(bb) for (f, args, kw, eng) in st for bb in kw["writes"])
                        if any(owner.get(x) not in (None, c) for x in W):
                            continue
                        out.append(st)
                        progressed = True
                        for x in W:
                            if x in spans[c] and any(x in wsets[j] for j in range(k) if j != c):
                                owner[x] = c
                        for x in list(owner):
                            if owner[x] == c and pos[c] >= spans[c][x][1]:
                                owner[x] = None
                        pos[c] += 1
                    assert progressed, "chain lock deadlock"
                return out

            def stages(items):
                segs = [[[]]]
                for it in items:
                    if it == "SEG":
                        segs.append([[]])
                    elif it == "CHAIN":
                        segs[-1].append([])
                    else:
                        segs[-1][-1].append(it)
                out = []
                for seg in segs:
                    out += zip_locked([stages1(ch) for ch in seg if ch])  if any(seg) else []
                return out

            def spread(genfunc, ti, n, off=0):
                sts = stages(collect(genfunc, ti))
                assert len(sts) <= n - off, (len(sts), n, off)
                k = 0
                for t in range(n):
                    while t >= off and k < len(sts) and k * (n - off) < (t - off + 1) * len(sts):
                        for f, args, kw, eng in sts[k]:
                            f(*args, **kw)
                        k += 1
                    yield

            counts = [len(stages(collect(g, 1))) for g in (gen_F, gen_Mc, gen_Ml, gen_E)]
            NSTG = max(counts) + 2

            def both(g1, g2):
                for _ in g1:
                    next(g2)
                    yield

            def tile_gen(ti):
                yield from spread(gen_F, ti, NSTG)
                yield from both(spread(gen_Mc, ti, NSTG, MC_OFF), spread(gen_Ml, ti, NSTG))
                yield from spread(gen_E, ti, NSTG)

            run_pipelined((tile_gen(ti) for ti in range(NT)), depth=3, skew=NSTG)

            if debug:
                P.dma(sp, lambda e: e.dma_start(out=ri_d, in_=rinfo[:, :, :, :].rearrange("p a b c -> p (a b c)")), rinfo, reads=[rinfo])
            P.barrier()
            P.flush(block)

        with ExitStack() as sb:
            B = lambda shape, dt=F32, name=None, dma=False: P.buf(sb, shape, dt, name, dma)
            gffnbc = B([128, 8], F32, "gffnbc", dma=True)
            P.dma(sp, lambda e: e.dma_start(out=gffnbc[:, :], in_=gbc_d[1]), gffnbc, writes=[gffnbc])
            zt = B([128, D], F32, "zt", dma=True)
            P.op(dve, lambda e: e.memset(zt[:, :], 0.0), writes=[zt])
            P.dma(sp, lambda e: e.dma_start(out=y_d[NSLOT:NSLOT + 128, :], in_=zt[:, :]), zt, reads=[zt])
            w1 = [B([128, 8, 512], BF16, "w1") for _ in range(2)]
            w3 = [B([128, 8, 512], BF16, "w3") for _ in range(2)]
            w2 = [B([128, 4, D], BF16, "w2") for _ in range(2)]
            w3s = B([128, 8, 512], F32, "w3s", dma=True)
            w1s = B([128, 8, 512], F32, "w1s", dma=True)
            w2s = B([128, 4, D], F32, "w2s", dma=True)
            hst = [B([128, NSUB, D], BF16, "hst", dma=True) for _ in range(2)]
            hfTe = [B([128, 8, CAP], BF16, "hfTe") for _ in range(2)]
            actT = B([128, 4, CAP], BF16, "actT")
            tb = [B([128, 512], F32, "tb") for _ in range(2)]
            tc = [B([128, 512], F32, "tc") for _ in range(2)]
            yt = [B([128, D], F32, "yt", dma=True) for _ in range(3)]
            ntiles = [(0, 512)] if CAP == 512 else ([(n0, min(512, CAP - n0)) for n0 in range(0, CAP, 512)])
            yi = 0
            ci = 0

            def load_expert(ex):
                sl = ex % 2
                P.dma(sp, lambda e: e.dma_start(out=hst[sl][:, :, :], in_=hs_d[ex * CAP:(ex + 1) * CAP, :].rearrange("(s p) n -> p s n", p=128)),
                      hst[sl], writes=[hst[sl]])
                P.dma(sp, lambda e: e.dma_start(out=w1s[:, :, :], in_=w1_d[ex].rearrange("(k p) n -> p k n", p=128)), w1s, writes=[w1s])
                P.dma(sp, lambda e: e.dma_start(out=w3s[:, :, :], in_=w3_d[ex].rearrange("(k p) n -> p k n", p=128)), w3s, writes=[w3s])
                P.dma(sp, lambda e: e.dma_start(out=w2s[:, :, :], in_=w2_d[ex].rearrange("(k p) n -> p k n", p=128)), w2s, writes=[w2s])

            def cast_expert(ex):
                sl = ex % 2
                for k in range(8):
                    P.op(pool, lambda e, k=k: e.tensor_tensor(w1[sl][:, k, :], w1s[:, k, :], gffnbc[:, k:k + 1].broadcast_to([128, 512]), ALU.mult),
                         reads=[w1s, gffnbc], writes=[w1[sl]])
                for k in range(8):
                    P.op(act, lambda e, k=k: e.activation(out=w3[sl][:, k, :], in_=w3s[:, k, :], func=AF.Copy, scale=gffnbc[:, k:k + 1]), reads=[w3s, gffnbc], writes=[w3[sl]])
                for k in range(4):
                    P.op(act, lambda e, k=k: e.activation(out=w2[sl][:, k, :], in_=w2s[:, k, :], func=AF.Copy), reads=[w2s], writes=[w2[sl]])

            pool6 = pmm + [ps1, ps2]
            p6 = [0]

            def next6():
                bb = pool6[p6[0] % 6]
                p6[0] += 1
                return bb

            def do_T(ex):
                sl = ex % 2
                hT_e = hfTe[sl]
                for sbt in range(NSUB):
                    pt_ = ptr if sbt % 2 == 0 else ptr2

                    def fn(e, sbt=sbt, pt_=pt_, sl=sl):
                        ins = None
                        for c in range(8):
                            ins = e.transpose(pt_[:, c * 128:(c + 1) * 128], hst[sl][:, sbt, c * 128:(c + 1) * 128], ident[:, :])
                        return ins
                    P.op(pe, fn, reads=[hst[sl], ident], writes=[pt_])
                    P.op(dve, lambda e, sbt=sbt, pt_=pt_, hT_e=hT_e: e.tensor_copy(hT_e[:, :, sbt * 128:(sbt + 1) * 128], pt_[:, :].rearrange("p (c j) -> p c j", c=8)),
                         reads=[pt_], writes=[hT_e])

            def do_H(ex):
                sl = ex % 2
                hT_e = hfTe[sl]
                for (n0, nn) in ntiles:
                    for m in range(4):
                        p1, p3 = next6(), next6()
                        for (pb, wt) in ((p1, w1[sl]), (p3, w3[sl])):
                            def fn(e, pb=pb, wt=wt, m=m, n0=n0, nn=nn, hT_e=hT_e):
                                ins = None
                                for k in range(8):
                                    ins = e.matmul(pb[:, 0:nn], wt[:, k, m * 128:(m + 1) * 128], hT_e[:, k, n0:n0 + nn], start=(k == 0), stop=(k == 7))
                                return ins
                            P.op(pe, fn, reads=[wt, hT_e], writes=[pb])
                        tb_, tc_ = tb[ci_[0] % 2], tc[ci_[0] % 2]
                        ci_[0] += 1
                        P.op(act, lambda e, p1=p1, tb_=tb_, nn=nn: e.activation(out=tb_[:, 0:nn], in_=p1[:, 0:nn], func=AF.Tanh, scale=0.5), reads=[p1], writes=[tb_])
                        P.op(dve, lambda e, p1=p1, tb_=tb_, tc_=tc_, nn=nn: e.scalar_tensor_tensor(tc_[:, 0:nn], tb_[:, 0:nn], 1.0, p1[:, 0:nn], ALU.add, ALU.mult),
                             reads=[tb_, p1], writes=[tc_])
                        P.op(dve, lambda e, p3=p3, tc_=tc_, nn=nn, m=m, n0=n0: e.scalar_tensor_tensor(actT[:, m, n0:n0 + nn], tc_[:, 0:nn], 0.5, p3[:, 0:nn], ALU.mult, ALU.mult),
                             reads=[tc_, p3], writes=[actT])

            def do_Y(ex):
                sl = ex % 2
                for sbt in range(NSUB):
                    yb = yt[yi_[0] % 3]
                    yi_[0] += 1
                    for h in range(2):
                        pb = next6()

                        def fn(e, pb=pb, h=h, sbt=sbt, sl=sl):
                            ins = None
                            for m in range(4):
                                ins = e.matmul(pb[:, :], actT[:, m, sbt * 128:(sbt + 1) * 128], w2[sl][:, m, h * 512:(h + 1) * 512], start=(m == 0), stop=(m == 3))
                            return ins
                        P.op(pe, fn, reads=[actT, w2[sl]], writes=[pb])
                        P.op(act, lambda e, pb=pb, h=h, yb=yb: e.activation(out=yb[:, h * 512:(h + 1) * 512], in_=pb[:, :], func=AF.Copy), reads=[pb], writes=[yb])
                    r0 = ex * CAP + sbt * 128
                    P.dma(sp, lambda e, yb=yb, r0=r0: e.dma_start(out=y_d[r0:r0 + 128, :], in_=yb[:, :]), yb, reads=[yb])

            ci_, yi_ = [0], [0]
            load_expert(0)
            cast_expert(0)
            do_T(0)
            for ex in range(32):
                if ex + 1 < 32:
                    load_expert(ex + 1)
                do_H(ex)
                if ex + 1 < 32:
                    cast_expert(ex + 1)
                    do_T(ex + 1)
                do_Y(ex)
            P.barrier()
            P.flush(block)

        with ExitStack() as sc:
            B = lambda shape, dt=F32, name=None, dma=False: P.buf(sc, shape, dt, name, dma)
            gplebc = B([128, 8], F32, "gplebc", dma=True)
            gpp = B([128, D], F32, "gpp", dma=True)
            gfin = B([128, D], F32, "gfin", dma=True)
            wple = B([128, 2, D], BF16, "wple", dma=True)
            wpg = B([128, 8, D], BF16, "wpg", dma=True)
            P.dma(sp, lambda e: e.dma_start(out=gplebc[:, :], in_=gbc_d[2]), gplebc, writes=[gplebc])
            P.dma(sp, lambda e: e.dma_start(out=gpp[:, :], in_=rowbc_d[0]), gpp, writes=[gpp])
            P.dma(sp, lambda e: e.dma_start(out=gfin[:, :], in_=rowbc_d[1]), gfin, writes=[gfin])
            for k in range(2):
                P.dma(pool, lambda e, k=k: e.dma_start(out=wple[:, k, :], in_=wple_d[k * 128:(k + 1) * 128, :]), wple, writes=[wple])
            for k in range(8):
                P.dma(pool, lambda e, k=k: e.dma_start(out=wpg[:, k, :], in_=wpg_d[k * 128:(k + 1) * 128, :]), wpg, writes=[wpg])
            for k in range(8):
                P.op(dve, lambda e, k=k: e.tensor_scalar(wpg[:, k, :], wpg[:, k, :], gplebc[:, k:k + 1], None, ALU.mult), reads=[wpg, gplebc], writes=[wpg])
            x1t_ = [B([128, D], F32, "x1c", dma=True) for _ in range(NP_C)]
            pt_b = [B([128, 256], F32, "pc", dma=True) for _ in range(NP_C)]
            y1_ = [B([128, D], F32, "y1c", dma=True) for _ in range(NP_C)]
            y2_ = [B([128, D], F32, "y2c", dma=True) for _ in range(NP_C)]
            NP = NP_C
            ssC_ = [B([128, 16], F32, "ssC") for _ in range(NP)]
            xn3 = [B([128, D], BF16, "xn3") for _ in range(NP)]
            junk, te, ob, thg = xn3, y2_, x1t_, y1_
            x3T = [B([128, 8, 128], BF16, "x3T") for _ in range(NP)]
            pbf = [B([128, 256], BF16, "pbf") for _ in range(NP)]
            pT = [B([128, 2, 128], BF16, "pT") for _ in range(NP)]

            def rsq(ssb, i_v, i_l, i_o):
                P.op(act, lambda e: e.activation(out=ssb[:, i_l:i_l + 1], in_=ssb[:, i_v:i_v + 1], func=AF.Ln), reads=[ssb], writes=[ssb])
                P.op(act, lambda e: e.activation(out=ssb[:, i_o:i_o + 1], in_=ssb[:, i_l:i_l + 1], func=AF.Exp, scale=-0.5), reads=[ssb], writes=[ssb])

            def subtile_gen(st):
                ti, q = divmod(st, Q)
                r0 = st * 128
                i3 = st % NP
                xx, pp, y1, y2 = x1t_[i3], pt_b[i3], y1_[i3], y2_[i3]
                jk, ssC = junk[i3], ssC_[i3]
                xn_, x3_, pb_, pT_, tg_, te_, ob_ = xn3[i3], x3T[i3], pbf[i3], pT[i3], thg[i3], te[i3], ob[i3]
                P.dma(sp, lambda e: e.dma_start(out=xx[:, :], in_=x1_d[r0:r0 + 128, :]), xx, writes=[xx])
                P.dma(sp, lambda e: e.dma_start(out=pp[:, :], in_=p_d[r0:r0 + 128, :]), pp, writes=[pp])
                for (yy, kk) in ((y1, 0), (y2, 1)):
                    P.dma(pool, lambda e, yy=yy, kk=kk: e.indirect_dma_start(
                        out=yy[:, :], out_offset=None, in_=y_d[:, :], in_offset=IOA(ap=sloti[:, ti, kk, q:q + 1], axis=0)),
                        yy, reads=[sloti], writes=[yy])
                yield
                for (yy, kk) in ((y1, 0), (y2, 1)):
                    P.op(dve, lambda e, yy=yy, kk=kk: e.scalar_tensor_tensor(xx[:, :], yy[:, :], rinfo[:, ti, 2 + kk, q:q + 1], xx[:, :], ALU.mult, ALU.add),
                         reads=[yy, rinfo, xx], writes=[xx])
                yield
                P.op(act, lambda e: e.activation(out=jk[:, :], in_=xx[:, :], func=AF.Square, accum_out=ssC[:, 0:1]), reads=[xx], writes=[jk, ssC])
                P.op(act, lambda e: e.activation(out=pb_[:, :], in_=pp[:, :], func=AF.Copy), reads=[pp], writes=[pb_])
                yield
                P.op(dve, lambda e: e.tensor_scalar(ssC[:, 1:2], ssC[:, 0:1], 1.0 / D, EPS, ALU.mult, ALU.add), reads=[ssC], writes=[ssC])
                yield
                rsq(ssC, 1, 3, 2)
                P.op(act, lambda e: e.activation(out=xn_[:, :], in_=xx[:, :], func=AF.Copy, scale=ssC[:, 2:3]), reads=[xx, ssC], writes=[xn_])
                yield
                transposes(xn_, 8, ptr)
                P.op(dve, lambda e: e.tensor_copy(x3_[:, :, :], ptr[:, :].rearrange("p (c j) -> p c j", c=8)), reads=[ptr], writes=[x3_])
                transposes(pb_, 2, ptr2)
                P.op(act, lambda e: e.activation(out=pT_[:, :, :], in_=ptr2[:, 0:256].rearrange("p (c j) -> p c j", c=2), func=AF.Copy), reads=[ptr2], writes=[pT_])
                yield
                pes = []
                for h in range(2):
                    pg_ = pmm[h]

                    def fn(e, pg_=pg_, h=h):
                        ins = None
                        for k in range(8):
                            ins = e.matmul(pg_[:, :], x3_[:, k, :], wpg[:, k, h * 512:(h + 1) * 512], start=(k == 0), stop=(k == 7))
                        return ins
                    P.op(pe, fn, reads=[x3_, wpg], writes=[pg_])
                    P.op(act, lambda e, pg_=pg_, h=h: e.activation(out=tg_[:, h * 512:(h + 1) * 512], in_=pg_[:, :], func=AF.Tanh, scale=0.5), reads=[pg_], writes=[tg_])
                    pe_ = (pmm[2], pmm[3], ps1, ps2)[(2 * st + h) % 4]

                    def fn2(e, pe_=pe_, h=h):
                        ins = None
                        for k in range(2):
                            ins = e.matmul(pe_[:, :], pT_[:, k, :], wple[:, k, h * 512:(h + 1) * 512], start=(k == 0), stop=(k == 1))
                        return ins
                    P.op(pe, fn2, reads=[pT_, wple], writes=[pe_])
                    P.op(act, lambda e, pe_=pe_, h=h: e.activation(out=jk[:, 0:512], in_=pe_[:, :], func=AF.Square, accum_out=ssC[:, 4 + h:5 + h]), reads=[pe_], writes=[jk, ssC])
                    pes.append(pe_)
                yield
                P.op(dve, lambda e: e.tensor_tensor(ssC[:, 6:7], ssC[:, 4:5], ssC[:, 5:6], ALU.add), reads=[ssC], writes=[ssC])
                P.op(dve, lambda e: e.tensor_scalar(ssC[:, 7:8], ssC[:, 6:7], 4.0 / D, 4.0 * EPS, ALU.mult, ALU.add), reads=[ssC], writes=[ssC])
                yield
                rsq(ssC, 7, 9, 8)
                yield
                for h in range(2):
                    P.op(dve, lambda e, h=h, pe_=pes[h]: e.scalar_tensor_tensor(te_[:, h * 512:(h + 1) * 512], pe_[:, :], ssC[:, 8:9], gpp[:, h * 512:(h + 1) * 512], ALU.mult, ALU.mult),
                         reads=[pes[h], ssC, gpp], writes=[te_])
                P.op(dve, lambda e: e.scalar_tensor_tensor(te_[:, :], tg_[:, :], 1.0, te_[:, :], ALU.add, ALU.mult), reads=[tg_, te_], writes=[te_])
                yield
                P.op(pool, lambda e: e.tensor_tensor(xx[:, :], xx[:, :], te_[:, :], ALU.add), reads=[te_, xx], writes=[xx])
                yield
                P.op(act, lambda e: e.activation(out=jk[:, :], in_=xx[:, :], func=AF.Square, accum_out=ssC[:, 10:11]), reads=[xx], writes=[jk, ssC])
                yield
                P.op(dve, lambda e: e.tensor_scalar(ssC[:, 11:12], ssC[:, 10:11], 1.0 / D, EPS, ALU.mult, ALU.add), reads=[ssC], writes=[ssC])
                yield
                rsq(ssC, 11, 13, 12)
                yield
                P.op(dve, lambda e: e.scalar_tensor_tensor(ob_[:, :], xx[:, :], ssC[:, 12:13], gfin[:, :], ALU.mult, ALU.mult), reads=[xx, ssC, gfin], writes=[ob_])
                P.dma(sp, lambda e: e.dma_start(out=out_d[r0:r0 + 128, :], in_=ob_[:, :]), ob_, reads=[ob_])

            run_pipelined((subtile_gen(st) for st in range(T // 128)), depth=NP, skew=2)
            P.barrier()
            P.flush(block)
    return nc


def _chan(v):
    return np.ascontiguousarray(np.asarray(v, np.float32).reshape(4, 128).T)


def _gbc(g):
    return np.ascontiguousarray(np.asarray(g, np.float32).reshape(8, 128).T)


def prep_shared(inp):
    f = lambda a: np.ascontiguousarray(np.asarray(a, np.float32))
    cpar = np.zeros((128, NCP), np.float32)
    cw = f(inp["conv_dw_w"][0])
    for c in range(4):
        cpar[:, CW + c * 31:CW + (c + 1) * 31] = cw[:, c * 128:(c + 1) * 128].T
    cpar[:, CB:CB + 4] = _chan(inp["conv_dw_b"][0])
    cpar[:, LG:LG + 4] = _chan(inp["conv_ln_g"][0])
    cpar[:, LB:LB + 4] = _chan(inp["conv_ln_b"][0])
    lw = f(inp["lru_conv_w"][0])
    for c in range(4):
        cpar[:, LW + c * 4:LW + (c + 1) * 4] = lw[:, c * 128:(c + 1) * 128].T
    cpar[:, LBB:LBB + 4] = _chan(inp["lru_conv_b"][0])
    cpar[:, BR:BR + 4] = _chan(inp["lru_b_r"][0])
    cpar[:, BI:BI + 4] = _chan(inp["lru_b_i"][0])
    cpar[:, LAM:LAM + 4] = _chan(inp["lru_lambda"][0])
    wr_, wi_ = f(inp["lru_w_r"][0]), f(inp["lru_w_i"][0])
    gbd = np.zeros((4, 128, 256), np.float32)
    for c in range(4):
        for hh in range(2):
            gbd[c, hh * 64:(hh + 1) * 64, hh * 64:(hh + 1) * 64] = wr_[2 * c + hh]
            gbd[c, hh * 64:(hh + 1) * 64, 128 + hh * 64:128 + (hh + 1) * 64] = wi_[2 * c + hh]
    rb = np.concatenate([f(inp["b_group"][0]), f(inp["b_expert"][0])])
    shared = {
        "w_in": f(inp["w_in"][0]), "w_out": f(inp["w_out"][0]),
        "w_route": np.ascontiguousarray(np.concatenate([f(inp["w_group"][0]), f(inp["w_expert"][0])], axis=1)),
        "gate_bd": gbd, "cpar": cpar,
        "gbc": np.stack([_gbc(inp["g_mix"][0]), _gbc(inp["g_ffn"][0]), _gbc(inp["g_ple"][0])]),
        "rowbc": np.stack([np.ascontiguousarray(np.broadcast_to(f(inp["g_ple_proj"][0]), (128, D))),
                           np.ascontiguousarray(np.broadcast_to(f(inp["g_final"]), (128, D)))]),
        "rbias": np.ascontiguousarray(np.broadcast_to(np.tile(rb, Q), (128, Q * 36))),
        "iota_e": np.ascontiguousarray(np.broadcast_to(np.tile(np.arange(32, dtype=np.float32), Q), (128, Q * 32))),
        "ident": np.eye(128, dtype=np.float32),
        "tri": np.ascontiguousarray(np.triu(np.ones((128, 128), np.float32), 1)),
        "w1": f(inp["w1"][0]), "w3": f(inp["w3"][0]), "w2": f(inp["w2"][0]),
        "w_ple": f(inp["w_ple"][0]), "w_ple_gate": f(inp["w_ple_gate"][0]),
    }
    return shared


def kernel(**inputs):
    NSEQ, CAP = 4, 1024
    x = np.asarray(inputs["x"], np.float32)
    p = np.asarray(inputs["p"], np.float32)[0]
    shared = prep_shared(inputs)
    nc = build(NSEQ, CAP)
    in_maps = []
    for i in range(N_CORES):
        m = dict(shared)
        m["x"] = np.ascontiguousarray(x[i * NSEQ:(i + 1) * NSEQ].reshape(NSEQ * SEQ, D))
        m["p"] = np.ascontiguousarray(p[i * NSEQ:(i + 1) * NSEQ].reshape(NSEQ * SEQ, 256))
        in_maps.append(m)
    res = run_bass_kernel_spmd(nc, in_maps, core_ids=list(range(N_CORES)))
    out = np.concatenate([np.asarray(r["out"], np.float32).reshape(NSEQ, SEQ, D) for r in res.results], axis=0)
    return out
```

```python
import numpy as np
from contextlib import ExitStack
import concourse.bass as bass
import concourse.mybir as mybir
from concourse.bass_utils import run_bass_kernel_spmd

F32 = mybir.dt.float32
BF16 = mybir.dt.bfloat16
I32 = mybir.dt.int32
ALU = mybir.AluOpType
AF = mybir.ActivationFunctionType
AX = mybir.AxisListType

N_CORES = 8
HOP = True
NP_C = 8
LOADLAG = 3
MC_OFF = 0
SEQ = 2048
D = 1024
EPS = 1e-6
Q = 4

CW = 0
CB = CW + 124
LG = CB + 4
LB = LG + 4
LW = LB + 4
LBB = LW + 16
BR = LBB + 4
BI = BR + 4
LAM = BI + 4
NCP = LAM + 4
D_CWH = 0
D_GH = 124
D_BH = 128
D_BRH = 132
D_BIH = 136
D_N4 = 140
D_N8 = 144
NDP = 148


class Src:
    def __init__(self, sem, name, is_dma):
        self.sem, self.name, self.is_dma, self.total = sem, name, is_dma, 0


class Eng(Src):
    def __init__(self, sem, name, blockname, same_wait=True):
        super().__init__(sem, name, False)
        self.blockname, self.ops, self.seen, self.same_wait = blockname, [], {}, same_wait


class Buf:
    def __init__(self, t, dsem=None):
        self.t, self.w, self.r, self.dsem = t, None, {}, dsem

    def __getitem__(self, k):
        return self.t[k]


class Prog:
    def __init__(self, nc, stack):
        self.nc, self.stack = nc, stack
        self.srcs = []
        mk = lambda n, b, sw=True: self._reg(Eng(self._sem("e_" + n), n, b, sw))
        self.pe = mk("pe", "tensor", False)
        self.act = mk("act", "scalar")
        self.dve = mk("dve", "vector")
        self.pool = mk("pool", "gpsimd")
        self.sp = mk("sp", "sync")
        self.engs = [self.pe, self.act, self.dve, self.pool, self.sp]
        self.nbuf = 0

    def _sem(self, name):
        return self.stack.enter_context(self.nc.semaphore(name))

    def _reg(self, s):
        self.srcs.append(s)
        return s

    def buf(self, stack, shape, dt, name=None, dma=False, psum=False):
        self.nbuf += 1
        name = "%s_%d" % (name or "b", self.nbuf)
        if psum:
            t = stack.enter_context(self.nc.psum_tensor(name, shape, dt))
        else:
            t = stack.enter_context(self.nc.sbuf_tensor(name, shape, dt))
        ds = self._reg(Src(self._sem("d_" + name), name, True)) if dma else None
        return Buf(t, ds)

    def _deps(self, eng, reads, writes):
        need = {}

        def add(src, val):
            if src.is_dma:
                val = src.total
            if need.get(src, 0) < val:
                need[src] = val

        for b in reads:
            if b.w is not None:
                add(*b.w)
        for b in writes:
            if b.w is not None:
                add(*b.w)
            for s, v in b.r.items():
                add(s, v)
        waits = []
        for src, val in need.items():
            if src is eng and not eng.same_wait:
                continue
            if eng.seen.get(src, 0) >= val:
                continue
            eng.seen[src] = val
            waits.append((src.sem, val))
        return waits

    def _mark(self, src, val, reads, writes):
        for b in reads:
            b.r[src] = val
        for b in writes:
            b.w = (src, val)
            b.r = {}

    def op(self, eng, fn, reads=(), writes=()):
        waits = self._deps(eng, reads, writes)
        eng.total += 1
        sem = eng.sem

        def emit(e):
            for s, v in waits:
                e.wait_ge(s, v)
            fn(e).then_inc(sem, 1)

        eng.ops.append(emit)
        self._mark(eng, eng.total, reads, writes)

    def dma(self, q, fn, sem_buf, reads=(), writes=()):
        src = sem_buf.dsem
        waits = self._deps(q, reads, writes)
        src.total += 16
        sem = src.sem

        def emit(e):
            for s, v in waits:
                e.wait_ge(s, v)
            fn(e).then_inc(sem, 16)

        q.ops.append(emit)
        self._mark(src, src.total, reads, writes)

    def barrier(self):
        for E in self.engs:
            waits = []
            for S in self.srcs:
                if S is E or S.total == 0:
                    continue
                if E.seen.get(S, 0) >= S.total:
                    continue
                E.seen[S] = S.total
                waits.append((S.sem, S.total))

            def emit(e, waits=waits):
                for s, v in waits:
                    e.wait_ge(s, v)

            E.ops.append(emit)

    def flush(self, block):
        for E in self.engs:
            if not E.ops:
                continue
            ops = E.ops
            E.ops = []

            def body(e, ops=ops):
                for f in ops:
                    f(e)

            getattr(block, E.blockname)(body)


def run_pipelined(gens, depth, skew):
    it = iter(gens)
    active, pending, tick = [], True, 0
    while pending or active:
        for g in list(active):
            try:
                next(g)
            except StopIteration:
                active.remove(g)
        if pending and tick % skew == 0 and len(active) < depth:
            try:
                g = next(it)
                active.append(g)
                next(g)
            except StopIteration:
                pending = False
        tick += 1


def build(NSEQ=4, CAP=640, debug=False):
    T = NSEQ * SEQ
    NT = T // 512
    NSUB = CAP // 128
    NSLOT = 32 * CAP
    TRASH = NSLOT
    nc = bass.Bass("TRN2", target_bir_lowering=False)

    def dr(name, shape, dt=F32, kind="ExternalInput"):
        return nc.dram_tensor(name, shape, dt, kind=kind).ap()

    x_d = dr("x", [T, D])
    p_d = dr("p", [T, 256])
    win_d = dr("w_in", [D, 2048])
    wout_d = dr("w_out", [D, D])
    wr_d = dr("w_route", [D, 36])
    gbd_d = dr("gate_bd", [4, 128, 256])
    cpar_d = dr("cpar", [128, NCP])
    gbc_d = dr("gbc", [3, 128, 8])
    rowbc_d = dr("rowbc", [2, 128, D])
    rb_d = dr("rbias", [128, Q * 36])
    iota_d = dr("iota_e", [128, Q * 32])
    ident_d = dr("ident", [128, 128])
    tri_d = dr("tri", [128, 128])
    w1_d = dr("w1", [32, D, 512])
    w3_d = dr("w3", [32, D, 512])
    w2_d = dr("w2", [32, 512, D])
    wple_d = dr("w_ple", [256, D])
    wpg_d = dr("w_ple_gate", [D, D])
    out_d = dr("out", [T, D], kind="ExternalOutput")
    sk = "ExternalOutput" if debug else "Internal"
    x1_d = dr("x1s", [T, D], kind=sk)
    hs_d = dr("hss", [NSLOT + 128, D], BF16, kind=sk)
    y_d = dr("yss", [NSLOT + 128, D], kind=sk)
    if debug:
        ri_d = dr("rinfo_o", [128, NT * 4 * Q], kind="ExternalOutput")

    IOA = bass.IndirectOffsetOnAxis

    with ExitStack() as top:
        P = Prog(nc, top)
        pe, act, dve, pool, sp = P.pe, P.act, P.dve, P.pool, P.sp
        block = top.enter_context(nc.Block())

        pmm = [P.buf(top, [128, 512], F32, "pmm", psum=True) for _ in range(4)]
        ptr = P.buf(top, [128, 1024], BF16, "ptr", psum=True)
        ptr2 = P.buf(top, [128, 1024], BF16, "ptr2", psum=True)
        ps1 = P.buf(top, [128, 512], F32, "ps1", psum=True)
        ps2 = P.buf(top, [128, 512], F32, "ps2", psum=True)
        pmm_i = [0]

        def next_pmm():
            b = pmm[pmm_i[0] % 4]
            pmm_i[0] += 1
            return b

        rinfo = P.buf(top, [128, NT, 4, Q], F32, "rinfo")
        sloti = P.buf(top, [128, NT, 2, Q], I32, "sloti")
        if debug:
            rinfo.dsem = P._reg(Src(P._sem("d_rinfo"), "rinfo", True))
        ident = P.buf(top, [128, 128], BF16, "ident", dma=True)
        P.dma(pool, lambda e: e.dma_start(out=ident[:, :], in_=ident_d), ident, writes=[ident])

        def transposes(src, n, dst_ps):
            def fn(e):
                ins = None
                for c in range(n):
                    ins = e.transpose(dst_ps[:, c * 128:(c + 1) * 128], src[:, c * 128:(c + 1) * 128], ident[:, :])
                return ins
            P.op(pe, fn, reads=[src, ident], writes=[dst_ps])

        def rstd_from_ss(ss, out, n):
            P.op(dve, lambda e: e.tensor_scalar(out, ss, 1.0 / n, EPS, ALU.mult, ALU.add), reads=[], writes=[])

        with ExitStack() as sa:
            B = lambda shape, dt=F32, name=None, dma=False: P.buf(sa, shape, dt, name, dma)
            win = B([128, 8, 2048], BF16, "win", dma=True)
            wout = B([128, 8, D], BF16, "wout", dma=True)
            wr = B([128, 8, 36], BF16, "wr", dma=True)
            gbd = B([128, 4, 256], BF16, "gbd", dma=True)
            tri = B([128, 128], BF16, "tri", dma=True)
            cpar = B([128, NCP], F32, "cpar", dma=True)
            dpar = B([128, NDP], F32, "dpar")
            gmixbc = B([128, 8], F32, "gmixbc", dma=True)
            gffnbc = B([128, 8], F32, "gffnbc", dma=True)
            rbias = B([128, Q, 36], F32, "rbias", dma=True)
            iota = B([128, Q, 32], F32, "iota", dma=True)
            ones = B([128, 128], BF16, "ones")
            identF = B([128, 128], F32, "identF", dma=True)
            P.dma(sp, lambda e: e.dma_start(out=identF[:, :], in_=ident_d), identF, writes=[identF])
            cntbc = B([128, 32], F32, "cntbc")

            P.dma(sp, lambda e: e.dma_start(out=cpar[:, :], in_=cpar_d), cpar, writes=[cpar])
            P.dma(sp, lambda e: e.dma_start(out=gmixbc[:, :], in_=gbc_d[0]), gmixbc, writes=[gmixbc])
            P.dma(sp, lambda e: e.dma_start(out=gffnbc[:, :], in_=gbc_d[1]), gffnbc, writes=[gffnbc])
            P.dma(sp, lambda e: e.dma_start(out=rbias[:, :, :], in_=rb_d.rearrange("p (q n) -> p q n", q=Q)), rbias, writes=[rbias])
            P.dma(sp, lambda e: e.dma_start(out=iota[:, :, :], in_=iota_d.rearrange("p (q n) -> p q n", q=Q)), iota, writes=[iota])
            P.dma(pool, lambda e: e.dma_start(out=tri[:, :], in_=tri_d), tri, writes=[tri])
            for k in range(8):
                P.dma(pool, lambda e, k=k: e.dma_start(out=win[:, k, :], in_=win_d[k * 128:(k + 1) * 128, :]), win, writes=[win])
            P.dma(pool, lambda e: e.dma_start(out=gbd[:, :, :], in_=gbd_d.rearrange("c p n -> p c n")), gbd, writes=[gbd])
            for k in range(8):
                P.dma(pool, lambda e, k=k: e.dma_start(out=wout[:, k, :], in_=wout_d[k * 128:(k + 1) * 128, :]), wout, writes=[wout])
            P.dma(pool, lambda e: e.dma_start(out=wr[:, :, :], in_=wr_d.rearrange("(k p) n -> p k n", p=128)), wr, writes=[wr])

            P.op(dve, lambda e: e.memset(ones[:, :], 1.0), writes=[ones])
            P.op(dve, lambda e: e.memset(cntbc[:, :], 0.0), writes=[cntbc])
            for k in range(8):
                P.op(dve, lambda e, k=k: e.tensor_scalar(win[:, k, :], win[:, k, :], gmixbc[:, k:k + 1], None, ALU.mult), reads=[win, gmixbc], writes=[win])
                P.op(dve, lambda e, k=k: e.tensor_scalar(wr[:, k, :], wr[:, k, :], gffnbc[:, k:k + 1], None, ALU.mult), reads=[wr, gffnbc], writes=[wr])

            tsm = B([128, 8, 4], F32, "tsm")

            def dv(fn, reads, writes):
                P.op(dve, fn, reads=reads, writes=writes)

            dv(lambda e: e.tensor_scalar(dpar[:, D_CWH:D_CWH + 124], cpar[:, CW:CW + 124], 0.5, None, ALU.mult), [cpar], [dpar])
            dv(lambda e: e.tensor_scalar(dpar[:, D_GH:D_GH + 8], cpar[:, LG:LG + 8], 0.5, None, ALU.mult), [cpar], [dpar])
            dv(lambda e: e.tensor_scalar(dpar[:, D_BRH:D_BRH + 8], cpar[:, BR:BR + 8], 0.5, None, ALU.mult), [cpar], [dpar])
            z_, az, ee, LL, tt, mk_, zp = [tsm[:, i, :] for i in range(7)]
            dv(lambda e: e.tensor_scalar(z_, cpar[:, LAM:LAM + 4], -1.0, None, ALU.mult), [cpar], [tsm])
            dv(lambda e: e.tensor_tensor(az, z_, cpar[:, LAM:LAM + 4], ALU.max), [tsm, cpar], [tsm])
            P.op(act, lambda e: e.activation(out=ee, in_=az, func=AF.Exp, scale=-1.0), reads=[tsm], writes=[tsm])
            P.op(act, lambda e: e.activation(out=LL, in_=ee, func=AF.Ln, bias=1.0, scale=1.0), reads=[tsm], writes=[tsm])
            dv(lambda e: e.tensor_scalar(tt, ee, -0.25, 1.0 / 3.0, ALU.mult, ALU.add), [tsm], [tsm])
            dv(lambda e: e.tensor_tensor(tt, tt, ee, ALU.mult), [tsm], [tsm])
            dv(lambda e: e.tensor_scalar(tt, tt, -1.0, 0.5, ALU.mult, ALU.add), [tsm], [tsm])
            dv(lambda e: e.tensor_tensor(tt, tt, ee, ALU.mult), [tsm], [tsm])
            dv(lambda e: e.tensor_scalar(tt, tt, -1.0, 1.0, ALU.mult, ALU.add), [tsm], [tsm])
            dv(lambda e: e.tensor_tensor(tt, tt, ee, ALU.mult), [tsm], [tsm])
            dv(lambda e: e.tensor_single_scalar(mk_, ee, 0.05, ALU.is_lt), [tsm], [tsm])
            dv(lambda e: e.tensor_tensor(tt, tt, LL, ALU.subtract), [tsm], [tsm])
            dv(lambda e: e.tensor_tensor(tt, tt, mk_, ALU.mult), [tsm], [tsm])
            dv(lambda e: e.tensor_tensor(tt, tt, LL, ALU.add), [tsm], [tsm])
            dv(lambda e: e.tensor_single_scalar(zp, z_, 0.0, ALU.max), [tsm], [tsm])
            dv(lambda e: e.tensor_tensor(tt, tt, zp, ALU.add), [tsm], [tsm])
            dv(lambda e: e.tensor_scalar(dpar[:, D_N4:D_N4 + 4], tt, -4.0, None, ALU.mult), [tsm], [dpar])
            dv(lambda e: e.tensor_scalar(dpar[:, D_N8:D_N8 + 4], tt, -8.0, None, ALU.mult), [tsm], [dpar])

            xa = [B([128, D], F32, "xa", dma=True) for _ in range(2)]
            ssF = [B([128, 8], F32, "ssF") for _ in range(2)]
            xn = [B([128, D], BF16, "xn") for _ in range(2)]
            hT = B([128, 8, 512], BF16, "hT")
            gth = [B([128, 512], F32, "gth") for _ in range(2)]
            gvs = [B([128, 512], F32, "gvs") for _ in range(2)]
            ga = B([128, 512], F32, "ga")
            gb = B([128, 512], F32, "gb")
            ub = [[B([128, 542], BF16, "ub") for _ in range(4)] for _ in range(2)]
            xbuf = [[B([128, 515], BF16, "xbuf") for _ in range(4)] for _ in range(2)]
            qg = [[B([128, 512], BF16, "qg") for _ in range(4)] for _ in range(2)]
            acc2 = [[B([128, 512], F32, "acc") for _ in range(4)] for _ in range(2)]
            cvbf = [B([128, 512], BF16, "cvbf") for _ in range(4)]
            sqbf = [B([128, 512], BF16, "sqbf") for _ in range(4)]
            mean = B([128, 512], F32, "mean")
            msq = B([128, 512], F32, "msq")
            xr = [B([128, 512], F32, "xr") for _ in range(2)]
            xrbf = [B([128, 512], BF16, "xrbf") for _ in range(2)]
            t1 = [B([128, 512], F32, "t1") for _ in range(2)]
            t2 = [B([128, 512], F32, "t2") for _ in range(2)]
            t3 = [B([128, 512], F32, "t3") for _ in range(2)]
            lh, th = t1, t2
            ab = [B([128, 512], F32, "ab")] * 2
            hb = [B([128, 512], F32, "hb")] * 2
            carry = [B([128, 1], F32, "carry") for _ in range(4)]
            yT = [B([128, 8, 512], BF16, "yT") for _ in range(2)]
            xb = B([128, D], F32, "xb", dma=True)
            ssE = [B([128, 8], F32, "ssE") for _ in range(2)]
            x1 = [B([128, D], F32, "x1", dma=True) for _ in range(2)]
            hfn = [B([128, D], BF16, "hfn", dma=True) for _ in range(4)]
            hfT = [B([128, 8, 128], BF16, "hfT") for _ in range(2)]
            rs = B([128, 40, Q], F32, "rs")
            r36 = B([128, Q, 36], F32, "r36")
            r32 = [B([128, Q, 32], F32, "r32") for _ in range(5)]
            mbf = B([128, Q, 32], BF16, "mbf")
            r8 = [B([128, Q, 8], F32, "r8") for _ in range(4)]
            r4 = [B([128, Q, 4], F32, "r4") for _ in range(3)]

            cwh = lambda c, k: dpar[:, D_CWH + c * 31 + k:D_CWH + c * 31 + k + 1]
            NJ = SEQ // 512

            def rsqA(ssb, i_v, i_l, i_o):
                P.op(act, lambda e: e.activation(out=ssb[:, i_l:i_l + 1], in_=ssb[:, i_v:i_v + 1], func=AF.Ln), reads=[ssb], writes=[ssb])
                P.op(act, lambda e: e.activation(out=ssb[:, i_o:i_o + 1], in_=ssb[:, i_l:i_l + 1], func=AF.Exp, scale=-0.5), reads=[ssb], writes=[ssb])

            def evac_scaled(dst3, ps, gvec):
                P.op(act, lambda e: e.activation(out=dst3.all(), in_=ps[:, :].rearrange("p (c j) -> p c j", c=8), func=AF.Copy),
                     reads=[ps], writes=[dst3.buf])

            class View3:
                def __init__(self, buf, fn, allfn=None):
                    self.buf, self.fn, self.all = buf, fn, allfn

                def __call__(self, c):
                    return self.fn(c)

            def gen_F(ti):
                s_, j = divmod(ti, NJ)
                row0 = ti * 512
                par = ti % 2
                ub_, xbuf_, qg_ = ub[par], xbuf[par], qg[par]
                if j == 0:
                    for c in range(4):
                        P.op(pool, lambda e, c=c: e.memset(ub_[c][:, 0:30], 0.0), writes=[ub_[c]])
                        P.op(pool, lambda e, c=c: e.memset(xbuf_[c][:, 0:3], 0.0), writes=[xbuf_[c]])
                for q in range(Q):
                    yield ("SEG" if q % 2 == 0 else "CHAIN")
                    xt, xnb, ss = xa[q % 2], xn[q % 2], ssF[q % 2]
                    r0 = row0 + q * 128
                    P.dma(sp, lambda e, xt=xt, r0=r0: e.dma_start(out=xt[:, :], in_=x_d[r0:r0 + 128, :]), xt, writes=[xt])
                    P.op(act, lambda e, xt=xt, ss=ss, xnb=xnb: e.activation(out=xnb[:, :], in_=xt[:, :], func=AF.Square, accum_out=ss[:, 0:1]),
                         reads=[xt], writes=[xnb, ss])
                    P.op(dve, lambda e, ss=ss: e.tensor_scalar(ss[:, 1:2], ss[:, 0:1], 1.0 / D, EPS, ALU.mult, ALU.add), reads=[ss], writes=[ss])
                    rsqA(ss, 1, 3, 2)
                    P.op(act, lambda e, xt=xt, xnb=xnb, ss=ss: e.activation(out=xnb[:, :], in_=xt[:, :], func=AF.Copy, scale=ss[:, 2:3]),
                         reads=[xt, ss], writes=[xnb])
                    transposes(xnb, 8, ptr)
                    evac_scaled(View3(hT, None, lambda q=q: hT[:, :, q * 128:(q + 1) * 128]), ptr, gmixbc)

                fbank = [pmm[0], ps2]

                def zmm(m, bi):
                    pb = fbank[bi % 2]

                    def fn(e, m=m, pb=pb):
                        ins = None
                        for k in range(8):
                            ins = e.matmul(pb[:, :], win[:, k, m * 128:(m + 1) * 128], hT[:, k, :], start=(k == 0), stop=(k == 7))
                        return ins
                    P.op(pe, fn, reads=[win, hT], writes=[pb])
                    return pb

                for c in range(4):
                    yield ("SEG" if c % 2 == 0 else "CHAIN")
                    pg = zmm(4 + c, c)
                    ta, vs = gth[c % 2], gvs[c % 2]
                    P.op(act, lambda e, pg=pg, ta=ta: e.activation(out=ta[:, :], in_=pg[:, :], func=AF.Tanh, scale=0.5), reads=[pg], writes=[ta])
                    pv = zmm(c, c)
                    P.op(act, lambda e, pv=pv, vs=vs: e.activation(out=vs[:, :], in_=pv[:, :], func=AF.Copy), reads=[pv], writes=[vs])
                    P.op(pool, lambda e, ta=ta, vs=vs: e.tensor_tensor(ta[:, :], ta[:, :], vs[:, :], ALU.mult), reads=[ta, vs], writes=[ta])
                    P.op(pool, lambda e, ta=ta, vs=vs, c=c: e.tensor_tensor(ub_[c][:, 30:542], ta[:, :], vs[:, :], ALU.add), reads=[ta, vs], writes=[ub_[c]])
                for c in range(4):
                    yield ("SEG" if c % 2 == 0 else "CHAIN")
                    px = zmm(8 + c, c)
                    P.op(act, lambda e, px=px, c=c: e.activation(out=xbuf_[c][:, 3:515], in_=px[:, :], func=AF.Copy), reads=[px], writes=[xbuf_[c]])
                for c in range(4):
                    yield "SEG"
                    pgl = zmm(12 + c, c)
                    P.op(act, lambda e, pgl=pgl, c=c: e.activation(out=qg_[c][:, :], in_=pgl[:, :], func=AF.Gelu_apprx_tanh), reads=[pgl], writes=[qg_[c]])

            def gen_Mc(ti):
                s_, j = divmod(ti, NJ)
                par = ti % 2
                ub_, xbuf_, qg_, yT_ = ub[par], xbuf[par], qg[par], yT[par]
                ubn, xbufn = ub[1 - par], xbuf[1 - par]
                acc = acc2[par]
                for k in range(31):
                    for c in range(4):
                        if k == 0:
                            P.op(dve, lambda e, c=c: e.tensor_scalar(acc[c][:, :], ub_[c][:, 0:512], cwh(c, 0), cpar[:, CB + c:CB + c + 1], ALU.mult, ALU.add),
                                 reads=[ub_[c], dpar, cpar], writes=[acc[c]])
                        else:
                            P.op(dve, lambda e, c=c, k=k: e.scalar_tensor_tensor(acc[c][:, :], ub_[c][:, k:k + 512], cwh(c, k), acc[c][:, :], ALU.mult, ALU.add),
                                 reads=[ub_[c], dpar, acc[c]], writes=[acc[c]])
                    yield
                for c in range(4):
                    if j < NJ - 1:
                        P.op(pool, lambda e, c=c: e.tensor_copy(ubn[c][:, 0:30], ub_[c][:, 512:542]), reads=[ub_[c]], writes=[ubn[c]])
                    P.op(act, lambda e, c=c: e.activation(out=cvbf[c][:, :], in_=acc[c][:, :], func=AF.Copy), reads=[acc[c]], writes=[cvbf[c]])
                    P.op(act, lambda e, c=c: e.activation(out=sqbf[c][:, :], in_=acc[c][:, :], func=AF.Square), reads=[acc[c]], writes=[sqbf[c]])

                def stat_mm(dst, srcs):
                    def fn(e):
                        ins = None
                        for c in range(4):
                            ins = e.matmul(dst[:, :], ones[:, :], srcs[c][:, :], start=(c == 0), stop=(c == 3))
                        return ins
                    P.op(pe, fn, reads=[ones] + srcs, writes=[dst])
                stat_mm(ps1, cvbf)
                P.op(act, lambda e: e.activation(out=mean[:, :], in_=ps1[:, :], func=AF.Copy, scale=1.0 / 512), reads=[ps1], writes=[mean])
                stat_mm(ps1, sqbf)
                P.op(pool, lambda e: e.tensor_tensor(msq[:, :], mean[:, :], mean[:, :], ALU.mult), reads=[mean], writes=[msq])
                P.op(dve, lambda e: e.scalar_tensor_tensor(msq[:, :], ps1[:, :], 1.0 / 512, msq[:, :], ALU.mult, ALU.subtract), reads=[ps1, msq], writes=[msq])
                P.op(dve, lambda e: e.tensor_scalar(msq[:, :], msq[:, :], EPS, None, ALU.add), reads=[msq], writes=[msq])
                P.op(act, lambda e: e.activation(out=msq[:, :], in_=msq[:, :], func=AF.Ln), reads=[msq], writes=[msq])
                P.op(act, lambda e: e.activation(out=msq[:, :], in_=msq[:, :], func=AF.Exp, scale=-0.5), reads=[msq], writes=[msq])
                yield
                for c in range(4):
                    P.op(pool, lambda e, c=c: e.tensor_tensor(acc[c][:, :], acc[c][:, :], mean[:, :], ALU.subtract), reads=[acc[c], mean], writes=[acc[c]])
                    P.op(pool, lambda e, c=c: e.tensor_tensor(acc[c][:, :], acc[c][:, :], msq[:, :], ALU.mult), reads=[acc[c], msq], writes=[acc[c]])
                for c in range(4):
                    t_ = (mean, msq)[c % 2]
                    P.op(act, lambda e, c=c: e.activation(out=acc[c][:, :], in_=acc[c][:, :], func=AF.Identity,
                                                          bias=dpar[:, D_BH + c:D_BH + c + 1], scale=dpar[:, D_GH + c:D_GH + c + 1]),
                         reads=[acc[c], dpar], writes=[acc[c]])
                    P.op(act, lambda e, c=c, t_=t_: e.activation(out=t_[:, :], in_=acc[c][:, :], func=AF.Tanh), reads=[acc[c]], writes=[t_])
                    P.op(dve, lambda e, c=c, t_=t_: e.scalar_tensor_tensor(yT_[:, c, :], t_[:, :], 1.0, acc[c][:, :], ALU.add, ALU.mult),
                         reads=[acc[c], t_], writes=[yT_])
            def gen_Ml(ti):
                s_, j = divmod(ti, NJ)
                par = ti % 2
                xbuf_, qg_, yT_ = xbuf[par], qg[par], yT[par]
                xbufn = xbuf[1 - par]
                if j == 0:
                    for c in range(4):
                        P.op(pool, lambda e, c=c: e.memset(carry[c][:, :], 0.0), writes=[carry[c]])
                for c in range(4):
                    xr_, xrb_ = xr[c % 2], xrbf[c % 2]
                    lw = lambda k, c=c: cpar[:, LW + c * 4 + k:LW + c * 4 + k + 1]
                    P.op(dve, lambda e, c=c, xr_=xr_, lw=lw: e.tensor_scalar(xr_[:, :], xbuf_[c][:, 0:512], lw(0), cpar[:, LBB + c:LBB + c + 1], ALU.mult, ALU.add),
                         reads=[xbuf_[c], cpar], writes=[xr_])
                    for k in range(1, 4):
                        P.op(dve, lambda e, c=c, k=k, xr_=xr_, lw=lw: e.scalar_tensor_tensor(xr_[:, :], xbuf_[c][:, k:k + 512], lw(k), xr_[:, :], ALU.mult, ALU.add),
                             reads=[xbuf_[c], cpar, xr_], writes=[xr_])
                    if j < NJ - 1:
                        P.op(pool, lambda e, c=c: e.tensor_copy(xbufn[c][:, 0:3], xbuf_[c][:, 512:515]), reads=[xbuf_[c]], writes=[xbufn[c]])
                    P.op(act, lambda e, xr_=xr_, xrb_=xrb_: e.activation(out=xrb_[:, :], in_=xr_[:, :], func=AF.Copy), reads=[xr_], writes=[xrb_])
                    pr, pi = pmm[1], pmm[1]
                    P.op(pe, lambda e, c=c, pr=pr, xrb_=xrb_: e.matmul(pr[:, :], gbd[:, c, 0:128], xrb_[:, :], start=True, stop=True), reads=[gbd, xrb_], writes=[pr])
                    a1, a2, a3, aa, hh = t1[c % 2], t2[c % 2], t3[c % 2], ab[c % 2], hb[c % 2]
                    dp = lambda o, c=c: dpar[:, o + c:o + c + 1]
                    yield
                    P.op(act, lambda e, pr=pr, a1=a1, dp=dp: e.activation(out=a1[:, :], in_=pr[:, :], func=AF.Tanh, bias=dp(D_BRH), scale=0.5), reads=[pr, dpar], writes=[a1])
                    P.op(pe, lambda e, c=c, pi=pi, xrb_=xrb_: e.matmul(pi[:, :], gbd[:, c, 128:256], xrb_[:, :], start=True, stop=True), reads=[gbd, xrb_], writes=[pi])
                    P.op(act, lambda e, pi=pi, a3=a3, dp=dp: e.activation(out=a3[:, :], in_=pi[:, :], func=AF.Tanh, bias=dp(D_BIH), scale=0.5), reads=[pi, dpar], writes=[a3])
                    P.op(act, lambda e, a1=a1, aa=aa, dp=dp: e.activation(out=aa[:, :], in_=a1[:, :], func=AF.Exp, bias=dp(D_N4), scale=dp(D_N4)), reads=[a1, dpar], writes=[aa])
                    P.op(act, lambda e, a1=a1, a2=a2, dp=dp: e.activation(out=a2[:, :], in_=a1[:, :], func=AF.Exp, bias=dp(D_N8), scale=dp(D_N8)), reads=[a1, dpar], writes=[a2])
                    P.op(dve, lambda e, a2=a2: e.tensor_scalar(a2[:, :], a2[:, :], 0.99999994, -1.0, ALU.min, ALU.mult), reads=[a2], writes=[a2])
                    P.op(act, lambda e, a2=a2: e.activation(out=a2[:, :], in_=a2[:, :], func=AF.Ln, bias=1.0, scale=1.0), reads=[a2], writes=[a2])
                    P.op(act, lambda e, a2=a2: e.activation(out=a2[:, :], in_=a2[:, :], func=AF.Exp, scale=0.5), reads=[a2], writes=[a2])
                    P.op(dve, lambda e, a3=a3, xr_=xr_: e.scalar_tensor_tensor(a3[:, :], a3[:, :], 1.0, xr_[:, :], ALU.add, ALU.mult), reads=[a3, xr_], writes=[a3])
                    yield
                    P.op(dve, lambda e, a2=a2, a3=a3: e.tensor_tensor(a3[:, :], a3[:, :], a2[:, :], ALU.mult), reads=[a2, a3], writes=[a3])
                    P.op(dve, lambda e, c=c, aa=aa, a3=a3, hh=hh: e.tensor_tensor_scan(hh[:, :], aa[:, :], a3[:, :], carry[c][:, 0:1], ALU.mult, ALU.add),
                         reads=[aa, a3, carry[c]], writes=[hh])
                    P.op(dve, lambda e, c=c, hh=hh: e.tensor_copy(carry[c][:, :], hh[:, 511:512]), reads=[hh], writes=[carry[c]])
                    P.op(dve, lambda e, c=c, hh=hh: e.scalar_tensor_tensor(yT_[:, 4 + c, :], hh[:, :], 0.5, qg_[c][:, :], ALU.mult, ALU.mult),
                         reads=[hh, qg_[c]], writes=[yT_])
                    yield

            def gen_E(ti):
                row0 = ti * 512
                yT_ = yT[ti % 2]
                for q in range(Q):
                    yield ("SEG" if q % 2 == 0 else "CHAIN")
                    r0 = row0 + q * 128
                    x1t, ss = x1[q % 2], ssE[q % 2]
                    hf = hfn[q]
                    hft = hfT[q % 2]
                    P.dma(sp, lambda e, r0=r0: e.dma_start(out=xb[:, :], in_=x_d[r0:r0 + 128, :]), xb, writes=[xb])
                    for h in range(2):
                        pb = pmm[2]

                        def fn(e, pb=pb, h=h, q=q):
                            ins = e.matmul(pb[:, :], identF[:, :], xb[:, h * 512:(h + 1) * 512], start=True, stop=False)
                            for k in range(8):
                                ins = e.matmul(pb[:, :], yT_[:, k, q * 128:(q + 1) * 128], wout[:, k, h * 512:(h + 1) * 512], start=False, stop=(k == 7))
                            return ins
                        P.op(pe, fn, reads=[yT_, wout, identF, xb], writes=[pb])
                        P.op(act, lambda e, pb=pb, h=h, x1t=x1t: e.activation(out=x1t[:, h * 512:(h + 1) * 512], in_=pb[:, :], func=AF.Copy),
                             reads=[pb], writes=[x1t])
                    P.dma(sp, lambda e, x1t=x1t, r0=r0: e.dma_start(out=x1_d[r0:r0 + 128, :], in_=x1t[:, :]), x1t, reads=[x1t])
                    P.op(act, lambda e, x1t=x1t, ss=ss, hf=hf: e.activation(out=hf[:, :], in_=x1t[:, :], func=AF.Square, accum_out=ss[:, 0:1]), reads=[x1t], writes=[hf, ss])
                    P.op(dve, lambda e, ss=ss: e.tensor_scalar(ss[:, 1:2], ss[:, 0:1], 1.0 / D, EPS, ALU.mult, ALU.add), reads=[ss], writes=[ss])
                    rsqA(ss, 1, 3, 2)
                    P.op(act, lambda e, x1t=x1t, hf=hf, ss=ss: e.activation(out=hf[:, :], in_=x1t[:, :], func=AF.Copy, scale=ss[:, 2:3]), reads=[x1t, ss], writes=[hf])
                    transposes(hf, 8, ptr2)
                    evac_scaled(View3(hft, None, lambda hft=hft: hft[:, :, :]), ptr2, gffnbc)

                    def fnl(e, hft=hft, q=q):
                        ins = None
                        for k in range(8):
                            ins = e.matmul(pmm[3][:, q * 36:(q + 1) * 36], hft[:, k, :], wr[:, k, :], start=(k == 0), stop=(k == 7))
                        return ins
                    P.op(pe, fnl, reads=[hft, wr], writes=[pmm[3]])

                yield "SEG"
                S = lambda i: rs[:, i, :]
                bc = lambda ap, n: ap.unsqueeze(2).broadcast_to([128, Q, n])
                lgb = r36
                P.op(dve, lambda e: e.tensor_tensor(lgb[:, :, :], pmm[3][:, 0:Q * 36].rearrange("p (q n) -> p q n", q=Q), rbias[:, :, :], ALU.add),
                     reads=[pmm[3], rbias], writes=[lgb])
                gmask, gsh, gex = r4
                P.op(dve, lambda e: e.tensor_reduce(S(0), lgb[:, :, 0:4], AX.X, ALU.max), reads=[lgb], writes=[rs])
                P.op(dve, lambda e: e.tensor_tensor(gmask[:, :, :], lgb[:, :, 0:4], bc(S(0), 4), ALU.is_equal), reads=[lgb, rs], writes=[gmask])
                P.op(dve, lambda e: e.tensor_tensor(gsh[:, :, :], lgb[:, :, 0:4], bc(S(0), 4), ALU.subtract), reads=[lgb, rs], writes=[gsh])
                P.op(act, lambda e: e.activation(out=gex[:, :, :], in_=gsh[:, :, :], func=AF.Exp), reads=[gsh], writes=[gex])
                P.op(dve, lambda e: e.tensor_reduce(S(1), gex[:, :, :], AX.X, ALU.add), reads=[gex], writes=[rs])
                P.op(dve, lambda e: e.reciprocal(S(2), S(1)), reads=[rs], writes=[rs])
                le4 = lgb[:, :, 4:36].rearrange("p q (g j) -> p q g j", g=4)
                tmp32 = r32[0]
                P.op(dve, lambda e: e.tensor_tensor(tmp32[:, :, :].rearrange("p q (g j) -> p q g j", g=4), le4,
                                                    gmask[:, :, :].unsqueeze(3).broadcast_to([128, Q, 4, 8]), ALU.mult), reads=[lgb, gmask], writes=[tmp32])
                sel, top8, oh1, oh2 = r8
                P.op(dve, lambda e: e.tensor_reduce(sel[:, :, :], tmp32[:, :, :].rearrange("p q (g j) -> p q j g", g=4), AX.X, ALU.add), reads=[tmp32], writes=[sel])
                yield
                for q in range(Q):
                    P.op(dve, lambda e, q=q: e.max(top8[:, q, :], sel[:, q, :]), reads=[sel], writes=[top8])
                P.op(dve, lambda e: e.tensor_tensor(oh1[:, :, :], sel[:, :, :], top8[:, :, 0:1].broadcast_to([128, Q, 8]), ALU.is_equal), reads=[sel, top8], writes=[oh1])
                P.op(dve, lambda e: e.tensor_tensor(oh2[:, :, :], sel[:, :, :], top8[:, :, 1:2].broadcast_to([128, Q, 8]), ALU.is_equal), reads=[sel, top8], writes=[oh2])
                P.op(dve, lambda e: e.tensor_tensor(S(3), top8[:, :, 1], top8[:, :, 0], ALU.subtract), reads=[top8], writes=[rs])
                P.op(act, lambda e: e.activation(out=S(4), in_=S(3), func=AF.Exp), reads=[rs], writes=[rs])
                P.op(dve, lambda e: e.tensor_scalar(S(5), S(4), 1.0, None, ALU.add), reads=[rs], writes=[rs])
                P.op(dve, lambda e: e.reciprocal(S(6), S(5)), reads=[rs], writes=[rs])
                P.op(dve, lambda e: e.tensor_tensor(S(7), S(6), S(2), ALU.mult), reads=[rs], writes=[rs])
                P.op(dve, lambda e: e.tensor_tensor(S(8), S(2), S(7), ALU.subtract), reads=[rs], writes=[rs])
                E1, E2 = r32[1], r32[2]
                for Ek, oh in ((E1, oh1), (E2, oh2)):
                    P.op(dve, lambda e, Ek=Ek, oh=oh: e.tensor_tensor(Ek[:, :, :].rearrange("p q (g j) -> p q g j", g=4),
                                                                      gmask[:, :, :].unsqueeze(3).broadcast_to([128, Q, 4, 8]),
                                                                      oh[:, :, :].unsqueeze(2).broadcast_to([128, Q, 4, 8]), ALU.mult),
                         reads=[gmask, oh], writes=[Ek])
                P.op(dve, lambda e: e.tensor_tensor(mbf[:, :, :], E1[:, :, :], E2[:, :, :], ALU.add), reads=[E1, E2], writes=[mbf])

                def fnc(e):
                    ins = None
                    for q in range(Q):
                        ins = e.matmul(pmm[3][:, 160 + q * 32:160 + (q + 1) * 32], tri[:, :], mbf[:, q, :], start=True, stop=(q == 0))
                        for q2 in range(q):
                            ins = e.matmul(pmm[3][:, 160 + q * 32:160 + (q + 1) * 32], ones[:, :], mbf[:, q2, :], start=False, stop=(q2 == q - 1))
                    for q in range(Q):
                        ins = e.matmul(pmm[3][:, 288:320], ones[:, :], mbf[:, q, :], start=(q == 0), stop=(q == Q - 1))
                    return ins
                P.op(pe, fnc, reads=[tri, ones, mbf], writes=[pmm[3]])
                yield
                tot = r32[3]
                P.op(dve, lambda e: e.tensor_tensor(tot[:, :, :], pmm[3][:, 160:288].rearrange("p (q n) -> p q n", q=Q),
                                                    cntbc[:, :].unsqueeze(1).broadcast_to([128, Q, 32]), ALU.add), reads=[pmm[3], cntbc], writes=[tot])
                P.op(dve, lambda e: e.tensor_tensor(cntbc[:, :], cntbc[:, :], pmm[3][:, 288:320], ALU.add), reads=[pmm[3], cntbc], writes=[cntbc])
                tm = r32[4]
                for kk, Ek in ((0, E1), (1, E2)):
                    P.op(dve, lambda e, Ek=Ek: e.tensor_tensor(tm[:, :, :], Ek[:, :, :], tot[:, :, :], ALU.mult), reads=[Ek, tot], writes=[tm])
                    P.op(dve, lambda e, kk=kk: e.tensor_reduce(S(10 + kk), tm[:, :, :], AX.X, ALU.add), reads=[tm], writes=[rs])
                    P.op(dve, lambda e, Ek=Ek: e.tensor_tensor(tm[:, :, :], Ek[:, :, :], iota[:, :, :], ALU.mult), reads=[Ek, iota], writes=[tm])
                    P.op(dve, lambda e, kk=kk: e.tensor_reduce(S(12 + kk), tm[:, :, :], AX.X, ALU.add), reads=[tm], writes=[rs])
                    P.op(dve, lambda e, kk=kk: e.scalar_tensor_tensor(S(14 + kk), S(12 + kk), float(CAP), S(10 + kk), ALU.mult, ALU.add), reads=[rs], writes=[rs])
                    P.op(dve, lambda e, kk=kk: e.tensor_single_scalar(S(16 + kk), S(10 + kk), float(CAP), ALU.is_lt), reads=[rs], writes=[rs])
                    P.op(dve, lambda e, kk=kk: e.tensor_scalar(S(14 + kk), S(14 + kk), float(-TRASH), None, ALU.add), reads=[rs], writes=[rs])
                    P.op(dve, lambda e, kk=kk: e.tensor_tensor(S(14 + kk), S(14 + kk), S(16 + kk), ALU.mult), reads=[rs], writes=[rs])
                    P.op(dve, lambda e, kk=kk: e.tensor_scalar(S(14 + kk), S(14 + kk), float(TRASH), 0.0, ALU.add, ALU.max), reads=[rs], writes=[rs])
                    P.op(dve, lambda e, kk=kk: e.tensor_scalar(rinfo[:, ti, kk, :], S(14 + kk), float(TRASH), None, ALU.min), reads=[rs], writes=[rinfo])
                    P.op(dve, lambda e, kk=kk: e.tensor_tensor(rinfo[:, ti, 2 + kk, :], S(7 + kk), S(16 + kk), ALU.mult), reads=[rs], writes=[rinfo])
                    yield
                P.op(dve, lambda e: e.tensor_copy(sloti[:, ti, :, :], rinfo[:, ti, 0:2, :]), reads=[rinfo], writes=[sloti])
                for q in range(Q):
                    for kk in range(2):
                        P.dma(pool, lambda e, q=q, kk=kk: e.indirect_dma_start(
                            out=hs_d[:, :], out_offset=IOA(ap=sloti[:, ti, kk, q:q + 1], axis=0), in_=hfn[q][:, :], in_offset=None),
                            hfn[q], reads=[hfn[q], sloti])

            def collect(genfunc, ti):
                items = []
                orig_op, orig_dma = P.op, P.dma
                P.op = lambda eng, fn, reads=(), writes=(): items.append((orig_op, (eng, fn), dict(reads=reads, writes=writes), eng))
                P.dma = lambda q, fn, sem_buf, reads=(), writes=(): items.append((orig_dma, (q, fn, sem_buf), dict(reads=reads, writes=writes), q))
                try:
                    for tok in genfunc(ti):
                        items.append(tok)
                finally:
                    del P.op, P.dma
                return items

            def stages1(items):
                out, cur, prev = [], [], None
                for it in items:
                    if it is None or (HOP and prev is not None and it[3] is not prev):
                        out.append(cur)
                        cur = []
                    if it is None:
                        prev = None
                    else:
                        cur.append(it)
                        prev = it[3]
                out.append(cur)
                res = []
                for st in out:
                    if not st:
                        continue
                    res.append(st)
                    if LOADLAG and all(it[3] is sp and it[2]["writes"] for it in st):
                        res.extend([[] for _ in range(LOADLAG)])
                return res

            def zip_locked(chains):
                k = len(chains)
                if k == 1:
                    return chains[0]
                spans, wsets = [], []
                for L in chains:
                    fw, lr, ws = {}, {}, set()
                    for si, st in enumerate(L):
                        for (f, args, kw, eng) in st:
                            for bb in kw["writes"]:
                                fw.setdefault(id(bb), si)
                                ws.add(id(bb))
                            for bb in kw["reads"]:
                                lr[id(bb)] = si
                    spans.append({x: (fw[x], lr[x]) for x in fw if x in lr and lr[x] > fw[x]})
                    wsets.append(ws)
                out, pos, owner = [], [0] * k, {}
                while any(pos[c] < len(chains[c]) for c in range(k)):
                    progressed = False
                    for c in range(k):
                        if pos[c] >= len(chains[c]):
                            continue
                        st = chains[c][pos[c]]
                        W = set(id(bb) for (f, args, kw, eng) in st for bb in kw["writes"])
                        if any(owner.get(x) not in (None, c) for x in W):
                            continue
                        out.append(st)
                        progressed = True
                        for x in W:
                            if x in spans[c] and any(x in wsets[j] for j in range(k) if j != c):
                                owner[x] = c
                        for x in list(owner):
                            if owner[x] == c and pos[c] >= spans[c][x][1]:
                                owner[x] = None
                        pos[c] += 1
                    assert progressed, "chain lock deadlock"
                return out

            def stages(items):
                segs = [[[]]]
                for it in items:
                    if it == "SEG":
                        segs.append([[]])
                    elif it == "CHAIN":
                        segs[-1].append([])
                    else:
                        segs[-1][-1].append(it)
                out = []
                for seg in segs:
                    out += zip_locked([stages1(ch) for ch in seg if ch])  if any(seg) else []
                return out

            def spread(genfunc, ti, n, off=0):
                sts = stages(collect(genfunc, ti))
                assert len(sts) <= n - off, (len(sts), n, off)
                k = 0
                for t in range(n):
                    while t >= off and k < len(sts) and k * (n - off) < (t - off + 1) * len(sts):
                        for f, args, kw, eng in sts[k]:
                            f(*args, **kw)
                        k += 1
                    yield

            counts = [len(stages(collect(g, 1))) for g in (gen_F, gen_Mc, gen_Ml, gen_E)]
            NSTG = max(counts) + 2

            def both(g1, g2):
                for _ in g1:
                    next(g2)
                    yield

            def tile_gen(ti):
                yield from spread(gen_F, ti, NSTG)
                yield from both(spread(gen_Mc, ti, NSTG, MC_OFF), spread(gen_Ml, ti, NSTG))
                yield from spread(gen_E, ti, NSTG)

            run_pipelined((tile_gen(ti) for ti in range(NT)), depth=3, skew=NSTG)

            if debug:
                P.dma(sp, lambda e: e.dma_start(out=ri_d, in_=rinfo[:, :, :, :].rearrange("p a b c -> p (a b c)")), rinfo, reads=[rinfo])
            P.barrier()
            P.flush(block)

        with ExitStack() as sb:
            B = lambda shape, dt=F32, name=None, dma=False: P.buf(sb, shape, dt, name, dma)
            gffnbc = B([128, 8], F32, "gffnbc", dma=True)
            P.dma(sp, lambda e: e.dma_start(out=gffnbc[:, :], in_=gbc_d[1]), gffnbc, writes=[gffnbc])
            zt = B([128, D], F32, "zt", dma=True)
            P.op(dve, lambda e: e.memset(zt[:, :], 0.0), writes=[zt])
            P.dma(sp, lambda e: e.dma_start(out=y_d[NSLOT:NSLOT + 128, :], in_=zt[:, :]), zt, reads=[zt])
            w1 = [B([128, 8, 512], BF16, "w1") for _ in range(2)]
            w3 = [B([128, 8, 512], BF16, "w3") for _ in range(2)]
            w2 = [B([128, 4, D], BF16, "w2") for _ in range(2)]
            w3s = B([128, 8, 512], F32, "w3s", dma=True)
            w1s = B([128, 8, 512], F32, "w1s", dma=True)
            w2s = B([128, 4, D], F32, "w2s", dma=True)
            hst = [B([128, NSUB, D], BF16, "hst", dma=True) for _ in range(2)]
            hfTe = [B([128, 8, CAP], BF16, "hfTe") for _ in range(2)]
            actT = B([128, 4, CAP], BF16, "actT")
            tb = [B([128, 512], F32, "tb") for _ in range(2)]
            tc = [B([128, 512], F32, "tc") for _ in range(2)]
            yt = [B([128, D], F32, "yt", dma=True) for _ in range(3)]
            ntiles = [(0, 512)] if CAP == 512 else ([(n0, min(512, CAP - n0)) for n0 in range(0, CAP, 512)])
            yi = 0
            ci = 0

            def load_expert(ex):
                sl = ex % 2
                P.dma(sp, lambda e: e.dma_start(out=hst[sl][:, :, :], in_=hs_d[ex * CAP:(ex + 1) * CAP, :].rearrange("(s p) n -> p s n", p=128)),
                      hst[sl], writes=[hst[sl]])
                P.dma(sp, lambda e: e.dma_start(out=w1s[:, :, :], in_=w1_d[ex].rearrange("(k p) n -> p k n", p=128)), w1s, writes=[w1s])
                P.dma(sp, lambda e: e.dma_start(out=w3s[:, :, :], in_=w3_d[ex].rearrange("(k p) n -> p k n", p=128)), w3s, writes=[w3s])
                P.dma(sp, lambda e: e.dma_start(out=w2s[:, :, :], in_=w2_d[ex].rearrange("(k p) n -> p k n", p=128)), w2s, writes=[w2s])

            def cast_expert(ex):
                sl = ex % 2
                for k in range(8):
                    P.op(pool, lambda e, k=k: e.tensor_tensor(w1[sl][:, k, :], w1s[:, k, :], gffnbc[:, k:k + 1].broadcast_to([128, 512]), ALU.mult),
                         reads=[w1s, gffnbc], writes=[w1[sl]])
                for k in range(8):
                    P.op(act, lambda e, k=k: e.activation(out=w3[sl][:, k, :], in_=w3s[:, k, :], func=AF.Copy, scale=gffnbc[:, k:k + 1]), reads=[w3s, gffnbc], writes=[w3[sl]])
                for k in range(4):
                    P.op(act, lambda e, k=k: e.activation(out=w2[sl][:, k, :], in_=w2s[:, k, :], func=AF.Copy), reads=[w2s], writes=[w2[sl]])

            pool6 = pmm + [ps1, ps2]
            p6 = [0]

            def next6():
                bb = pool6[p6[0] % 6]
                p6[0] += 1
                return bb

            def do_T(ex):
                sl = ex % 2
                hT_e = hfTe[sl]
                for sbt in range(NSUB):
                    pt_ = ptr if sbt % 2 == 0 else ptr2

                    def fn(e, sbt=sbt, pt_=pt_, sl=sl):
                        ins = None
                        for c in range(8):
                            ins = e.transpose(pt_[:, c * 128:(c + 1) * 128], hst[sl][:, sbt, c * 128:(c + 1) * 128], ident[:, :])
                        return ins
                    P.op(pe, fn, reads=[hst[sl], ident], writes=[pt_])
                    P.op(dve, lambda e, sbt=sbt, pt_=pt_, hT_e=hT_e: e.tensor_copy(hT_e[:, :, sbt * 128:(sbt + 1) * 128], pt_[:, :].rearrange("p (c j) -> p c j", c=8)),
                         reads=[pt_], writes=[hT_e])

            def do_H(ex):
                sl = ex % 2
                hT_e = hfTe[sl]
                for (n0, nn) in ntiles:
                    for m in range(4):
                        p1, p3 = next6(), next6()
                        for (pb, wt) in ((p1, w1[sl]), (p3, w3[sl])):
                            def fn(e, pb=pb, wt=wt, m=m, n0=n0, nn=nn, hT_e=hT_e):
                                ins = None
                                for k in range(8):
                                    ins = e.matmul(pb[:, 0:nn], wt[:, k, m * 128:(m + 1) * 128], hT_e[:, k, n0:n0 + nn], start=(k == 0), stop=(k == 7))
                                return ins
                            P.op(pe, fn, reads=[wt, hT_e], writes=[pb])
                        tb_, tc_ = tb[ci_[0] % 2], tc[ci_[0] % 2]
                        ci_[0] += 1
                        P.op(act, lambda e, p1=p1, tb_=tb_, nn=nn: e.activation(out=tb_[:, 0:nn], in_=p1[:, 0:nn], func=AF.Tanh, scale=0.5), reads=[p1], writes=[tb_])
                        P.op(dve, lambda e, p1=p1, tb_=tb_, tc_=tc_, nn=nn: e.scalar_tensor_tensor(tc_[:, 0:nn], tb_[:, 0:nn], 1.0, p1[:, 0:nn], ALU.add, ALU.mult),
                             reads=[tb_, p1], writes=[tc_])
                        P.op(dve, lambda e, p3=p3, tc_=tc_, nn=nn, m=m, n0=n0: e.scalar_tensor_tensor(actT[:, m, n0:n0 + nn], tc_[:, 0:nn], 0.5, p3[:, 0:nn], ALU.mult, ALU.mult),
                             reads=[tc_, p3], writes=[actT])

            def do_Y(ex):
                sl = ex % 2
                for sbt in range(NSUB):
                    yb = yt[yi_[0] % 3]
                    yi_[0] += 1
                    for h in range(2):
                        pb = next6()

                        def fn(e, pb=pb, h=h, sbt=sbt, sl=sl):
                            ins = None
                            for m in range(4):
                                ins = e.matmul(pb[:, :], actT[:, m, sbt * 128:(sbt + 1) * 128], w2[sl][:, m, h * 512:(h + 1) * 512], start=(m == 0), stop=(m == 3))
                            return ins
                        P.op(pe, fn, reads=[actT, w2[sl]], writes=[pb])
                        P.op(act, lambda e, pb=pb, h=h, yb=yb: e.activation(out=yb[:, h * 512:(h + 1) * 512], in_=pb[:, :], func=AF.Copy), reads=[pb], writes=[yb])
                    r0 = ex * CAP + sbt * 128
                    P.dma(sp, lambda e, yb=yb, r0=r0: e.dma_start(out=y_d[r0:r0 + 128, :], in_=yb[:, :]), yb, reads=[yb])

            ci_, yi_ = [0], [0]
            load_expert(0)
            cast_expert(0)
            do_T(0)
            for ex in range(32):
                if ex + 1 < 32:
                    load_expert(ex + 1)
                do_H(ex)
                if ex + 1 < 32:
                    cast_expert(ex + 1)
                    do_T(ex + 1)
                do_Y(ex)
            P.barrier()
            P.flush(block)

        with ExitStack() as sc:
            B = lambda shape, dt=F32, name=None, dma=False: P.buf(sc, shape, dt, name, dma)
            gplebc = B([128, 8], F32, "gplebc", dma=True)
            gpp = B([128, D], F32, "gpp", dma=True)
            gfin = B([128, D], F32, "gfin", dma=True)
            wple = B([128, 2, D], BF16, "wple", dma=True)
            wpg = B([128, 8, D], BF16, "wpg", dma=True)
            P.dma(sp, lambda e: e.dma_start(out=gplebc[:, :], in_=gbc_d[2]), gplebc, writes=[gplebc])
            P.dma(sp, lambda e: e.dma_start(out=gpp[:, :], in_=rowbc_d[0]), gpp, writes=[gpp])
            P.dma(sp, lambda e: e.dma_start(out=gfin[:, :], in_=rowbc_d[1]), gfin, writes=[gfin])
            for k in range(2):
                P.dma(pool, lambda e, k=k: e.dma_start(out=wple[:, k, :], in_=wple_d[k * 128:(k + 1) * 128, :]), wple, writes=[wple])
            for k in range(8):
                P.dma(pool, lambda e, k=k: e.dma_start(out=wpg[:, k, :], in_=wpg_d[k * 128:(k + 1) * 128, :]), wpg, writes=[wpg])
            for k in range(8):
                P.op(dve, lambda e, k=k: e.tensor_scalar(wpg[:, k, :], wpg[:, k, :], gplebc[:, k:k + 1], None, ALU.mult), reads=[wpg, gplebc], writes=[wpg])
            x1t_ = [B([128, D], F32, "x1c", dma=True) for _ in range(NP_C)]
            pt_b = [B([128, 256], F32, "pc", dma=True) for _ in range(NP_C)]
            y1_ = [B([128, D], F32, "y1c", dma=True) for _ in range(NP_C)]
            y2_ = [B([128, D], F32, "y2c", dma=True) for _ in range(NP_C)]
            NP = NP_C
            ssC_ = [B([128, 16], F32, "ssC") for _ in range(NP)]
            xn3 = [B([128, D], BF16, "xn3") for _ in range(NP)]
            junk, te, ob, thg = xn3, y2_, x1t_, y1_
            x3T = [B([128, 8, 128], BF16, "x3T") for _ in range(NP)]
            pbf = [B([128, 256], BF16, "pbf") for _ in range(NP)]
            pT = [B([128, 2, 128], BF16, "pT") for _ in range(NP)]

            def rsq(ssb, i_v, i_l, i_o):
                P.op(act, lambda e: e.activation(out=ssb[:, i_l:i_l + 1], in_=ssb[:, i_v:i_v + 1], func=AF.Ln), reads=[ssb], writes=[ssb])
                P.op(act, lambda e: e.activation(out=ssb[:, i_o:i_o + 1], in_=ssb[:, i_l:i_l + 1], func=AF.Exp, scale=-0.5), reads=[ssb], writes=[ssb])

            def subtile_gen(st):
                ti, q = divmod(st, Q)
                r0 = st * 128
                i3 = st % NP
                xx, pp, y1, y2 = x1t_[i3], pt_b[i3], y1_[i3], y2_[i3]
                jk, ssC = junk[i3], ssC_[i3]
                xn_, x3_, pb_, pT_, tg_, te_, ob_ = xn3[i3], x3T[i3], pbf[i3], pT[i3], thg[i3], te[i3], ob[i3]
                P.dma(sp, lambda e: e.dma_start(out=xx[:, :], in_=x1_d[r0:r0 + 128, :]), xx, writes=[xx])
                P.dma(sp, lambda e: e.dma_start(out=pp[:, :], in_=p_d[r0:r0 + 128, :]), pp, writes=[pp])
                for (yy, kk) in ((y1, 0), (y2, 1)):
                    P.dma(pool, lambda e, yy=yy, kk=kk: e.indirect_dma_start(
                        out=yy[:, :], out_offset=None, in_=y_d[:, :], in_offset=IOA(ap=sloti[:, ti, kk, q:q + 1], axis=0)),
                        yy, reads=[sloti], writes=[yy])
                yield
                for (yy, kk) in ((y1, 0), (y2, 1)):
                    P.op(dve, lambda e, yy=yy, kk=kk: e.scalar_tensor_tensor(xx[:, :], yy[:, :], rinfo[:, ti, 2 + kk, q:q + 1], xx[:, :], ALU.mult, ALU.add),
                         reads=[yy, rinfo, xx], writes=[xx])
                yield
                P.op(act, lambda e: e.activation(out=jk[:, :], in_=xx[:, :], func=AF.Square, accum_out=ssC[:, 0:1]), reads=[xx], writes=[jk, ssC])
                P.op(act, lambda e: e.activation(out=pb_[:, :], in_=pp[:, :], func=AF.Copy), reads=[pp], writes=[pb_])
                yield
                P.op(dve, lambda e: e.tensor_scalar(ssC[:, 1:2], ssC[:, 0:1], 1.0 / D, EPS, ALU.mult, ALU.add), reads=[ssC], writes=[ssC])
                yield
                rsq(ssC, 1, 3, 2)
                P.op(act, lambda e: e.activation(out=xn_[:, :], in_=xx[:, :], func=AF.Copy, scale=ssC[:, 2:3]), reads=[xx, ssC], writes=[xn_])
                yield
                transposes(xn_, 8, ptr)
                P.op(dve, lambda e: e.tensor_copy(x3_[:, :, :], ptr[:, :].rearrange("p (c j) -> p c j", c=8)), reads=[ptr], writes=[x3_])
                transposes(pb_, 2, ptr2)
                P.op(act, lambda e: e.activation(out=pT_[:, :, :], in_=ptr2[:, 0:256].rearrange("p (c j) -> p c j", c=2), func=AF.Copy), reads=[ptr2], writes=[pT_])
                yield
                pes = []
                for h in range(2):
                    pg_ = pmm[h]

                    def fn(e, pg_=pg_, h=h):
                        ins = None
                        for k in range(8):
                            ins = e.matmul(pg_[:, :], x3_[:, k, :], wpg[:, k, h * 512:(h + 1) * 512], start=(k == 0), stop=(k == 7))
                        return ins
                    P.op(pe, fn, reads=[x3_, wpg], writes=[pg_])
                    P.op(act, lambda e, pg_=pg_, h=h: e.activation(out=tg_[:, h * 512:(h + 1) * 512], in_=pg_[:, :], func=AF.Tanh, scale=0.5), reads=[pg_], writes=[tg_])
                    pe_ = (pmm[2], pmm[3], ps1, ps2)[(2 * st + h) % 4]

                    def fn2(e, pe_=pe_, h=h):
                        ins = None
                        for k in range(2):
                            ins = e.matmul(pe_[:, :], pT_[:, k, :], wple[:, k, h * 512:(h + 1) * 512], start=(k == 0), stop=(k == 1))
                        return ins
                    P.op(pe, fn2, reads=[pT_, wple], writes=[pe_])
                    P.op(act, lambda e, pe_=pe_, h=h: e.activation(out=jk[:, 0:512], in_=pe_[:, :], func=AF.Square, accum_out=ssC[:, 4 + h:5 + h]), reads=[pe_], writes=[jk, ssC])
                    pes.append(pe_)
                yield
                P.op(dve, lambda e: e.tensor_tensor(ssC[:, 6:7], ssC[:, 4:5], ssC[:, 5:6], ALU.add), reads=[ssC], writes=[ssC])
                P.op(dve, lambda e: e.tensor_scalar(ssC[:, 7:8], ssC[:, 6:7], 4.0 / D, 4.0 * EPS, ALU.mult, ALU.add), reads=[ssC], writes=[ssC])
                yield
                rsq(ssC, 7, 9, 8)
                yield
                for h in range(2):
                    P.op(dve, lambda e, h=h, pe_=pes[h]: e.scalar_tensor_tensor(te_[:, h * 512:(h + 1) * 512], pe_[:, :], ssC[:, 8:9], gpp[:, h * 512:(h + 1) * 512], ALU.mult, ALU.mult),
                         reads=[pes[h], ssC, gpp], writes=[te_])
                P.op(dve, lambda e: e.scalar_tensor_tensor(te_[:, :], tg_[:, :], 1.0, te_[:, :], ALU.add, ALU.mult), reads=[tg_, te_], writes=[te_])
                yield
                P.op(pool, lambda e: e.tensor_tensor(xx[:, :], xx[:, :], te_[:, :], ALU.add), reads=[te_, xx], writes=[xx])
                yield
                P.op(act, lambda e: e.activation(out=jk[:, :], in_=xx[:, :], func=AF.Square, accum_out=ssC[:, 10:11]), reads=[xx], writes=[jk, ssC])
                yield
                P.op(dve, lambda e: e.tensor_scalar(ssC[:, 11:12], ssC[:, 10:11], 1.0 / D, EPS, ALU.mult, ALU.add), reads=[ssC], writes=[ssC])
                yield
                rsq(ssC, 11, 13, 12)
                yield
                P.op(dve, lambda e: e.scalar_tensor_tensor(ob_[:, :], xx[:, :], ssC[:, 12:13], gfin[:, :], ALU.mult, ALU.mult), reads=[xx, ssC, gfin], writes=[ob_])
                P.dma(sp, lambda e: e.dma_start(out=out_d[r0:r0 + 128, :], in_=ob_[:, :]), ob_, reads=[ob_])

            run_pipelined((subtile_gen(st) for st in range(T // 128)), depth=NP, skew=2)
            P.barrier()
            P.flush(block)
    return nc


def _chan(v):
    return np.ascontiguousarray(np.asarray(v, np.float32).reshape(4, 128).T)


def _gbc(g):
    return np.ascontiguousarray(np.asarray(g, np.float32).reshape(8, 128).T)


def prep_shared(inp):
    f = lambda a: np.ascontiguousarray(np.asarray(a, np.float32))
    cpar = np.zeros((128, NCP), np.float32)
    cw = f(inp["conv_dw_w"][0])
    for c in range(4):
        cpar[:, CW + c * 31:CW + (c + 1) * 31] = cw[:, c * 128:(c + 1) * 128].T
    cpar[:, CB:CB + 4] = _chan(inp["conv_dw_b"][0])
    cpar[:, LG:LG + 4] = _chan(inp["conv_ln_g"][0])
    cpar[:, LB:LB + 4] = _chan(inp["conv_ln_b"][0])
    lw = f(inp["lru_conv_w"][0])
    for c in range(4):
        cpar[:, LW + c * 4:LW + (c + 1) * 4] = lw[:, c * 128:(c + 1) * 128].T
    cpar[:, LBB:LBB + 4] = _chan(inp["lru_conv_b"][0])
    cpar[:, BR:BR + 4] = _chan(inp["lru_b_r"][0])
    cpar[:, BI:BI + 4] = _chan(inp["lru_b_i"][0])
    cpar[:, LAM:LAM + 4] = _chan(inp["lru_lambda"][0])
    wr_, wi_ = f(inp["lru_w_r"][0]), f(inp["lru_w_i"][0])
    gbd = np.zeros((4, 128, 256), np.float32)
    for c in range(4):
        for hh in range(2):
            gbd[c, hh * 64:(hh + 1) * 64, hh * 64:(hh + 1) * 64] = wr_[2 * c + hh]
            gbd[c, hh * 64:(hh + 1) * 64, 128 + hh * 64:128 + (hh + 1) * 64] = wi_[2 * c + hh]
    rb = np.concatenate([f(inp["b_group"][0]), f(inp["b_expert"][0])])
    shared = {
        "w_in": f(inp["w_in"][0]), "w_out": f(inp["w_out"][0]),
        "w_route": np.ascontiguousarray(np.concatenate([f(inp["w_group"][0]), f(inp["w_expert"][0])], axis=1)),
        "gate_bd": gbd, "cpar": cpar,
        "gbc": np.stack([_gbc(inp["g_mix"][0]), _gbc(inp["g_ffn"][0]), _gbc(inp["g_ple"][0])]),
        "rowbc": np.stack([np.ascontiguousarray(np.broadcast_to(f(inp["g_ple_proj"][0]), (128, D))),
                           np.ascontiguousarray(np.broadcast_to(f(inp["g_final"]), (128, D)))]),
        "rbias": np.ascontiguousarray(np.broadcast_to(np.tile(rb, Q), (128, Q * 36))),
        "iota_e": np.ascontiguousarray(np.broadcast_to(np.tile(np.arange(32, dtype=np.float32), Q), (128, Q * 32))),
        "ident": np.eye(128, dtype=np.float32),
        "tri": np.ascontiguousarray(np.triu(np.ones((128, 128), np.float32), 1)),
        "w1": f(inp["w1"][0]), "w3": f(inp["w3"][0]), "w2": f(inp["w2"][0]),
        "w_ple": f(inp["w_ple"][0]), "w_ple_gate": f(inp["w_ple_gate"][0]),
    }
    return shared


def kernel(**inputs):
    NSEQ, CAP = 4, 1024
    x = np.asarray(inputs["x"], np.float32)
    p = np.asarray(inputs["p"], np.float32)[0]
    shared = prep_shared(inputs)
    nc = build(NSEQ, CAP)
    in_maps = []
    for i in range(N_CORES):
        m = dict(shared)
        m["x"] = np.ascontiguousarray(x[i * NSEQ:(i + 1) * NSEQ].reshape(NSEQ * SEQ, D))
        m["p"] = np.ascontiguousarray(p[i * NSEQ:(i + 1) * NSEQ].reshape(NSEQ * SEQ, 256))
        in_maps.append(m)
    res = run_bass_kernel_spmd(nc, in_maps, core_ids=list(range(N_CORES)))
    out = np.concatenate([np.asarray(r["out"], np.float32).reshape(NSEQ, SEQ, D) for r in res.results], axis=0)
    return out
```

```python
import numpy as np
from contextlib import ExitStack
import concourse.bass as bass
import concourse.mybir as mybir
from concourse.bass_utils import run_bass_kernel_spmd

F32 = mybir.dt.float32
BF16 = mybir.dt.bfloat16
I32 = mybir.dt.int32
ALU = mybir.AluOpType
AF = mybir.ActivationFunctionType
AX = mybir.AxisListType

N_CORES = 8
HOP = True
NP_C = 8
LOADLAG = 3
MC_OFF = 0
SEQ = 2048
D = 1024
EPS = 1e-6
Q = 4

CW = 0
CB = CW + 124
LG = CB + 4
LB = LG + 4
LW = LB + 4
LBB = LW + 16
BR = LBB + 4
BI = BR + 4
LAM = BI + 4
NCP = LAM + 4
D_CWH = 0
D_GH = 124
D_BH = 128
D_BRH = 132
D_BIH = 136
D_N4 = 140
D_N8 = 144
NDP = 148


class Src:
    def __init__(self, sem, name, is_dma):
        self.sem, self.name, self.is_dma, self.total = sem, name, is_dma, 0


class Eng(Src):
    def __init__(self, sem, name, blockname, same_wait=True):
        super().__init__(sem, name, False)
        self.blockname, self.ops, self.seen, self.same_wait = blockname, [], {}, same_wait


class Buf:
    def __init__(self, t, dsem=None):
        self.t, self.w, self.r, self.dsem = t, None, {}, dsem

    def __getitem__(self, k):
        return self.t[k]


class Prog:
    def __init__(self, nc, stack):
        self.nc, self.stack = nc, stack
        self.srcs = []
        mk = lambda n, b, sw=True: self._reg(Eng(self._sem("e_" + n), n, b, sw))
        self.pe = mk("pe", "tensor", False)
        self.act = mk("act", "scalar")
        self.dve = mk("dve", "vector")
        self.pool = mk("pool", "gpsimd")
        self.sp = mk("sp", "sync")
        self.engs = [self.pe, self.act, self.dve, self.pool, self.sp]
        self.nbuf = 0

    def _sem(self, name):
        return self.stack.enter_context(self.nc.semaphore(name))

    def _reg(self, s):
        self.srcs.append(s)
        return s

    def buf(self, stack, shape, dt, name=None, dma=False, psum=False):
        self.nbuf += 1
        name = "%s_%d" % (name or "b", self.nbuf)
        if psum:
            t = stack.enter_context(self.nc.psum_tensor(name, shape, dt))
        else:
            t = stack.enter_context(self.nc.sbuf_tensor(name, shape, dt))
        ds = self._reg(Src(self._sem("d_" + name), name, True)) if dma else None
        return Buf(t, ds)

    def _deps(self, eng, reads, writes):
        need = {}

        def add(src, val):
            if src.is_dma:
                val = src.total
            if need.get(src, 0) < val:
                need[src] = val

        for b in reads:
            if b.w is not None:
                add(*b.w)
        for b in writes:
            if b.w is not None:
                add(*b.w)
            for s, v in b.r.items():
                add(s, v)
        waits = []
        for src, val in need.items():
            if src is eng and not eng.same_wait:
                continue
            if eng.seen.get(src, 0) >= val:
                continue
            eng.seen[src] = val
            waits.append((src.sem, val))
        return waits

    def _mark(self, src, val, reads, writes):
        for b in reads:
            b.r[src] = val
        for b in writes:
            b.w = (src, val)
            b.r = {}

    def op(self, eng, fn, reads=(), writes=()):
        waits = self._deps(eng, reads, writes)
        eng.total += 1
        sem = eng.sem

        def emit(e):
            for s, v in waits:
                e.wait_ge(s, v)
            fn(e).then_inc(sem, 1)

        eng.ops.append(emit)
        self._mark(eng, eng.total, reads, writes)

    def dma(self, q, fn, sem_buf, reads=(), writes=()):
        src = sem_buf.dsem
        waits = self._deps(q, reads, writes)
        src.total += 16
        sem = src.sem

        def emit(e):
            for s, v in waits:
                e.wait_ge(s, v)
            fn(e).then_inc(sem, 16)

        q.ops.append(emit)
        self._mark(src, src.total, reads, writes)

    def barrier(self):
        for E in self.engs:
            waits = []
            for S in self.srcs:
                if S is E or S.total == 0:
                    continue
                if E.seen.get(S, 0) >= S.total:
                    continue
                E.seen[S] = S.total
                waits.append((S.sem, S.total))

            def emit(e, waits=waits):
                for s, v in waits:
                    e.wait_ge(s, v)

            E.ops.append(emit)

    def flush(self, block):
        for E in self.engs:
            if not E.ops:
                continue
            ops = E.ops
            E.ops = []

            def body(e, ops=ops):
                for f in ops:
                    f(e)

            getattr(block, E.blockname)(body)


def run_pipelined(gens, depth, skew):
    it = iter(gens)
    active, pending, tick = [], True, 0
    while pending or active:
        for g in list(active):
            try:
                next(g)
            except StopIteration:
                active.remove(g)
        if pending and tick % skew == 0 and len(active) < depth:
            try:
                g = next(it)
                active.append(g)
                next(g)
            except StopIteration:
                pending = False
        tick += 1


def build(NSEQ=4, CAP=640, debug=False):
    T = NSEQ * SEQ
    NT = T // 512
    NSUB = CAP // 128
    NSLOT = 32 * CAP
    TRASH = NSLOT
    nc = bass.Bass("TRN2", target_bir_lowering=False)

    def dr(name, shape, dt=F32, kind="ExternalInput"):
        return nc.dram_tensor(name, shape, dt, kind=kind).ap()

    x_d = dr("x", [T, D])
    p_d = dr("p", [T, 256])
    win_d = dr("w_in", [D, 2048])
    wout_d = dr("w_out", [D, D])
    wr_d = dr("w_route", [D, 36])
    gbd_d = dr("gate_bd", [4, 128, 256])
    cpar_d = dr("cpar", [128, NCP])
    gbc_d = dr("gbc", [3, 128, 8])
    rowbc_d = dr("rowbc", [2, 128, D])
    rb_d = dr("rbias", [128, Q * 36])
    iota_d = dr("iota_e", [128, Q * 32])
    ident_d = dr("ident", [128, 128])
    tri_d = dr("tri", [128, 128])
    w1_d = dr("w1", [32, D, 512])
    w3_d = dr("w3", [32, D, 512])
    w2_d = dr("w2", [32, 512, D])
    wple_d = dr("w_ple", [256, D])
    wpg_d = dr("w_ple_gate", [D, D])
    out_d = dr("out", [T, D], kind="ExternalOutput")
    sk = "ExternalOutput" if debug else "Internal"
    x1_d = dr("x1s", [T, D], kind=sk)
    hs_d = dr("hss", [NSLOT + 128, D], BF16, kind=sk)
    y_d = dr("yss", [NSLOT + 128, D], kind=sk)
    if debug:
        ri_d = dr("rinfo_o", [128, NT * 4 * Q], kind="ExternalOutput")

    IOA = bass.IndirectOffsetOnAxis

    with ExitStack() as top:
        P = Prog(nc, top)
        pe, act, dve, pool, sp = P.pe, P.act, P.dve, P.pool, P.sp
        block = top.enter_context(nc.Block())

        pmm = [P.buf(top, [128, 512], F32, "pmm", psum=True) for _ in range(4)]
        ptr = P.buf(top, [128, 1024], BF16, "ptr", psum=True)
        ptr2 = P.buf(top, [128, 1024], BF16, "ptr2", psum=True)
        ps1 = P.buf(top, [128, 512], F32, "ps1", psum=True)
        ps2 = P.buf(top, [128, 512], F32, "ps2", psum=True)
        pmm_i = [0]

        def next_pmm():
            b = pmm[pmm_i[0] % 4]
            pmm_i[0] += 1
            return b

        rinfo = P.buf(top, [128, NT, 4, Q], F32, "rinfo")
        sloti = P.buf(top, [128, NT, 2, Q], I32, "sloti")
        if debug:
            rinfo.dsem = P._reg(Src(P._sem("d_rinfo"), "rinfo", True))
        ident = P.buf(top, [128, 128], BF16, "ident", dma=True)
        P.dma(pool, lambda e: e.dma_start(out=ident[:, :], in_=ident_d), ident, writes=[ident])

        def transposes(src, n, dst_ps):
            def fn(e):
                ins = None
                for c in range(n):
                    ins = e.transpose(dst_ps[:, c * 128:(c + 1) * 128], src[:, c * 128:(c + 1) * 128], ident[:, :])
                return ins
            P.op(pe, fn, reads=[src, ident], writes=[dst_ps])

        def rstd_from_ss(ss, out, n):
            P.op(dve, lambda e: e.tensor_scalar(out, ss, 1.0 / n, EPS, ALU.mult, ALU.add), reads=[], writes=[])

        with ExitStack() as sa:
            B = lambda shape, dt=F32, name=None, dma=False: P.buf(sa, shape, dt, name, dma)
            win = B([128, 8, 2048], BF16, "win", dma=True)
            wout = B([128, 8, D], BF16, "wout", dma=True)
            wr = B([128, 8, 36], BF16, "wr", dma=True)
            gbd = B([128, 4, 256], BF16, "gbd", dma=True)
            tri = B([128, 128], BF16, "tri", dma=True)
            cpar = B([128, NCP], F32, "cpar", dma=True)
            dpar = B([128, NDP], F32, "dpar")
            gmixbc = B([128, 8], F32, "gmixbc", dma=True)
            gffnbc = B([128, 8], F32, "gffnbc", dma=True)
            rbias = B([128, Q, 36], F32, "rbias", dma=True)
            iota = B([128, Q, 32], F32, "iota", dma=True)
            ones = B([128, 128], BF16, "ones")
            identF = B([128, 128], F32, "identF", dma=True)
            P.dma(sp, lambda e: e.dma_start(out=identF[:, :], in_=ident_d), identF, writes=[identF])
            cntbc = B([128, 32], F32, "cntbc")

            P.dma(sp, lambda e: e.dma_start(out=cpar[:, :], in_=cpar_d), cpar, writes=[cpar])
            P.dma(sp, lambda e: e.dma_start(out=gmixbc[:, :], in_=gbc_d[0]), gmixbc, writes=[gmixbc])
            P.dma(sp, lambda e: e.dma_start(out=gffnbc[:, :], in_=gbc_d[1]), gffnbc, writes=[gffnbc])
            P.dma(sp, lambda e: e.dma_start(out=rbias[:, :, :], in_=rb_d.rearrange("p (q n) -> p q n", q=Q)), rbias, writes=[rbias])
            P.dma(sp, lambda e: e.dma_start(out=iota[:, :, :], in_=iota_d.rearrange("p (q n) -> p q n", q=Q)), iota, writes=[iota])
            P.dma(pool, lambda e: e.dma_start(out=tri[:, :], in_=tri_d), tri, writes=[tri])
            for k in range(8):
                P.dma(pool, lambda e, k=k: e.dma_start(out=win[:, k, :], in_=win_d[k * 128:(k + 1) * 128, :]), win, writes=[win])
            P.dma(pool, lambda e: e.dma_start(out=gbd[:, :, :], in_=gbd_d.rearrange("c p n -> p c n")), gbd, writes=[gbd])
            for k in range(8):
                P.dma(pool, lambda e, k=k: e.dma_start(out=wout[:, k, :], in_=wout_d[k * 128:(k + 1) * 128, :]), wout, writes=[wout])
            P.dma(pool, lambda e: e.dma_start(out=wr[:, :, :], in_=wr_d.rearrange("(k p) n -> p k n", p=128)), wr, writes=[wr])

            P.op(dve, lambda e: e.memset(ones[:, :], 1.0), writes=[ones])
            P.op(dve, lambda e: e.memset(cntbc[:, :], 0.0), writes=[cntbc])
            for k in range(8):
                P.op(dve, lambda e, k=k: e.tensor_scalar(win[:, k, :], win[:, k, :], gmixbc[:, k:k + 1], None, ALU.mult), reads=[win, gmixbc], writes=[win])
                P.op(dve, lambda e, k=k: e.tensor_scalar(wr[:, k, :], wr[:, k, :], gffnbc[:, k:k + 1], None, ALU.mult), reads=[wr, gffnbc], writes=[wr])

            tsm = B([128, 8, 4], F32, "tsm")

            def dv(fn, reads, writes):
                P.op(dve, fn, reads=reads, writes=writes)

            dv(lambda e: e.tensor_scalar(dpar[:, D_CWH:D_CWH + 124], cpar[:, CW:CW + 124], 0.5, None, ALU.mult), [cpar], [dpar])
            dv(lambda e: e.tensor_scalar(dpar[:, D_GH:D_GH + 8], cpar[:, LG:LG + 8], 0.5, None, ALU.mult), [cpar], [dpar])
            dv(lambda e: e.tensor_scalar(dpar[:, D_BRH:D_BRH + 8], cpar[:, BR:BR + 8], 0.5, None, ALU.mult), [cpar], [dpar])
            z_, az, ee, LL, tt, mk_, zp = [tsm[:, i, :] for i in range(7)]
            dv(lambda e: e.tensor_scalar(z_, cpar[:, LAM:LAM + 4], -1.0, None, ALU.mult), [cpar], [tsm])
            dv(lambda e: e.tensor_tensor(az, z_, cpar[:, LAM:LAM + 4], ALU.max), [tsm, cpar], [tsm])
            P.op(act, lambda e: e.activation(out=ee, in_=az, func=AF.Exp, scale=-1.0), reads=[tsm], writes=[tsm])
            P.op(act, lambda e: e.activation(out=LL, in_=ee, func=AF.Ln, bias=1.0, scale=1.0), reads=[tsm], writes=[tsm])
            dv(lambda e: e.tensor_scalar(tt, ee, -0.25, 1.0 / 3.0, ALU.mult, ALU.add), [tsm], [tsm])
            dv(lambda e: e.tensor_tensor(tt, tt, ee, ALU.mult), [tsm], [tsm])
            dv(lambda e: e.tensor_scalar(tt, tt, -1.0, 0.5, ALU.mult, ALU.add), [tsm], [tsm])
            dv(lambda e: e.tensor_tensor(tt, tt, ee, ALU.mult), [tsm], [tsm])
            dv(lambda e: e.tensor_scalar(tt, tt, -1.0, 1.0, ALU.mult, ALU.add), [tsm], [tsm])
            dv(lambda e: e.tensor_tensor(tt, tt, ee, ALU.mult), [tsm], [tsm])
            dv(lambda e: e.tensor_single_scalar(mk_, ee, 0.05, ALU.is_lt), [tsm], [tsm])
            dv(lambda e: e.tensor_tensor(tt, tt, LL, ALU.subtract), [tsm], [tsm])
            dv(lambda e: e.tensor_tensor(tt, tt, mk_, ALU.mult), [tsm], [tsm])
            dv(lambda e: e.tensor_tensor(tt, tt, LL, ALU.add), [tsm], [tsm])
            dv(lambda e: e.tensor_single_scalar(zp, z_, 0.0, ALU.max), [tsm], [tsm])
            dv(lambda e: e.tensor_tensor(tt, tt, zp, ALU.add), [tsm], [tsm])
            dv(lambda e: e.tensor_scalar(dpar[:, D_N4:D_N4 + 4], tt, -4.0, None, ALU.mult), [tsm], [dpar])
            dv(lambda e: e.tensor_scalar(dpar[:, D_N8:D_N8 + 4], tt, -8.0, None, ALU.mult), [tsm], [dpar])

            xa = [B([128, D], F32, "xa", dma=True) for _ in range(2)]
            ssF = [B([128, 8], F32, "ssF") for _ in range(2)]
            xn = [B([128, D], BF16, "xn") for _ in range(2)]
            hT = B([128, 8, 512], BF16, "hT")
            gth = [B([128, 512], F32, "gth") for _ in range(2)]
            gvs = [B([128, 512], F32, "gvs") for _ in range(2)]
            ga = B([128, 512], F32, "ga")
            gb = B([128, 512], F32, "gb")
            ub = [[B([128, 542], BF16, "ub") for _ in range(4)] for _ in range(2)]
            xbuf = [[B([128, 515], BF16, "xbuf") for _ in range(4)] for _ in range(2)]
            qg = [[B([128, 512], BF16, "qg") for _ in range(4)] for _ in range(2)]
            acc2 = [[B([128, 512], F32, "acc") for _ in range(4)] for _ in range(2)]
            cvbf = [B([128, 512], BF16, "cvbf") for _ in range(4)]
            sqbf = [B([128, 512], BF16, "sqbf") for _ in range(4)]
            mean = B([128, 512], F32, "mean")
            msq = B([128, 512], F32, "msq")
            xr = [B([128, 512], F32, "xr") for _ in range(2)]
            xrbf = [B([128, 512], BF16, "xrbf") for _ in range(2)]
            t1 = [B([128, 512], F32, "t1") for _ in range(2)]
            t2 = [B([128, 512], F32, "t2") for _ in range(2)]
            t3 = [B([128, 512], F32, "t3") for _ in range(2)]
            lh, th = t1, t2
            ab = [B([128, 512], F32, "ab")] * 2
            hb = [B([128, 512], F32, "hb")] * 2
            carry = [B([128, 1], F32, "carry") for _ in range(4)]
            yT = [B([128, 8, 512], BF16, "yT") for _ in range(2)]
            xb = B([128, D], F32, "xb", dma=True)
            ssE = [B([128, 8], F32, "ssE") for _ in range(2)]
            x1 = [B([128, D], F32, "x1", dma=True) for _ in range(2)]
            hfn = [B([128, D], BF16, "hfn", dma=True) for _ in range(4)]
            hfT = [B([128, 8, 128], BF16, "hfT") for _ in range(2)]
            rs = B([128, 40, Q], F32, "rs")
            r36 = B([128, Q, 36], F32, "r36")
            r32 = [B([128, Q, 32], F32, "r32") for _ in range(5)]
            mbf = B([128, Q, 32], BF16, "mbf")
            r8 = [B([128, Q, 8], F32, "r8") for _ in range(4)]
            r4 = [B([128, Q, 4], F32, "r4") for _ in range(3)]

            cwh = lambda c, k: dpar[:, D_CWH + c * 31 + k:D_CWH + c * 31 + k + 1]
            NJ = SEQ // 512

            def rsqA(ssb, i_v, i_l, i_o):
                P.op(act, lambda e: e.activation(out=ssb[:, i_l:i_l + 1], in_=ssb[:, i_v:i_v + 1], func=AF.Ln), reads=[ssb], writes=[ssb])
                P.op(act, lambda e: e.activation(out=ssb[:, i_o:i_o + 1], in_=ssb[:, i_l:i_l + 1], func=AF.Exp, scale=-0.5), reads=[ssb], writes=[ssb])

            def evac_scaled(dst3, ps, gvec):
                P.op(act, lambda e: e.activation(out=dst3.all(), in_=ps[:, :].rearrange("p (c j) -> p c j", c=8), func=AF.Copy),
                     reads=[ps], writes=[dst3.buf])

            class View3:
                def __init__(self, buf, fn, allfn=None):
                    self.buf, self.fn, self.all = buf, fn, allfn

                def __call__(self, c):
                    return self.fn(c)

            def gen_F(ti):
                s_, j = divmod(ti, NJ)
                row0 = ti * 512
                par = ti % 2
                ub_, xbuf_, qg_ = ub[par], xbuf[par], qg[par]
                if j == 0:
                    for c in range(4):
                        P.op(pool, lambda e, c=c: e.memset(ub_[c][:, 0:30], 0.0), writes=[ub_[c]])
                        P.op(pool, lambda e, c=c: e.memset(xbuf_[c][:, 0:3], 0.0), writes=[xbuf_[c]])
                for q in range(Q):
                    yield ("SEG" if q % 2 == 0 else "CHAIN")
                    xt, xnb, ss = xa[q % 2], xn[q % 2], ssF[q % 2]
                    r0 = row0 + q * 128
                    P.dma(sp, lambda e, xt=xt, r0=r0: e.dma_start(out=xt[:, :], in_=x_d[r0:r0 + 128, :]), xt, writes=[xt])
                    P.op(act, lambda e, xt=xt, ss=ss, xnb=xnb: e.activation(out=xnb[:, :], in_=xt[:, :], func=AF.Square, accum_out=ss[:, 0:1]),
                         reads=[xt], writes=[xnb, ss])
                    P.op(dve, lambda e, ss=ss: e.tensor_scalar(ss[:, 1:2], ss[:, 0:1], 1.0 / D, EPS, ALU.mult, ALU.add), reads=[ss], writes=[ss])
                    rsqA(ss, 1, 3, 2)
                    P.op(act, lambda e, xt=xt, xnb=xnb, ss=ss: e.activation(out=xnb[:, :], in_=xt[:, :], func=AF.Copy, scale=ss[:, 2:3]),
                         reads=[xt, ss], writes=[xnb])
                    transposes(xnb, 8, ptr)
                    evac_scaled(View3(hT, None, lambda q=q: hT[:, :, q * 128:(q + 1) * 128]), ptr, gmixbc)

                fbank = [pmm[0], ps2]

                def zmm(m, bi):
                    pb = fbank[bi % 2]

                    def fn(e, m=m, pb=pb):
                        ins = None
                        for k in range(8):
                            ins = e.matmul(pb[:, :], win[:, k, m * 128:(m + 1) * 128], hT[:, k, :], start=(k == 0), stop=(k == 7))
                        return ins
                    P.op(pe, fn, reads=[win, hT], writes=[pb])
                    return pb

                for c in range(4):
                    yield ("SEG" if c % 2 == 0 else "CHAIN")
                    pg = zmm(4 + c, c)
                    ta, vs = gth[c % 2], gvs[c % 2]
                    P.op(act, lambda e, pg=pg, ta=ta: e.activation(out=ta[:, :], in_=pg[:, :], func=AF.Tanh, scale=0.5), reads=[pg], writes=[ta])
                    pv = zmm(c, c)
                    P.op(act, lambda e, pv=pv, vs=vs: e.activation(out=vs[:, :], in_=pv[:, :], func=AF.Copy), reads=[pv], writes=[vs])
                    P.op(pool, lambda e, ta=ta, vs=vs: e.tensor_tensor(ta[:, :], ta[:, :], vs[:, :], ALU.mult), reads=[ta, vs], writes=[ta])
                    P.op(pool, lambda e, ta=ta, vs=vs, c=c: e.tensor_tensor(ub_[c][:, 30:542], ta[:, :], vs[:, :], ALU.add), reads=[ta, vs], writes=[ub_[c]])
                for c in range(4):
                    yield ("SEG" if c % 2 == 0 else "CHAIN")
                    px = zmm(8 + c, c)
                    P.op(act, lambda e, px=px, c=c: e.activation(out=xbuf_[c][:, 3:515], in_=px[:, :], func=AF.Copy), reads=[px], writes=[xbuf_[c]])
                for c in range(4):
                    yield "SEG"
                    pgl = zmm(12 + c, c)
                    P.op(act, lambda e, pgl=pgl, c=c: e.activation(out=qg_[c][:, :], in_=pgl[:, :], func=AF.Gelu_apprx_tanh), reads=[pgl], writes=[qg_[c]])

            def gen_Mc(ti):
                s_, j = divmod(ti, NJ)
                par = ti % 2
                ub_, xbuf_, qg_, yT_ = ub[par], xbuf[par], qg[par], yT[par]
                ubn, xbufn = ub[1 - par], xbuf[1 - par]
                acc = acc2[par]
                for k in range(31):
                    for c in range(4):
                        if k == 0:
                            P.op(dve, lambda e, c=c: e.tensor_scalar(acc[c][:, :], ub_[c][:, 0:512], cwh(c, 0), cpar[:, CB + c:CB + c + 1], ALU.mult, ALU.add),
                                 reads=[ub_[c], dpar, cpar], writes=[acc[c]])
                        else:
                            P.op(dve, lambda e, c=c, k=k: e.scalar_tensor_tensor(acc[c][:, :], ub_[c][:, k:k + 512], cwh(c, k), acc[c][:, :], ALU.mult, ALU.add),
                                 reads=[ub_[c], dpar, acc[c]], writes=[acc[c]])
                    yield
                for c in range(4):
                    if j < NJ - 1:
                        P.op(pool, lambda e, c=c: e.tensor_copy(ubn[c][:, 0:30], ub_[c][:, 512:542]), reads=[ub_[c]], writes=[ubn[c]])
                    P.op(act, lambda e, c=c: e.activation(out=cvbf[c][:, :], in_=acc[c][:, :], func=AF.Copy), reads=[acc[c]], writes=[cvbf[c]])
                    P.op(act, lambda e, c=c: e.activation(out=sqbf[c][:, :], in_=acc[c][:, :], func=AF.Square), reads=[acc[c]], writes=[sqbf[c]])

                def stat_mm(dst, srcs):
                    def fn(e):
                        ins = None
                        for c in range(4):
                            ins = e.matmul(dst[:, :], ones[:, :], srcs[c][:, :], start=(c == 0), stop=(c == 3))
                        return ins
                    P.op(pe, fn, reads=[ones] + srcs, writes=[dst])
                stat_mm(ps1, cvbf)
                P.op(act, lambda e: e.activation(out=mean[:, :], in_=ps1[:, :], func=AF.Copy, scale=1.0 / 512), reads=[ps1], writes=[mean])
                stat_mm(ps1, sqbf)
                P.op(pool, lambda e: e.tensor_tensor(msq[:, :], mean[:, :], mean[:, :], ALU.mult), reads=[mean], writes=[msq])
                P.op(dve, lambda e: e.scalar_tensor_tensor(msq[:, :], ps1[:, :], 1.0 / 512, msq[:, :], ALU.mult, ALU.subtract), reads=[ps1, msq], writes=[msq])
                P.op(dve, lambda e: e.tensor_scalar(msq[:, :], msq[:, :], EPS, None, ALU.add), reads=[msq], writes=[msq])
                P.op(act, lambda e: e.activation(out=msq[:, :], in_=msq[:, :], func=AF.Ln), reads=[msq], writes=[msq])
                P.op(act, lambda e: e.activation(out=msq[:, :], in_=msq[:, :], func=AF.Exp, scale=-0.5), reads=[msq], writes=[msq])
                yield
                for c in range(4):
                    P.op(pool, lambda e, c=c: e.tensor_tensor(acc[c][:, :], acc[c][:, :], mean[:, :], ALU.subtract), reads=[acc[c], mean], writes=[acc[c]])
                    P.op(pool, lambda e, c=c: e.tensor_tensor(acc[c][:, :], acc[c][:, :], msq[:, :], ALU.mult), reads=[acc[c], msq], writes=[acc[c]])
                for c in range(4):
                    P.op(act, lambda e, c=c: e.activation(out=yT_[:, c, :], in_=acc[c][:, :], func=AF.Silu,
                                                          bias=cpar[:, LB + c:LB + c + 1], scale=cpar[:, LG + c:LG + c + 1]),
                         reads=[acc[c], cpar], writes=[yT_])
            def gen_Ml(ti):
                s_, j = divmod(ti, NJ)
                par = ti % 2
                xbuf_, qg_, yT_ = xbuf[par], qg[par], yT[par]
                xbufn = xbuf[1 - par]
                if j == 0:
                    for c in range(4):
                        P.op(pool, lambda e, c=c: e.memset(carry[c][:, :], 0.0), writes=[carry[c]])
                for c in range(4):
                    xr_, xrb_ = xr[c % 2], xrbf[c % 2]
                    lw = lambda k, c=c: cpar[:, LW + c * 4 + k:LW + c * 4 + k + 1]
                    P.op(dve, lambda e, c=c, xr_=xr_, lw=lw: e.tensor_scalar(xr_[:, :], xbuf_[c][:, 0:512], lw(0), cpar[:, LBB + c:LBB + c + 1], ALU.mult, ALU.add),
                         reads=[xbuf_[c], cpar], writes=[xr_])
                    for k in range(1, 4):
                        P.op(dve, lambda e, c=c, k=k, xr_=xr_, lw=lw: e.scalar_tensor_tensor(xr_[:, :], xbuf_[c][:, k:k + 512], lw(k), xr_[:, :], ALU.mult, ALU.add),
                             reads=[xbuf_[c], cpar, xr_], writes=[xr_])
                    if j < NJ - 1:
                        P.op(pool, lambda e, c=c: e.tensor_copy(xbufn[c][:, 0:3], xbuf_[c][:, 512:515]), reads=[xbuf_[c]], writes=[xbufn[c]])
                    P.op(act, lambda e, xr_=xr_, xrb_=xrb_: e.activation(out=xrb_[:, :], in_=xr_[:, :], func=AF.Copy), reads=[xr_], writes=[xrb_])
                    pr, pi = pmm[1], pmm[1]
                    P.op(pe, lambda e, c=c, pr=pr, xrb_=xrb_: e.matmul(pr[:, :], gbd[:, c, 0:128], xrb_[:, :], start=True, stop=True), reads=[gbd, xrb_], writes=[pr])
                    a1, a2, a3, aa, hh = t1[c % 2], t2[c % 2], t3[c % 2], ab[c % 2], hb[c % 2]
                    dp = lambda o, c=c: dpar[:, o + c:o + c + 1]
                    yield
                    P.op(act, lambda e, pr=pr, a1=a1, dp=dp: e.activation(out=a1[:, :], in_=pr[:, :], func=AF.Tanh, bias=dp(D_BRH), scale=0.5), reads=[pr, dpar], writes=[a1])
                    P.op(pe, lambda e, c=c, pi=pi, xrb_=xrb_: e.matmul(pi[:, :], gbd[:, c, 128:256], xrb_[:, :], start=True, stop=True), reads=[gbd, xrb_], writes=[pi])
                    P.op(act, lambda e, pi=pi, a3=a3, dp=dp: e.activation(out=a3[:, :], in_=pi[:, :], func=AF.Tanh, bias=dp(D_BIH), scale=0.5), reads=[pi, dpar], writes=[a3])
                    P.op(act, lambda e, a1=a1, aa=aa, dp=dp: e.activation(out=aa[:, :], in_=a1[:, :], func=AF.Exp, bias=dp(D_N4), scale=dp(D_N4)), reads=[a1, dpar], writes=[aa])
                    P.op(act, lambda e, a1=a1, a2=a2, dp=dp: e.activation(out=a2[:, :], in_=a1[:, :], func=AF.Exp, bias=dp(D_N8), scale=dp(D_N8)), reads=[a1, dpar], writes=[a2])
                    P.op(dve, lambda e, a2=a2: e.tensor_scalar(a2[:, :], a2[:, :], 0.99999994, -1.0, ALU.min, ALU.mult), reads=[a2], writes=[a2])
                    P.op(act, lambda e, a2=a2: e.activation(out=a2[:, :], in_=a2[:, :], func=AF.Ln, bias=1.0, scale=1.0), reads=[a2], writes=[a2])
                    P.op(act, lambda e, a2=a2: e.activation(out=a2[:, :], in_=a2[:, :], func=AF.Exp, scale=0.5), reads=[a2], writes=[a2])
                    P.op(dve, lambda e, a3=a3, xr_=xr_: e.scalar_tensor_tensor(a3[:, :], a3[:, :], 1.0, xr_[:, :], ALU.add, ALU.mult), reads=[a3, xr_], writes=[a3])
                    yield
                    P.op(dve, lambda e, a2=a2, a3=a3: e.tensor_tensor(a3[:, :], a3[:, :], a2[:, :], ALU.mult), reads=[a2, a3], writes=[a3])
                    P.op(dve, lambda e, c=c, aa=aa, a3=a3, hh=hh: e.tensor_tensor_scan(hh[:, :], aa[:, :], a3[:, :], carry[c][:, 0:1], ALU.mult, ALU.add),
                         reads=[aa, a3, carry[c]], writes=[hh])
                    P.op(dve, lambda e, c=c, hh=hh: e.tensor_copy(carry[c][:, :], hh[:, 511:512]), reads=[hh], writes=[carry[c]])
                    P.op(dve, lambda e, c=c, hh=hh: e.scalar_tensor_tensor(yT_[:, 4 + c, :], hh[:, :], 0.5, qg_[c][:, :], ALU.mult, ALU.mult),
                         reads=[hh, qg_[c]], writes=[yT_])
                    yield

            def gen_E(ti):
                row0 = ti * 512
                yT_ = yT[ti % 2]
                for q in range(Q):
                    yield ("SEG" if q % 2 == 0 else "CHAIN")
                    r0 = row0 + q * 128
                    x1t, ss = x1[q % 2], ssE[q % 2]
                    hf = hfn[q]
                    hft = hfT[q % 2]
                    P.dma(sp, lambda e, r0=r0: e.dma_start(out=xb[:, :], in_=x_d[r0:r0 + 128, :]), xb, writes=[xb])
                    for h in range(2):
                        pb = pmm[2]

                        def fn(e, pb=pb, h=h, q=q):
                            ins = e.matmul(pb[:, :], identF[:, :], xb[:, h * 512:(h + 1) * 512], start=True, stop=False)
                            for k in range(8):
                                ins = e.matmul(pb[:, :], yT_[:, k, q * 128:(q + 1) * 128], wout[:, k, h * 512:(h + 1) * 512], start=False, stop=(k == 7))
                            return ins
                        P.op(pe, fn, reads=[yT_, wout, identF, xb], writes=[pb])
                        P.op(act, lambda e, pb=pb, h=h, x1t=x1t: e.activation(out=x1t[:, h * 512:(h + 1) * 512], in_=pb[:, :], func=AF.Copy),
                             reads=[pb], writes=[x1t])
                    P.dma(sp, lambda e, x1t=x1t, r0=r0: e.dma_start(out=x1_d[r0:r0 + 128, :], in_=x1t[:, :]), x1t, reads=[x1t])
                    P.op(act, lambda e, x1t=x1t, ss=ss, hf=hf: e.activation(out=hf[:, :], in_=x1t[:, :], func=AF.Square, accum_out=ss[:, 0:1]), reads=[x1t], writes=[hf, ss])
                    P.op(dve, lambda e, ss=ss: e.tensor_scalar(ss[:, 1:2], ss[:, 0:1], 1.0 / D, EPS, ALU.mult, ALU.add), reads=[ss], writes=[ss])
                    rsqA(ss, 1, 3, 2)
                    P.op(act, lambda e, x1t=x1t, hf=hf, ss=ss: e.activation(out=hf[:, :], in_=x1t[:, :], func=AF.Copy, scale=ss[:, 2:3]), reads=[x1t, ss], writes=[hf])
                    transposes(hf, 8, ptr2)
                    evac_scaled(View3(hft, None, lambda hft=hft: hft[:, :, :]), ptr2, gffnbc)

                    def fnl(e, hft=hft, q=q):
                        ins = None
                        for k in range(8):
                            ins = e.matmul(pmm[3][:, q * 36:(q + 1) * 36], hft[:, k, :], wr[:, k, :], start=(k == 0), stop=(k == 7))
                        return ins
                    P.op(pe, fnl, reads=[hft, wr], writes=[pmm[3]])

                yield "SEG"
                S = lambda i: rs[:, i, :]
                bc = lambda ap, n: ap.unsqueeze(2).broadcast_to([128, Q, n])
                lgb = r36
                P.op(dve, lambda e: e.tensor_tensor(lgb[:, :, :], pmm[3][:, 0:Q * 36].rearrange("p (q n) -> p q n", q=Q), rbias[:, :, :], ALU.add),
                     reads=[pmm[3], rbias], writes=[lgb])
                gmask, gsh, gex = r4
                P.op(dve, lambda e: e.tensor_reduce(S(0), lgb[:, :, 0:4], AX.X, ALU.max), reads=[lgb], writes=[rs])
                P.op(dve, lambda e: e.tensor_tensor(gmask[:, :, :], lgb[:, :, 0:4], bc(S(0), 4), ALU.is_equal), reads=[lgb, rs], writes=[gmask])
                P.op(dve, lambda e: e.tensor_tensor(gsh[:, :, :], lgb[:, :, 0:4], bc(S(0), 4), ALU.subtract), reads=[lgb, rs], writes=[gsh])
                P.op(act, lambda e: e.activation(out=gex[:, :, :], in_=gsh[:, :, :], func=AF.Exp), reads=[gsh], writes=[gex])
                P.op(dve, lambda e: e.tensor_reduce(S(1), gex[:, :, :], AX.X, ALU.add), reads=[gex], writes=[rs])
                P.op(dve, lambda e: e.reciprocal(S(2), S(1)), reads=[rs], writes=[rs])
                le4 = lgb[:, :, 4:36].rearrange("p q (g j) -> p q g j", g=4)
                tmp32 = r32[0]
                P.op(dve, lambda e: e.tensor_tensor(tmp32[:, :, :].rearrange("p q (g j) -> p q g j", g=4), le4,
                                                    gmask[:, :, :].unsqueeze(3).broadcast_to([128, Q, 4, 8]), ALU.mult), reads=[lgb, gmask], writes=[tmp32])
                sel, top8, oh1, oh2 = r8
                P.op(dve, lambda e: e.tensor_reduce(sel[:, :, :], tmp32[:, :, :].rearrange("p q (g j) -> p q j g", g=4), AX.X, ALU.add), reads=[tmp32], writes=[sel])
                yield
                for q in range(Q):
                    P.op(dve, lambda e, q=q: e.max(top8[:, q, :], sel[:, q, :]), reads=[sel], writes=[top8])
                P.op(dve, lambda e: e.tensor_tensor(oh1[:, :, :], sel[:, :, :], top8[:, :, 0:1].broadcast_to([128, Q, 8]), ALU.is_equal), reads=[sel, top8], writes=[oh1])
                P.op(dve, lambda e: e.tensor_tensor(oh2[:, :, :], sel[:, :, :], top8[:, :, 1:2].broadcast_to([128, Q, 8]), ALU.is_equal), reads=[sel, top8], writes=[oh2])
                P.op(dve, lambda e: e.tensor_tensor(S(3), top8[:, :, 1], top8[:, :, 0], ALU.subtract), reads=[top8], writes=[rs])
                P.op(act, lambda e: e.activation(out=S(4), in_=S(3), func=AF.Exp), reads=[rs], writes=[rs])
                P.op(dve, lambda e: e.tensor_scalar(S(5), S(4), 1.0, None, ALU.add), reads=[rs], writes=[rs])
                P.op(dve, lambda e: e.reciprocal(S(6), S(5)), reads=[rs], writes=[rs])
                P.op(dve, lambda e: e.tensor_tensor(S(7), S(6), S(2), ALU.mult), reads=[rs], writes=[rs])
                P.op(dve, lambda e: e.tensor_tensor(S(8), S(2), S(7), ALU.subtract), reads=[rs], writes=[rs])
                E1, E2 = r32[1], r32[2]
                for Ek, oh in ((E1, oh1), (E2, oh2)):
                    P.op(dve, lambda e, Ek=Ek, oh=oh: e.tensor_tensor(Ek[:, :, :].rearrange("p q (g j) -> p q g j", g=4),
                                                                      gmask[:, :, :].unsqueeze(3).broadcast_to([128, Q, 4, 8]),
                                                                      oh[:, :, :].unsqueeze(2).broadcast_to([128, Q, 4, 8]), ALU.mult),
                         reads=[gmask, oh], writes=[Ek])
                P.op(dve, lambda e: e.tensor_tensor(mbf[:, :, :], E1[:, :, :], E2[:, :, :], ALU.add), reads=[E1, E2], writes=[mbf])

                def fnc(e):
                    ins = None
                    for q in range(Q):
                        ins = e.matmul(pmm[3][:, 160 + q * 32:160 + (q + 1) * 32], tri[:, :], mbf[:, q, :], start=True, stop=(q == 0))
                        for q2 in range(q):
                            ins = e.matmul(pmm[3][:, 160 + q * 32:160 + (q + 1) * 32], ones[:, :], mbf[:, q2, :], start=False, stop=(q2 == q - 1))
                    for q in range(Q):
                        ins = e.matmul(pmm[3][:, 288:320], ones[:, :], mbf[:, q, :], start=(q == 0), stop=(q == Q - 1))
                    return ins
                P.op(pe, fnc, reads=[tri, ones, mbf], writes=[pmm[3]])
                yield
                tot = r32[3]
                P.op(dve, lambda e: e.tensor_tensor(tot[:, :, :], pmm[3][:, 160:288].rearrange("p (q n) -> p q n", q=Q),
                                                    cntbc[:, :].unsqueeze(1).broadcast_to([128, Q, 32]), ALU.add), reads=[pmm[3], cntbc], writes=[tot])
                P.op(dve, lambda e: e.tensor_tensor(cntbc[:, :], cntbc[:, :], pmm[3][:, 288:320], ALU.add), reads=[pmm[3], cntbc], writes=[cntbc])
                tm = r32[4]
                for kk, Ek in ((0, E1), (1, E2)):
                    P.op(dve, lambda e, Ek=Ek: e.tensor_tensor(tm[:, :, :], Ek[:, :, :], tot[:, :, :], ALU.mult), reads=[Ek, tot], writes=[tm])
                    P.op(dve, lambda e, kk=kk: e.tensor_reduce(S(10 + kk), tm[:, :, :], AX.X, ALU.add), reads=[tm], writes=[rs])
                    P.op(dve, lambda e, Ek=Ek: e.tensor_tensor(tm[:, :, :], Ek[:, :, :], iota[:, :, :], ALU.mult), reads=[Ek, iota], writes=[tm])
                    P.op(dve, lambda e, kk=kk: e.tensor_reduce(S(12 + kk), tm[:, :, :], AX.X, ALU.add), reads=[tm], writes=[rs])
                    P.op(dve, lambda e, kk=kk: e.scalar_tensor_tensor(S(14 + kk), S(12 + kk), float(CAP), S(10 + kk), ALU.mult, ALU.add), reads=[rs], writes=[rs])
                    P.op(dve, lambda e, kk=kk: e.tensor_single_scalar(S(16 + kk), S(10 + kk), float(CAP), ALU.is_lt), reads=[rs], writes=[rs])
                    P.op(dve, lambda e, kk=kk: e.tensor_scalar(S(14 + kk), S(14 + kk), float(-TRASH), None, ALU.add), reads=[rs], writes=[rs])
                    P.op(dve, lambda e, kk=kk: e.tensor_tensor(S(14 + kk), S(14 + kk), S(16 + kk), ALU.mult), reads=[rs], writes=[rs])
                    P.op(dve, lambda e, kk=kk: e.tensor_scalar(S(14 + kk), S(14 + kk), float(TRASH), 0.0, ALU.add, ALU.max), reads=[rs], writes=[rs])
                    P.op(dve, lambda e, kk=kk: e.tensor_scalar(rinfo[:, ti, kk, :], S(14 + kk), float(TRASH), None, ALU.min), reads=[rs], writes=[rinfo])
                    P.op(dve, lambda e, kk=kk: e.tensor_tensor(rinfo[:, ti, 2 + kk, :], S(7 + kk), S(16 + kk), ALU.mult), reads=[rs], writes=[rinfo])
                    yield
                P.op(dve, lambda e: e.tensor_copy(sloti[:, ti, :, :], rinfo[:, ti, 0:2, :]), reads=[rinfo], writes=[sloti])
                for q in range(Q):
                    for kk in range(2):
                        P.dma(pool, lambda e, q=q, kk=kk: e.indirect_dma_start(
                            out=hs_d[:, :], out_offset=IOA(ap=sloti[:, ti, kk, q:q + 1], axis=0), in_=hfn[q][:, :], in_offset=None),
                            hfn[q], reads=[hfn[q], sloti])

            def collect(genfunc, ti):
                items = []
                orig_op, orig_dma = P.op, P.dma
                P.op = lambda eng, fn, reads=(), writes=(): items.append((orig_op, (eng, fn), dict(reads=reads, writes=writes), eng))
                P.dma = lambda q, fn, sem_buf, reads=(), writes=(): items.append((orig_dma, (q, fn, sem_buf), dict(reads=reads, writes=writes), q))
                try:
                    for tok in genfunc(ti):
                        items.append(tok)
                finally:
                    del P.op, P.dma
                return items

            def stages1(items):
                out, cur, prev = [], [], None
                for it in items:
                    if it is None or (HOP and prev is not None and it[3] is not prev):
                        out.append(cur)
                        cur = []
                    if it is None:
                        prev = None
                    else:
                        cur.append(it)
                        prev = it[3]
                out.append(cur)
                res = []
                for st in out:
                    if not st:
                        continue
                    res.append(st)
                    if LOADLAG and all(it[3] is sp and it[2]["writes"] for it in st):
                        res.extend([[] for _ in range(LOADLAG)])
                return res

            def zip_locked(chains):
                k = len(chains)
                if k == 1:
                    return chains[0]
                spans, wsets = [], []
                for L in chains:
                    fw, lr, ws = {}, {}, set()
                    for si, st in enumerate(L):
                        for (f, args, kw, eng) in st:
                            for bb in kw["writes"]:
                                fw.setdefault(id(bb), si)
                                ws.add(id(bb))
                            for bb in kw["reads"]:
                                lr[id(bb)] = si
                    spans.append({x: (fw[x], lr[x]) for x in fw if x in lr and lr[x] > fw[x]})
                    wsets.append(ws)
                out, pos, owner = [], [0] * k, {}
                while any(pos[c] < len(chains[c]) for c in range(k)):
                    progressed = False
                    for c in range(k):
                        if pos[c] >= len(chains[c]):
                            continue
                        st = chains[c][pos[c]]
                        W = set(id(bb) for (f, args, kw, eng) in st for bb in kw["writes"])
                        if any(owner.get(x) not in (None, c) for x in W):
                            continue
                        out.append(st)
                        progressed = True
                        for x in W:
                            if x in spans[c] and any(x in wsets[j] for j in range(k) if j != c):
                                owner[x] = c
                        for x in list(owner):
                            if owner[x] == c and pos[c] >= spans[c][x][1]:
                                owner[x] = None
                        pos[c] += 1
                    assert progressed, "chain lock deadlock"
                return out

            def stages(items):
                segs = [[[]]]
                for it in items:
                    if it == "SEG":
                        segs.append([[]])
                    elif it == "CHAIN":
                        segs[-1].append([])
                    else:
                        segs[-1][-1].append(it)
                out = []
                for seg in segs:
                    out += zip_locked([stages1(ch) for ch in seg if ch])  if any(seg) else []
                return out

            def spread(genfunc, ti, n, off=0):
                sts = stages(collect(genfunc, ti))
                assert len(sts) <= n - off, (len(sts), n, off)
                k = 0
                for t in range(n):
                    while t >= off and k < len(sts) and k * (n - off) < (t - off + 1) * len(sts):
                        for f, args, kw, eng in sts[k]:
                            f(*args, **kw)
                        k += 1
                    yield

            counts = [len(stages(collect(g, 1))) for g in (gen_F, gen_Mc, gen_Ml, gen_E)]
            NSTG = max(counts) + 2

            def both(g1, g2):
                for _ in g1:
                    next(g2)
                    yield

            def tile_gen(ti):
                yield from spread(gen_F, ti, NSTG)
                yield from both(spread(gen_Mc, ti, NSTG, MC_OFF), spread(gen_Ml, ti, NSTG))
                yield from spread(gen_E, ti, NSTG)

            run_pipelined((tile_gen(ti) for ti in range(NT)), depth=3, skew=NSTG)

            if debug:
                P.dma(sp, lambda e: e.dma_start(out=ri_d, in_=rinfo[:, :, :, :].rearrange("p a b c -> p (a b c)")), rinfo, reads=[rinfo])
            P.barrier()
            P.flush(block)

        with ExitStack() as sb:
            B = lambda shape, dt=F32, name=None, dma=False: P.buf(sb, shape, dt, name, dma)
            gffnbc = B([128, 8], F32, "gffnbc", dma=True)
            P.dma(sp, lambda e: e.dma_start(out=gffnbc[:, :], in_=gbc_d[1]), gffnbc, writes=[gffnbc])
            zt = B([128, D], F32, "zt", dma=True)
            P.op(dve, lambda e: e.memset(zt[:, :], 0.0), writes=[zt])
            P.dma(sp, lambda e: e.dma_start(out=y_d[NSLOT:NSLOT + 128, :], in_=zt[:, :]), zt, reads=[zt])
            w1 = [B([128, 8, 512], BF16, "w1") for _ in range(2)]
            w3 = [B([128, 8, 512], BF16, "w3") for _ in range(2)]
            w2 = [B([128, 4, D], BF16, "w2") for _ in range(2)]
            w3s = B([128, 8, 512], F32, "w3s", dma=True)
            w1s = B([128, 8, 512], F32, "w1s", dma=True)
            w2s = B([128, 4, D], F32, "w2s", dma=True)
            hst = [B([128, NSUB, D], BF16, "hst", dma=True) for _ in range(2)]
            hfTe = [B([128, 8, CAP], BF16, "hfTe") for _ in range(2)]
            actT = B([128, 4, CAP], BF16, "actT")
            tb = [B([128, 512], F32, "tb") for _ in range(2)]
            tc = [B([128, 512], F32, "tc") for _ in range(2)]
            yt = [B([128, D], F32, "yt", dma=True) for _ in range(3)]
            ntiles = [(0, 512)] if CAP == 512 else ([(n0, min(512, CAP - n0)) for n0 in range(0, CAP, 512)])
            yi = 0
            ci = 0

            def load_expert(ex):
                sl = ex % 2
                P.dma(sp, lambda e: e.dma_start(out=hst[sl][:, :, :], in_=hs_d[ex * CAP:(ex + 1) * CAP, :].rearrange("(s p) n -> p s n", p=128)),
                      hst[sl], writes=[hst[sl]])
                P.dma(sp, lambda e: e.dma_start(out=w1s[:, :, :], in_=w1_d[ex].rearrange("(k p) n -> p k n", p=128)), w1s, writes=[w1s])
                P.dma(sp, lambda e: e.dma_start(out=w3s[:, :, :], in_=w3_d[ex].rearrange("(k p) n -> p k n", p=128)), w3s, writes=[w3s])
                P.dma(sp, lambda e: e.dma_start(out=w2s[:, :, :], in_=w2_d[ex].rearrange("(k p) n -> p k n", p=128)), w2s, writes=[w2s])

            def cast_expert(ex):
                sl = ex % 2
                for k in range(8):
                    P.op(pool, lambda e, k=k: e.tensor_tensor(w1[sl][:, k, :], w1s[:, k, :], gffnbc[:, k:k + 1].broadcast_to([128, 512]), ALU.mult),
                         reads=[w1s, gffnbc], writes=[w1[sl]])
                for k in range(8):
                    P.op(act, lambda e, k=k: e.activation(out=w3[sl][:, k, :], in_=w3s[:, k, :], func=AF.Copy, scale=gffnbc[:, k:k + 1]), reads=[w3s, gffnbc], writes=[w3[sl]])
                for k in range(4):
                    P.op(act, lambda e, k=k: e.activation(out=w2[sl][:, k, :], in_=w2s[:, k, :], func=AF.Copy), reads=[w2s], writes=[w2[sl]])

            pool6 = pmm + [ps1, ps2]
            p6 = [0]

            def next6():
                bb = pool6[p6[0] % 6]
                p6[0] += 1
                return bb

            def do_T(ex):
                sl = ex % 2
                hT_e = hfTe[sl]
                for sbt in range(NSUB):
                    pt_ = ptr if sbt % 2 == 0 else ptr2

                    def fn(e, sbt=sbt, pt_=pt_, sl=sl):
                        ins = None
                        for c in range(8):
                            ins = e.transpose(pt_[:, c * 128:(c + 1) * 128], hst[sl][:, sbt, c * 128:(c + 1) * 128], ident[:, :])
                        return ins
                    P.op(pe, fn, reads=[hst[sl], ident], writes=[pt_])
                    P.op(dve, lambda e, sbt=sbt, pt_=pt_, hT_e=hT_e: e.tensor_copy(hT_e[:, :, sbt * 128:(sbt + 1) * 128], pt_[:, :].rearrange("p (c j) -> p c j", c=8)),
                         reads=[pt_], writes=[hT_e])

            def do_H(ex):
                sl = ex % 2
                hT_e = hfTe[sl]
                for (n0, nn) in ntiles:
                    for m in range(4):
                        p1, p3 = next6(), next6()
                        for (pb, wt) in ((p1, w1[sl]), (p3, w3[sl])):
                            def fn(e, pb=pb, wt=wt, m=m, n0=n0, nn=nn, hT_e=hT_e):
                                ins = None
                                for k in range(8):
                                    ins = e.matmul(pb[:, 0:nn], wt[:, k, m * 128:(m + 1) * 128], hT_e[:, k, n0:n0 + nn], start=(k == 0), stop=(k == 7))
                                return ins
                            P.op(pe, fn, reads=[wt, hT_e], writes=[pb])
                        tb_, tc_ = tb[ci_[0] % 2], tc[ci_[0] % 2]
                        ci_[0] += 1
                        P.op(act, lambda e, p1=p1, tb_=tb_, nn=nn: e.activation(out=tb_[:, 0:nn], in_=p1[:, 0:nn], func=AF.Tanh, scale=0.5), reads=[p1], writes=[tb_])
                        P.op(dve, lambda e, p1=p1, tb_=tb_, tc_=tc_, nn=nn: e.scalar_tensor_tensor(tc_[:, 0:nn], tb_[:, 0:nn], 1.0, p1[:, 0:nn], ALU.add, ALU.mult),
                             reads=[tb_, p1], writes=[tc_])
                        P.op(dve, lambda e, p3=p3, tc_=tc_, nn=nn, m=m, n0=n0: e.scalar_tensor_tensor(actT[:, m, n0:n0 + nn], tc_[:, 0:nn], 0.5, p3[:, 0:nn], ALU.mult, ALU.mult),
                             reads=[tc_, p3], writes=[actT])

            def do_Y(ex):
                sl = ex % 2
                for sbt in range(NSUB):
                    yb = yt[yi_[0] % 3]
                    yi_[0] += 1
                    for h in range(2):
                        pb = next6()

                        def fn(e, pb=pb, h=h, sbt=sbt, sl=sl):
                            ins = None
                            for m in range(4):
                                ins = e.matmul(pb[:, :], actT[:, m, sbt * 128:(sbt + 1) * 128], w2[sl][:, m, h * 512:(h + 1) * 512], start=(m == 0), stop=(m == 3))
                            return ins
                        P.op(pe, fn, reads=[actT, w2[sl]], writes=[pb])
                        P.op(act, lambda e, pb=pb, h=h, yb=yb: e.activation(out=yb[:, h * 512:(h + 1) * 512], in_=pb[:, :], func=AF.Copy), reads=[pb], writes=[yb])
                    r0 = ex * CAP + sbt * 128
                    P.dma(sp, lambda e, yb=yb, r0=r0: e.dma_start(out=y_d[r0:r0 + 128, :], in_=yb[:, :]), yb, reads=[yb])

            ci_, yi_ = [0], [0]
            load_expert(0)
            cast_expert(0)
            do_T(0)
            for ex in range(32):
                if ex + 1 < 32:
                    load_expert(ex + 1)
                do_H(ex)
                if ex + 1 < 32:
                    cast_expert(ex + 1)
                    do_T(ex + 1)
                do_Y(ex)
            P.barrier()
            P.flush(block)

        with ExitStack() as sc:
            B = lambda shape, dt=F32, name=None, dma=False: P.buf(sc, shape, dt, name, dma)
            gplebc = B([128, 8], F32, "gplebc", dma=True)
            gpp = B([128, D], F32, "gpp", dma=True)
            gfin = B([128, D], F32, "gfin", dma=True)
            wple = B([128, 2, D], BF16, "wple", dma=True)
            wpg = B([128, 8, D], BF16, "wpg", dma=True)
            P.dma(sp, lambda e: e.dma_start(out=gplebc[:, :], in_=gbc_d[2]), gplebc, writes=[gplebc])
            P.dma(sp, lambda e: e.dma_start(out=gpp[:, :], in_=rowbc_d[0]), gpp, writes=[gpp])
            P.dma(sp, lambda e: e.dma_start(out=gfin[:, :], in_=rowbc_d[1]), gfin, writes=[gfin])
            for k in range(2):
                P.dma(pool, lambda e, k=k: e.dma_start(out=wple[:, k, :], in_=wple_d[k * 128:(k + 1) * 128, :]), wple, writes=[wple])
            for k in range(8):
                P.dma(pool, lambda e, k=k: e.dma_start(out=wpg[:, k, :], in_=wpg_d[k * 128:(k + 1) * 128, :]), wpg, writes=[wpg])
            for k in range(8):
                P.op(dve, lambda e, k=k: e.tensor_scalar(wpg[:, k, :], wpg[:, k, :], gplebc[:, k:k + 1], None, ALU.mult), reads=[wpg, gplebc], writes=[wpg])
            x1t_ = [B([128, D], F32, "x1c", dma=True) for _ in range(NP_C)]
            pt_b = [B([128, 256], F32, "pc", dma=True) for _ in range(NP_C)]
            y1_ = [B([128, D], F32, "y1c", dma=True) for _ in range(NP_C)]
            y2_ = [B([128, D], F32, "y2c", dma=True) for _ in range(NP_C)]
            NP = NP_C
            ssC_ = [B([128, 16], F32, "ssC") for _ in range(NP)]
            xn3 = [B([128, D], BF16, "xn3") for _ in range(NP)]
            junk, te, ob, thg = xn3, y2_, x1t_, y1_
            x3T = [B([128, 8, 128], BF16, "x3T") for _ in range(NP)]
            pbf = [B([128, 256], BF16, "pbf") for _ in range(NP)]
            pT = [B([128, 2, 128], BF16, "pT") for _ in range(NP)]

            def rsq(ssb, i_v, i_l, i_o):
                P.op(act, lambda e: e.activation(out=ssb[:, i_l:i_l + 1], in_=ssb[:, i_v:i_v + 1], func=AF.Ln), reads=[ssb], writes=[ssb])
                P.op(act, lambda e: e.activation(out=ssb[:, i_o:i_o + 1], in_=ssb[:, i_l:i_l + 1], func=AF.Exp, scale=-0.5), reads=[ssb], writes=[ssb])

            def subtile_gen(st):
                ti, q = divmod(st, Q)
                r0 = st * 128
                i3 = st % NP
                xx, pp, y1, y2 = x1t_[i3], pt_b[i3], y1_[i3], y2_[i3]
                jk, ssC = junk[i3], ssC_[i3]
                xn_, x3_, pb_, pT_, tg_, te_, ob_ = xn3[i3], x3T[i3], pbf[i3], pT[i3], thg[i3], te[i3], ob[i3]
                P.dma(sp, lambda e: e.dma_start(out=xx[:, :], in_=x1_d[r0:r0 + 128, :]), xx, writes=[xx])
                P.dma(sp, lambda e: e.dma_start(out=pp[:, :], in_=p_d[r0:r0 + 128, :]), pp, writes=[pp])
                for (yy, kk) in ((y1, 0), (y2, 1)):
                    P.dma(pool, lambda e, yy=yy, kk=kk: e.indirect_dma_start(
                        out=yy[:, :], out_offset=None, in_=y_d[:, :], in_offset=IOA(ap=sloti[:, ti, kk, q:q + 1], axis=0)),
                        yy, reads=[sloti], writes=[yy])
                yield
                for (yy, kk) in ((y1, 0), (y2, 1)):
                    P.op(dve, lambda e, yy=yy, kk=kk: e.scalar_tensor_tensor(xx[:, :], yy[:, :], rinfo[:, ti, 2 + kk, q:q + 1], xx[:, :], ALU.mult, ALU.add),
                         reads=[yy, rinfo, xx], writes=[xx])
                yield
                P.op(act, lambda e: e.activation(out=jk[:, :], in_=xx[:, :], func=AF.Square, accum_out=ssC[:, 0:1]), reads=[xx], writes=[jk, ssC])
                P.op(act, lambda e: e.activation(out=pb_[:, :], in_=pp[:, :], func=AF.Copy), reads=[pp], writes=[pb_])
                yield
                P.op(dve, lambda e: e.tensor_scalar(ssC[:, 1:2], ssC[:, 0:1], 1.0 / D, EPS, ALU.mult, ALU.add), reads=[ssC], writes=[ssC])
                yield
                rsq(ssC, 1, 3, 2)
                P.op(act, lambda e: e.activation(out=xn_[:, :], in_=xx[:, :], func=AF.Copy, scale=ssC[:, 2:3]), reads=[xx, ssC], writes=[xn_])
                yield
                transposes(xn_, 8, ptr)
                P.op(dve, lambda e: e.tensor_copy(x3_[:, :, :], ptr[:, :].rearrange("p (c j) -> p c j", c=8)), reads=[ptr], writes=[x3_])
                transposes(pb_, 2, ptr2)
                P.op(act, lambda e: e.activation(out=pT_[:, :, :], in_=ptr2[:, 0:256].rearrange("p (c j) -> p c j", c=2), func=AF.Copy), reads=[ptr2], writes=[pT_])
                yield
                pes = []
                for h in range(2):
                    pg_ = pmm[h]

                    def fn(e, pg_=pg_, h=h):
                        ins = None
                        for k in range(8):
                            ins = e.matmul(pg_[:, :], x3_[:, k, :], wpg[:, k, h * 512:(h + 1) * 512], start=(k == 0), stop=(k == 7))
                        return ins
                    P.op(pe, fn, reads=[x3_, wpg], writes=[pg_])
                    P.op(act, lambda e, pg_=pg_, h=h: e.activation(out=tg_[:, h * 512:(h + 1) * 512], in_=pg_[:, :], func=AF.Tanh, scale=0.5), reads=[pg_], writes=[tg_])
                    pe_ = (pmm[2], pmm[3], ps1, ps2)[(2 * st + h) % 4]

                    def fn2(e, pe_=pe_, h=h):
                        ins = None
                        for k in range(2):
                            ins = e.matmul(pe_[:, :], pT_[:, k, :], wple[:, k, h * 512:(h + 1) * 512], start=(k == 0), stop=(k == 1))
                        return ins
                    P.op(pe, fn2, reads=[pT_, wple], writes=[pe_])
                    P.op(act, lambda e, pe_=pe_, h=h: e.activation(out=jk[:, 0:512], in_=pe_[:, :], func=AF.Square, accum_out=ssC[:, 4 + h:5 + h]), reads=[pe_], writes=[jk, ssC])
                    pes.append(pe_)
                yield
                P.op(dve, lambda e: e.tensor_tensor(ssC[:, 6:7], ssC[:, 4:5], ssC[:, 5:6], ALU.add), reads=[ssC], writes=[ssC])
                P.op(dve, lambda e: e.tensor_scalar(ssC[:, 7:8], ssC[:, 6:7], 4.0 / D, 4.0 * EPS, ALU.mult, ALU.add), reads=[ssC], writes=[ssC])
                yield
                rsq(ssC, 7, 9, 8)
                yield
                for h in range(2):
                    P.op(dve, lambda e, h=h, pe_=pes[h]: e.scalar_tensor_tensor(te_[:, h * 512:(h + 1) * 512], pe_[:, :], ssC[:, 8:9], gpp[:, h * 512:(h + 1) * 512], ALU.mult, ALU.mult),
                         reads=[pes[h], ssC, gpp], writes=[te_])
                P.op(dve, lambda e: e.scalar_tensor_tensor(te_[:, :], tg_[:, :], 1.0, te_[:, :], ALU.add, ALU.mult), reads=[tg_, te_], writes=[te_])
                yield
                P.op(pool, lambda e: e.tensor_tensor(xx[:, :], xx[:, :], te_[:, :], ALU.add), reads=[te_, xx], writes=[xx])
                yield
                P.op(act, lambda e: e.activation(out=jk[:, :], in_=xx[:, :], func=AF.Square, accum_out=ssC[:, 10:11]), reads=[xx], writes=[jk, ssC])
                yield
                P.op(dve, lambda e: e.tensor_scalar(ssC[:, 11:12], ssC[:, 10:11], 1.0 / D, EPS, ALU.mult, ALU.add), reads=[ssC], writes=[ssC])
                yield
                rsq(ssC, 11, 13, 12)
                yield
                P.op(dve, lambda e: e.scalar_tensor_tensor(ob_[:, :], xx[:, :], ssC[:, 12:13], gfin[:, :], ALU.mult, ALU.mult), reads=[xx, ssC, gfin], writes=[ob_])
                P.dma(sp, lambda e: e.dma_start(out=out_d[r0:r0 + 128, :], in_=ob_[:, :]), ob_, reads=[ob_])

            run_pipelined((subtile_gen(st) for st in range(T // 128)), depth=NP, skew=2)
            P.barrier()
            P.flush(block)
    return nc


def _chan(v):
    return np.ascontiguousarray(np.asarray(v, np.float32).reshape(4, 128).T)


def _gbc(g):
    return np.ascontiguousarray(np.asarray(g, np.float32).reshape(8, 128).T)


def prep_shared(inp):
    f = lambda a: np.ascontiguousarray(np.asarray(a, np.float32))
    cpar = np.zeros((128, NCP), np.float32)
    cw = f(inp["conv_dw_w"][0])
    for c in range(4):
        cpar[:, CW + c * 31:CW + (c + 1) * 31] = cw[:, c * 128:(c + 1) * 128].T
    cpar[:, CB:CB + 4] = _chan(inp["conv_dw_b"][0])
    cpar[:, LG:LG + 4] = _chan(inp["conv_ln_g"][0])
    cpar[:, LB:LB + 4] = _chan(inp["conv_ln_b"][0])
    lw = f(inp["lru_conv_w"][0])
    for c in range(4):
        cpar[:, LW + c * 4:LW + (c + 1) * 4] = lw[:, c * 128:(c + 1) * 128].T
    cpar[:, LBB:LBB + 4] = _chan(inp["lru_conv_b"][0])
    cpar[:, BR:BR + 4] = _chan(inp["lru_b_r"][0])
    cpar[:, BI:BI + 4] = _chan(inp["lru_b_i"][0])
    cpar[:, LAM:LAM + 4] = _chan(inp["lru_lambda"][0])
    wr_, wi_ = f(inp["lru_w_r"][0]), f(inp["lru_w_i"][0])
    gbd = np.zeros((4, 128, 256), np.float32)
    for c in range(4):
        for hh in range(2):
            gbd[c, hh * 64:(hh + 1) * 64, hh * 64:(hh + 1) * 64] = wr_[2 * c + hh]
            gbd[c, hh * 64:(hh + 1) * 64, 128 + hh * 64:128 + (hh + 1) * 64] = wi_[2 * c + hh]
    rb = np.concatenate([f(inp["b_group"][0]), f(inp["b_expert"][0])])
    shared = {
        "w_in": f(inp["w_in"][0]), "w_out": f(inp["w_out"][0]),
        "w_route": np.ascontiguousarray(np.concatenate([f(inp["w_group"][0]), f(inp["w_expert"][0])], axis=1)),
        "gate_bd": gbd, "cpar": cpar,
        "gbc": np.stack([_gbc(inp["g_mix"][0]), _gbc(inp["g_ffn"][0]), _gbc(inp["g_ple"][0])]),
        "rowbc": np.stack([np.ascontiguousarray(np.broadcast_to(f(inp["g_ple_proj"][0]), (128, D))),
                           np.ascontiguousarray(np.broadcast_to(f(inp["g_final"]), (128, D)))]),
        "rbias": np.ascontiguousarray(np.broadcast_to(np.tile(rb, Q), (128, Q * 36))),
        "iota_e": np.ascontiguousarray(np.broadcast_to(np.tile(np.arange(32, dtype=np.float32), Q), (128, Q * 32))),
        "ident": np.eye(128, dtype=np.float32),
        "tri": np.ascontiguousarray(np.triu(np.ones((128, 128), np.float32), 1)),
        "w1": f(inp["w1"][0]), "w3": f(inp["w3"][0]), "w2": f(inp["w2"][0]),
        "w_ple": f(inp["w_ple"][0]), "w_ple_gate": f(inp["w_ple_gate"][0]),
    }
    return shared


def kernel(**inputs):
    NSEQ, CAP = 4, 1024
    x = np.asarray(inputs["x"], np.float32)
    p = np.asarray(inputs["p"], np.float32)[0]
    shared = prep_shared(inputs)
    nc = build(NSEQ, CAP)
    in_maps = []
    for i in range(N_CORES):
        m = dict(shared)
        m["x"] = np.ascontiguousarray(x[i * NSEQ:(i + 1) * NSEQ].reshape(NSEQ * SEQ, D))
        m["p"] = np.ascontiguousarray(p[i * NSEQ:(i + 1) * NSEQ].reshape(NSEQ * SEQ, 256))
        in_maps.append(m)
    res = run_bass_kernel_spmd(nc, in_maps, core_ids=list(range(N_CORES)))
    out = np.concatenate([np.asarray(r["out"], np.float32).reshape(NSEQ, SEQ, D) for r in res.results], axis=0)
    return out
```

```python
import numpy as np
from contextlib import ExitStack
import concourse.bass as bass
import concourse.mybir as mybir
from concourse.bass_utils import run_bass_kernel_spmd

F32 = mybir.dt.float32
BF16 = mybir.dt.bfloat16
I32 = mybir.dt.int32
ALU = mybir.AluOpType
AF = mybir.ActivationFunctionType
AX = mybir.AxisListType

N_CORES = 8
HOP = True
NP_C = 8
LOADLAG = 3
MC_OFF = 0
SEQ = 2048
D = 1024
EPS = 1e-6
Q = 4

CW = 0
CB = CW + 124
LG = CB + 4
LB = LG + 4
LW = LB + 4
LBB = LW + 16
BR = LBB + 4
BI = BR + 4
LAM = BI + 4
NCP = LAM + 4
D_CWH = 0
D_GH = 124
D_BH = 128
D_BRH = 132
D_BIH = 136
D_N4 = 140
D_N8 = 144
NDP = 148


class Src:
    def __init__(self, sem, name, is_dma):
        self.sem, self.name, self.is_dma, self.total = sem, name, is_dma, 0


class Eng(Src):
    def __init__(self, sem, name, blockname, same_wait=True):
        super().__init__(sem, name, False)
        self.blockname, self.ops, self.seen, self.same_wait = blockname, [], {}, same_wait


class Buf:
    def __init__(self, t, dsem=None):
        self.t, self.w, self.r, self.dsem = t, None, {}, dsem

    def __getitem__(self, k):
        return self.t[k]


class Prog:
    def __init__(self, nc, stack):
        self.nc, self.stack = nc, stack
        self.srcs = []
        mk = lambda n, b, sw=True: self._reg(Eng(self._sem("e_" + n), n, b, sw))
        self.pe = mk("pe", "tensor", False)
        self.act = mk("act", "scalar")
        self.dve = mk("dve", "vector")
        self.pool = mk("pool", "gpsimd")
        self.sp = mk("sp", "sync")
        self.engs = [self.pe, self.act, self.dve, self.pool, self.sp]
        self.nbuf = 0

    def _sem(self, name):
        return self.stack.enter_context(self.nc.semaphore(name))

    def _reg(self, s):
        self.srcs.append(s)
        return s

    def buf(self, stack, shape, dt, name=None, dma=False, psum=False):
        self.nbuf += 1
        name = "%s_%d" % (name or "b", self.nbuf)
        if psum:
            t = stack.enter_context(self.nc.psum_tensor(name, shape, dt))
        else:
            t = stack.enter_context(self.nc.sbuf_tensor(name, shape, dt))
        ds = self._reg(Src(self._sem("d_" + name), name, True)) if dma else None
        return Buf(t, ds)

    def _deps(self, eng, reads, writes):
        need = {}

        def add(src, val):
            if src.is_dma:
                val = src.total
            if need.get(src, 0) < val:
                need[src] = val

        for b in reads:
            if b.w is not None:
                add(*b.w)
        for b in writes:
            if b.w is not None:
                add(*b.w)
            for s, v in b.r.items():
                add(s, v)
        waits = []
        for src, val in need.items():
            if src is eng and not eng.same_wait:
                continue
            if eng.seen.get(src, 0) >= val:
                continue
            eng.seen[src] = val
            waits.append((src.sem, val))
        return waits

    def _mark(self, src, val, reads, writes):
        for b in reads:
            b.r[src] = val
        for b in writes:
            b.w = (src, val)
            b.r = {}

    def op(self, eng, fn, reads=(), writes=()):
        waits = self._deps(eng, reads, writes)
        eng.total += 1
        sem = eng.sem

        def emit(e):
            for s, v in waits:
                e.wait_ge(s, v)
            fn(e).then_inc(sem, 1)

        eng.ops.append(emit)
        self._mark(eng, eng.total, reads, writes)

    def dma(self, q, fn, sem_buf, reads=(), writes=()):
        src = sem_buf.dsem
        waits = self._deps(q, reads, writes)
        src.total += 16
        sem = src.sem

        def emit(e):
            for s, v in waits:
                e.wait_ge(s, v)
            fn(e).then_inc(sem, 16)

        q.ops.append(emit)
        self._mark(src, src.total, reads, writes)

    def barrier(self):
        for E in self.engs:
            waits = []
            for S in self.srcs:
                if S is E or S.total == 0:
                    continue
                if E.seen.get(S, 0) >= S.total:
                    continue
                E.seen[S] = S.total
                waits.append((S.sem, S.total))

            def emit(e, waits=waits):
                for s, v in waits:
                    e.wait_ge(s, v)

            E.ops.append(emit)

    def flush(self, block):
        for E in self.engs:
            if not E.ops:
                continue
            ops = E.ops
            E.ops = []

            def body(e, ops=ops):
                for f in ops:
                    f(e)

            getattr(block, E.blockname)(body)


def run_pipelined(gens, depth, skew):
    it = iter(gens)
    active, pending, tick = [], True, 0
    while pending or active:
        for g in list(active):
            try:
                next(g)
            except StopIteration:
                active.remove(g)
        if pending and tick % skew == 0 and len(active) < depth:
            try:
                g = next(it)
                active.append(g)
                next(g)
            except StopIteration:
                pending = False
        tick += 1


def build(NSEQ=4, CAP=640, debug=False):
    T = NSEQ * SEQ
    NT = T // 512
    NSUB = CAP // 128
    NSLOT = 32 * CAP
    TRASH = NSLOT
    nc = bass.Bass("TRN2", target_bir_lowering=False)

    def dr(name, shape, dt=F32, kind="ExternalInput"):
        return nc.dram_tensor(name, shape, dt, kind=kind).ap()

    x_d = dr("x", [T, D])
    p_d = dr("p", [T, 256])
    win_d = dr("w_in", [D, 2048])
    wout_d = dr("w_out", [D, D])
    wr_d = dr("w_route", [D, 36])
    gbd_d = dr("gate_bd", [4, 128, 256])
    cpar_d = dr("cpar", [128, NCP])
    gbc_d = dr("gbc", [3, 128, 8])
    rowbc_d = dr("rowbc", [2, 128, D])
    rb_d = dr("rbias", [128, Q * 36])
    iota_d = dr("iota_e", [128, Q * 32])
    ident_d = dr("ident", [128, 128])
    tri_d = dr("tri", [128, 128])
    w1_d = dr("w1", [32, D, 512])
    w3_d = dr("w3", [32, D, 512])
    w2_d = dr("w2", [32, 512, D])
    wple_d = dr("w_ple", [256, D])
    wpg_d = dr("w_ple_gate", [D, D])
    out_d = dr("out", [T, D], kind="ExternalOutput")
    sk = "ExternalOutput" if debug else "Internal"
    x1_d = dr("x1s", [T, D], kind=sk)
    hs_d = dr("hss", [NSLOT + 128, D], BF16, kind=sk)
    y_d = dr("yss", [NSLOT + 128, D], kind=sk)
    if debug:
        ri_d = dr("rinfo_o", [128, NT * 4 * Q], kind="ExternalOutput")

    IOA = bass.IndirectOffsetOnAxis

    with ExitStack() as top:
        P = Prog(nc, top)
        pe, act, dve, pool, sp = P.pe, P.act, P.dve, P.pool, P.sp
        block = top.enter_context(nc.Block())

        pmm = [P.buf(top, [128, 512], F32, "pmm", psum=True) for _ in range(4)]
        ptr = P.buf(top, [128, 1024], BF16, "ptr", psum=True)
        ptr2 = P.buf(top, [128, 1024], BF16, "ptr2", psum=True)
        ps1 = P.buf(top, [128, 512], F32, "ps1", psum=True)
        ps2 = P.buf(top, [128, 512], F32, "ps2", psum=True)
        pmm_i = [0]

        def next_pmm():
            b = pmm[pmm_i[0] % 4]
            pmm_i[0] += 1
            return b

        rinfo = P.buf(top, [128, NT, 4, Q], F32, "rinfo")
        sloti = P.buf(top, [128, NT, 2, Q], I32, "sloti")
        if debug:
            rinfo.dsem = P._reg(Src(P._sem("d_rinfo"), "rinfo", True))
        ident = P.buf(top, [128, 128], BF16, "ident", dma=True)
        P.dma(pool, lambda e: e.dma_start(out=ident[:, :], in_=ident_d), ident, writes=[ident])

        def transposes(src, n, dst_ps):
            def fn(e):
                ins = None
                for c in range(n):
                    ins = e.transpose(dst_ps[:, c * 128:(c + 1) * 128], src[:, c * 128:(c + 1) * 128], ident[:, :])
                return ins
            P.op(pe, fn, reads=[src, ident], writes=[dst_ps])

        def rstd_from_ss(ss, out, n):
            P.op(dve, lambda e: e.tensor_scalar(out, ss, 1.0 / n, EPS, ALU.mult, ALU.add), reads=[], writes=[])

        with ExitStack() as sa:
            B = lambda shape, dt=F32, name=None, dma=False: P.buf(sa, shape, dt, name, dma)
            win = B([128, 8, 2048], BF16, "win", dma=True)
            wout = B([128, 8, D], BF16, "wout", dma=True)
            wr = B([128, 8, 36], BF16, "wr", dma=True)
            gbd = B([128, 4, 256], BF16, "gbd", dma=True)
            tri = B([128, 128], BF16, "tri", dma=True)
            cpar = B([128, NCP], F32, "cpar", dma=True)
            dpar = B([128, NDP], F32, "dpar")
            gmixbc = B([128, 8], F32, "gmixbc", dma=True)
            gffnbc = B([128, 8], F32, "gffnbc", dma=True)
            rbias = B([128, Q, 36], F32, "rbias", dma=True)
            iota = B([128, Q, 32], F32, "iota", dma=True)
            ones = B([128, 128], BF16, "ones")
            identF = B([128, 128], F32, "identF", dma=True)
            P.dma(sp, lambda e: e.dma_start(out=identF[:, :], in_=ident_d), identF, writes=[identF])
            cntbc = B([128, 32], F32, "cntbc")

            P.dma(sp, lambda e: e.dma_start(out=cpar[:, :], in_=cpar_d), cpar, writes=[cpar])
            P.dma(sp, lambda e: e.dma_start(out=gmixbc[:, :], in_=gbc_d[0]), gmixbc, writes=[gmixbc])
            P.dma(sp, lambda e: e.dma_start(out=gffnbc[:, :], in_=gbc_d[1]), gffnbc, writes=[gffnbc])
            P.dma(sp, lambda e: e.dma_start(out=rbias[:, :, :], in_=rb_d.rearrange("p (q n) -> p q n", q=Q)), rbias, writes=[rbias])
            P.dma(sp, lambda e: e.dma_start(out=iota[:, :, :], in_=iota_d.rearrange("p (q n) -> p q n", q=Q)), iota, writes=[iota])
            P.dma(pool, lambda e: e.dma_start(out=tri[:, :], in_=tri_d), tri, writes=[tri])
            for k in range(8):
                P.dma(pool, lambda e, k=k: e.dma_start(out=win[:, k, :], in_=win_d[k * 128:(k + 1) * 128, :]), win, writes=[win])
            P.dma(pool, lambda e: e.dma_start(out=gbd[:, :, :], in_=gbd_d.rearrange("c p n -> p c n")), gbd, writes=[gbd])
            for k in range(8):
                P.dma(pool, lambda e, k=k: e.dma_start(out=wout[:, k, :], in_=wout_d[k * 128:(k + 1) * 128, :]), wout, writes=[wout])
            P.dma(pool, lambda e: e.dma_start(out=wr[:, :, :], in_=wr_d.rearrange("(k p) n -> p k n", p=128)), wr, writes=[wr])

            P.op(dve, lambda e: e.memset(ones[:, :], 1.0), writes=[ones])
            P.op(dve, lambda e: e.memset(cntbc[:, :], 0.0), writes=[cntbc])
            for k in range(8):
                P.op(dve, lambda e, k=k: e.tensor_scalar(win[:, k, :], win[:, k, :], gmixbc[:, k:k + 1], None, ALU.mult), reads=[win, gmixbc], writes=[win])
                P.op(dve, lambda e, k=k: e.tensor_scalar(wr[:, k, :], wr[:, k, :], gffnbc[:, k:k + 1], None, ALU.mult), reads=[wr, gffnbc], writes=[wr])

            tsm = B([128, 8, 4], F32, "tsm")

            def dv(fn, reads, writes):
                P.op(dve, fn, reads=reads, writes=writes)

            dv(lambda e: e.tensor_scalar(dpar[:, D_CWH:D_CWH + 124], cpar[:, CW:CW + 124], 0.5, None, ALU.mult), [cpar], [dpar])
            dv(lambda e: e.tensor_scalar(dpar[:, D_GH:D_GH + 8], cpar[:, LG:LG + 8], 0.5, None, ALU.mult), [cpar], [dpar])
            dv(lambda e: e.tensor_scalar(dpar[:, D_BRH:D_BRH + 8], cpar[:, BR:BR + 8], 0.5, None, ALU.mult), [cpar], [dpar])
            z_, az, ee, LL, tt, mk_, zp = [tsm[:, i, :] for i in range(7)]
            dv(lambda e: e.tensor_scalar(z_, cpar[:, LAM:LAM + 4], -1.0, None, ALU.mult), [cpar], [tsm])
            dv(lambda e: e.tensor_tensor(az, z_, cpar[:, LAM:LAM + 4], ALU.max), [tsm, cpar], [tsm])
            P.op(act, lambda e: e.activation(out=ee, in_=az, func=AF.Exp, scale=-1.0), reads=[tsm], writes=[tsm])
            P.op(act, lambda e: e.activation(out=LL, in_=ee, func=AF.Ln, bias=1.0, scale=1.0), reads=[tsm], writes=[tsm])
            dv(lambda e: e.tensor_scalar(tt, ee, -0.25, 1.0 / 3.0, ALU.mult, ALU.add), [tsm], [tsm])
            dv(lambda e: e.tensor_tensor(tt, tt, ee, ALU.mult), [tsm], [tsm])
            dv(lambda e: e.tensor_scalar(tt, tt, -1.0, 0.5, ALU.mult, ALU.add), [tsm], [tsm])
            dv(lambda e: e.tensor_tensor(tt, tt, ee, ALU.mult), [tsm], [tsm])
            dv(lambda e: e.tensor_scalar(tt, tt, -1.0, 1.0, ALU.mult, ALU.add), [tsm], [tsm])
            dv(lambda e: e.tensor_tensor(tt, tt, ee, ALU.mult), [tsm], [tsm])
            dv(lambda e: e.tensor_single_scalar(mk_, ee, 0.05, ALU.is_lt), [tsm], [tsm])
            dv(lambda e: e.tensor_tensor(tt, tt, LL, ALU.subtract), [tsm], [tsm])
            dv(lambda e: e.tensor_tensor(tt, tt, mk_, ALU.mult), [tsm], [tsm])
            dv(lambda e: e.tensor_tensor(tt, tt, LL, ALU.add), [tsm], [tsm])
            dv(lambda e: e.tensor_single_scalar(zp, z_, 0.0, ALU.max), [tsm], [tsm])
            dv(lambda e: e.tensor_tensor(tt, tt, zp, ALU.add), [tsm], [tsm])
            dv(lambda e: e.tensor_scalar(dpar[:, D_N4:D_N4 + 4], tt, -4.0, None, ALU.mult), [tsm], [dpar])
            dv(lambda e: e.tensor_scalar(dpar[:, D_N8:D_N8 + 4], tt, -8.0, None, ALU.mult), [tsm], [dpar])

            xa = [B([128, D], F32, "xa", dma=True) for _ in range(2)]
            ssF = [B([128, 8], F32, "ssF") for _ in range(2)]
            xn = [B([128, D], BF16, "xn") for _ in range(2)]
            hT = B([128, 8, 512], BF16, "hT")
            gth = [B([128, 512], F32, "gth") for _ in range(2)]
            gvs = [B([128, 512], F32, "gvs") for _ in range(2)]
            ga = B([128, 512], F32, "ga")
            gb = B([128, 512], F32, "gb")
            ub = [[B([128, 542], BF16, "ub") for _ in range(4)] for _ in range(2)]
            xbuf = [[B([128, 515], BF16, "xbuf") for _ in range(4)] for _ in range(2)]
            qg = [[B([128, 512], BF16, "qg") for _ in range(4)] for _ in range(2)]
            acc2 = [[B([128, 512], F32, "acc") for _ in range(4)] for _ in range(2)]
            cvbf = [B([128, 512], BF16, "cvbf") for _ in range(4)]
            sqbf = [B([128, 512], BF16, "sqbf") for _ in range(4)]
            mean = B([128, 512], F32, "mean")
            msq = B([128, 512], F32, "msq")
            xr = [B([128, 512], F32, "xr") for _ in range(2)]
            xrbf = [B([128, 512], BF16, "xrbf") for _ in range(2)]
            t1 = [B([128, 512], F32, "t1") for _ in range(2)]
            t2 = [B([128, 512], F32, "t2") for _ in range(2)]
            t3 = [B([128, 512], F32, "t3") for _ in range(2)]
            lh, th = t1, t2
            ab = [B([128, 512], F32, "ab")] * 2
            hb = [B([128, 512], F32, "hb")] * 2
            carry = [B([128, 1], F32, "carry") for _ in range(4)]
            yT = [B([128, 8, 512], BF16, "yT") for _ in range(2)]
            xb = B([128, D], F32, "xb", dma=True)
            ssE = [B([128, 8], F32, "ssE") for _ in range(2)]
            x1 = [B([128, D], F32, "x1", dma=True) for _ in range(2)]
            hfn = [B([128, D], BF16, "hfn", dma=True) for _ in range(4)]
            hfT = [B([128, 8, 128], BF16, "hfT") for _ in range(2)]
            rs = B([128, 40, Q], F32, "rs")
            r36 = B([128, Q, 36], F32, "r36")
            r32 = [B([128, Q, 32], F32, "r32") for _ in range(5)]
            mbf = B([128, Q, 32], BF16, "mbf")
            r8 = [B([128, Q, 8], F32, "r8") for _ in range(4)]
            r4 = [B([128, Q, 4], F32, "r4") for _ in range(3)]

            cwh = lambda c, k: dpar[:, D_CWH + c * 31 + k:D_CWH + c * 31 + k + 1]
            NJ = SEQ // 512

            def rsqA(ssb, i_v, i_l, i_o):
                P.op(act, lambda e: e.activation(out=ssb[:, i_l:i_l + 1], in_=ssb[:, i_v:i_v + 1], func=AF.Ln), reads=[ssb], writes=[ssb])
                P.op(act, lambda e: e.activation(out=ssb[:, i_o:i_o + 1], in_=ssb[:, i_l:i_l + 1], func=AF.Exp, scale=-0.5), reads=[ssb], writes=[ssb])

            def evac_scaled(dst3, ps, gvec):
                P.op(act, lambda e: e.activation(out=dst3.all(), in_=ps[:, :].rearrange("p (c j) -> p c j", c=8), func=AF.Copy),
                     reads=[ps], writes=[dst3.buf])

            class View3:
                def __init__(self, buf, fn, allfn=None):
                    self.buf, self.fn, self.all = buf, fn, allfn

                def __call__(self, c):
                    return self.fn(c)

            def gen_F(ti):
                s_, j = divmod(ti, NJ)
                row0 = ti * 512
                par = ti % 2
                ub_, xbuf_, qg_ = ub[par], xbuf[par], qg[par]
                if j == 0:
                    for c in range(4):
                        P.op(pool, lambda e, c=c: e.memset(ub_[c][:, 0:30], 0.0), writes=[ub_[c]])
                        P.op(pool, lambda e, c=c: e.memset(xbuf_[c][:, 0:3], 0.0), writes=[xbuf_[c]])
                for q in range(Q):
                    yield ("SEG" if q % 2 == 0 else "CHAIN")
                    xt, xnb, ss = xa[q % 2], xn[q % 2], ssF[q % 2]
                    r0 = row0 + q * 128
                    P.dma(sp, lambda e, xt=xt, r0=r0: e.dma_start(out=xt[:, :], in_=x_d[r0:r0 + 128, :]), xt, writes=[xt])
                    P.op(act, lambda e, xt=xt, ss=ss, xnb=xnb: e.activation(out=xnb[:, :], in_=xt[:, :], func=AF.Square, accum_out=ss[:, 0:1]),
                         reads=[xt], writes=[xnb, ss])
                    P.op(dve, lambda e, ss=ss: e.tensor_scalar(ss[:, 1:2], ss[:, 0:1], 1.0 / D, EPS, ALU.mult, ALU.add), reads=[ss], writes=[ss])
                    rsqA(ss, 1, 3, 2)
                    P.op(act, lambda e, xt=xt, xnb=xnb, ss=ss: e.activation(out=xnb[:, :], in_=xt[:, :], func=AF.Copy, scale=ss[:, 2:3]),
                         reads=[xt, ss], writes=[xnb])
                    transposes(xnb, 8, ptr)
                    evac_scaled(View3(hT, None, lambda q=q: hT[:, :, q * 128:(q + 1) * 128]), ptr, gmixbc)

                fbank = [pmm[0], ps2]

                def zmm(m, bi):
                    pb = fbank[bi % 2]

                    def fn(e, m=m, pb=pb):
                        ins = None
                        for k in range(8):
                            ins = e.matmul(pb[:, :], win[:, k, m * 128:(m + 1) * 128], hT[:, k, :], start=(k == 0), stop=(k == 7))
                        return ins
                    P.op(pe, fn, reads=[win, hT], writes=[pb])
                    return pb

                for c in range(4):
                    yield ("SEG" if c % 2 == 0 else "CHAIN")
                    pg = zmm(4 + c, c)
                    ta, vs = gth[c % 2], gvs[c % 2]
                    P.op(act, lambda e, pg=pg, ta=ta: e.activation(out=ta[:, :], in_=pg[:, :], func=AF.Tanh, scale=0.5), reads=[pg], writes=[ta])
                    pv = zmm(c, c)
                    P.op(act, lambda e, pv=pv, vs=vs: e.activation(out=vs[:, :], in_=pv[:, :], func=AF.Copy), reads=[pv], writes=[vs])
                    P.op(pool, lambda e, ta=ta, vs=vs: e.tensor_tensor(ta[:, :], ta[:, :], vs[:, :], ALU.mult), reads=[ta, vs], writes=[ta])
                    P.op(pool, lambda e, ta=ta, vs=vs, c=c: e.tensor_tensor(ub_[c][:, 30:542], ta[:, :], vs[:, :], ALU.add), reads=[ta, vs], writes=[ub_[c]])
                for c in range(4):
                    yield ("SEG" if c % 2 == 0 else "CHAIN")
                    px = zmm(8 + c, c)
                    P.op(act, lambda e, px=px, c=c: e.activation(out=xbuf_[c][:, 3:515], in_=px[:, :], func=AF.Copy), reads=[px], writes=[xbuf_[c]])
                for c in range(4):
                    yield "SEG"
                    pgl = zmm(12 + c, c)
                    P.op(act, lambda e, pgl=pgl, c=c: e.activation(out=qg_[c][:, :], in_=pgl[:, :], func=AF.Gelu_apprx_tanh), reads=[pgl], writes=[qg_[c]])

            def gen_Mc(ti):
                s_, j = divmod(ti, NJ)
                par = ti % 2
                ub_, xbuf_, qg_, yT_ = ub[par], xbuf[par], qg[par], yT[par]
                ubn, xbufn = ub[1 - par], xbuf[1 - par]
                acc = acc2[par]
                for k in range(31):
                    for c in range(4):
                        if k == 0:
                            P.op(dve, lambda e, c=c: e.tensor_scalar(acc[c][:, :], ub_[c][:, 0:512], cwh(c, 0), cpar[:, CB + c:CB + c + 1], ALU.mult, ALU.add),
                                 reads=[ub_[c], dpar, cpar], writes=[acc[c]])
                        else:
                            P.op(dve, lambda e, c=c, k=k: e.scalar_tensor_tensor(acc[c][:, :], ub_[c][:, k:k + 512], cwh(c, k), acc[c][:, :], ALU.mult, ALU.add),
                                 reads=[ub_[c], dpar, acc[c]], writes=[acc[c]])
                    yield
                for c in range(4):
                    if j < NJ - 1:
                        P.op(pool, lambda e, c=c: e.tensor_copy(ubn[c][:, 0:30], ub_[c][:, 512:542]), reads=[ub_[c]], writes=[ubn[c]])
                    P.op(act, lambda e, c=c: e.activation(out=cvbf[c][:, :], in_=acc[c][:, :], func=AF.Copy), reads=[acc[c]], writes=[cvbf[c]])
                    P.op(act, lambda e, c=c: e.activation(out=sqbf[c][:, :], in_=acc[c][:, :], func=AF.Square), reads=[acc[c]], writes=[sqbf[c]])

                def stat_mm(dst, srcs):
                    def fn(e):
                        ins = None
                        for c in range(4):
                            ins = e.matmul(dst[:, :], ones[:, :], srcs[c][:, :], start=(c == 0), stop=(c == 3))
                        return ins
                    P.op(pe, fn, reads=[ones] + srcs, writes=[dst])
                stat_mm(ps1, cvbf)
                P.op(act, lambda e: e.activation(out=mean[:, :], in_=ps1[:, :], func=AF.Copy, scale=1.0 / 512), reads=[ps1], writes=[mean])
                stat_mm(ps1, sqbf)
                P.op(pool, lambda e: e.tensor_tensor(msq[:, :], mean[:, :], mean[:, :], ALU.mult), reads=[mean], writes=[msq])
                P.op(dve, lambda e: e.scalar_tensor_tensor(msq[:, :], ps1[:, :], 1.0 / 512, msq[:, :], ALU.mult, ALU.subtract), reads=[ps1, msq], writes=[msq])
                P.op(dve, lambda e: e.tensor_scalar(msq[:, :], msq[:, :], EPS, None, ALU.add), reads=[msq], writes=[msq])
                P.op(act, lambda e: e.activation(out=msq[:, :], in_=msq[:, :], func=AF.Ln), reads=[msq], writes=[msq])
                P.op(act, lambda e: e.activation(out=msq[:, :], in_=msq[:, :], func=AF.Exp, scale=-0.5), reads=[msq], writes=[msq])
                yield
                for c in range(4):
                    P.op(pool, lambda e, c=c: e.tensor_tensor(acc[c][:, :], acc[c][:, :], mean[:, :], ALU.subtract), reads=[acc[c], mean], writes=[acc[c]])
                    P.op(pool, lambda e, c=c: e.tensor_tensor(acc[c][:, :], acc[c][:, :], msq[:, :], ALU.mult), reads=[acc[c], msq], writes=[acc[c]])
                for c in range(4):
                    P.op(act, lambda e, c=c: e.activation(out=yT_[:, c, :], in_=acc[c][:, :], func=AF.Silu,
                                                          bias=cpar[:, LB + c:LB + c + 1], scale=cpar[:, LG + c:LG + c + 1]),
                         reads=[acc[c], cpar], writes=[yT_])
            def gen_Ml(ti):
                s_, j = divmod(ti, NJ)
                par = ti % 2
                xbuf_, qg_, yT_ = xbuf[par], qg[par], yT[par]
                xbufn = xbuf[1 - par]
                if j == 0:
                    for c in range(4):
                        P.op(pool, lambda e, c=c: e.memset(carry[c][:, :], 0.0), writes=[carry[c]])
                for c in range(4):
                    xr_, xrb_ = xr[c % 2], xrbf[c % 2]
                    lw = lambda k, c=c: cpar[:, LW + c * 4 + k:LW + c * 4 + k + 1]
                    P.op(dve, lambda e, c=c, xr_=xr_, lw=lw: e.tensor_scalar(xr_[:, :], xbuf_[c][:, 0:512], lw(0), cpar[:, LBB + c:LBB + c + 1], ALU.mult, ALU.add),
                         reads=[xbuf_[c], cpar], writes=[xr_])
                    for k in range(1, 4):
                        P.op(dve, lambda e, c=c, k=k, xr_=xr_, lw=lw: e.scalar_tensor_tensor(xr_[:, :], xbuf_[c][:, k:k + 512], lw(k), xr_[:, :], ALU.mult, ALU.add),
                             reads=[xbuf_[c], cpar, xr_], writes=[xr_])
                    if j < NJ - 1:
                        P.op(pool, lambda e, c=c: e.tensor_copy(xbufn[c][:, 0:3], xbuf_[c][:, 512:515]), reads=[xbuf_[c]], writes=[xbufn[c]])
                    P.op(act, lambda e, xr_=xr_, xrb_=xrb_: e.activation(out=xrb_[:, :], in_=xr_[:, :], func=AF.Copy), reads=[xr_], writes=[xrb_])
                    pr, pi = pmm[1], pmm[1]
                    P.op(pe, lambda e, c=c, pr=pr, xrb_=xrb_: e.matmul(pr[:, :], gbd[:, c, 0:128], xrb_[:, :], start=True, stop=True), reads=[gbd, xrb_], writes=[pr])
                    a1, a2, a3, aa, hh = t1[c % 2], t2[c % 2], t3[c % 2], ab[c % 2], hb[c % 2]
                    dp = lambda o, c=c: dpar[:, o + c:o + c + 1]
                    yield
                    P.op(act, lambda e, pr=pr, a1=a1, dp=dp: e.activation(out=a1[:, :], in_=pr[:, :], func=AF.Tanh, bias=dp(D_BRH), scale=0.5), reads=[pr, dpar], writes=[a1])
                    P.op(pe, lambda e, c=c, pi=pi, xrb_=xrb_: e.matmul(pi[:, :], gbd[:, c, 128:256], xrb_[:, :], start=True, stop=True), reads=[gbd, xrb_], writes=[pi])
                    P.op(act, lambda e, pi=pi, a3=a3, dp=dp: e.activation(out=a3[:, :], in_=pi[:, :], func=AF.Tanh, bias=dp(D_BIH), scale=0.5), reads=[pi, dpar], writes=[a3])
                    P.op(act, lambda e, a1=a1, aa=aa, dp=dp: e.activation(out=aa[:, :], in_=a1[:, :], func=AF.Exp, bias=dp(D_N4), scale=dp(D_N4)), reads=[a1, dpar], writes=[aa])
                    P.op(act, lambda e, a1=a1, a2=a2, dp=dp: e.activation(out=a2[:, :], in_=a1[:, :], func=AF.Exp, bias=dp(D_N8), scale=dp(D_N8)), reads=[a1, dpar], writes=[a2])
                    P.op(dve, lambda e, a2=a2: e.tensor_scalar(a2[:, :], a2[:, :], 0.99999994, -1.0, ALU.min, ALU.mult), reads=[a2], writes=[a2])
                    P.op(act, lambda e, a2=a2: e.activation(out=a2[:, :], in_=a2[:, :], func=AF.Ln, bias=1.0, scale=1.0), reads=[a2], writes=[a2])
                    P.op(act, lambda e, a2=a2: e.activation(out=a2[:, :], in_=a2[:, :], func=AF.Exp, scale=0.5), reads=[a2], writes=[a2])
                    P.op(dve, lambda e, a3=a3, xr_=xr_: e.scalar_tensor_tensor(a3[:, :], a3[:, :], 1.0, xr_[:, :], ALU.add, ALU.mult), reads=[a3, xr_], writes=[a3])
                    yield
                    P.op(dve, lambda e, a2=a2, a3=a3: e.tensor_tensor(a3[:, :], a3[:, :], a2[:, :], ALU.mult), reads=[a2, a3], writes=[a3])
                    P.op(dve, lambda e, c=c, aa=aa, a3=a3, hh=hh: e.tensor_tensor_scan(hh[:, :], aa[:, :], a3[:, :], carry[c][:, 0:1], ALU.mult, ALU.add),
                         reads=[aa, a3, carry[c]], writes=[hh])
                    P.op(dve, lambda e, c=c, hh=hh: e.tensor_copy(carry[c][:, :], hh[:, 511:512]), reads=[hh], writes=[carry[c]])
                    P.op(dve, lambda e, c=c, hh=hh: e.scalar_tensor_tensor(yT_[:, 4 + c, :], hh[:, :], 0.5, qg_[c][:, :], ALU.mult, ALU.mult),
                         reads=[hh, qg_[c]], writes=[yT_])
                    yield

            def gen_E(ti):
                row0 = ti * 512
                yT_ = yT[ti % 2]
                for q in range(Q):
                    yield ("SEG" if q % 2 == 0 else "CHAIN")
                    r0 = row0 + q * 128
                    x1t, ss = x1[q % 2], ssE[q % 2]
                    hf = hfn[q]
                    hft = hfT[q % 2]
                    P.dma(sp, lambda e, r0=r0: e.dma_start(out=xb[:, :], in_=x_d[r0:r0 + 128, :]), xb, writes=[xb])
                    for h in range(2):
                        pb = pmm[2]

                        def fn(e, pb=pb, h=h, q=q):
                            ins = e.matmul(pb[:, :], identF[:, :], xb[:, h * 512:(h + 1) * 512], start=True, stop=False)
                            for k in range(8):
                                ins = e.matmul(pb[:, :], yT_[:, k, q * 128:(q + 1) * 128], wout[:, k, h * 512:(h + 1) * 512], start=False, stop=(k == 7))
                            return ins
                        P.op(pe, fn, reads=[yT_, wout, identF, xb], writes=[pb])
                        P.op(act, lambda e, pb=pb, h=h, x1t=x1t: e.activation(out=x1t[:, h * 512:(h + 1) * 512], in_=pb[:, :], func=AF.Copy),
                             reads=[pb], writes=[x1t])
                    P.dma(sp, lambda e, x1t=x1t, r0=r0: e.dma_start(out=x1_d[r0:r0 + 128, :], in_=x1t[:, :]), x1t, reads=[x1t])
                    P.op(act, lambda e, x1t=x1t, ss=ss, hf=hf: e.activation(out=hf[:, :], in_=x1t[:, :], func=AF.Square, accum_out=ss[:, 0:1]), reads=[x1t], writes=[hf, ss])
                    P.op(dve, lambda e, ss=ss: e.tensor_scalar(ss[:, 1:2], ss[:, 0:1], 1.0 / D, EPS, ALU.mult, ALU.add), reads=[ss], writes=[ss])
                    rsqA(ss, 1, 3, 2)
                    P.op(act, lambda e, x1t=x1t, hf=hf, ss=ss: e.activation(out=hf[:, :], in_=x1t[:, :], func=AF.Copy, scale=ss[:, 2:3]), reads=[x1t, ss], writes=[hf])
                    transposes(hf, 8, ptr2)
                    evac_scaled(View3(hft, None, lambda hft=hft: hft[:, :, :]), ptr2, gffnbc)

                    def fnl(e, hft=hft, q=q):
                        ins = None
                        for k in range(8):
                            ins = e.matmul(pmm[3][:, q * 36:(q + 1) * 36], hft[:, k, :], wr[:, k, :], start=(k == 0), stop=(k == 7))
                        return ins
                    P.op(pe, fnl, reads=[hft, wr], writes=[pmm[3]])

                yield "SEG"
                S = lambda i: rs[:, i, :]
                bc = lambda ap, n: ap.unsqueeze(2).broadcast_to([128, Q, n])
                lgb = r36
                P.op(dve, lambda e: e.tensor_tensor(lgb[:, :, :], pmm[3][:, 0:Q * 36].rearrange("p (q n) -> p q n", q=Q), rbias[:, :, :], ALU.add),
                     reads=[pmm[3], rbias], writes=[lgb])
                gmask, gsh, gex = r4
                P.op(dve, lambda e: e.tensor_reduce(S(0), lgb[:, :, 0:4], AX.X, ALU.max), reads=[lgb], writes=[rs])
                P.op(dve, lambda e: e.tensor_tensor(gmask[:, :, :], lgb[:, :, 0:4], bc(S(0), 4), ALU.is_equal), reads=[lgb, rs], writes=[gmask])
                P.op(dve, lambda e: e.tensor_tensor(gsh[:, :, :], lgb[:, :, 0:4], bc(S(0), 4), ALU.subtract), reads=[lgb, rs], writes=[gsh])
                P.op(act, lambda e: e.activation(out=gex[:, :, :], in_=gsh[:, :, :], func=AF.Exp), reads=[gsh], writes=[gex])
                P.op(dve, lambda e: e.tensor_reduce(S(1), gex[:, :, :], AX.X, ALU.add), reads=[gex], writes=[rs])
                P.op(dve, lambda e: e.reciprocal(S(2), S(1)), reads=[rs], writes=[rs])
                le4 = lgb[:, :, 4:36].rearrange("p q (g j) -> p q g j", g=4)
                tmp32 = r32[0]
                P.op(dve, lambda e: e.tensor_tensor(tmp32[:, :, :].rearrange("p q (g j) -> p q g j", g=4), le4,
                                                    gmask[:, :, :].unsqueeze(3).broadcast_to([128, Q, 4, 8]), ALU.mult), reads=[lgb, gmask], writes=[tmp32])
                sel, top8, oh1, oh2 = r8
                P.op(dve, lambda e: e.tensor_reduce(sel[:, :, :], tmp32[:, :, :].rearrange("p q (g j) -> p q j g", g=4), AX.X, ALU.add), reads=[tmp32], writes=[sel])
                yield
                for q in range(Q):
                    P.op(dve, lambda e, q=q: e.max(top8[:, q, :], sel[:, q, :]), reads=[sel], writes=[top8])
                P.op(dve, lambda e: e.tensor_tensor(oh1[:, :, :], sel[:, :, :], top8[:, :, 0:1].broadcast_to([128, Q, 8]), ALU.is_equal), reads=[sel, top8], writes=[oh1])
                P.op(dve, lambda e: e.tensor_tensor(oh2[:, :, :], sel[:, :, :], top8[:, :, 1:2].broadcast_to([128, Q, 8]), ALU.is_equal), reads=[sel, top8], writes=[oh2])
                P.op(dve, lambda e: e.tensor_tensor(S(3), top8[:, :, 1], top8[:, :, 0], ALU.subtract), reads=[top8], writes=[rs])
                P.op(act, lambda e: e.activation(out=S(4), in_=S(3), func=AF.Exp), reads=[rs], writes=[rs])
                P.op(dve, lambda e: e.tensor_scalar(S(5), S(4), 1.0, None, ALU.add), reads=[rs], writes=[rs])
                P.op(dve, lambda e: e.reciprocal(S(6), S(5)), reads=[rs], writes=[rs])
                P.op(dve, lambda e: e.tensor_tensor(S(7), S(6), S(2), ALU.mult), reads=[rs], writes=[rs])
                P.op(dve, lambda e: e.tensor_tensor(S(8), S(2), S(7), ALU.subtract), reads=[rs], writes=[rs])
                E1, E2 = r32[1], r32[2]
                for Ek, oh in ((E1, oh1), (E2, oh2)):
                    P.op(dve, lambda e, Ek=Ek, oh=oh: e.tensor_tensor(Ek[:, :, :].rearrange("p q (g j) -> p q g j", g=4),
                                                                      gmask[:, :, :].unsqueeze(3).broadcast_to([128, Q, 4, 8]),
                                                                      oh[:, :, :].unsqueeze(2).broadcast_to([128, Q, 4, 8]), ALU.mult),
                         reads=[gmask, oh], writes=[Ek])
                P.op(dve, lambda e: e.tensor_tensor(mbf[:, :, :], E1[:, :, :], E2[:, :, :], ALU.add), reads=[E1, E2], writes=[mbf])

                def fnc(e):
                    ins = None
                    for q in range(Q):
                        ins = e.matmul(pmm[3][:, 160 + q * 32:160 + (q + 1) * 32], tri[:, :], mbf[:, q, :], start=True, stop=(q == 0))
                        for q2 in range(q):
                            ins = e.matmul(pmm[3][:, 160 + q * 32:160 + (q + 1) * 32], ones[:, :], mbf[:, q2, :], start=False, stop=(q2 == q - 1))
                    for q in range(Q):
                        ins = e.matmul(pmm[3][:, 288:320], ones[:, :], mbf[:, q, :], start=(q == 0), stop=(q == Q - 1))
                    return ins
                P.op(pe, fnc, reads=[tri, ones, mbf], writes=[pmm[3]])
                yield
                tot = r32[3]
                P.op(dve, lambda e: e.tensor_tensor(tot[:, :, :], pmm[3][:, 160:288].rearrange("p (q n) -> p q n", q=Q),
                                                    cntbc[:, :].unsqueeze(1).broadcast_to([128, Q, 32]), ALU.add), reads=[pmm[3], cntbc], writes=[tot])
                P.op(dve, lambda e: e.tensor_tensor(cntbc[:, :], cntbc[:, :], pmm[3][:, 288:320], ALU.add), reads=[pmm[3], cntbc], writes=[cntbc])
                tm = r32[4]
                for kk, Ek in ((0, E1), (1, E2)):
                    P.op(dve, lambda e, Ek=Ek: e.tensor_tensor(tm[:, :, :], Ek[:, :, :], tot[:, :, :], ALU.mult), reads=[Ek, tot], writes=[tm])
                    P.op(dve, lambda e, kk=kk: e.tensor_reduce(S(10 + kk), tm[:, :, :], AX.X, ALU.add), reads=[tm], writes=[rs])
                    P.op(dve, lambda e, Ek=Ek: e.tensor_tensor(tm[:, :, :], Ek[:, :, :], iota[:, :, :], ALU.mult), reads=[Ek, iota], writes=[tm])
                    P.op(dve, lambda e, kk=kk: e.tensor_reduce(S(12 + kk), tm[:, :, :], AX.X, ALU.add), reads=[tm], writes=[rs])
                    P.op(dve, lambda e, kk=kk: e.scalar_tensor_tensor(S(14 + kk), S(12 + kk), float(CAP), S(10 + kk), ALU.mult, ALU.add), reads=[rs], writes=[rs])
                    P.op(dve, lambda e, kk=kk: e.tensor_single_scalar(S(16 + kk), S(10 + kk), float(CAP), ALU.is_lt), reads=[rs], writes=[rs])
                    P.op(dve, lambda e, kk=kk: e.tensor_scalar(S(14 + kk), S(14 + kk), float(-TRASH), None, ALU.add), reads=[rs], writes=[rs])
                    P.op(dve, lambda e, kk=kk: e.tensor_tensor(S(14 + kk), S(14 + kk), S(16 + kk), ALU.mult), reads=[rs], writes=[rs])
                    P.op(dve, lambda e, kk=kk: e.tensor_scalar(S(14 + kk), S(14 + kk), float(TRASH), 0.0, ALU.add, ALU.max), reads=[rs], writes=[rs])
                    P.op(dve, lambda e, kk=kk: e.tensor_scalar(rinfo[:, ti, kk, :], S(14 + kk), float(TRASH), None, ALU.min), reads=[rs], writes=[rinfo])
                    P.op(dve, lambda e, kk=kk: e.tensor_tensor(rinfo[:, ti, 2 + kk, :], S(7 + kk), S(16 + kk), ALU.mult), reads=[rs], writes=[rinfo])
                    yield
                P.op(dve, lambda e: e.tensor_copy(sloti[:, ti, :, :], rinfo[:, ti, 0:2, :]), reads=[rinfo], writes=[sloti])
                for q in range(Q):
                    for kk in range(2):
                        P.dma(pool, lambda e, q=q, kk=kk: e.indirect_dma_start(
                            out=hs_d[:, :], out_offset=IOA(ap=sloti[:, ti, kk, q:q + 1], axis=0), in_=hfn[q][:, :], in_offset=None),
                            hfn[q], reads=[hfn[q], sloti])

            def collect(genfunc, ti):
                items = []
                orig_op, orig_dma = P.op, P.dma
                P.op = lambda eng, fn, reads=(), writes=(): items.append((orig_op, (eng, fn), dict(reads=reads, writes=writes), eng))
                P.dma = lambda q, fn, sem_buf, reads=(), writes=(): items.append((orig_dma, (q, fn, sem_buf), dict(reads=reads, writes=writes), q))
                try:
                    for tok in genfunc(ti):
                        items.append(tok)
                finally:
                    del P.op, P.dma
                return items

            def stages1(items):
                out, cur, prev = [], [], None
                for it in items:
                    if it is None or (HOP and prev is not None and it[3] is not prev):
                        out.append(cur)
                        cur = []
                    if it is None:
                        prev = None
                    else:
                        cur.append(it)
                        prev = it[3]
                out.append(cur)
                res = []
                for st in out:
                    if not st:
                        continue
                    res.append(st)
                    if LOADLAG and all(it[3] is sp and it[2]["writes"] for it in st):
                        res.extend([[] for _ in range(LOADLAG)])
                return res

            def zip_locked(chains):
                k = len(chains)
                if k == 1:
                    return chains[0]
                spans, wsets = [], []
                for L in chains:
                    fw, lr, ws = {}, {}, set()
                    for si, st in enumerate(L):
                        for (f, args, kw, eng) in st:
                            for bb in kw["writes"]:
                                fw.setdefault(id(bb), si)
                                ws.add(id(bb))
                            for bb in kw["reads"]:
                                lr[id(bb)] = si
                    spans.append({x: (fw[x], lr[x]) for x in fw if x in lr and lr[x] > fw[x]})
                    wsets.append(ws)
                out, pos, owner = [], [0] * k, {}
                while any(pos[c] < len(chains[c]) for c in range(k)):
                    progressed = False
                    for c in range(k):
                        if pos[c] >= len(chains[c]):
                            continue
                        st = chains[c][pos[c]]
                        W = set(id(bb) for (f, args, kw, eng) in st for bb in kw["writes"])
                        if any(owner.get(x) not in (None, c) for x in W):
                            continue
                        out.append(st)
                        progressed = True
                        for x in W:
                            if x in spans[c] and any(x in wsets[j] for j in range(k) if j != c):
                                owner[x] = c
                        for x in list(owner):
                            if owner[x] == c and pos[c] >= spans[c][x][1]:
                                owner[x] = None
                        pos[c] += 1
                    assert progressed, "chain lock deadlock"
                return out

            def stages(items):
                segs = [[[]]]
                for it in items:
                    if it == "SEG":
                        segs.append([[]])
                    elif it == "CHAIN":
                        segs[-1].append([])
                    else:
                        segs[-1][-1].append(it)
                out = []
                for seg in segs:
                    out += zip_locked([stages1(ch) for ch in seg if ch])  if any(seg) else []
                return out

            def spread(genfunc, ti, n, off=0):
                sts = stages(collect(genfunc, ti))
                assert len(sts) <= n - off, (len(sts), n, off)
                k = 0
                for t in range(n):
                    while t >= off and k < len(sts) and k * (n - off) < (t - off + 1) * len(sts):
                        for f, args, kw, eng in sts[k]:
                            f(*args, **kw)
                        k += 1
                    yield

            counts = [len(stages(collect(g, 1))) for g in (gen_F, gen_Mc, gen_Ml, gen_E)]
            NSTG = max(counts) + 2

            def both(g1, g2):
                for _ in g1:
                    next(g2)
                    yield

            def tile_gen(ti):
                yield from spread(gen_F, ti, NSTG)
                yield from both(spread(gen_Mc, ti, NSTG, MC_OFF), spread(gen_Ml, ti, NSTG))
                yield from spread(gen_E, ti, NSTG)

            run_pipelined((tile_gen(ti) for ti in range(NT)), depth=3, skew=NSTG)

            if debug:
                P.dma(sp, lambda e: e.dma_start(out=ri_d, in_=rinfo[:, :, :, :].rearrange("p a b c -> p (a b c)")), rinfo, reads=[rinfo])
            P.barrier()
            P.flush(block)

        with ExitStack() as sb:
            B = lambda shape, dt=F32, name=None, dma=False: P.buf(sb, shape, dt, name, dma)
            gffnbc = B([128, 8], F32, "gffnbc", dma=True)
            P.dma(sp, lambda e: e.dma_start(out=gffnbc[:, :], in_=gbc_d[1]), gffnbc, writes=[gffnbc])
            zt = B([128, D], F32, "zt", dma=True)
            P.op(dve, lambda e: e.memset(zt[:, :], 0.0), writes=[zt])
            P.dma(sp, lambda e: e.dma_start(out=y_d[NSLOT:NSLOT + 128, :], in_=zt[:, :]), zt, reads=[zt])
            w1 = [B([128, 8, 512], BF16, "w1") for _ in range(2)]
            w3 = [B([128, 8, 512], BF16, "w3") for _ in range(2)]
            w2 = [B([128, 4, D], BF16, "w2") for _ in range(2)]
            w3s = B([128, 8, 512], F32, "w3s", dma=True)
            w1s = B([128, 8, 512], F32, "w1s", dma=True)
            w2s = B([128, 4, D], F32, "w2s", dma=True)
            hst = [B([128, NSUB, D], BF16, "hst", dma=True) for _ in range(2)]
            hfTe = [B([128, 8, CAP], BF16, "hfTe") for _ in range(2)]
            actT = B([128, 4, CAP], BF16, "actT")
            tb = [B([128, 512], F32, "tb") for _ in range(2)]
            tc = [B([128, 512], F32, "tc") for _ in range(2)]
            yt = [B([128, D], F32, "yt", dma=True) for _ in range(3)]
            ntiles = [(0, 512)] if CAP == 512 else ([(n0, min(512, CAP - n0)) for n0 in range(0, CAP, 512)])
            yi = 0
            ci = 0

            def load_expert(ex):
                sl = ex % 2
                P.dma(sp, lambda e: e.dma_start(out=hst[sl][:, :, :], in_=hs_d[ex * CAP:(ex + 1) * CAP, :].rearrange("(s p) n -> p s n", p=128)),
                      hst[sl], writes=[hst[sl]])
                P.dma(sp, lambda e: e.dma_start(out=w1s[:, :, :], in_=w1_d[ex].rearrange("(k p) n -> p k n", p=128)), w1s, writes=[w1s])
                P.dma(sp, lambda e: e.dma_start(out=w3s[:, :, :], in_=w3_d[ex].rearrange("(k p) n -> p k n", p=128)), w3s, writes=[w3s])
                P.dma(sp, lambda e: e.dma_start(out=w2s[:, :, :], in_=w2_d[ex].rearrange("(k p) n -> p k n", p=128)), w2s, writes=[w2s])

            def cast_expert(ex):
                sl = ex % 2
                for k in range(8):
                    P.op(pool, lambda e, k=k: e.tensor_tensor(w1[sl][:, k, :], w1s[:, k, :], gffnbc[:, k:k + 1].broadcast_to([128, 512]), ALU.mult),
                         reads=[w1s, gffnbc], writes=[w1[sl]])
                for k in range(8):
                    P.op(act, lambda e, k=k: e.activation(out=w3[sl][:, k, :], in_=w3s[:, k, :], func=AF.Copy, scale=gffnbc[:, k:k + 1]), reads=[w3s, gffnbc], writes=[w3[sl]])
                for k in range(4):
                    P.op(act, lambda e, k=k: e.activation(out=w2[sl][:, k, :], in_=w2s[:, k, :], func=AF.Copy), reads=[w2s], writes=[w2[sl]])

            pool6 = pmm + [ps1, ps2]
            p6 = [0]

            def next6():
                bb = pool6[p6[0] % 6]
                p6[0] += 1
                return bb

            def do_T(ex):
                sl = ex % 2
                hT_e = hfTe[sl]
                for sbt in range(NSUB):
                    pt_ = ptr if sbt % 2 == 0 else ptr2

                    def fn(e, sbt=sbt, pt_=pt_, sl=sl):
                        ins = None
                        for c in range(8):
                            ins = e.transpose(pt_[:, c * 128:(c + 1) * 128], hst[sl][:, sbt, c * 128:(c + 1) * 128], ident[:, :])
                        return ins
                    P.op(pe, fn, reads=[hst[sl], ident], writes=[pt_])
                    P.op(dve, lambda e, sbt=sbt, pt_=pt_, hT_e=hT_e: e.tensor_copy(hT_e[:, :, sbt * 128:(sbt + 1) * 128], pt_[:, :].rearrange("p (c j) -> p c j", c=8)),
                         reads=[pt_], writes=[hT_e])

            def do_H(ex):
                sl = ex % 2
                hT_e = hfTe[sl]
                for (n0, nn) in ntiles:
                    for m in range(4):
                        p1, p3 = next6(), next6()
                        for (pb, wt) in ((p1, w1[sl]), (p3, w3[sl])):
                            def fn(e, pb=pb, wt=wt, m=m, n0=n0, nn=nn, hT_e=hT_e):
                                ins = None
                                for k in range(8):
                                    ins = e.matmul(pb[:, 0:nn], wt[:, k, m * 128:(m + 1) * 128], hT_e[:, k, n0:n0 + nn], start=(k == 0), stop=(k == 7))
                                return ins
                            P.op(pe, fn, reads=[wt, hT_e], writes=[pb])
                        tb_, tc_ = tb[ci_[0] % 2], tc[ci_[0] % 2]
                        ci_[0] += 1
                        P.op(act, lambda e, p1=p1, tb_=tb_, nn=nn: e.activation(out=tb_[:, 0:nn], in_=p1[:, 0:nn], func=AF.Silu), reads=[p1], writes=[tb_])
                        P.op(dve, lambda e, p3=p3, tb_=tb_, nn=nn, m=m, n0=n0: e.tensor_tensor(actT[:, m, n0:n0 + nn], tb_[:, 0:nn], p3[:, 0:nn], ALU.mult),
                             reads=[tb_, p3], writes=[actT])

            def do_Y(ex):
                sl = ex % 2
                for sbt in range(NSUB):
                    yb = yt[yi_[0] % 3]
                    yi_[0] += 1
                    for h in range(2):
                        pb = next6()

                        def fn(e, pb=pb, h=h, sbt=sbt, sl=sl):
                            ins = None
                            for m in range(4):
                                ins = e.matmul(pb[:, :], actT[:, m, sbt * 128:(sbt + 1) * 128], w2[sl][:, m, h * 512:(h + 1) * 512], start=(m == 0), stop=(m == 3))
                            return ins
                        P.op(pe, fn, reads=[actT, w2[sl]], writes=[pb])
                        P.op(act, lambda e, pb=pb, h=h, yb=yb: e.activation(out=yb[:, h * 512:(h + 1) * 512], in_=pb[:, :], func=AF.Copy), reads=[pb], writes=[yb])
                    r0 = ex * CAP + sbt * 128
                    P.dma(sp, lambda e, yb=yb, r0=r0: e.dma_start(out=y_d[r0:r0 + 128, :], in_=yb[:, :]), yb, reads=[yb])

            ci_, yi_ = [0], [0]
            load_expert(0)
            cast_expert(0)
            do_T(0)
            for ex in range(32):
                if ex + 1 < 32:
                    load_expert(ex + 1)
                do_H(ex)
                if ex + 1 < 32:
                    cast_expert(ex + 1)
                    do_T(ex + 1)
                do_Y(ex)
            P.barrier()
            P.flush(block)

        with ExitStack() as sc:
            B = lambda shape, dt=F32, name=None, dma=False: P.buf(sc, shape, dt, name, dma)
            gplebc = B([128, 8], F32, "gplebc", dma=True)
            gpp = B([128, D], F32, "gpp", dma=True)
            gfin = B([128, D], F32, "gfin", dma=True)
            wple = B([128, 2, D], BF16, "wple", dma=True)
            wpg = B([128, 8, D], BF16, "wpg", dma=True)
            P.dma(sp, lambda e: e.dma_start(out=gplebc[:, :], in_=gbc_d[2]), gplebc, writes=[gplebc])
            P.dma(sp, lambda e: e.dma_start(out=gpp[:, :], in_=rowbc_d[0]), gpp, writes=[gpp])
            P.dma(sp, lambda e: e.dma_start(out=gfin[:, :], in_=rowbc_d[1]), gfin, writes=[gfin])
            for k in range(2):
                P.dma(pool, lambda e, k=k: e.dma_start(out=wple[:, k, :], in_=wple_d[k * 128:(k + 1) * 128, :]), wple, writes=[wple])
            for k in range(8):
                P.dma(pool, lambda e, k=k: e.dma_start(out=wpg[:, k, :], in_=wpg_d[k * 128:(k + 1) * 128, :]), wpg, writes=[wpg])
            for k in range(8):
                P.op(dve, lambda e, k=k: e.tensor_scalar(wpg[:, k, :], wpg[:, k, :], gplebc[:, k:k + 1], None, ALU.mult), reads=[wpg, gplebc], writes=[wpg])
            x1t_ = [B([128, D], F32, "x1c", dma=True) for _ in range(NP_C)]
            pt_b = [B([128, 256], F32, "pc", dma=True) for _ in range(NP_C)]
            y1_ = [B([128, D], F32, "y1c", dma=True) for _ in range(NP_C)]
            y2_ = [B([128, D], F32, "y2c", dma=True) for _ in range(NP_C)]
            NP = NP_C
            ssC_ = [B([128, 16], F32, "ssC") for _ in range(NP)]
            xn3 = [B([128, D], BF16, "xn3") for _ in range(NP)]
            junk, te, ob, thg = xn3, y2_, x1t_, y1_
            x3T = [B([128, 8, 128], BF16, "x3T") for _ in range(NP)]
            pbf = [B([128, 256], BF16, "pbf") for _ in range(NP)]
            pT = [B([128, 2, 128], BF16, "pT") for _ in range(NP)]

            def rsq(ssb, i_v, i_l, i_o):
                P.op(act, lambda e: e.activation(out=ssb[:, i_l:i_l + 1], in_=ssb[:, i_v:i_v + 1], func=AF.Ln), reads=[ssb], writes=[ssb])
                P.op(act, lambda e: e.activation(out=ssb[:, i_o:i_o + 1], in_=ssb[:, i_l:i_l + 1], func=AF.Exp, scale=-0.5), reads=[ssb], writes=[ssb])

            def subtile_gen(st):
                ti, q = divmod(st, Q)
                r0 = st * 128
                i3 = st % NP
                xx, pp, y1, y2 = x1t_[i3], pt_b[i3], y1_[i3], y2_[i3]
                jk, ssC = junk[i3], ssC_[i3]
                xn_, x3_, pb_, pT_, tg_, te_, ob_ = xn3[i3], x3T[i3], pbf[i3], pT[i3], thg[i3], te[i3], ob[i3]
                P.dma(sp, lambda e: e.dma_start(out=xx[:, :], in_=x1_d[r0:r0 + 128, :]), xx, writes=[xx])
                P.dma(sp, lambda e: e.dma_start(out=pp[:, :], in_=p_d[r0:r0 + 128, :]), pp, writes=[pp])
                for (yy, kk) in ((y1, 0), (y2, 1)):
                    P.dma(pool, lambda e, yy=yy, kk=kk: e.indirect_dma_start(
                        out=yy[:, :], out_offset=None, in_=y_d[:, :], in_offset=IOA(ap=sloti[:, ti, kk, q:q + 1], axis=0)),
                        yy, reads=[sloti], writes=[yy])
                yield
                for (yy, kk) in ((y1, 0), (y2, 1)):
                    P.op(dve, lambda e, yy=yy, kk=kk: e.scalar_tensor_tensor(xx[:, :], yy[:, :], rinfo[:, ti, 2 + kk, q:q + 1], xx[:, :], ALU.mult, ALU.add),
                         reads=[yy, rinfo, xx], writes=[xx])
                yield
                P.op(act, lambda e: e.activation(out=jk[:, :], in_=xx[:, :], func=AF.Square, accum_out=ssC[:, 0:1]), reads=[xx], writes=[jk, ssC])
                P.op(act, lambda e: e.activation(out=pb_[:, :], in_=pp[:, :], func=AF.Copy), reads=[pp], writes=[pb_])
                yield
                P.op(dve, lambda e: e.tensor_scalar(ssC[:, 1:2], ssC[:, 0:1], 1.0 / D, EPS, ALU.mult, ALU.add), reads=[ssC], writes=[ssC])
                yield
                rsq(ssC, 1, 3, 2)
                P.op(act, lambda e: e.activation(out=xn_[:, :], in_=xx[:, :], func=AF.Copy, scale=ssC[:, 2:3]), reads=[xx, ssC], writes=[xn_])
                yield
                transposes(xn_, 8, ptr)
                P.op(dve, lambda e: e.tensor_copy(x3_[:, :, :], ptr[:, :].rearrange("p (c j) -> p c j", c=8)), reads=[ptr], writes=[x3_])
                transposes(pb_, 2, ptr2)
                P.op(act, lambda e: e.activation(out=pT_[:, :, :], in_=ptr2[:, 0:256].rearrange("p (c j) -> p c j", c=2), func=AF.Copy), reads=[ptr2], writes=[pT_])
                yield
                pes = []
                for h in range(2):
                    pg_ = pmm[h]

                    def fn(e, pg_=pg_, h=h):
                        ins = None
                        for k in range(8):
                            ins = e.matmul(pg_[:, :], x3_[:, k, :], wpg[:, k, h * 512:(h + 1) * 512], start=(k == 0), stop=(k == 7))
                        return ins
                    P.op(pe, fn, reads=[x3_, wpg], writes=[pg_])
                    P.op(act, lambda e, pg_=pg_, h=h: e.activation(out=tg_[:, h * 512:(h + 1) * 512], in_=pg_[:, :], func=AF.Tanh, scale=0.5), reads=[pg_], writes=[tg_])
                    pe_ = (pmm[2], pmm[3], ps1, ps2)[(2 * st + h) % 4]

                    def fn2(e, pe_=pe_, h=h):
                        ins = None
                        for k in range(2):
                            ins = e.matmul(pe_[:, :], pT_[:, k, :], wple[:, k, h * 512:(h + 1) * 512], start=(k == 0), stop=(k == 1))
                        return ins
                    P.op(pe, fn2, reads=[pT_, wple], writes=[pe_])
                    P.op(act, lambda e, pe_=pe_, h=h: e.activation(out=jk[:, 0:512], in_=pe_[:, :], func=AF.Square, accum_out=ssC[:, 4 + h:5 + h]), reads=[pe_], writes=[jk, ssC])
                    pes.append(pe_)
                yield
                P.op(dve, lambda e: e.tensor_tensor(ssC[:, 6:7], ssC[:, 4:5], ssC[:, 5:6], ALU.add), reads=[ssC], writes=[ssC])
                P.op(dve, lambda e: e.tensor_scalar(ssC[:, 7:8], ssC[:, 6:7], 4.0 / D, 4.0 * EPS, ALU.mult, ALU.add), reads=[ssC], writes=[ssC])
                yield
                rsq(ssC, 7, 9, 8)
                yield
                for h in range(2):
                    P.op(dve, lambda e, h=h, pe_=pes[h]: e.scalar_tensor_tensor(te_[:, h * 512:(h + 1) * 512], pe_[:, :], ssC[:, 8:9], gpp[:, h * 512:(h + 1) * 512], ALU.mult, ALU.mult),
                         reads=[pes[h], ssC, gpp], writes=[te_])
                P.op(dve, lambda e: e.scalar_tensor_tensor(te_[:, :], tg_[:, :], 1.0, te_[:, :], ALU.add, ALU.mult), reads=[tg_, te_], writes=[te_])
                yield
                P.op(pool, lambda e: e.tensor_tensor(xx[:, :], xx[:, :], te_[:, :], ALU.add), reads=[te_, xx], writes=[xx])
                yield
                P.op(act, lambda e: e.activation(out=jk[:, :], in_=xx[:, :], func=AF.Square, accum_out=ssC[:, 10:11]), reads=[xx], writes=[jk, ssC])
                yield
                P.op(dve, lambda e: e.tensor_scalar(ssC[:, 11:12], ssC[:, 10:11], 1.0 / D, EPS, ALU.mult, ALU.add), reads=[ssC], writes=[ssC])
                yield
                rsq(ssC, 11, 13, 12)
                yield
                P.op(dve, lambda e: e.scalar_tensor_tensor(ob_[:, :], xx[:, :], ssC[:, 12:13], gfin[:, :], ALU.mult, ALU.mult), reads=[xx, ssC, gfin], writes=[ob_])
                P.dma(sp, lambda e: e.dma_start(out=out_d[r0:r0 + 128, :], in_=ob_[:, :]), ob_, reads=[ob_])

            run_pipelined((subtile_gen(st) for st in range(T // 128)), depth=NP, skew=2)
            P.barrier()
            P.flush(block)
    return nc


def _chan(v):
    return np.ascontiguousarray(np.asarray(v, np.float32).reshape(4, 128).T)


def _gbc(g):
    return np.ascontiguousarray(np.asarray(g, np.float32).reshape(8, 128).T)


def prep_shared(inp):
    f = lambda a: np.ascontiguousarray(np.asarray(a, np.float32))
    cpar = np.zeros((128, NCP), np.float32)
    cw = f(inp["conv_dw_w"][0])
    for c in range(4):
        cpar[:, CW + c * 31:CW + (c + 1) * 31] = cw[:, c * 128:(c + 1) * 128].T
    cpar[:, CB:CB + 4] = _chan(inp["conv_dw_b"][0])
    cpar[:, LG:LG + 4] = _chan(inp["conv_ln_g"][0])
    cpar[:, LB:LB + 4] = _chan(inp["conv_ln_b"][0])
    lw = f(inp["lru_conv_w"][0])
    for c in range(4):
        cpar[:, LW + c * 4:LW + (c + 1) * 4] = lw[:, c * 128:(c + 1) * 128].T
    cpar[:, LBB:LBB + 4] = _chan(inp["lru_conv_b"][0])
    cpar[:, BR:BR + 4] = _chan(inp["lru_b_r"][0])
    cpar[:, BI:BI + 4] = _chan(inp["lru_b_i"][0])
    cpar[:, LAM:LAM + 4] = _chan(inp["lru_lambda"][0])
    wr_, wi_ = f(inp["lru_w_r"][0]), f(inp["lru_w_i"][0])
    gbd = np.zeros((4, 128, 256), np.float32)
    for c in range(4):
        for hh in range(2):
            gbd[c, hh * 64:(hh + 1) * 64, hh * 64:(hh + 1) * 64] = wr_[2 * c + hh]
            gbd[c, hh * 64:(hh + 1) * 64, 128 + hh * 64:128 + (hh + 1) * 64] = wi_[2 * c + hh]
    rb = np.concatenate([f(inp["b_group"][0]), f(inp["b_expert"][0])])
    shared = {
        "w_in": f(inp["w_in"][0]), "w_out": f(inp["w_out"][0]),
        "w_route": np.ascontiguousarray(np.concatenate([f(inp["w_group"][0]), f(inp["w_expert"][0])], axis=1)),
        "gate_bd": gbd, "cpar": cpar,
        "gbc": np.stack([_gbc(inp["g_mix"][0]), _gbc(inp["g_ffn"][0]), _gbc(inp["g_ple"][0])]),
        "rowbc": np.stack([np.ascontiguousarray(np.broadcast_to(f(inp["g_ple_proj"][0]), (128, D))),
                           np.ascontiguousarray(np.broadcast_to(f(inp["g_final"]), (128, D)))]),
        "rbias": np.ascontiguousarray(np.broadcast_to(np.tile(rb, Q), (128, Q * 36))),
        "iota_e": np.ascontiguousarray(np.broadcast_to(np.tile(np.arange(32, dtype=np.float32), Q), (128, Q * 32))),
        "ident": np.eye(128, dtype=np.float32),
        "tri": np.ascontiguousarray(np.triu(np.ones((128, 128), np.float32), 1)),
        "w1": f(inp["w1"][0]), "w3": f(inp["w3"][0]), "w2": f(inp["w2"][0]),
        "w_ple": f(inp["w_ple"][0]), "w_ple_gate": f(inp["w_ple_gate"][0]),
    }
    return shared


def kernel(**inputs):
    NSEQ, CAP = 4, 1024
    x = np.asarray(inputs["x"], np.float32)
    p = np.asarray(inputs["p"], np.float32)[0]
    shared = prep_shared(inputs)
    nc = build(NSEQ, CAP)
    in_maps = []
    for i in range(N_CORES):
        m = dict(shared)
        m["x"] = np.ascontiguousarray(x[i * NSEQ:(i + 1) * NSEQ].reshape(NSEQ * SEQ, D))
        m["p"] = np.ascontiguousarray(p[i * NSEQ:(i + 1) * NSEQ].reshape(NSEQ * SEQ, 256))
        in_maps.append(m)
    res = run_bass_kernel_spmd(nc, in_maps, core_ids=list(range(N_CORES)))
    out = np.concatenate([np.asarray(r["out"], np.float32).reshape(NSEQ, SEQ, D) for r in res.results], axis=0)
    return out
```
